# Optimizing a Trainium2 kernel written in Bass

```python
import jax, jax.numpy as jnp
from jax import lax
import numpy as np

D_MODEL = 1024
BATCH = 8
SEQ = 2048
DEPTH = 4

N_MIXERS = 4
HEAD_DIM = 128
CHUNK = 64
GLA_HEADS = 4
GLA_DK = D_MODEL // 2 // GLA_HEADS
GLA_DV = D_MODEL // GLA_HEADS
GLA_GATE_RANK = 16
GLA_GATE_NORMALIZER = 16.0
GLA_COLS = (GLA_HEADS * GLA_DK, GLA_HEADS * GLA_DK, GLA_HEADS * GLA_DV, GLA_HEADS * GLA_DV, GLA_GATE_RANK)
MOBA_HEADS = D_MODEL // HEAD_DIM
MOBA_BLOCK = 256
MOBA_TOPK = 3
MOBA_QCHUNK = 16
MOBA_COLS = (D_MODEL, D_MODEL, D_MODEL)
ROPE_THETA = 500000.0
ROPE_DIM = HEAD_DIM // 4
NEG_INF = -1e30
GDN_HEADS = D_MODEL // HEAD_DIM
GDN_DK = HEAD_DIM
GDN_WIDTH = GDN_HEADS * GDN_DK
GDN_CONV = 4
GDN_COLS = (3 * GDN_WIDTH, GDN_WIDTH, GDN_HEADS, GDN_HEADS)
HGRN_HEADS = 8
HGRN_F_DIM = 128
HGRN_F = HGRN_HEADS * HGRN_F_DIM
HGRN_COLS = (HGRN_F, HGRN_F, D_MODEL, D_MODEL)
FFN_HIDDEN = -(-8 * D_MODEL // (3 * 256)) * 256
DEEPNORM_ALPHA = (2.0 * DEPTH) ** 0.25
DEEPNORM_BETA = (8.0 * DEPTH) ** -0.25
N_GLA = len(range(0, DEPTH, N_MIXERS))
N_MOBA = len(range(1, DEPTH, N_MIXERS))
N_GDN = len(range(2, DEPTH, N_MIXERS))
N_HGRN = len(range(3, DEPTH, N_MIXERS))

kernel_name = "hybrid_gla_moba_gdn_hgrn2_deepnorm"


def split_cols(x, cols):
    return jnp.split(x, [int(c) for c in np.cumsum(cols)[:-1]], axis=-1)


def to_heads(x, h):
    b, s, _ = x.shape
    return x.reshape(b, s, h, -1).transpose(0, 2, 1, 3)


def from_heads(x):
    b, h, s, d = x.shape
    return x.transpose(0, 2, 1, 3).reshape(b, s, h * d)


def layer_norm(x, g, b, eps=1e-5):
    xf = x.astype(jnp.float32)
    mu = jnp.mean(xf, -1, keepdims=True)
    var = jnp.mean(jnp.square(xf - mu), -1, keepdims=True)
    return ((xf - mu) * lax.rsqrt(var + eps) * g + b).astype(x.dtype)


def rms_norm(x, g, eps=1e-6):
    xf = x.astype(jnp.float32)
    return (xf * lax.rsqrt(jnp.mean(xf * xf, -1, keepdims=True) + eps) * g).astype(x.dtype)


def l2_normalize(x, eps=1e-6):
    xf = x.astype(jnp.float32)
    return (xf * lax.rsqrt(jnp.sum(xf * xf, -1, keepdims=True) + eps)).astype(x.dtype)


def partial_rope(x, positions):
    inv_freq = ROPE_THETA ** (-jnp.arange(0, ROPE_DIM, 2, dtype=jnp.float32) / ROPE_DIM)
    ang = positions.astype(jnp.float32)[:, None, :, None] * inv_freq
    cos, sin = jnp.cos(ang), jnp.sin(ang)
    xr = x[..., :ROPE_DIM].astype(jnp.float32)
    x1, x2 = xr[..., :ROPE_DIM // 2], xr[..., ROPE_DIM // 2:]
    rot = jnp.concatenate([x1 * cos - x2 * sin, x2 * cos + x1 * sin], -1).astype(x.dtype)
    return jnp.concatenate([rot, x[..., ROPE_DIM:]], -1)


def causal_depthwise_conv(x, w):
    k, c = w.shape
    return lax.conv_general_dilated(x, w.reshape(k, 1, c).astype(x.dtype), window_strides=(1,),
                                    padding=((k - 1, 0),), dimension_numbers=("NWC", "WIO", "NWC"),
                                    feature_group_count=c)


def swiglu_ffn(x, w_gu, w_down):
    gate, up = jnp.split(x @ w_gu, 2, axis=-1)
    return (jax.nn.silu(gate) * up) @ w_down


def chunk_gla(q, k, v, log_g):
    b, h, s, dk = q.shape
    dv = v.shape[-1]
    n = s // CHUNK
    f32 = jnp.float32
    q, k, log_g = (t.astype(f32).reshape(b, h, n, CHUNK, dk) for t in (q, k, log_g))
    v = v.astype(f32).reshape(b, h, n, CHUNK, dv)
    cum = jnp.cumsum(log_g, axis=3)
    last = cum[:, :, :, -1:, :]
    q_dec = q * jnp.exp(cum)
    k_inv = k * jnp.exp(-cum)
    k_to_end = k * jnp.exp(last - cum)
    causal = jnp.tril(jnp.ones((CHUNK, CHUNK), bool))
    scores = jnp.where(causal, jnp.einsum("bhnik,bhnjk->bhnij", q_dec, k_inv), 0.0)
    o_intra = jnp.einsum("bhnij,bhnjv->bhniv", scores, v)
    upd = jnp.einsum("bhnjk,bhnjv->bhnkv", k_to_end, v)
    decay = jnp.exp(last[:, :, :, 0, :])

    def step(state, inp):
        dec, u = inp
        return state * dec[..., None] + u, state

    _, states = lax.scan(step, jnp.zeros((b, h, dk, dv), f32),
                         (jnp.moveaxis(decay, 2, 0), jnp.moveaxis(upd, 2, 0)))
    states = jnp.moveaxis(states, 0, 2)
    o_inter = jnp.einsum("bhnik,bhnkv->bhniv", q_dec, states)
    return (o_intra + o_inter).reshape(b, h, s, dv)


def chunk_gated_delta(q, k, v, g, beta):
    b, h, s, dk = q.shape
    dv = v.shape[-1]
    n = s // CHUNK
    f32 = jnp.float32
    q = q.astype(f32).reshape(b, h, n, CHUNK, dk)
    k = k.astype(f32).reshape(b, h, n, CHUNK, dk)
    v = v.astype(f32).reshape(b, h, n, CHUNK, dv)
    g = g.astype(f32).reshape(b, h, n, CHUNK)
    beta = beta.astype(f32).reshape(b, h, n, CHUNK)
    cum = jnp.cumsum(g, axis=-1)
    incl = jnp.tril(jnp.ones((CHUNK, CHUNK), bool))
    strict = jnp.tril(jnp.ones((CHUNK, CHUNK), bool), -1)
    diff = cum[..., :, None] - cum[..., None, :]
    decay = jnp.where(incl, jnp.exp(jnp.where(incl, diff, 0.0)), 0.0)
    k_beta = k * beta[..., None]
    lower = jnp.where(strict, jnp.einsum("bhnid,bhnjd->bhnij", k_beta, k) * decay, 0.0)
    eye = jnp.eye(CHUNK, dtype=f32)
    rhs = jnp.concatenate([v * beta[..., None], k_beta * jnp.exp(cum)[..., None]], axis=-1)
    sol = lax.linalg.triangular_solve(lower + eye, rhs, left_side=True, lower=True)
    u, w = sol[..., :dv], sol[..., dv:]
    qk = jnp.einsum("bhnid,bhnjd->bhnij", q, k) * decay
    q_dec = q * jnp.exp(cum)[..., None]
    k_end = k * jnp.exp(cum[..., -1:] - cum)[..., None]
    chunk_decay = jnp.exp(cum[..., -1])

    def step(state, inp):
        u_c, w_c, qk_c, qd_c, ke_c, dec_c = inp
        v_new = u_c - jnp.einsum("bhck,bhkv->bhcv", w_c, state)
        o_c = jnp.einsum("bhck,bhkv->bhcv", qd_c, state) + jnp.einsum("bhij,bhjv->bhiv", qk_c, v_new)
        state = state * dec_c[..., None, None] + jnp.einsum("bhck,bhcv->bhkv", ke_c, v_new)
        return state, o_c

    xs = tuple(jnp.moveaxis(t, 2, 0) for t in (u, w, qk, q_dec, k_end, chunk_decay))
    _, o = lax.scan(step, jnp.zeros((b, h, dk, dv), f32), xs)
    return jnp.moveaxis(o, 0, 2).reshape(b, h, s, dv)


def moba_attention(q, k, v):
    b, h, s, d = q.shape
    nb = -(-s // MOBA_BLOCK)
    s_pad = nb * MOBA_BLOCK
    pad = ((0, 0), (0, 0), (0, s_pad - s), (0, 0))
    q, k, v = (jnp.pad(t, pad) for t in (q, k, v))
    scale = HEAD_DIM ** -0.5
    k_blocks = k.reshape(b, h, nb, MOBA_BLOCK, d)
    v_blocks = v.reshape(b, h, nb, MOBA_BLOCK, d)
    k_mean = jnp.mean(k_blocks.astype(jnp.float32), axis=3)
    gate = jnp.einsum("bhsd,bhnd->bhsn", q.astype(jnp.float32), k_mean)
    q_block = jnp.arange(s_pad) // MOBA_BLOCK
    past = jnp.arange(nb)[None, :] < q_block[:, None]
    gate = jnp.where(past, gate, NEG_INF)
    n_sel = min(MOBA_TOPK, nb)
    _, sel = lax.top_k(gate, n_sel)
    bi = jnp.arange(b)[:, None, None, None]
    hi = jnp.arange(h)[None, :, None, None]

    def chunk_fn(ci):
        q0 = ci * MOBA_QCHUNK
        blk = q0 // MOBA_BLOCK
        q_c = lax.dynamic_slice_in_dim(q, q0, MOBA_QCHUNK, axis=2)
        k_own = lax.dynamic_slice_in_dim(k, blk * MOBA_BLOCK, MOBA_BLOCK, axis=2)
        v_own = lax.dynamic_slice_in_dim(v, blk * MOBA_BLOCK, MOBA_BLOCK, axis=2)
        sel_c = lax.dynamic_slice_in_dim(sel, q0, MOBA_QCHUNK, axis=2)
        k_sel = k_blocks[bi, hi, sel_c]
        v_sel = v_blocks[bi, hi, sel_c]
        own = jnp.einsum("bhqd,bhkd->bhqk", q_c, k_own, preferred_element_type=jnp.float32) * scale
        q_pos = q0 + jnp.arange(MOBA_QCHUNK)
        k_pos = blk * MOBA_BLOCK + jnp.arange(MOBA_BLOCK)
        own = jnp.where(k_pos[None, :] <= q_pos[:, None], own, NEG_INF)
        far = jnp.einsum("bhqd,bhqnkd->bhqnk", q_c, k_sel, preferred_element_type=jnp.float32) * scale
        far = jnp.where((jnp.arange(n_sel) < blk)[:, None], far, NEG_INF)
        logits = jnp.concatenate([own, far.reshape(b, h, MOBA_QCHUNK, n_sel * MOBA_BLOCK)], axis=-1)
        p = jax.nn.softmax(logits, axis=-1).astype(v.dtype)
        p_own = p[..., :MOBA_BLOCK]
        p_far = p[..., MOBA_BLOCK:].reshape(b, h, MOBA_QCHUNK, n_sel, MOBA_BLOCK)
        return (jnp.einsum("bhqk,bhkd->bhqd", p_own, v_own)
                + jnp.einsum("bhqnk,bhqnkd->bhqd", p_far, v_sel))

    out = lax.map(chunk_fn, jnp.arange(s_pad // MOBA_QCHUNK))
    out = jnp.moveaxis(out, 0, 2).reshape(b, h, s_pad, d)
    return out[:, :, :s]


def gla_mixer(x, w_in, w_gk, b_gk, norm_g, w_out):
    q, k, v, g, gk_low = split_cols(x @ w_in, GLA_COLS)
    log_a = jax.nn.log_sigmoid((gk_low @ w_gk + b_gk).astype(jnp.float32)) / GLA_GATE_NORMALIZER
    q = to_heads(q, GLA_HEADS) * (GLA_DK ** -0.5)
    o = chunk_gla(q, to_heads(k, GLA_HEADS), to_heads(v, GLA_HEADS), to_heads(log_a, GLA_HEADS))
    o = from_heads(rms_norm(o.astype(x.dtype), norm_g)) * jax.nn.silu(g)
    return o @ w_out


def moba_mixer(x, positions, w_in, w_out):
    q, k, v = split_cols(x @ w_in, MOBA_COLS)
    q = partial_rope(to_heads(q, MOBA_HEADS), positions)
    k = partial_rope(to_heads(k, MOBA_HEADS), positions)
    o = moba_attention(q, k, to_heads(v, MOBA_HEADS))
    return from_heads(o) @ w_out


def gdn_mixer(x, w_in, conv_w, a_log, dt_bias, norm_g, w_out):
    qkv, z, a, b_logit = split_cols(x @ w_in, GDN_COLS)
    qkv = jax.nn.silu(causal_depthwise_conv(qkv, conv_w))
    q, k, v = jnp.split(qkv, 3, axis=-1)
    q = l2_normalize(to_heads(q, GDN_HEADS)) * (GDN_DK ** -0.5)
    k = l2_normalize(to_heads(k, GDN_HEADS))
    v = to_heads(v, GDN_HEADS)
    beta = jax.nn.sigmoid(b_logit.astype(jnp.float32)).transpose(0, 2, 1)
    g = (-jnp.exp(a_log.astype(jnp.float32))
         * jax.nn.softplus(a.astype(jnp.float32) + dt_bias)).transpose(0, 2, 1)
    o = chunk_gated_delta(q, k, v, g, beta).astype(x.dtype)
    o = from_heads(rms_norm(o, norm_g)) * jax.nn.silu(z)
    return o @ w_out


def hgrn2_mixer(x, lb, w_in, norm_g, w_out):
    q, f_logit, i, g = split_cols(x @ w_in, HGRN_COLS)
    f_logit = f_logit.astype(jnp.float32)
    forget = lb + (1.0 - lb) * jax.nn.sigmoid(f_logit)
    k_in = (1.0 - lb) * jax.nn.sigmoid(-f_logit)
    o = chunk_gla(to_heads(q, HGRN_HEADS), to_heads(k_in, HGRN_HEADS), to_heads(i, HGRN_HEADS),
                  to_heads(jnp.log(forget), HGRN_HEADS))
    o = from_heads(rms_norm(o.astype(x.dtype), norm_g)) * jax.nn.silu(g)
    return o @ w_out


def setup_inputs(seed: int = 0) -> dict:
    key = jax.random.key(seed)
    ks = iter(jax.random.split(key, 32))
    f32 = jnp.float32

    def normal(shape):
        return jax.random.normal(next(ks), shape, f32)

    def dense(shape, fan_in, scale=1.0):
        return normal(shape) * (scale * fan_in ** -0.5)

    def gain(shape):
        return 1.0 + 0.02 * normal(shape)

    def small(shape):
        return 0.02 * normal(shape)

    x = normal((BATCH, SEQ, D_MODEL))
    offset = jax.random.randint(next(ks), (BATCH,), 0, 4096, dtype=jnp.int32)
    positions = offset[:, None] + jnp.arange(SEQ, dtype=jnp.int32)[None, :]
    a_init = jax.random.uniform(next(ks), (N_GDN, GDN_HEADS), f32, 1.0, 16.0)
    dt = jnp.exp(jax.random.uniform(next(ks), (N_GDN, GDN_HEADS), f32, float(np.log(1e-3)), float(np.log(1e-1))))
    return {
        "x": x,
        "positions": positions,
        "gla_w_in": dense((N_GLA, D_MODEL, sum(GLA_COLS)), D_MODEL),
        "gla_w_gk": dense((N_GLA, GLA_GATE_RANK, GLA_HEADS * GLA_DK), GLA_GATE_RANK),
        "gla_b_gk": small((N_GLA, GLA_HEADS * GLA_DK)),
        "gla_norm_g": gain((N_GLA, GLA_DV)),
        "gla_w_out": dense((N_GLA, GLA_HEADS * GLA_DV, D_MODEL), GLA_HEADS * GLA_DV, DEEPNORM_BETA),
        "moba_w_in": dense((N_MOBA, D_MODEL, sum(MOBA_COLS)), D_MODEL),
        "moba_w_out": dense((N_MOBA, D_MODEL, D_MODEL), D_MODEL, DEEPNORM_BETA),
        "gdn_w_in": dense((N_GDN, D_MODEL, sum(GDN_COLS)), D_MODEL),
        "gdn_conv_w": dense((N_GDN, GDN_CONV, 3 * GDN_WIDTH), GDN_CONV),
        "gdn_a_log": jnp.log(a_init),
        "gdn_dt_bias": dt + jnp.log(-jnp.expm1(-dt)),
        "gdn_norm_g": gain((N_GDN, GDN_DK)),
        "gdn_w_out": dense((N_GDN, GDN_WIDTH, D_MODEL), GDN_WIDTH, DEEPNORM_BETA),
        "hgrn_lower_bounds": small((DEPTH, HGRN_F)),
        "hgrn_w_in": dense((N_HGRN, D_MODEL, sum(HGRN_COLS)), D_MODEL),
        "hgrn_norm_g": gain((N_HGRN, D_MODEL // HGRN_HEADS)),
        "hgrn_w_out": dense((N_HGRN, D_MODEL, D_MODEL), D_MODEL, DEEPNORM_BETA),
        "ffn_w_gu": dense((DEPTH, D_MODEL, 2 * FFN_HIDDEN), D_MODEL),
        "ffn_w_down": dense((DEPTH, FFN_HIDDEN, D_MODEL), FFN_HIDDEN, DEEPNORM_BETA),
        "ln_g": gain((DEPTH, 2, D_MODEL)),
        "ln_b": small((DEPTH, 2, D_MODEL)),
    }


def reference(x, positions, gla_w_in, gla_w_gk, gla_b_gk, gla_norm_g, gla_w_out, moba_w_in, moba_w_out,
              gdn_w_in, gdn_conv_w, gdn_a_log, gdn_dt_bias, gdn_norm_g, gdn_w_out, hgrn_lower_bounds,
              hgrn_w_in, hgrn_norm_g, hgrn_w_out, ffn_w_gu, ffn_w_down, ln_g, ln_b):
    lb_soft = jax.nn.softmax(hgrn_lower_bounds.astype(jnp.float32), axis=0)
    lower_bounds = jnp.cumsum(lb_soft, axis=0) - lb_soft[0]
    for i in range(DEPTH):
        kind, j = i % N_MIXERS, i // N_MIXERS
        if kind == 0:
            y = gla_mixer(x, gla_w_in[j], gla_w_gk[j], gla_b_gk[j], gla_norm_g[j], gla_w_out[j])
        elif kind == 1:
            y = moba_mixer(x, positions, moba_w_in[j], moba_w_out[j])
        elif kind == 2:
            y = gdn_mixer(x, gdn_w_in[j], gdn_conv_w[j], gdn_a_log[j], gdn_dt_bias[j], gdn_norm_g[j], gdn_w_out[j])
        else:
            y = hgrn2_mixer(x, lower_bounds[i], hgrn_w_in[j], hgrn_norm_g[j], hgrn_w_out[j])
        x = layer_norm(DEEPNORM_ALPHA * x + y, ln_g[i, 0], ln_b[i, 0])
        x = layer_norm(DEEPNORM_ALPHA * x + swiglu_ffn(x, ffn_w_gu[i], ffn_w_down[i]), ln_g[i, 1], ln_b[i, 1])
    return x
```

```python
import contextlib
import numpy as np
import concourse.bass as bass
import concourse.mybir as mybir
from concourse.bass_utils import run_bass_kernel_spmd

F32 = mybir.dt.float32
BF16 = mybir.dt.bfloat16
I32 = mybir.dt.int32
AF = mybir.ActivationFunctionType
ALU = mybir.AluOpType
AX = mybir.AxisListType

D = 1024
S = 2048
NT = S // 128
DEPTH = 4
FFN_H = 2816
ALPHA = (2.0 * DEPTH) ** 0.25
NCORES = 8


class T:
    __slots__ = ("name", "w", "r", "excl")

    def __init__(self, name, excl=False):
        self.name = name
        self.w = None
        self.r = []
        self.excl = excl


class Prog:
    ENGS = ("pe", "dve", "act", "pool", "sp")
    NLANES = 6

    def __init__(self, nc):
        self.nc = nc
        self.ins = []
        self.pending = {e: set() for e in self.ENGS}
        self.last_barrier = 0
        self.eng_obj = {"pe": nc.tensor, "dve": nc.vector, "act": nc.scalar, "pool": nc.gpsimd, "sp": nc.sync}

    def op(self, eng, fn, reads=(), writes=(), dma=False):
        idx = len(self.ins)
        deps = set()
        for t in reads:
            if t.w is not None:
                deps.add(t.w)
            if t.excl:
                deps.update(r for r in t.r if self.ins[r]["eng"] != eng)
        for t in writes:
            if t.w is not None:
                deps.add(t.w)
            deps.update(t.r)
        if self.pending[eng]:
            deps |= self.pending[eng]
            self.pending[eng] = set()
        if eng == "pe":
            deps = {d for d in deps if self.ins[d]["eng"] != "pe" or self.ins[d]["dma"]}
        self.ins.append(dict(eng=eng, fn=fn, deps=deps, dma=dma))
        for t in reads:
            t.r.append(idx)
        for t in writes:
            t.w = idx
            t.r = []
        return idx

    def barrier(self):
        last = {}
        deps = set()
        for i in range(self.last_barrier, len(self.ins)):
            ins = self.ins[i]
            if ins["dma"]:
                deps.add(i)
            else:
                last[ins["eng"]] = i
        deps |= set(last.values())
        for e in self.ENGS:
            self.pending[e] |= deps
        self.last_barrier = len(self.ins)

    def dma(self, eng, out, in_, reads=(), writes=(), **kw):
        return self.op(eng, lambda e: e.dma_start(out=out, in_=in_, **kw), reads, writes, dma=True)

    def finalize(self):
        nc = self.nc
        waited = set()
        for ins in self.ins:
            waited.update(ins["deps"])
        sems = {}
        with contextlib.ExitStack() as es:
            for e in self.ENGS:
                sems[e] = es.enter_context(nc.semaphore("s_" + e))
            for q in ("sp", "act", "pool"):
                for l in range(self.NLANES):
                    sems[(q, l)] = es.enter_context(nc.semaphore("d_%s%d" % (q, l)))
            cnt = {k: 0 for k in sems}
            sig = {}
            lane_rr = {"sp": 0, "act": 0, "pool": 0}
            lane_prev = {}
            for idx, ins in enumerate(self.ins):
                if ins["dma"]:
                    q = ins["eng"]
                    lane = (q, lane_rr[q] % self.NLANES)
                    lane_rr[q] += 1
                    ins["lane_prev"] = cnt[lane]
                    cnt[lane] += 16
                    sig[idx] = (lane, cnt[lane])
                elif idx in waited:
                    cnt[ins["eng"]] += 1
                    sig[idx] = (ins["eng"], cnt[ins["eng"]])
            self.sig_counts = dict(cnt)
            self.wait_hist = {}
            with nc.Block() as block:
                for eng in self.ENGS:
                    my = [(i, ins) for i, ins in enumerate(self.ins) if ins["eng"] == eng]
                    if not my:
                        continue

                    def body(e, my=my, eng=eng):
                        seen = {}
                        for idx, ins in my:
                            needs = {}
                            for d in ins["deps"]:
                                sk, val = sig[d]
                                if needs.get(sk, 0) < val:
                                    needs[sk] = val
                            if ins["dma"]:
                                sk, val = sig[idx]
                                if ins["lane_prev"] > 0 and needs.get(sk, 0) < ins["lane_prev"]:
                                    needs[sk] = ins["lane_prev"]
                            nw = 0
                            for sk, val in needs.items():
                                if seen.get(sk, 0) < val:
                                    e.wait_ge(sems[sk], val)
                                    seen[sk] = val
                                    nw += 1
                            self.wait_hist[nw] = self.wait_hist.get(nw, 0) + 1
                            r = ins["fn"](e)
                            if idx in sig:
                                sk, val = sig[idx]
                                r.then_inc(sems[sk], 16 if ins["dma"] else 1)
                        for l in range(self.NLANES):
                            sk = (eng, l)
                            if sk in cnt and cnt[sk] > 0 and seen.get(sk, 0) < cnt[sk]:
                                e.wait_ge(sems[sk], cnt[sk])

                    getattr(block, {"pe": "tensor", "dve": "vector", "act": "scalar", "pool": "gpsimd", "sp": "sync"}[eng])(body)


class Ctx:
    pass


def host_consts():
    c = {}
    c["ident"] = np.eye(128, dtype=np.float32)
    i = np.arange(128)
    c["m_incl"] = (i[:, None] <= i[None, :]).astype(np.float32)
    c["m_rev"] = (i[:, None] > i[None, :]).astype(np.float32)
    c["m_caus"] = (i[:, None] <= i[None, :]).astype(np.float32)
    inv = (500000.0 ** (-np.arange(0, 32, 2, dtype=np.float32) / 32.0)).astype(np.float32)
    c["invf"] = np.concatenate([inv, inv]).reshape(32, 1).astype(np.float32)
    c["sgn"] = np.concatenate([-np.ones(16), np.ones(16)]).reshape(32, 1).astype(np.float32)
    E = np.zeros((8, 8, 128), np.float32)
    for n in range(8):
        E[n, n, :] = 30000.0
    c["E"] = E.reshape(8, 1024)
    c["causneg"] = np.where(i[:, None] > i[None, :], -30000.0, 0.0).astype(np.float32)
    npast = np.where(np.arange(8)[None, :] < np.arange(8)[:, None], 0.0, -1e30).astype(np.float32)
    c["negpast"] = npast.reshape(1, 64)
    BIG = 1.0e5
    c["g_pos"] = np.where(i[None, :] >= i[:, None], BIG, 0.0).astype(np.float32)
    c["g_negt"] = np.where(i[None, :] < i[:, None], -BIG, 0.0).astype(np.float32)
    c["ones"] = np.ones((128, 128), np.float32)
    c["m_incl_gla"] = c["m_incl"] * np.float32(-1.0 / 16.0)
    c["m_rev_gla"] = c["m_rev"] * np.float32(-1.0 / 16.0)
    return c


def build(n_layers=DEPTH, dbg=None, layers=None):
    dbg = dbg or {}
    nc = bass.Bass("TRN2", target_bir_lowering=False)
    dr = {}

    def din(name, shape, dt=F32):
        dr[name] = nc.dram_tensor(name, list(shape), dt, kind="ExternalInput").ap()
        return dr[name]

    din("x", [S, D])
    din("positions", [S, 1], I32)
    din("gla_w_in", [D, 3088]); din("gla_w_gk", [16, 512]); din("gla_b_gk", [1, 512]); din("gla_norm_g", [1, 256])
    din("gla_w_out", [D, D])
    din("moba_w_in", [D, 3072]); din("moba_w_out", [D, D])
    din("gdn_w_in", [D, 4112]); din("gdn_conv_w", [4, 3072]); din("gdn_a_log", [1, 8]); din("gdn_dt_bias", [1, 8])
    din("gdn_norm_g", [1, 128]); din("gdn_w_out", [D, D])
    din("hgrn_lower_bounds", [4, 1024]); din("hgrn_w_in", [D, 4096]); din("hgrn_norm_g", [1, 128])
    din("hgrn_w_out", [D, D])
    din("ffn_w_gu", [DEPTH, D, 2 * FFN_H]); din("ffn_w_down", [DEPTH, FFN_H, D])
    din("ln_g", [DEPTH * 2, D]); din("ln_b", [DEPTH * 2, D])
    for k, v in host_consts().items():
        din("c_" + k, v.shape)
    y_out = nc.dram_tensor("y", [S, D], F32, kind="ExternalOutput").ap()

    P = Prog(nc)
    C = Ctx()
    C.nc, C.P, C.dr, C.dbg = nc, P, dr, dbg
    with contextlib.ExitStack() as es:
        C.es = es
        C.X = es.enter_context(nc.sbuf_tensor("X", [128, NT, D], F32))
        C.Xt = [T("X%d" % t) for t in range(NT)]
        C.ps = [es.enter_context(nc.psum_tensor("ps%d" % i, [128, 512], F32)) for i in range(8)]
        C.pst = [T("ps%d" % i, excl=True) for i in range(8)]
        C.ident = es.enter_context(nc.sbuf_tensor("ident", [128, 128], F32))
        C.ident_t = T("ident")
        P.dma("sp", C.ident[:, :], dr["c_ident"][:, :], writes=[C.ident_t])
        C.eps5 = es.enter_context(nc.sbuf_tensor("eps5", [128, 1], F32))
        C.eps_t = T("eps5")
        P.op("pool", lambda e: e.memset(C.eps5[:, :], 1e-5), writes=[C.eps_t])
        C.ps_rr = 0
        C.ps_pool_rr = {}
        C.cm = {}
        C.cm_t = {}
        for nm in ("m_incl", "m_rev", "m_caus", "m_incl_gla", "m_rev_gla", "g_pos", "g_negt", "ones"):
            C.cm[nm] = es.enter_context(nc.sbuf_tensor("k_" + nm, [128, 128], F32))
            C.cm_t[nm] = T("c_" + nm)
            P.dma("sp", C.cm[nm][:, :], dr["c_" + nm][:, :], writes=[C.cm_t[nm]])
        C.identb = es.enter_context(nc.sbuf_tensor("identb", [128, 128], BF16))
        C.identb_t = T("identb")
        P.op("dve", lambda e: e.tensor_copy(out=C.identb[:, :], in_=C.ident[:, :]), reads=[C.ident_t], writes=[C.identb_t])
        C.eps6 = es.enter_context(nc.sbuf_tensor("eps6", [128, 1], F32))
        C.eps6_t = T("eps6")
        P.op("pool", lambda e: e.memset(C.eps6[:, :], 1e-6), writes=[C.eps6_t])
        C.ones1 = es.enter_context(nc.sbuf_tensor("ones1", [1, 128], F32))
        C.ones1_t = T("ones1")
        P.op("pool", lambda e: e.memset(C.ones1[:, :], 1.0), writes=[C.ones1_t])
        for t in range(NT):
            P.dma("sp", C.X[:, t, :], dr["x"][t * 128:(t + 1) * 128, :], writes=[C.Xt[t]])
        for l in (layers if layers is not None else range(n_layers)):
            if not dbg.get("skip_mixer"):
                MIXERS[l % 4](C, l)
            if not dbg.get("skip_ffn"):
                ffn_phase(C, l)
        for t in range(NT):
            P.dma("sp", y_out[t * 128:(t + 1) * 128, :], C.X[:, t, :], reads=[C.Xt[t]])
        P.finalize()
    C.P = P
    build.last_prog = P
    return nc


def load_bcast_row(C, dst, dst_t, src_row_ap, eng="sp"):
    n = src_row_ap.shape[-1]
    C.P.dma(eng, dst, src_row_ap.to_broadcast([128, n]), writes=[dst_t])


def layer_norm_tile(C, t, zsrc, G, Bv, G_t, B_t, wk):
    P, nc = C.P, C.nc
    z, z_t, st, st_t, mv, mv_t, sc, sc_t = wk
    xt = C.Xt[t]
    for h, (pap, pT) in enumerate(zsrc):
        sl = slice(h * 512, (h + 1) * 512)
        P.op("dve", lambda e, sl=sl, pap=pap: e.scalar_tensor_tensor(
            out=z[:, sl], in0=C.X[:, t, sl], scalar=ALPHA, in1=pap, op0=ALU.mult, op1=ALU.add),
            reads=[xt, pT], writes=[z_t])
    for h in range(2):
        sl = slice(h * 512, (h + 1) * 512)
        P.op("dve", lambda e, sl=sl, h=h: e.bn_stats(out=st[:, h * 6:(h + 1) * 6], in_=z[:, sl]),
             reads=[z_t], writes=[st_t])
    P.op("dve", lambda e: e.bn_aggr(out=mv[:, 0:2], in_=st[:, 0:12]), reads=[st_t], writes=[mv_t])
    P.op("act", lambda e: e.activation(out=sc[:, 0:1], in_=mv[:, 1:2], func=AF.Ln, bias=C.eps5[:, 0:1], scale=1.0),
         reads=[mv_t, C.eps_t], writes=[sc_t])
    P.op("act", lambda e: e.activation(out=sc[:, 1:2], in_=sc[:, 0:1], func=AF.Exp, scale=-0.5),
         reads=[sc_t], writes=[sc_t])
    P.op("dve", lambda e: e.scalar_tensor_tensor(out=sc[:, 2:3], in0=mv[:, 0:1], scalar=-1.0, in1=sc[:, 1:2],
                                                 op0=ALU.mult, op1=ALU.mult), reads=[mv_t, sc_t], writes=[sc_t])
    P.op("act", lambda e: e.activation(out=z[:, :], in_=z[:, :], func=AF.Identity, bias=sc[:, 2:3], scale=sc[:, 1:2]),
         reads=[z_t, sc_t], writes=[z_t])
    P.op("dve", lambda e: e.tensor_tensor(out=z[:, :], in0=z[:, :], in1=G[:, :], op=ALU.mult),
         reads=[z_t, G_t], writes=[z_t])
    P.op("dve", lambda e: e.tensor_tensor(out=C.X[:, t, :], in0=z[:, :], in1=Bv[:, :], op=ALU.add),
         reads=[z_t, B_t], writes=[xt])


_ALLOC_CTR = [0]


def alloc(C, st, name, shape, dt):
    _ALLOC_CTR[0] += 1
    return st.enter_context(C.nc.sbuf_tensor("%s_%d" % (name, _ALLOC_CTR[0]), list(shape), dt))


def barrier(C):
    C.P.barrier()


def build_xT(C, XT, XT_t, tiles, evac_eng="act"):
    P = C.P
    for t in tiles:
        for half in range(2):
            pi = C.ps_rr % 8
            C.ps_rr += 1
            ps, pt = C.ps[pi], C.pst[pi]
            for c4 in range(4):
                c = half * 4 + c4
                P.op("pe", lambda e, ps=ps, c=c, c4=c4, t=t: e.transpose(
                    out=ps[:, c4 * 128:(c4 + 1) * 128], in_=C.X[:, t, c * 128:(c + 1) * 128], identity=C.ident[:, :]),
                    reads=[C.Xt[t], C.ident_t], writes=[pt])
            dst = XT[:, half * 4:(half + 1) * 4, t * 128:(t + 1) * 128]
            src = ps[:, :].rearrange("p (c n) -> p c n", c=4)
            if evac_eng == "act":
                P.op("act", lambda e, dst=dst, src=src: e.activation(out=dst, in_=src, func=AF.Copy),
                     reads=[pt], writes=[XT_t[t]])
            else:
                P.op("dve", lambda e, dst=dst, src=src: e.tensor_copy(out=dst, in_=src), reads=[pt], writes=[XT_t[t]])


def ffn_phase(C, l):
    P, nc, dr = C.P, C.nc, C.dr
    HG = 256
    NG = FFN_H // HG
    NHC = FFN_H // 128
    with contextlib.ExitStack() as st:
        XT = alloc(C, st, "f_XT", [128, 8, S], BF16)
        XT_t = {t: T("f_XT%d" % t) for t in range(NT)}
        Wd = alloc(C, st, "f_Wd", [128, NHC, D], BF16)
        Wd_t = [T("f_Wd%d" % i) for i in range(NHC)]
        Wg = [alloc(C, st, "f_Wg%d" % i, [128, 8, 2 * HG], BF16) for i in range(2)]
        Wg_t = [(T("f_Wgg%d" % i), T("f_Wgu%d" % i)) for i in range(2)]
        hT = alloc(C, st, "f_hT", [128, NHC, 512], BF16)
        hT_t = [T("f_hT%d" % i) for i in range(NHC)]
        sg = [alloc(C, st, "f_sg%d" % i, [128, 512], F32) for i in range(2)]
        sg_t = [T("f_sg%d" % i) for i in range(2)]
        G = alloc(C, st, "f_G", [128, D], F32); G_t = T("f_G")
        Bv = alloc(C, st, "f_B", [128, D], F32); B_t = T("f_B")
        z = alloc(C, st, "f_z", [128, D], F32); z_t = T("f_z")
        stt = alloc(C, st, "f_st", [128, 12], F32); st_t = T("f_st")
        mv = alloc(C, st, "f_mv", [128, 2], F32); mv_t = T("f_mv")
        sc = alloc(C, st, "f_sc", [128, 4], F32); sc_t = T("f_sc")
        wk = (z, z_t, stt, st_t, mv, mv_t, sc, sc_t)
        load_bcast_row(C, G[:, :], G_t, dr["ln_g"][2 * l + 1:2 * l + 2, :])
        load_bcast_row(C, Bv[:, :], B_t, dr["ln_b"][2 * l + 1:2 * l + 2, :])
        wd_src = dr["ffn_w_down"][l].rearrange("(c p) n -> p c n", p=128)
        for c in range(0, NHC, 2):
            P.dma("pool", Wd[:, c:c + 2, :], wd_src[:, c:c + 2, :], writes=Wd_t[c:c + 2])
        wgu = dr["ffn_w_gu"][l].rearrange("(k p) n -> p k n", p=128)
        gi = 0
        for tb in range(4):
            build_xT(C, XT, XT_t, range(tb * 4, tb * 4 + 4))
            xts = [XT_t[t] for t in range(tb * 4, tb * 4 + 4)]
            for g in range(NG):
                b = gi % 2
                gi += 1
                P.dma("pool", Wg[b][:, :, 0:HG], wgu[:, :, g * HG:(g + 1) * HG], writes=[Wg_t[b][0]])
                P.dma("pool", Wg[b][:, :, HG:2 * HG], wgu[:, :, FFN_H + g * HG:FFN_H + (g + 1) * HG], writes=[Wg_t[b][1]])
                for cc in range(HG // 128):
                    hc = g * (HG // 128) + cc
                    pg_i, pu_i = C.ps_rr % 8, (C.ps_rr + 1) % 8
                    C.ps_rr += 2
                    for (pi, off, wt) in ((pg_i, 0, Wg_t[b][0]), (pu_i, HG, Wg_t[b][1])):
                        for k in range(8):
                            P.op("pe", lambda e, pi=pi, off=off, k=k, b=b, cc=cc, tb=tb: e.matmul(
                                C.ps[pi][:, :], lhsT=Wg[b][:, k, off + cc * 128:off + (cc + 1) * 128],
                                rhs=XT[:, k, tb * 512:(tb + 1) * 512], start=(k == 0), stop=(k == 7)),
                                reads=[wt] + xts, writes=[C.pst[pi]])
                    sb = hc % 2
                    P.op("act", lambda e, sb=sb, pg_i=pg_i: e.activation(out=sg[sb][:, :], in_=C.ps[pg_i][:, :], func=AF.Silu),
                         reads=[C.pst[pg_i]], writes=[sg_t[sb]])
                    P.op("dve", lambda e, sb=sb, pu_i=pu_i, hc=hc: e.tensor_tensor(
                        out=hT[:, hc, :], in0=sg[sb][:, :], in1=C.ps[pu_i][:, :], op=ALU.mult),
                        reads=[sg_t[sb], C.pst[pu_i]], writes=[hT_t[hc]])
            for tt in range(4):
                t = tb * 4 + tt
                zs = []
                for cb in range(2):
                    pi = C.ps_rr % 8
                    C.ps_rr += 1
                    for hc in range(NHC):
                        P.op("pe", lambda e, pi=pi, hc=hc, tt=tt, cb=cb: e.matmul(
                            C.ps[pi][:, :], lhsT=hT[:, hc, tt * 128:(tt + 1) * 128],
                            rhs=Wd[:, hc, cb * 512:(cb + 1) * 512], start=(hc == 0), stop=(hc == NHC - 1)),
                            reads=[hT_t[hc], Wd_t[hc]], writes=[C.pst[pi]])
                    zs.append((C.ps[pi][:, :], C.pst[pi]))
                layer_norm_tile(C, t, zs, G, Bv, G_t, B_t, wk)
        barrier(C)


def next_ps(C, pool=None):
    if pool is None:
        i = C.ps_rr % 8
        C.ps_rr += 1
    else:
        k = C.ps_pool_rr.get(pool, 0)
        C.ps_pool_rr[pool] = k + 1
        i = pool[k % len(pool)]
    return C.ps[i], C.pst[i]


def outproj_phase(C, l, O, O_t, w_out_ap):
    P, nc, dr = C.P, C.nc, C.dr
    with contextlib.ExitStack() as st:
        Wo = alloc(C, st, "o_Wo", [128, 8, D], BF16)
        Wo_t = [T("o_Wo%d" % i) for i in range(8)]
        src = w_out_ap.rearrange("(c p) n -> p c n", p=128)
        for c in range(0, 8, 2):
            P.dma("pool", Wo[:, c:c + 2, :], src[:, c:c + 2, :], writes=Wo_t[c:c + 2])
        G = alloc(C, st, "o_G", [128, D], F32); G_t = T("o_G")
        Bv = alloc(C, st, "o_B", [128, D], F32); B_t = T("o_B")
        z = alloc(C, st, "o_z", [128, D], F32); z_t = T("o_z")
        stt = alloc(C, st, "o_st", [128, 12], F32); st_t = T("o_st")
        mv = alloc(C, st, "o_mv", [128, 2], F32); mv_t = T("o_mv")
        sc = alloc(C, st, "o_sc", [128, 4], F32); sc_t = T("o_sc")
        wk = (z, z_t, stt, st_t, mv, mv_t, sc, sc_t)
        load_bcast_row(C, G[:, :], G_t, dr["ln_g"][2 * l:2 * l + 1, :])
        load_bcast_row(C, Bv[:, :], B_t, dr["ln_b"][2 * l:2 * l + 1, :])
        oT = [alloc(C, st, "o_oT%d" % i, [128, 8, 128], BF16) for i in range(2)]
        oT_t = [T("o_oT%d" % i) for i in range(2)]
        for t in range(NT):
            b = t % 2
            ps, pt = next_ps(C)
            psb = ps[:, :].bitcast(BF16)
            for c in range(8):
                P.op("pe", lambda e, psb=psb, c=c, t=t: e.transpose(
                    out=psb[:, c * 128:(c + 1) * 128], in_=O[:, t, c * 128:(c + 1) * 128], identity=C.identb[:, :]),
                    reads=[O_t[t], C.identb_t], writes=[pt])
            P.op("act", lambda e, psb=psb, b=b: e.activation(
                out=oT[b][:, :, :], in_=psb.rearrange("p (c n) -> p c n", c=8), func=AF.Copy),
                reads=[pt], writes=[oT_t[b]])
            zs = []
            for cb in range(2):
                ps2, pt2 = next_ps(C)
                for c in range(8):
                    P.op("pe", lambda e, ps2=ps2, c=c, cb=cb, b=b: e.matmul(
                        ps2[:, :], lhsT=oT[b][:, c, :], rhs=Wo[:, c, cb * 512:(cb + 1) * 512],
                        start=(c == 0), stop=(c == 7)), reads=[oT_t[b], Wo_t[c]], writes=[pt2])
                zs.append((ps2[:, :], pt2))
            layer_norm_tile(C, t, zs, G, Bv, G_t, B_t, wk)
        barrier(C)


def gla_phase(C, l):
    P, nc, dr = C.P, C.nc, C.dr
    H, DK, DV = 4, 128, 256
    with contextlib.ExitStack() as st:
        O = alloc(C, st, "g_O", [128, NT, D], BF16)
        O_t = [T("g_O%d" % t) for t in range(NT)]
        with contextlib.ExitStack() as st2:
            gla_heads(C, l, st2, O, O_t)
            barrier(C)
        outproj_phase(C, l, O, O_t, dr["gla_w_out"])


def gla_heads(C, l, st, O, O_t):
    P, nc, dr = C.P, C.nc, C.dr
    H, DK, DV = 4, 128, 256
    XT = alloc(C, st, "g_XT", [128, 8, S], BF16)
    XT_t = {t: T("g_XT%d" % t) for t in range(NT)}
    build_xT(C, XT, XT_t, range(NT))
    win = dr["gla_w_in"].rearrange("(k p) n -> p k n", p=128)
    Wlow = alloc(C, st, "g_Wlow", [128, 8, 16], BF16); Wlow_t = T("g_Wlow")
    P.dma("pool", Wlow[:, :, :], win[:, :, 3072:3088], writes=[Wlow_t])
    gkT = alloc(C, st, "g_gkT", [16, S], F32); gkT_t = T("g_gkT")
    for tb in range(4):
        ps, pt = next_ps(C)
        for k in range(8):
            P.op("pe", lambda e, ps=ps, k=k, tb=tb: e.matmul(ps[0:16, :], lhsT=Wlow[:, k, :], rhs=XT[:, k, tb * 512:(tb + 1) * 512],
                                                          start=(k == 0), stop=(k == 7)),
                 reads=[Wlow_t] + [XT_t[t] for t in range(tb * 4, tb * 4 + 4)], writes=[pt])
        P.op("act", lambda e, ps=ps, tb=tb: e.activation(out=gkT[:, tb * 512:(tb + 1) * 512], in_=ps[0:16, :], func=AF.Copy),
             reads=[pt], writes=[gkT_t])
    wgk = alloc(C, st, "g_wgk", [16, 512], F32); wgk_t = T("g_wgk")
    P.dma("sp", wgk[:, :], dr["gla_w_gk"][:, :], writes=[wgk_t])
    bgk = alloc(C, st, "g_bgk", [1, 512], F32); bgk_t = T("g_bgk")
    P.dma("sp", bgk[:, :], dr["gla_b_gk"][:, :], writes=[bgk_t])
    ng = alloc(C, st, "g_ng", [128, DV], F32); ng_t = T("g_ng")
    load_bcast_row(C, ng[:, :], ng_t, dr["gla_norm_g"][0:1, :])
    Wh = [alloc(C, st, "g_Wh%d" % i, [128, 8, 768], BF16) for i in range(2)]
    Wh_t = [[T("g_Wh%d_%d" % (i, j)) for j in range(4)] for i in range(2)]
    state = alloc(C, st, "g_state", [128, DV], F32); state_t = T("g_state")
    state_bf = alloc(C, st, "g_statebf", [128, DV], BF16); statebf_t = T("g_statebf")
    NB = 2
    def mk(name, shape, dt):
        return [alloc(C, st, "%s%d" % (name, i), shape, dt) for i in range(NB)], [T("%s%d" % (name, i)) for i in range(NB)]
    ex, ex_t = mk("g_ex", [128, 128], F32)
    lt, lt_t = mk("g_l", [128, 128], F32)
    ecT, ecT_t = mk("g_ecT", [128, 128], F32)
    eiT, eiT_t = mk("g_eiT", [128, 128], F32)
    eR, eR_t = mk("g_eR", [128, 128], F32)
    qd, qd_t = mk("g_qd", [128, 128], BF16)
    ki, ki_t = mk("g_ki", [128, 128], BF16)
    ke, ke_t = mk("g_ke", [128, 128], BF16)
    vb, vb_t = mk("g_vb", [128, DV], BF16)
    sg, sg_t = mk("g_sg", [128, DV], F32)
    sT, sT_t = mk("g_sT", [128, 128], BF16)
    junk, junk_t = mk("g_junk", [128, DV], F32)
    ss, ss_t = mk("g_ss", [128, 4], F32)
    cols = [(0, 128), (512, 128), (1024, 256), (2048, 256)]
    for h in range(H):
        wb = h % 2
        off = 0
        for j, (base, wdt) in enumerate(cols):
            P.dma("pool", Wh[wb][:, :, off:off + wdt], win[:, :, base + h * wdt:base + (h + 1) * wdt], writes=[Wh_t[wb][j]])
            off += wdt
        P.op("pool", lambda e: e.memset(state[:, :], 0.0), writes=[state_t])
        P.op("pool", lambda e: e.memset(state_bf[:, :], 0.0), writes=[statebf_t])
        for t in range(NT):
            b = t % NB
            tok = slice(t * 128, (t + 1) * 128)
            p1, p1t = next_ps(C)
            for j in range(2):
                for k in range(8):
                    P.op("pe", lambda e, p1=p1, j=j, k=k, wb=wb, tok=tok: e.matmul(
                        p1[:, j * 128:(j + 1) * 128], lhsT=Wh[wb][:, k, j * 128:(j + 1) * 128], rhs=XT[:, k, tok],
                        start=(k == 0), stop=(k == 7)), reads=[Wh_t[wb][j], XT_t[t]], writes=[p1t])
            p2, p2t = next_ps(C)
            for k in range(8):
                P.op("pe", lambda e, p2=p2, k=k, wb=wb, tok=tok: e.matmul(
                    p2[:, 0:384], lhsT=XT[:, k, tok], rhs=Wh[wb][:, k, 128:512], start=(k == 0), stop=(k == 7)),
                    reads=[Wh_t[wb][1], Wh_t[wb][2], XT_t[t]], writes=[p2t])
            p3, p3t = next_ps(C)
            for k in range(8):
                P.op("pe", lambda e, p3=p3, k=k, wb=wb, tok=tok: e.matmul(
                    p3[:, 0:256], lhsT=XT[:, k, tok], rhs=Wh[wb][:, k, 512:768], start=(k == 0), stop=(k == 7)),
                    reads=[Wh_t[wb][3], XT_t[t]], writes=[p3t])
            P.op("pe", lambda e, p3=p3, tok=tok, h=h: e.matmul(
                p3[:, 256:384], lhsT=gkT[:, tok], rhs=wgk[:, h * 128:(h + 1) * 128], start=True, stop=False),
                reads=[gkT_t, wgk_t], writes=[p3t])
            P.op("pe", lambda e, p3=p3, h=h: e.matmul(
                p3[:, 256:384], lhsT=C.ones1[:, :], rhs=bgk[:, h * 128:(h + 1) * 128], start=False, stop=True),
                reads=[C.ones1_t, bgk_t], writes=[p3t])
            P.op("act", lambda e, p3=p3, b=b: e.activation(out=ex[b][:, :], in_=p3[:, 256:384], func=AF.Exp, scale=-1.0),
                 reads=[p3t], writes=[ex_t[b]])
            P.op("act", lambda e, b=b: e.activation(out=lt[b][:, :], in_=ex[b][:, :], func=AF.Ln, bias=1.0, scale=1.0),
                 reads=[ex_t[b]], writes=[lt_t[b]])
            p4, p4t = next_ps(C)
            P.op("pe", lambda e, p4=p4, b=b: e.matmul(p4[:, 0:128], lhsT=lt[b][:, :], rhs=C.cm["m_incl_gla"][:, :], start=True, stop=True),
                 reads=[lt_t[b], C.cm_t["m_incl_gla"]], writes=[p4t])
            P.op("pe", lambda e, p4=p4, b=b: e.matmul(p4[:, 128:256], lhsT=C.cm["m_rev_gla"][:, :], rhs=lt[b][:, :], start=True, stop=True),
                 reads=[lt_t[b], C.cm_t["m_rev_gla"]], writes=[p4t])
            P.op("act", lambda e, p4=p4, b=b: e.activation(out=ecT[b][:, :], in_=p4[:, 0:128], func=AF.Exp),
                 reads=[p4t], writes=[ecT_t[b]])
            P.op("act", lambda e, p4=p4, b=b: e.activation(out=eiT[b][:, :], in_=p4[:, 0:128], func=AF.Exp, scale=-1.0),
                 reads=[p4t], writes=[eiT_t[b]])
            P.op("act", lambda e, p4=p4, b=b: e.activation(out=eR[b][:, :], in_=p4[:, 128:256], func=AF.Exp),
                 reads=[p4t], writes=[eR_t[b]])
            P.op("dve", lambda e, p1=p1, b=b: e.scalar_tensor_tensor(
                out=qd[b][:, :], in0=p1[:, 0:128], scalar=float(DK ** -0.5), in1=ecT[b][:, :], op0=ALU.mult, op1=ALU.mult),
                reads=[p1t, ecT_t[b]], writes=[qd_t[b]])
            P.op("dve", lambda e, p1=p1, b=b: e.tensor_tensor(out=ki[b][:, :], in0=p1[:, 128:256], in1=eiT[b][:, :], op=ALU.mult),
                 reads=[p1t, eiT_t[b]], writes=[ki_t[b]])
            P.op("dve", lambda e, p2=p2, b=b: e.tensor_tensor(out=ke[b][:, :], in0=p2[:, 0:128], in1=eR[b][:, :], op=ALU.mult),
                 reads=[p2t, eR_t[b]], writes=[ke_t[b]])
            P.op("act", lambda e, p2=p2, b=b: e.activation(out=vb[b][:, :], in_=p2[:, 128:384], func=AF.Copy),
                 reads=[p2t], writes=[vb_t[b]])
            P.op("act", lambda e, p3=p3, b=b: e.activation(out=sg[b][:, :], in_=p3[:, 0:256], func=AF.Silu),
                 reads=[p3t], writes=[sg_t[b]])
            P.op("dve", lambda e, b=b: e.tensor_tensor(out=sg[b][:, :], in0=sg[b][:, :], in1=ng[:, :], op=ALU.mult),
                 reads=[sg_t[b], ng_t], writes=[sg_t[b]])
            p5, p5t = next_ps(C)
            P.op("pe", lambda e, p5=p5, b=b: e.matmul(p5[:, 0:128], lhsT=ki[b][:, :], rhs=qd[b][:, :], start=True, stop=True),
                 reads=[ki_t[b], qd_t[b]], writes=[p5t])
            P.op("dve", lambda e, p5=p5, b=b: e.tensor_tensor(out=sT[b][:, :], in0=p5[:, 0:128], in1=C.cm["m_caus"][:, :], op=ALU.mult),
                 reads=[p5t, C.cm_t["m_caus"]], writes=[sT_t[b]])
            p6, p6t = next_ps(C)
            P.op("pe", lambda e, p6=p6, b=b: e.matmul(p6[:, 0:DV], lhsT=sT[b][:, :], rhs=vb[b][:, :], start=True, stop=False),
                 reads=[sT_t[b], vb_t[b]], writes=[p6t])
            P.op("pe", lambda e, p6=p6, b=b: e.matmul(p6[:, 0:DV], lhsT=qd[b][:, :], rhs=state_bf[:, :], start=False, stop=True),
                 reads=[qd_t[b], statebf_t], writes=[p6t])
            p7, p7t = next_ps(C)
            P.op("pe", lambda e, p7=p7, b=b: e.matmul(p7[:, 0:DV], lhsT=ke[b][:, :], rhs=vb[b][:, :], start=True, stop=True),
                 reads=[ke_t[b], vb_t[b]], writes=[p7t])
            P.op("dve", lambda e, p7=p7, b=b: e.scalar_tensor_tensor(
                out=state[:, :], in0=state[:, :], scalar=ecT[b][:, 127:128], in1=p7[:, 0:DV], op0=ALU.mult, op1=ALU.add),
                reads=[state_t, ecT_t[b], p7t], writes=[state_t])
            P.op("act", lambda e: e.activation(out=state_bf[:, :], in_=state[:, :], func=AF.Copy),
                 reads=[state_t], writes=[statebf_t])
            P.op("act", lambda e, p6=p6, b=b: e.activation(out=junk[b][:, :], in_=p6[:, 0:DV], func=AF.Square, accum_out=ss[b][:, 0:1]),
                 reads=[p6t], writes=[junk_t[b], ss_t[b]])
            P.op("act", lambda e, b=b: e.activation(out=ss[b][:, 1:2], in_=ss[b][:, 0:1], func=AF.Ln, bias=C.eps6[:, 0:1], scale=1.0 / DV),
                 reads=[ss_t[b], C.eps6_t], writes=[ss_t[b]])
            P.op("act", lambda e, b=b: e.activation(out=ss[b][:, 2:3], in_=ss[b][:, 1:2], func=AF.Exp, scale=-0.5),
                 reads=[ss_t[b]], writes=[ss_t[b]])
            P.op("dve", lambda e, p6=p6, b=b, t=t, h=h: e.scalar_tensor_tensor(
                out=O[:, t, h * DV:(h + 1) * DV], in0=p6[:, 0:DV], scalar=ss[b][:, 2:3], in1=sg[b][:, :], op0=ALU.mult, op1=ALU.mult),
                reads=[p6t, ss_t[b], sg_t[b]], writes=[O_t[t]])


def moba_phase(C, l):
    P, nc, dr = C.P, C.nc, C.dr
    with contextlib.ExitStack() as st:
        O = alloc(C, st, "m_O", [128, NT, D], BF16)
        O_t = [T("m_O%d" % t) for t in range(NT)]
        with contextlib.ExitStack() as st2:
            moba_heads(C, l, st2, O, O_t)
            barrier(C)
        outproj_phase(C, l, O, O_t, dr["moba_w_out"])


def moba_heads(C, l, st, O, O_t):
    P, nc, dr = C.P, C.nc, C.dr
    H, DH = 8, 128
    PI = float(np.pi)
    XT = alloc(C, st, "m_XT", [128, 8, S], BF16)
    XT_t = {t: T("m_XT%d" % t) for t in range(NT)}
    build_xT(C, XT, XT_t, range(NT))
    win = dr["moba_w_in"].rearrange("(k p) n -> p k n", p=128)
    CT = alloc(C, st, "m_CT", [128, S], F32); CT_t = T("m_CT")
    ST = alloc(C, st, "m_ST", [128, S], F32); ST_t = T("m_ST")
    P.op("pool", lambda e: e.memset(CT[:, :], 1.0), writes=[CT_t])
    P.op("pool", lambda e: e.memset(ST[:, :], 0.0), writes=[ST_t])
    invf = alloc(C, st, "m_invf", [32, 1], F32); invf_t = T("m_invf")
    sgn = alloc(C, st, "m_sgn", [32, 1], F32); sgn_t = T("m_sgn")
    P.dma("sp", invf[:, :], dr["c_invf"][:, :], writes=[invf_t])
    P.dma("sp", sgn[:, :], dr["c_sgn"][:, :], writes=[sgn_t])
    posi = alloc(C, st, "m_posi", [32, 256], I32); posi_t = T("m_posi")
    ang = alloc(C, st, "m_ang", [32, 256], F32); ang_t = T("m_ang")
    r_ = alloc(C, st, "m_r", [32, 256], F32); r_t = T("m_r")
    ki_ = alloc(C, st, "m_ki", [32, 256], I32); ki_t = T("m_ki")
    th = alloc(C, st, "m_th", [32, 256], F32); th_t = T("m_th")
    mk_ = alloc(C, st, "m_mk", [32, 256], F32); mk_t = T("m_mk")
    pos_row = dr["positions"].rearrange("s o -> o s")
    for tb in range(8):
        cs = slice(tb * 256, (tb + 1) * 256)
        P.dma("sp", posi[:, :], pos_row[:, cs].to_broadcast([32, 256]), writes=[posi_t])
        P.op("dve", lambda e: e.tensor_copy(out=ang[:, :], in_=posi[:, :]), reads=[posi_t], writes=[ang_t])
        P.op("dve", lambda e: e.tensor_scalar(out=ang[:, :], in0=ang[:, :], scalar1=invf[:, 0:1], scalar2=None, op0=ALU.mult),
             reads=[ang_t, invf_t], writes=[ang_t])
        for (shift, dst, dst_t) in ((PI / 2, CT, CT_t), (0.0, ST, ST_t)):
            P.op("dve", lambda e, shift=shift: e.tensor_scalar(out=r_[:, :], in0=ang[:, :], scalar1=shift, scalar2=1.0 / (2 * PI),
                                                            op0=ALU.add, op1=ALU.mult), reads=[ang_t], writes=[r_t])
            P.op("dve", lambda e: e.tensor_copy(out=ki_[:, :], in_=r_[:, :]), reads=[r_t], writes=[ki_t])
            P.op("dve", lambda e: e.tensor_copy(out=r_[:, :], in_=ki_[:, :]), reads=[ki_t], writes=[r_t])
            P.op("dve", lambda e: e.scalar_tensor_tensor(out=th[:, :], in0=r_[:, :], scalar=-2 * PI, in1=ang[:, :],
                                                         op0=ALU.mult, op1=ALU.add), reads=[r_t, ang_t], writes=[th_t])
            if shift != 0.0:
                P.op("dve", lambda e, shift=shift: e.tensor_scalar(out=th[:, :], in0=th[:, :], scalar1=shift, scalar2=None, op0=ALU.add),
                     reads=[th_t], writes=[th_t])
            P.op("dve", lambda e: e.tensor_scalar(out=mk_[:, :], in0=th[:, :], scalar1=PI, scalar2=-2 * PI, op0=ALU.is_gt, op1=ALU.mult),
                 reads=[th_t], writes=[mk_t])
            P.op("dve", lambda e: e.tensor_tensor(out=th[:, :], in0=th[:, :], in1=mk_[:, :], op=ALU.add), reads=[th_t, mk_t], writes=[th_t])
            P.op("dve", lambda e: e.tensor_scalar(out=mk_[:, :], in0=th[:, :], scalar1=-PI, scalar2=2 * PI, op0=ALU.is_lt, op1=ALU.mult),
                 reads=[th_t], writes=[mk_t])
            P.op("dve", lambda e: e.tensor_tensor(out=th[:, :], in0=th[:, :], in1=mk_[:, :], op=ALU.add), reads=[th_t, mk_t], writes=[th_t])
            P.op("dve", lambda e: e.tensor_scalar(out=th[:, :], in0=th[:, :], scalar1=-PI, scalar2=PI, op0=ALU.max, op1=ALU.min),
                 reads=[th_t], writes=[th_t])
            P.op("act", lambda e, dst=dst, cs=cs: e.activation(out=dst[0:32, cs], in_=th[:, :], func=AF.Sin), reads=[th_t], writes=[dst_t])
    P.op("dve", lambda e: e.tensor_scalar(out=ST[0:32, :], in0=ST[0:32, :], scalar1=sgn[:, 0:1], scalar2=None, op0=ALU.mult),
         reads=[ST_t, sgn_t], writes=[ST_t])
    if C.dbg.get("moba_stop") == 1:
        return
    Ef = alloc(C, st, "m_Ef", [8, 1024], F32); Ef_t = T("m_Ef")
    P.dma("sp", Ef[:, :], dr["c_E"][:, :], writes=[Ef_t])
    Eb = alloc(C, st, "m_Eb", [8, 1024], BF16); Eb_t = T("m_Eb")
    P.op("dve", lambda e: e.tensor_copy(out=Eb[:, :], in_=Ef[:, :]), reads=[Ef_t], writes=[Eb_t])
    cnf = alloc(C, st, "m_cnf", [128, 128], F32); cnf_t = T("m_cnf")
    P.dma("sp", cnf[:, :], dr["c_causneg"][:, :], writes=[cnf_t])
    cnb = alloc(C, st, "m_cnb", [128, 128], BF16); cnb_t = T("m_cnb")
    P.op("dve", lambda e: e.tensor_copy(out=cnb[:, :], in_=cnf[:, :]), reads=[cnf_t], writes=[cnb_t])
    npast = alloc(C, st, "m_npast", [128, 64], F32); npast_t = T("m_npast")
    P.dma("sp", npast[:, :], dr["c_negpast"][:, :].to_broadcast([128, 64]), writes=[npast_t])
    if C.dbg.get("moba_stop") == 11:
        return
    Wh0 = alloc(C, st, "m_Wh", [128, 8, 640], BF16)
    Wh = [Wh0, Wh0]
    Wh_t0 = [T("m_Wh_%d" % j) for j in range(5)]
    Wh_t = [Wh_t0, Wh_t0]
    P.op("pool", lambda e: e.memset(Wh0[:, :, 384:640], 0.0), writes=[Wh_t0[3], Wh_t0[4]])
    qT0 = alloc(C, st, "m_qT", [128, S], BF16)
    kT0 = alloc(C, st, "m_kT", [128, S], BF16)
    qT = [qT0, qT0]
    kT = [kT0, kT0]
    qT_t0 = [T("m_qT_%d" % tb) for tb in range(4)]
    kT_t0 = [T("m_kT_%d" % tb) for tb in range(4)]
    qT_t = [qT_t0, qT_t0]
    kT_t = [kT_t0, kT_t0]
    vaug = [alloc(C, st, "m_va%d" % i, [128, NT, 130], BF16) for i in range(2)]
    va_t = [[T("m_va%d_%d" % (i, g)) for g in range(4)] for i in range(2)]
    for i in range(2):
        P.op("pool", lambda e, i=i: e.memset(vaug[i][:, :, 128:130], 1.0), writes=va_t[i])
    t1 = [alloc(C, st, "m_t1%d" % i, [128, 512], F32) for i in range(2)]
    t1_t = [T("m_t1%d" % i) for i in range(2)]
    t2 = [alloc(C, st, "m_t2%d" % i, [128, 512], F32) for i in range(2)]
    t2_t = [T("m_t2%d" % i) for i in range(2)]
    km = alloc(C, st, "m_km", [128, 8], F32); km_t = T("m_km")
    kmb = alloc(C, st, "m_kmb", [128, 8], BF16); kmb_t = T("m_kmb")
    NB = 2
    g_ = [alloc(C, st, "m_g%d" % i, [128, 8], F32) for i in range(NB)]; g_t = [T("m_g%d" % i) for i in range(NB)]
    mx = [alloc(C, st, "m_mx%d" % i, [128, 8], F32) for i in range(NB)]; mx_t = [T("m_mx%d" % i) for i in range(NB)]
    sm = [alloc(C, st, "m_sm%d" % i, [128, 8], F32) for i in range(NB)]; sm_t = [T("m_sm%d" % i) for i in range(NB)]
    selT = [alloc(C, st, "m_selT%d" % i, [8, 128], BF16) for i in range(NB)]; selT_t = [T("m_selT%d" % i) for i in range(NB)]
    rec = [alloc(C, st, "m_rec%d" % i, [128, 1], F32) for i in range(NB)]; rec_t = [T("m_rec%d" % i) for i in range(NB)]
    NPB = 4
    PT = [alloc(C, st, "m_PT%d" % i, [128, 128], BF16) for i in range(NPB)]; PT_t = [T("m_PT%d" % i) for i in range(NPB)]
    pt_rr = 0
    scale = float(DH ** -0.5)
    for h in range(H):
        wb = h % 2
        hb = h * 128
        segs = [(0, 128, hb), (128, 128, 1024 + hb), (256, 128, 2048 + hb)]
        for j, (o, w, src) in enumerate(segs):
            P.dma("pool", Wh[wb][:, :, o:o + w], win[:, :, src:src + w], writes=[Wh_t[wb][j]])
        P.dma("pool", Wh[wb][:, :, 384:400], win[:, :, hb + 16:hb + 32], writes=[Wh_t[wb][3]])
        P.dma("pool", Wh[wb][:, :, 400:416], win[:, :, hb:hb + 16], writes=[Wh_t[wb][3]])
        P.dma("pool", Wh[wb][:, :, 512:528], win[:, :, 1024 + hb + 16:1024 + hb + 32], writes=[Wh_t[wb][4]])
        P.dma("pool", Wh[wb][:, :, 528:544], win[:, :, 1024 + hb:1024 + hb + 16], writes=[Wh_t[wb][4]])
        if C.dbg.get("moba_stop") == 12:
            return
        for (dstT, dst_t, co, sw, wj, swj) in ((qT[wb], qT_t[wb], 0, 384, 0, 3), (kT[wb], kT_t[wb], 128, 512, 1, 4)):
            for tb in range(4):
                cs = slice(tb * 512, (tb + 1) * 512)
                xts = [XT_t[t] for t in range(tb * 4, tb * 4 + 4)]
                pq, pqt = next_ps(C, (2, 3, 4, 5, 6, 7))
                for k in range(8):
                    P.op("pe", lambda e, pq=pq, k=k, wb=wb, co=co, cs=cs: e.matmul(
                        pq[:, :], lhsT=Wh[wb][:, k, co:co + 128], rhs=XT[:, k, cs], start=(k == 0), stop=(k == 7)),
                        reads=[Wh_t[wb][wj]] + xts, writes=[pqt])
                pw, pwt = next_ps(C, (2, 3, 4, 5, 6, 7))
                for k in range(8):
                    P.op("pe", lambda e, pw=pw, k=k, wb=wb, sw=sw, cs=cs: e.matmul(
                        pw[:, :], lhsT=Wh[wb][:, k, sw:sw + 128], rhs=XT[:, k, cs], start=(k == 0), stop=(k == 7)),
                        reads=[Wh_t[wb][swj]] + xts, writes=[pwt])
                tbuf = tb % 2
                P.op("dve", lambda e, pq=pq, cs=cs, tbuf=tbuf: e.tensor_tensor(out=t1[tbuf][:, :], in0=pq[:, :], in1=CT[:, cs], op=ALU.mult),
                     reads=[pqt, CT_t], writes=[t1_t[tbuf]])
                P.op("dve", lambda e, pw=pw, cs=cs, tbuf=tbuf: e.tensor_tensor(out=t2[tbuf][:, :], in0=pw[:, :], in1=ST[:, cs], op=ALU.mult),
                     reads=[pwt, ST_t], writes=[t2_t[tbuf]])
                P.op("dve", lambda e, dstT=dstT, cs=cs, tbuf=tbuf: e.tensor_tensor(out=dstT[:, cs], in0=t1[tbuf][:, :], in1=t2[tbuf][:, :], op=ALU.add),
                     reads=[t1_t[tbuf], t2_t[tbuf]], writes=[dst_t[tb]])
        if C.dbg.get("moba_stop") == 2:
            return
        P.op("dve", lambda e, wb=wb: e.tensor_reduce(out=km[:, :], in_=kT[wb][:, :].rearrange("p (n b) -> p n b", b=256), axis=AX.X, op=ALU.add),
             reads=kT_t[wb], writes=[km_t])
        P.op("dve", lambda e: e.tensor_scalar(out=kmb[:, :], in0=km[:, :], scalar1=1.0 / 256.0, scalar2=None, op0=ALU.mult),
             reads=[km_t], writes=[kmb_t])
        for g4 in range(4):
            pv, pvt = next_ps(C, (2, 3, 4, 5, 6, 7))
            for j in range(4):
                t = g4 * 4 + j
                for k in range(8):
                    P.op("pe", lambda e, pv=pv, j=j, k=k, t=t, wb=wb: e.matmul(
                        pv[:, j * 128:(j + 1) * 128], lhsT=XT[:, k, t * 128:(t + 1) * 128], rhs=Wh[wb][:, k, 256:384],
                        start=(k == 0), stop=(k == 7)), reads=[Wh_t[wb][2], XT_t[t]], writes=[pvt])
            P.op("act", lambda e, pv=pv, g4=g4, wb=wb: e.activation(
                out=vaug[wb][:, g4 * 4:(g4 + 1) * 4, 0:128], in_=pv[:, :].rearrange("p (j n) -> p j n", j=4), func=AF.Copy),
                reads=[pvt], writes=[va_t[wb][g4]])
        if C.dbg.get("moba_stop") == 3:
            return
        for qt in range(C.dbg.get("moba_nqt", NT)):
            b = qt % NB
            blk = qt // 2
            qs = slice(qt * 128, (qt + 1) * 128)
            use_gate = blk >= 4
            if use_gate:
                pg, pgt = next_ps(C, (2, 3, 4, 5, 6, 7))
                P.op("pe", lambda e, pg=pg, wb=wb, qs=qs: e.matmul(pg[:, 0:8], lhsT=qT[wb][:, qs], rhs=kmb[:, :], start=True, stop=True),
                     reads=[qT_t[wb][qt // 4], kmb_t], writes=[pgt])
                P.op("dve", lambda e, pg=pg, b=b, blk=blk: e.tensor_tensor(out=g_[b][:, :], in0=pg[:, 0:8], in1=npast[:, blk * 8:(blk + 1) * 8], op=ALU.add),
                     reads=[pgt, npast_t], writes=[g_t[b]])
                P.op("dve", lambda e, b=b: e.max(out=mx[b][:, :], in_=g_[b][:, :]), reads=[g_t[b]], writes=[mx_t[b]])
                P.op("dve", lambda e, b=b: e.tensor_scalar(out=sm[b][:, :], in0=g_[b][:, :], scalar1=mx[b][:, 2:3], scalar2=-1.0,
                                                         op0=ALU.is_ge, op1=ALU.add), reads=[g_t[b], mx_t[b]], writes=[sm_t[b]])
                pt2, pt2t = next_ps(C, (2, 3, 4, 5, 6, 7))
                P.op("pe", lambda e, pt2=pt2, b=b: e.transpose(out=pt2[0:8, 0:128], in_=sm[b][:, 0:8], identity=C.ident[:, :]),
                     reads=[sm_t[b], C.ident_t], writes=[pt2t])
                P.op("act", lambda e, pt2=pt2, b=b: e.activation(out=selT[b][:, :], in_=pt2[0:8, 0:128], func=AF.Copy),
                     reads=[pt2t], writes=[selT_t[b]])
            chunks = list(range(0, 2 * blk)) + ([qt - 1, qt] if qt % 2 == 1 else [qt])
            po, pot = next_ps(C, (0, 1))
            for ci, kc in enumerate(chunks):
                past = kc < 2 * blk
                diag = kc == qt
                ks = slice(kc * 128, (kc + 1) * 128)
                ps_, pst_ = next_ps(C, (2, 3, 4, 5, 6, 7))
                extra = (past and use_gate) or diag
                P.op("pe", lambda e, ps_=ps_, wb=wb, ks=ks, qs=qs, extra=extra: e.matmul(
                    ps_[:, 0:128], lhsT=kT[wb][:, ks], rhs=qT[wb][:, qs], start=True, stop=not extra),
                    reads=[kT_t[wb][kc // 4], qT_t[wb][qt // 4]], writes=[pst_])
                if past and use_gate:
                    n = kc // 2
                    P.op("pe", lambda e, ps_=ps_, n=n, b=b: e.matmul(
                        ps_[:, 0:128], lhsT=Eb[:, n * 128:(n + 1) * 128], rhs=selT[b][:, :], start=False, stop=True),
                        reads=[Eb_t, selT_t[b]], writes=[pst_])
                if diag:
                    P.op("pe", lambda e, ps_=ps_: e.matmul(ps_[:, 0:128], lhsT=C.identb[:, :], rhs=cnb[:, :], start=False, stop=True),
                         reads=[C.identb_t, cnb_t], writes=[pst_])
                pb = pt_rr % NPB
                pt_rr += 1
                P.op("act", lambda e, ps_=ps_, pb=pb: e.activation(out=PT[pb][:, :], in_=ps_[:, 0:128], func=AF.Exp, scale=scale),
                     reads=[pst_], writes=[PT_t[pb]])
                P.op("pe", lambda e, po=po, pb=pb, wb=wb, kc=kc, ci=ci, nch=len(chunks): e.matmul(
                    po[:, 0:129], lhsT=PT[pb][:, :], rhs=vaug[wb][:, kc, 0:129], start=(ci == 0), stop=(ci == nch - 1)),
                    reads=[PT_t[pb], va_t[wb][kc // 4]], writes=[pot])
            P.op("dve", lambda e, po=po, b=b: e.reciprocal(out=rec[b][:, :], in_=po[:, 128:129]), reads=[pot], writes=[rec_t[b]])
            P.op("dve", lambda e, po=po, b=b, qt=qt, hb=hb: e.tensor_scalar(
                out=O[:, qt, hb:hb + 128], in0=po[:, 0:128], scalar1=rec[b][:, 0:1], scalar2=None, op0=ALU.mult),
                reads=[pot, rec_t[b]], writes=[O_t[qt]])


def gdn_phase(C, l):
    P, nc, dr = C.P, C.nc, C.dr
    with contextlib.ExitStack() as st:
        O = alloc(C, st, "d_O", [128, NT, D], BF16)
        O_t = [T("d_O%d" % t) for t in range(NT)]
        with contextlib.ExitStack() as st2:
            gdn_heads(C, l, st2, O, O_t)
            barrier(C)
        outproj_phase(C, l, O, O_t, dr["gdn_w_out"])


def gdn_heads(C, l, st, O, O_t):
    P, nc, dr = C.P, C.nc, C.dr
    H, DK = 8, 128
    XT = alloc(C, st, "d_XT", [128, 8, S], BF16)
    XT_t = {t: T("d_XT%d" % t) for t in range(NT)}
    build_xT(C, XT, XT_t, range(NT))
    allx = [XT_t[t] for t in range(NT)]
    win = dr["gdn_w_in"].rearrange("(k p) n -> p k n", p=128)
    PS6 = (2, 3, 4, 5, 6, 7)
    Wab = alloc(C, st, "d_Wab", [128, 8, 16], BF16); Wab_t = T("d_Wab")
    P.dma("pool", Wab[:, :, :], win[:, :, 4096:4112], writes=[Wab_t])
    alog = alloc(C, st, "d_alog", [128, 8], F32); alog_t = T("d_alog")
    dtb = alloc(C, st, "d_dtb", [128, 8], F32); dtb_t = T("d_dtb")
    P.dma("sp", alog[:, :], dr["gdn_a_log"][0:1, :].to_broadcast([128, 8]), writes=[alog_t])
    P.dma("sp", dtb[:, :], dr["gdn_dt_bias"][0:1, :].to_broadcast([128, 8]), writes=[dtb_t])
    P.op("act", lambda e: e.activation(out=alog[:, :], in_=alog[:, :], func=AF.Exp), reads=[alog_t], writes=[alog_t])
    ng = alloc(C, st, "d_ng", [128, 128], F32); ng_t = T("d_ng")
    load_bcast_row(C, ng[:, :], ng_t, dr["gdn_norm_g"][0:1, :])
    def sc(name):
        return alloc(C, st, name, [128, NT, 8], F32), T(name)
    g_all, g_t = sc("d_g"); beta, beta_t = sc("d_beta"); cum, cum_t = sc("d_cum"); tot, tot_t = sc("d_tot")
    ecum, ecum_t = sc("d_ecum"); ncum, ncum_t = sc("d_ncum"); bec, bec_t = sc("d_bec"); eend, eend_t = sc("d_eend")
    ecl, ecl_t = sc("d_ecl"); tmp, tmp_t = sc("d_tmp")
    pab, pabt = next_ps(C)
    for t in range(NT):
        for k in range(8):
            P.op("pe", lambda e, t=t, k=k: e.matmul(pab[:, t * 16:(t + 1) * 16], lhsT=XT[:, k, t * 128:(t + 1) * 128], rhs=Wab[:, k, :],
                                                    start=(k == 0), stop=(k == 7)), reads=[XT_t[t], Wab_t], writes=[pabt])
    pab3 = pab[:, 0:256].rearrange("p (t c) -> p t c", c=16)
    for t in range(NT):
        P.op("dve", lambda e, t=t: e.tensor_tensor(out=tmp[:, t, :], in0=pab3[:, t, 0:8], in1=dtb[:, :], op=ALU.add),
             reads=[pabt, dtb_t], writes=[tmp_t])
    P.op("act", lambda e: e.activation(out=tmp[:, :, :], in_=tmp[:, :, :], func=AF.Exp), reads=[tmp_t], writes=[tmp_t])
    P.op("act", lambda e: e.activation(out=tmp[:, :, :], in_=tmp[:, :, :], func=AF.Ln, bias=1.0, scale=1.0), reads=[tmp_t], writes=[tmp_t])
    for t in range(NT):
        P.op("dve", lambda e, t=t: e.scalar_tensor_tensor(out=g_all[:, t, :], in0=tmp[:, t, :], scalar=-1.0, in1=alog[:, :],
                                                          op0=ALU.mult, op1=ALU.mult), reads=[tmp_t, alog_t], writes=[g_t])
    P.op("act", lambda e: e.activation(out=beta[:, :, :], in_=pab3[:, :, 8:16], func=AF.Sigmoid), reads=[pabt], writes=[beta_t])
    pcm, pcmt = next_ps(C)
    for t in range(NT):
        P.op("pe", lambda e, t=t: e.matmul(pcm[:, t * 8:(t + 1) * 8], lhsT=C.cm["m_incl"][:, :], rhs=g_all[:, t, :], start=True, stop=True),
             reads=[g_t, C.cm_t["m_incl"]], writes=[pcmt])
        P.op("pe", lambda e, t=t: e.matmul(pcm[:, 128 + t * 8:128 + (t + 1) * 8], lhsT=C.cm["ones"][:, :], rhs=g_all[:, t, :], start=True, stop=True),
             reads=[g_t, C.cm_t["ones"]], writes=[pcmt])
    P.op("act", lambda e: e.activation(out=cum[:, :, :], in_=pcm[:, 0:128].rearrange("p (t c) -> p t c", c=8), func=AF.Copy),
         reads=[pcmt], writes=[cum_t])
    P.op("act", lambda e: e.activation(out=tot[:, :, :], in_=pcm[:, 128:256].rearrange("p (t c) -> p t c", c=8), func=AF.Copy),
         reads=[pcmt], writes=[tot_t])
    P.op("act", lambda e: e.activation(out=ecum[:, :, :], in_=cum[:, :, :], func=AF.Exp), reads=[cum_t], writes=[ecum_t])
    P.op("act", lambda e: e.activation(out=ecl[:, :, :], in_=tot[:, :, :], func=AF.Exp), reads=[tot_t], writes=[ecl_t])
    P.op("dve", lambda e: e.tensor_scalar(out=ncum[:, :, :], in0=cum[:, :, :], scalar1=-1.0, scalar2=None, op0=ALU.mult),
         reads=[cum_t], writes=[ncum_t])
    P.op("dve", lambda e: e.tensor_tensor(out=bec[:, :, :], in0=beta[:, :, :], in1=ecum[:, :, :], op=ALU.mult),
         reads=[beta_t, ecum_t], writes=[bec_t])
    P.op("dve", lambda e: e.tensor_tensor(out=eend[:, :, :], in0=tot[:, :, :], in1=cum[:, :, :], op=ALU.subtract),
         reads=[tot_t, cum_t], writes=[eend_t])
    P.op("act", lambda e: e.activation(out=eend[:, :, :], in_=eend[:, :, :], func=AF.Exp), reads=[eend_t], writes=[eend_t])
    sc_reads = [g_t, beta_t, cum_t, ecum_t, ncum_t, bec_t, eend_t, ecl_t]
    if C.dbg.get("gdn_stop") == 1:
        return
    Wh = alloc(C, st, "d_Wh", [128, 8, 512], BF16)
    Wh_t = [T("d_Wh_%d" % j) for j in range(4)]
    cw = alloc(C, st, "d_cw", [128, 12], F32); cw_t = T("d_cw")
    pre = alloc(C, st, "d_pre", [128, S + 3], F32); pre_t = T("d_pre")
    P.op("pool", lambda e: e.memset(pre[:, 0:3], 0.0), writes=[pre_t])
    yb = alloc(C, st, "d_y", [128, S], F32); yb_t = T("d_y")
    vT = alloc(C, st, "d_vT", [128, S], F32); vT_t = T("d_vT")
    qTn = alloc(C, st, "d_qTn", [128, S], BF16); qTn_t = T("d_qTn")
    kTn = alloc(C, st, "d_kTn", [128, S], BF16); kTn_t = T("d_kTn")
    rs = alloc(C, st, "d_rs", [128, 512], F32); rs_t = T("d_rs")
    Sst = alloc(C, st, "d_S", [128, 128], F32); S_t = T("d_S")
    Sbf = alloc(C, st, "d_Sbf", [128, 128], BF16); Sbf_t = T("d_Sbf")
    def mk(name, shape, dt, n=2):
        return [alloc(C, st, "%s%d" % (name, i), shape, dt) for i in range(n)], [T("%s%d" % (name, i)) for i in range(n)]
    Dm, Dm_t = mk("d_Dm", [128, 128], F32)
    DTm, DTm_t = mk("d_DTm", [128, 128], F32)
    Lm, Lm_t = mk("d_L", [128, 128], F32)
    LT, LT_t = mk("d_LT", [128, 128], F32)
    Pa, Pa_t = mk("d_Pa", [128, 128], F32)
    PTa, PTa_t = mk("d_PTa", [128, 128], F32)
    Pb, Pb_t = mk("d_Pb", [128, 128], F32)
    PTb, PTb_t = mk("d_PTb", [128, 128], F32)
    qkT, qkT_t = mk("d_qkT", [128, 128], BF16)
    kend, kend_t = mk("d_kend", [128, 128], BF16)
    Xa, Xa_t = mk("d_Xa", [128, 256], F32)
    Xb, Xb_t = mk("d_Xb", [128, 256], F32)
    wT, wT_t = mk("d_wT", [128, 128], BF16)
    vnew, vnew_t = mk("d_vnew", [128, 128], BF16)
    intra, intra_t = mk("d_intra", [128, 128], F32)
    osb, osb_t = mk("d_osb", [128, 128], F32)
    sg, sg_t = mk("d_sg", [128, 128], F32)
    junk, junk_t = mk("d_junk", [128, 128], F32)
    ss, ss_t = mk("d_ss", [128, 4], F32)
    cwsrc = dr["gdn_conv_w"].rearrange("j c -> c j")
    for h in range(H):
        hb = h * 128
        for j in range(4):
            P.dma("pool", Wh[:, :, j * 128:(j + 1) * 128], win[:, :, j * 1024 + hb:j * 1024 + hb + 128], writes=[Wh_t[j]])
        for s3 in range(3):
            P.dma("sp", cw[:, s3 * 4:(s3 + 1) * 4], cwsrc[s3 * 1024 + hb:s3 * 1024 + hb + 128, :], writes=[cw_t],
                  allow_slow_non_contiguous=True)
        P.op("pool", lambda e: e.memset(Sst[:, :], 0.0), writes=[S_t])
        P.op("pool", lambda e: e.memset(Sbf[:, :], 0.0), writes=[Sbf_t])
        for s3 in range(3):
            for tb in range(4):
                cs = slice(tb * 512, (tb + 1) * 512)
                pp, ppt = next_ps(C)
                for k in range(8):
                    P.op("pe", lambda e, pp=pp, k=k, s3=s3, cs=cs: e.matmul(
                        pp[:, :], lhsT=Wh[:, k, s3 * 128:(s3 + 1) * 128], rhs=XT[:, k, cs], start=(k == 0), stop=(k == 7)),
                        reads=[Wh_t[s3]] + allx[tb * 4:tb * 4 + 4], writes=[ppt])
                P.op("act", lambda e, pp=pp, tb=tb: e.activation(out=pre[:, 3 + tb * 512:3 + (tb + 1) * 512], in_=pp[:, :], func=AF.Copy),
                     reads=[ppt], writes=[pre_t])
            dst, dst_t = (vT, vT_t) if s3 == 2 else (yb, yb_t)
            P.op("dve", lambda e, dst=dst, s3=s3: e.tensor_scalar(out=dst[:, :], in0=pre[:, 3:3 + S], scalar1=cw[:, s3 * 4 + 3:s3 * 4 + 4],
                                                                 scalar2=None, op0=ALU.mult), reads=[pre_t, cw_t], writes=[dst_t])
            for j in range(3):
                P.op("dve", lambda e, dst=dst, s3=s3, j=j: e.scalar_tensor_tensor(
                    out=dst[:, :], in0=pre[:, j:j + S], scalar=cw[:, s3 * 4 + j:s3 * 4 + j + 1], in1=dst[:, :], op0=ALU.mult, op1=ALU.add),
                    reads=[pre_t, cw_t, dst_t], writes=[dst_t])
            P.op("act", lambda e, dst=dst: e.activation(out=dst[:, :], in_=dst[:, :], func=AF.Silu), reads=[dst_t], writes=[dst_t])
            if s3 < 2:
                outT, outT_t = (qTn, qTn_t) if s3 == 0 else (kTn, kTn_t)
                scl = float(DK ** -0.5) if s3 == 0 else 1.0
                P.op("dve", lambda e: e.tensor_tensor(out=pre[:, 3:3 + S], in0=yb[:, :], in1=yb[:, :], op=ALU.mult),
                     reads=[yb_t, pre_t], writes=[pre_t])
                for tb in range(4):
                    cs = slice(tb * 512, (tb + 1) * 512)
                    pq, pqt = next_ps(C)
                    P.op("pe", lambda e, pq=pq, tb=tb: e.matmul(pq[:, :], lhsT=C.cm["ones"][:, :], rhs=pre[:, 3 + tb * 512:3 + (tb + 1) * 512],
                                                               start=True, stop=True), reads=[pre_t, C.cm_t["ones"]], writes=[pqt])
                    P.op("act", lambda e, pq=pq: e.activation(out=rs[:, :], in_=pq[:, :], func=AF.Ln, bias=C.eps6[:, 0:1], scale=1.0),
                         reads=[pqt, C.eps6_t], writes=[rs_t])
                    P.op("act", lambda e: e.activation(out=rs[:, :], in_=rs[:, :], func=AF.Exp, scale=-0.5), reads=[rs_t], writes=[rs_t])
                    P.op("dve", lambda e, outT=outT, cs=cs, scl=scl: e.scalar_tensor_tensor(
                        out=outT[:, cs], in0=yb[:, cs], scalar=scl, in1=rs[:, :], op0=ALU.mult, op1=ALU.mult),
                        reads=[yb_t, rs_t], writes=[outT_t])
        if C.dbg.get("gdn_stop") == 2:
            return
        for t in range(C.dbg.get("gdn_nt", NT)):
            b = t % 2
            tok = slice(t * 128, (t + 1) * 128)
            col = lambda arr: arr[:, t, h:h + 1]
            pa, pat = next_ps(C)
            P.op("pe", lambda e, pa=pa, tok=tok: e.matmul(pa[:, 0:128], lhsT=kTn[:, tok], rhs=kTn[:, tok], start=True, stop=True),
                 reads=[kTn_t], writes=[pat])
            P.op("pe", lambda e, pa=pa, tok=tok: e.matmul(pa[:, 128:256], lhsT=kTn[:, tok], rhs=qTn[:, tok], start=True, stop=True),
                 reads=[kTn_t, qTn_t], writes=[pat])
            pb, pbt = next_ps(C)
            gbc = g_all[:, t, h:h + 1].to_broadcast([128, 128])
            for (o, mname) in ((0, "g_pos"), (128, "g_negt")):
                P.op("pe", lambda e, pb=pb, o=o, gbc=gbc: e.matmul(pb[:, o:o + 128], lhsT=gbc, rhs=C.cm["m_incl"][:, :], start=True, stop=False),
                     reads=[g_t, C.cm_t["m_incl"]], writes=[pbt])
                P.op("pe", lambda e, pb=pb, o=o, mname=mname: e.matmul(pb[:, o:o + 128], lhsT=C.ident[:, :], rhs=C.cm[mname][:, :], start=False, stop=True),
                     reads=[C.ident_t, C.cm_t[mname]], writes=[pbt])
            pc, pct = next_ps(C)
            P.op("pe", lambda e, pc=pc, tok=tok: e.transpose(out=pc[:, 0:128], in_=vT[:, tok], identity=C.ident[:, :]),
                 reads=[vT_t, C.ident_t], writes=[pct])
            pd, pdt = next_ps(C)
            pdb = pd[:, :].bitcast(BF16)
            P.op("pe", lambda e, pdb=pdb, tok=tok: e.transpose(out=pdb[:, 0:128], in_=kTn[:, tok], identity=C.identb[:, :]),
                 reads=[kTn_t, C.identb_t], writes=[pdt])
            P.op("act", lambda e, pb=pb, b=b, t=t, h=h: e.activation(out=Dm[b][:, :], in_=pb[:, 0:128], func=AF.Exp, bias=cum[:, t, h:h + 1], scale=-1.0),
                 reads=[pbt, cum_t], writes=[Dm_t[b]])
            P.op("act", lambda e, pb=pb, b=b, t=t, h=h: e.activation(out=DTm[b][:, :], in_=pb[:, 128:256], func=AF.Exp, bias=ncum[:, t, h:h + 1], scale=1.0),
                 reads=[pbt, ncum_t], writes=[DTm_t[b]])
            P.op("dve", lambda e, pa=pa, b=b, t=t, h=h: e.scalar_tensor_tensor(
                out=Lm[b][:, :], in0=pa[:, 0:128], scalar=beta[:, t, h:h + 1], in1=Dm[b][:, :], op0=ALU.mult, op1=ALU.mult),
                reads=[pat, beta_t, Dm_t[b]], writes=[Lm_t[b]])
            P.op("dve", lambda e, pa=pa, b=b: e.tensor_tensor(out=qkT[b][:, :], in0=pa[:, 128:256], in1=DTm[b][:, :], op=ALU.mult),
                 reads=[pat, DTm_t[b]], writes=[qkT_t[b]])
            P.op("dve", lambda e, pc=pc, b=b, t=t, h=h: e.tensor_scalar(out=Xa[b][:, 0:128], in0=pc[:, 0:128], scalar1=beta[:, t, h:h + 1],
                                                                     scalar2=None, op0=ALU.mult), reads=[pct, beta_t], writes=[Xa_t[b]])
            P.op("dve", lambda e, pdb=pdb, b=b, t=t, h=h: e.tensor_scalar(out=Xa[b][:, 128:256], in0=pdb[:, 0:128], scalar1=bec[:, t, h:h + 1],
                                                                      scalar2=None, op0=ALU.mult), reads=[pdt, bec_t], writes=[Xa_t[b]])
            P.op("act", lambda e, pdb=pdb, b=b, t=t, h=h: e.activation(out=kend[b][:, :], in_=pdb[:, 0:128], func=AF.Copy, scale=eend[:, t, h:h + 1]),
                 reads=[pdt, eend_t], writes=[kend_t[b]])
            if C.dbg.get("gdn_stop") == 3:
                return
            pe_, pet = next_ps(C)
            P.op("pe", lambda e, pe_=pe_, b=b: e.transpose(out=pe_[:, 0:128], in_=Lm[b][:, :], identity=C.ident[:, :]),
                 reads=[Lm_t[b], C.ident_t], writes=[pet])
            P.op("act", lambda e, pe_=pe_, b=b: e.activation(out=LT[b][:, :], in_=pe_[:, 0:128], func=AF.Copy), reads=[pet], writes=[LT_t[b]])
            if C.dbg.get("gdn_stop") == 5:
                return
            px, pxt = next_ps(C)
            P.op("pe", lambda e, px=px, b=b: e.matmul(px[:, 0:256], lhsT=LT[b][:, :], rhs=Xa[b][:, :], start=True, stop=True),
                 reads=[LT_t[b], Xa_t[b]], writes=[pxt])
            P.op("dve", lambda e, px=px, b=b: e.tensor_tensor(out=Xb[b][:, :], in0=Xa[b][:, :], in1=px[:, 0:256], op=ALU.subtract),
                 reads=[Xa_t[b], pxt], writes=[Xb_t[b]])
            if C.dbg.get("gdn_stop") == 6:
                return
            Xc, Xc_t, Xn, Xn_t = Xb[b], Xb_t[b], Xa[b], Xa_t[b]
            Pc, Pc_t, PTc, PTc_t = Lm[b], Lm_t[b], LT[b], LT_t[b]
            for lev in range(1, C.dbg.get("gdn_lev", 7)):
                if lev % 2 == 1:
                    Pn, Pn_t, PTn, PTn_t = Pa[b], Pa_t[b], PTa[b], PTa_t[b]
                else:
                    Pn, Pn_t, PTn, PTn_t = Pb[b], Pb_t[b], PTb[b], PTb_t[b]
                pp2, pp2t = next_ps(C)
                P.op("pe", lambda e, pp2=pp2, Pc=Pc, PTc=PTc: e.matmul(pp2[:, 0:128], lhsT=Pc[:, :], rhs=PTc[:, :], start=True, stop=True),
                     reads=[Pc_t, PTc_t], writes=[pp2t])
                if lev < 6 and C.dbg.get("gdn_var") != 2:
                    P.op("pe", lambda e, pp2=pp2, Pc=Pc, PTc=PTc: e.matmul(pp2[:, 128:256], lhsT=PTc[:, :], rhs=Pc[:, :], start=True, stop=True),
                         reads=[Pc_t, PTc_t], writes=[pp2t])
                P.op("act", lambda e, pp2=pp2, PTn=PTn: e.activation(out=PTn[:, :], in_=pp2[:, 0:128], func=AF.Copy), reads=[pp2t], writes=[PTn_t])
                if lev < 6 and C.dbg.get("gdn_var") != 2:
                    P.op("dve", lambda e, pp2=pp2, Pn=Pn: e.tensor_scalar(out=Pn[:, :], in0=pp2[:, 128:256], scalar1=1.0, scalar2=None, op0=ALU.mult),
                         reads=[pp2t], writes=[Pn_t])
                if C.dbg.get("gdn_var") == 1:
                    continue
                px2, px2t = next_ps(C)
                P.op("pe", lambda e, px2=px2, PTn=PTn, Xc=Xc: e.matmul(px2[:, 0:256], lhsT=PTn[:, :], rhs=Xc[:, :], start=True, stop=True),
                     reads=[PTn_t, Xc_t], writes=[px2t])
                P.op("dve", lambda e, px2=px2, Xc=Xc, Xn=Xn: e.tensor_tensor(out=Xn[:, :], in0=Xc[:, :], in1=px2[:, 0:256], op=ALU.add),
                     reads=[Xc_t, px2t], writes=[Xn_t])
                Xc, Xc_t, Xn, Xn_t = Xn, Xn_t, Xc, Xc_t
                Pc, Pc_t, PTc, PTc_t = Pn, Pn_t, PTn, PTn_t
            if C.dbg.get("gdn_stop") == 4:
                return
            pw_, pwt_ = next_ps(C)
            P.op("pe", lambda e, pw_=pw_, Xc=Xc: e.transpose(out=pw_[:, 0:128], in_=Xc[:, 128:256], identity=C.ident[:, :]),
                 reads=[Xc_t, C.ident_t], writes=[pwt_])
            P.op("act", lambda e, pw_=pw_, b=b: e.activation(out=wT[b][:, :], in_=pw_[:, 0:128], func=AF.Copy), reads=[pwt_], writes=[wT_t[b]])
            pv, pvt = next_ps(C)
            P.op("pe", lambda e, pv=pv, b=b: e.matmul(pv[:, 0:128], lhsT=wT[b][:, :], rhs=Sbf[:, :], start=True, stop=True),
                 reads=[wT_t[b], Sbf_t], writes=[pvt])
            P.op("dve", lambda e, pv=pv, b=b, Xc=Xc: e.tensor_tensor(out=vnew[b][:, :], in0=Xc[:, 0:128], in1=pv[:, 0:128], op=ALU.subtract),
                 reads=[Xc_t, pvt], writes=[vnew_t[b]])
            po, pot = next_ps(C)
            P.op("pe", lambda e, po=po, b=b: e.matmul(po[:, 0:128], lhsT=qkT[b][:, :], rhs=vnew[b][:, :], start=True, stop=True),
                 reads=[qkT_t[b], vnew_t[b]], writes=[pot])
            P.op("pe", lambda e, po=po, tok=tok: e.matmul(po[:, 128:256], lhsT=qTn[:, tok], rhs=Sbf[:, :], start=True, stop=True),
                 reads=[qTn_t, Sbf_t], writes=[pot])
            P.op("act", lambda e, po=po, b=b: e.activation(out=intra[b][:, :], in_=po[:, 0:128], func=AF.Copy), reads=[pot], writes=[intra_t[b]])
            P.op("dve", lambda e, po=po, b=b, t=t, h=h: e.scalar_tensor_tensor(
                out=osb[b][:, :], in0=po[:, 128:256], scalar=ecum[:, t, h:h + 1], in1=intra[b][:, :], op0=ALU.mult, op1=ALU.add),
                reads=[pot, ecum_t, intra_t[b]], writes=[osb_t[b]])
            pu, put = next_ps(C)
            P.op("pe", lambda e, pu=pu, b=b: e.matmul(pu[:, 0:128], lhsT=kend[b][:, :], rhs=vnew[b][:, :], start=True, stop=True),
                 reads=[kend_t[b], vnew_t[b]], writes=[put])
            P.op("dve", lambda e, pu=pu, t=t, h=h: e.scalar_tensor_tensor(
                out=Sst[:, :], in0=Sst[:, :], scalar=ecl[:, t, h:h + 1], in1=pu[:, 0:128], op0=ALU.mult, op1=ALU.add),
                reads=[S_t, ecl_t, put], writes=[S_t])
            P.op("act", lambda e: e.activation(out=Sbf[:, :], in_=Sst[:, :], func=AF.Copy), reads=[S_t], writes=[Sbf_t])
            pz, pzt = next_ps(C)
            for k in range(8):
                P.op("pe", lambda e, pz=pz, k=k, tok=tok: e.matmul(pz[:, 0:128], lhsT=XT[:, k, tok], rhs=Wh[:, k, 384:512], start=(k == 0), stop=(k == 7)),
                     reads=[XT_t[t], Wh_t[3]], writes=[pzt])
            P.op("act", lambda e, pz=pz, b=b: e.activation(out=sg[b][:, :], in_=pz[:, 0:128], func=AF.Silu), reads=[pzt], writes=[sg_t[b]])
            P.op("dve", lambda e, b=b: e.tensor_tensor(out=sg[b][:, :], in0=sg[b][:, :], in1=ng[:, :], op=ALU.mult),
                 reads=[sg_t[b], ng_t], writes=[sg_t[b]])
            P.op("act", lambda e, b=b: e.activation(out=junk[b][:, :], in_=osb[b][:, :], func=AF.Square, accum_out=ss[b][:, 0:1]),
                 reads=[osb_t[b]], writes=[junk_t[b], ss_t[b]])
            P.op("act", lambda e, b=b: e.activation(out=ss[b][:, 1:2], in_=ss[b][:, 0:1], func=AF.Ln, bias=C.eps6[:, 0:1], scale=1.0 / 128),
                 reads=[ss_t[b], C.eps6_t], writes=[ss_t[b]])
            P.op("act", lambda e, b=b: e.activation(out=ss[b][:, 2:3], in_=ss[b][:, 1:2], func=AF.Exp, scale=-0.5),
                 reads=[ss_t[b]], writes=[ss_t[b]])
            P.op("dve", lambda e, b=b, t=t, hb=hb: e.scalar_tensor_tensor(
                out=O[:, t, hb:hb + 128], in0=osb[b][:, :], scalar=ss[b][:, 2:3], in1=sg[b][:, :], op0=ALU.mult, op1=ALU.mult),
                reads=[osb_t[b], ss_t[b], sg_t[b]], writes=[O_t[t]])


def hgrn_phase(C, l):
    P, nc, dr = C.P, C.nc, C.dr
    with contextlib.ExitStack() as st:
        O = alloc(C, st, "h_O", [128, NT, D], BF16)
        O_t = [T("h_O%d" % t) for t in range(NT)]
        with contextlib.ExitStack() as st2:
            hgrn_heads(C, l, st2, O, O_t)
            barrier(C)
        outproj_phase(C, l, O, O_t, dr["hgrn_w_out"])


def hgrn_lb(C, st, l, src_ap, shape, name):
    P = C.P
    pn, n = shape
    hb = alloc(C, st, name + "_hb", [pn, 4, n], F32); hb_t = T(name + "_hb")
    P.dma("sp", hb[:, :, :], src_ap, writes=[hb_t], allow_slow_non_contiguous=True)
    mx = alloc(C, st, name + "_mx", [pn, n], F32); mx_t = T(name + "_mx")
    P.op("dve", lambda e: e.tensor_tensor(out=mx[:, :], in0=hb[:, 0, :], in1=hb[:, 1, :], op=ALU.max), reads=[hb_t], writes=[mx_t])
    for j in (2, 3):
        P.op("dve", lambda e, j=j: e.tensor_tensor(out=mx[:, :], in0=mx[:, :], in1=hb[:, j, :], op=ALU.max), reads=[hb_t, mx_t], writes=[mx_t])
    for j in range(4):
        P.op("dve", lambda e, j=j: e.tensor_tensor(out=hb[:, j, :], in0=hb[:, j, :], in1=mx[:, :], op=ALU.subtract), reads=[hb_t, mx_t], writes=[hb_t])
    P.op("act", lambda e: e.activation(out=hb[:, :, :], in_=hb[:, :, :], func=AF.Exp), reads=[hb_t], writes=[hb_t])
    den = mx
    P.op("dve", lambda e: e.tensor_tensor(out=den[:, :], in0=hb[:, 0, :], in1=hb[:, 1, :], op=ALU.add), reads=[hb_t, mx_t], writes=[mx_t])
    for j in (2, 3):
        P.op("dve", lambda e, j=j: e.tensor_tensor(out=den[:, :], in0=den[:, :], in1=hb[:, j, :], op=ALU.add), reads=[hb_t, mx_t], writes=[mx_t])
    P.op("dve", lambda e: e.reciprocal(out=den[:, :], in_=den[:, :]), reads=[mx_t], writes=[mx_t])
    lb = alloc(C, st, name + "_lb", [pn, n], F32); lb_t = T(name + "_lb")
    oml = alloc(C, st, name + "_oml", [pn, n], F32)
    if l == 0:
        P.op("dve", lambda e: e.memset(lb[:, :], 0.0), writes=[lb_t])
    else:
        P.op("dve", lambda e: e.tensor_copy(out=lb[:, :], in_=hb[:, 1, :]), reads=[hb_t], writes=[lb_t])
        for j in range(2, l + 1):
            P.op("dve", lambda e, j=j: e.tensor_tensor(out=lb[:, :], in0=lb[:, :], in1=hb[:, j, :], op=ALU.add), reads=[hb_t, lb_t], writes=[lb_t])
        P.op("dve", lambda e: e.tensor_tensor(out=lb[:, :], in0=lb[:, :], in1=den[:, :], op=ALU.mult), reads=[mx_t, lb_t], writes=[lb_t])
    P.op("dve", lambda e: e.tensor_scalar(out=oml[:, :], in0=lb[:, :], scalar1=-1.0, scalar2=1.0, op0=ALU.mult, op1=ALU.add),
         reads=[lb_t], writes=[lb_t])
    return lb, oml, lb_t


def hgrn_heads(C, l, st, O, O_t):
    P, nc, dr = C.P, C.nc, C.dr
    H, DK, DV = 8, 128, 128
    XT = alloc(C, st, "h_XT", [128, 8, S], BF16)
    XT_t = {t: T("h_XT%d" % t) for t in range(NT)}
    build_xT(C, XT, XT_t, range(NT))
    win = dr["hgrn_w_in"].rearrange("(k p) n -> p k n", p=128)
    lbb, omlb, lbb_t = hgrn_lb(C, st, l, dr["hgrn_lower_bounds"].rearrange("(o l) n -> o l n", o=1).to_broadcast([128, 4, D]),
                               (128, D), "h_b")
    lbT, omlT, lbT_t = hgrn_lb(C, st, l, dr["hgrn_lower_bounds"].rearrange("l (c p) -> p l c", p=128), (128, 8), "h_T")
    ng = alloc(C, st, "h_ng", [128, DV], F32); ng_t = T("h_ng")
    load_bcast_row(C, ng[:, :], ng_t, dr["hgrn_norm_g"][0:1, :])
    Wh = [alloc(C, st, "h_Wh%d" % i, [128, 8, 512], BF16) for i in range(2)]
    Wh_t = [[T("h_Wh%d_%d" % (i, j)) for j in range(4)] for i in range(2)]
    state = alloc(C, st, "h_state", [128, DV], F32); state_t = T("h_state")
    state_bf = alloc(C, st, "h_statebf", [128, DV], BF16); statebf_t = T("h_statebf")
    NB = 2
    def mk(name, shape, dt):
        return [alloc(C, st, "%s%d" % (name, i), shape, dt) for i in range(NB)], [T("%s%d" % (name, i)) for i in range(NB)]
    sig, sig_t = mk("h_sig", [128, 128], F32)
    a_, a_t = mk("h_a", [128, 128], F32)
    fg, fg_t = mk("h_fg", [128, 128], F32)
    kin, kin_t = mk("h_kin", [128, 128], F32)
    la, la_t = mk("h_la", [128, 128], F32)
    sgT, sgT_t = mk("h_sgT", [128, 128], F32)
    ecT, ecT_t = mk("h_ecT", [128, 128], F32)
    eiT, eiT_t = mk("h_eiT", [128, 128], F32)
    eR, eR_t = mk("h_eR", [128, 128], F32)
    qd, qd_t = mk("h_qd", [128, 128], BF16)
    ki, ki_t = mk("h_ki", [128, 128], BF16)
    ke, ke_t = mk("h_ke", [128, 128], BF16)
    vb, vb_t = mk("h_vb", [128, DV], BF16)
    sg, sg_t = mk("h_sg", [128, DV], F32)
    sT, sT_t = mk("h_sT", [128, 128], BF16)
    junk, junk_t = mk("h_junk", [128, DV], F32)
    ss, ss_t = mk("h_ss", [128, 4], F32)
    for h in range(H):
        wb = h % 2
        hs = slice(h * 128, (h + 1) * 128)
        for j in range(4):
            P.dma("pool", Wh[wb][:, :, j * 128:(j + 1) * 128], win[:, :, j * 1024 + h * 128:j * 1024 + (h + 1) * 128], writes=[Wh_t[wb][j]])
        P.op("pool", lambda e: e.memset(state[:, :], 0.0), writes=[state_t])
        P.op("pool", lambda e: e.memset(state_bf[:, :], 0.0), writes=[statebf_t])
        for t in range(NT):
            b = t % NB
            tok = slice(t * 128, (t + 1) * 128)
            p1, p1t = next_ps(C)
            for j in range(2):
                for k in range(8):
                    P.op("pe", lambda e, p1=p1, j=j, k=k, wb=wb, tok=tok: e.matmul(
                        p1[:, j * 128:(j + 1) * 128], lhsT=Wh[wb][:, k, j * 128:(j + 1) * 128], rhs=XT[:, k, tok],
                        start=(k == 0), stop=(k == 7)), reads=[Wh_t[wb][j], XT_t[t]], writes=[p1t])
            p2, p2t = next_ps(C)
            for k in range(8):
                P.op("pe", lambda e, p2=p2, k=k, wb=wb, tok=tok: e.matmul(
                    p2[:, 0:384], lhsT=XT[:, k, tok], rhs=Wh[wb][:, k, 128:512], start=(k == 0), stop=(k == 7)),
                    reads=[Wh_t[wb][1], Wh_t[wb][2], Wh_t[wb][3], XT_t[t]], writes=[p2t])
            P.op("act", lambda e, p2=p2, b=b: e.activation(out=sig[b][:, :], in_=p2[:, 0:128], func=AF.Sigmoid),
                 reads=[p2t], writes=[sig_t[b]])
            P.op("dve", lambda e, b=b, hs=hs: e.tensor_tensor(out=a_[b][:, :], in0=sig[b][:, :], in1=omlb[:, hs], op=ALU.mult),
                 reads=[sig_t[b], lbb_t], writes=[a_t[b]])
            P.op("dve", lambda e, b=b, hs=hs: e.tensor_tensor(out=fg[b][:, :], in0=a_[b][:, :], in1=lbb[:, hs], op=ALU.add),
                 reads=[a_t[b], lbb_t], writes=[fg_t[b]])
            P.op("dve", lambda e, b=b, hs=hs: e.tensor_tensor(out=kin[b][:, :], in0=omlb[:, hs], in1=a_[b][:, :], op=ALU.subtract),
                 reads=[a_t[b], lbb_t], writes=[kin_t[b]])
            P.op("act", lambda e, b=b: e.activation(out=la[b][:, :], in_=fg[b][:, :], func=AF.Ln),
                 reads=[fg_t[b]], writes=[la_t[b]])
            P.op("act", lambda e, p1=p1, b=b: e.activation(out=sgT[b][:, :], in_=p1[:, 128:256], func=AF.Sigmoid, scale=-1.0),
                 reads=[p1t], writes=[sgT_t[b]])
            p4, p4t = next_ps(C)
            P.op("pe", lambda e, p4=p4, b=b: e.matmul(p4[:, 0:128], lhsT=la[b][:, :], rhs=C.cm["m_incl"][:, :], start=True, stop=True),
                 reads=[la_t[b], C.cm_t["m_incl"]], writes=[p4t])
            P.op("pe", lambda e, p4=p4, b=b: e.matmul(p4[:, 128:256], lhsT=C.cm["m_rev"][:, :], rhs=la[b][:, :], start=True, stop=True),
                 reads=[la_t[b], C.cm_t["m_rev"]], writes=[p4t])
            P.op("act", lambda e, p4=p4, b=b: e.activation(out=ecT[b][:, :], in_=p4[:, 0:128], func=AF.Exp),
                 reads=[p4t], writes=[ecT_t[b]])
            P.op("act", lambda e, p4=p4, b=b: e.activation(out=eiT[b][:, :], in_=p4[:, 0:128], func=AF.Exp, scale=-1.0),
                 reads=[p4t], writes=[eiT_t[b]])
            P.op("act", lambda e, p4=p4, b=b: e.activation(out=eR[b][:, :], in_=p4[:, 128:256], func=AF.Exp),
                 reads=[p4t], writes=[eR_t[b]])
            P.op("dve", lambda e, p1=p1, b=b: e.tensor_tensor(out=qd[b][:, :], in0=p1[:, 0:128], in1=ecT[b][:, :], op=ALU.mult),
                 reads=[p1t, ecT_t[b]], writes=[qd_t[b]])
            P.op("dve", lambda e, b=b, h=h: e.scalar_tensor_tensor(
                out=ki[b][:, :], in0=sgT[b][:, :], scalar=omlT[:, h:h + 1], in1=eiT[b][:, :], op0=ALU.mult, op1=ALU.mult),
                reads=[sgT_t[b], lbT_t, eiT_t[b]], writes=[ki_t[b]])
            P.op("dve", lambda e, b=b: e.tensor_tensor(out=ke[b][:, :], in0=kin[b][:, :], in1=eR[b][:, :], op=ALU.mult),
                 reads=[kin_t[b], eR_t[b]], writes=[ke_t[b]])
            P.op("act", lambda e, p2=p2, b=b: e.activation(out=vb[b][:, :], in_=p2[:, 128:256], func=AF.Copy),
                 reads=[p2t], writes=[vb_t[b]])
            P.op("act", lambda e, p2=p2, b=b: e.activation(out=sg[b][:, :], in_=p2[:, 256:384], func=AF.Silu),
                 reads=[p2t], writes=[sg_t[b]])
            P.op("dve", lambda e, b=b: e.tensor_tensor(out=sg[b][:, :], in0=sg[b][:, :], in1=ng[:, :], op=ALU.mult),
                 reads=[sg_t[b], ng_t], writes=[sg_t[b]])
            p5, p5t = next_ps(C)
            P.op("pe", lambda e, p5=p5, b=b: e.matmul(p5[:, 0:128], lhsT=ki[b][:, :], rhs=qd[b][:, :], start=True, stop=True),
                 reads=[ki_t[b], qd_t[b]], writes=[p5t])
            P.op("dve", lambda e, p5=p5, b=b: e.tensor_tensor(out=sT[b][:, :], in0=p5[:, 0:128], in1=C.cm["m_caus"][:, :], op=ALU.mult),
                 reads=[p5t, C.cm_t["m_caus"]], writes=[sT_t[b]])
            p6, p6t = next_ps(C)
            P.op("pe", lambda e, p6=p6, b=b: e.matmul(p6[:, 0:DV], lhsT=sT[b][:, :], rhs=vb[b][:, :], start=True, stop=False),
                 reads=[sT_t[b], vb_t[b]], writes=[p6t])
            P.op("pe", lambda e, p6=p6, b=b: e.matmul(p6[:, 0:DV], lhsT=qd[b][:, :], rhs=state_bf[:, :], start=False, stop=True),
                 reads=[qd_t[b], statebf_t], writes=[p6t])
            p7, p7t = next_ps(C)
            P.op("pe", lambda e, p7=p7, b=b: e.matmul(p7[:, 0:DV], lhsT=ke[b][:, :], rhs=vb[b][:, :], start=True, stop=True),
                 reads=[ke_t[b], vb_t[b]], writes=[p7t])
            P.op("dve", lambda e, p7=p7, b=b: e.scalar_tensor_tensor(
                out=state[:, :], in0=state[:, :], scalar=ecT[b][:, 127:128], in1=p7[:, 0:DV], op0=ALU.mult, op1=ALU.add),
                reads=[state_t, ecT_t[b], p7t], writes=[state_t])
            P.op("act", lambda e: e.activation(out=state_bf[:, :], in_=state[:, :], func=AF.Copy),
                 reads=[state_t], writes=[statebf_t])
            P.op("act", lambda e, p6=p6, b=b: e.activation(out=junk[b][:, :], in_=p6[:, 0:DV], func=AF.Square, accum_out=ss[b][:, 0:1]),
                 reads=[p6t], writes=[junk_t[b], ss_t[b]])
            P.op("act", lambda e, b=b: e.activation(out=ss[b][:, 1:2], in_=ss[b][:, 0:1], func=AF.Ln, bias=C.eps6[:, 0:1], scale=1.0 / DV),
                 reads=[ss_t[b], C.eps6_t], writes=[ss_t[b]])
            P.op("act", lambda e, b=b: e.activation(out=ss[b][:, 2:3], in_=ss[b][:, 1:2], func=AF.Exp, scale=-0.5),
                 reads=[ss_t[b]], writes=[ss_t[b]])
            P.op("dve", lambda e, p6=p6, b=b, t=t, h=h: e.scalar_tensor_tensor(
                out=O[:, t, h * DV:(h + 1) * DV], in0=p6[:, 0:DV], scalar=ss[b][:, 2:3], in1=sg[b][:, :], op0=ALU.mult, op1=ALU.mult),
                reads=[p6t, ss_t[b], sg_t[b]], writes=[O_t[t]])


def make_in_map(inputs, b):
    m = {}
    m["x"] = np.ascontiguousarray(inputs["x"][b])
    m["positions"] = np.ascontiguousarray(inputs["positions"][b].reshape(S, 1).astype(np.int32))
    for k in ("gla_w_in", "gla_w_gk", "gla_b_gk", "gla_norm_g", "gla_w_out", "moba_w_in", "moba_w_out", "gdn_w_in",
              "gdn_conv_w", "gdn_a_log", "gdn_dt_bias", "gdn_norm_g", "gdn_w_out", "hgrn_w_in", "hgrn_norm_g",
              "hgrn_w_out"):
        v = np.asarray(inputs[k])[0]
        if v.ndim == 1:
            v = v.reshape(1, -1)
        m[k] = np.ascontiguousarray(v)
    m["hgrn_lower_bounds"] = np.ascontiguousarray(inputs["hgrn_lower_bounds"])
    m["ffn_w_gu"] = np.ascontiguousarray(inputs["ffn_w_gu"])
    m["ffn_w_down"] = np.ascontiguousarray(inputs["ffn_w_down"])
    m["ln_g"] = np.ascontiguousarray(np.asarray(inputs["ln_g"]).reshape(DEPTH * 2, D))
    m["ln_b"] = np.ascontiguousarray(np.asarray(inputs["ln_b"]).reshape(DEPTH * 2, D))
    for k, v in host_consts().items():
        m["c_" + k] = v
    return m


def _todo(C, l):
    raise NotImplementedError


MIXERS = [gla_phase, moba_phase, gdn_phase, hgrn_phase]
_NC_CACHE = {}


def kernel(**inputs):
    if "nc" not in _NC_CACHE:
        _NC_CACHE["nc"] = build()
    nc = _NC_CACHE["nc"]
    in_maps = [make_in_map(inputs, b) for b in range(NCORES)]
    res = run_bass_kernel_spmd(nc, in_maps, core_ids=list(range(NCORES)))
    return np.stack([np.asarray(r["y"]) for r in res.results], axis=0).astype(np.float32)
```

```python
import contextlib
import numpy as np
import concourse.bass as bass
import concourse.mybir as mybir
from concourse.bass_utils import run_bass_kernel_spmd

F32 = mybir.dt.float32
BF16 = mybir.dt.bfloat16
I32 = mybir.dt.int32
AF = mybir.ActivationFunctionType
ALU = mybir.AluOpType
AX = mybir.AxisListType

D = 1024
S = 2048
NT = S // 128
DEPTH = 4
FFN_H = 2816
ALPHA = (2.0 * DEPTH) ** 0.25
NCORES = 8


class T:
    __slots__ = ("name", "w", "r", "excl")

    def __init__(self, name, excl=False):
        self.name = name
        self.w = None
        self.r = []
        self.excl = excl


class Prog:
    ENGS = ("pe", "dve", "act", "pool", "sp")
    NLANES = 6

    def __init__(self, nc):
        self.nc = nc
        self.ins = []
        self.pending = {e: set() for e in self.ENGS}
        self.last_barrier = 0
        self.eng_obj = {"pe": nc.tensor, "dve": nc.vector, "act": nc.scalar, "pool": nc.gpsimd, "sp": nc.sync}

    def op(self, eng, fn, reads=(), writes=(), dma=False):
        idx = len(self.ins)
        deps = set()
        for t in reads:
            if t.w is not None:
                deps.add(t.w)
            if t.excl:
                deps.update(r for r in t.r if self.ins[r]["eng"] != eng)
        for t in writes:
            if t.w is not None:
                deps.add(t.w)
            deps.update(t.r)
        if self.pending[eng]:
            deps |= self.pending[eng]
            self.pending[eng] = set()
        if eng == "pe":
            deps = {d for d in deps if self.ins[d]["eng"] != "pe" or self.ins[d]["dma"]}
        self.ins.append(dict(eng=eng, fn=fn, deps=deps, dma=dma))
        for t in reads:
            t.r.append(idx)
        for t in writes:
            t.w = idx
            t.r = []
        return idx

    def barrier(self):
        last = {}
        deps = set()
        for i in range(self.last_barrier, len(self.ins)):
            ins = self.ins[i]
            if ins["dma"]:
                deps.add(i)
            else:
                last[ins["eng"]] = i
        deps |= set(last.values())
        for e in self.ENGS:
            self.pending[e] |= deps
        self.last_barrier = len(self.ins)

    def dma(self, eng, out, in_, reads=(), writes=(), **kw):
        return self.op(eng, lambda e: e.dma_start(out=out, in_=in_, **kw), reads, writes, dma=True)

    def finalize(self):
        nc = self.nc
        waited = set()
        for ins in self.ins:
            waited.update(ins["deps"])
        sems = {}
        with contextlib.ExitStack() as es:
            for e in self.ENGS:
                sems[e] = es.enter_context(nc.semaphore("s_" + e))
            for q in ("sp", "act", "pool"):
                for l in range(self.NLANES):
                    sems[(q, l)] = es.enter_context(nc.semaphore("d_%s%d" % (q, l)))
            cnt = {k: 0 for k in sems}
            sig = {}
            lane_rr = {"sp": 0, "act": 0, "pool": 0}
            lane_prev = {}
            for idx, ins in enumerate(self.ins):
                if ins["dma"]:
                    q = ins["eng"]
                    lane = (q, lane_rr[q] % self.NLANES)
                    lane_rr[q] += 1
                    ins["lane_prev"] = cnt[lane]
                    cnt[lane] += 16
                    sig[idx] = (lane, cnt[lane])
                elif idx in waited:
                    cnt[ins["eng"]] += 1
                    sig[idx] = (ins["eng"], cnt[ins["eng"]])
            self.sig_counts = dict(cnt)
            self.wait_hist = {}
            with nc.Block() as block:
                for eng in self.ENGS:
                    my = [(i, ins) for i, ins in enumerate(self.ins) if ins["eng"] == eng]
                    if not my:
                        continue

                    def body(e, my=my, eng=eng):
                        seen = {}
                        for idx, ins in my:
                            needs = {}
                            for d in ins["deps"]:
                                sk, val = sig[d]
                                if needs.get(sk, 0) < val:
                                    needs[sk] = val
                            if ins["dma"]:
                                sk, val = sig[idx]
                                if ins["lane_prev"] > 0 and needs.get(sk, 0) < ins["lane_prev"]:
                                    needs[sk] = ins["lane_prev"]
                            nw = 0
                            for sk, val in needs.items():
                                if seen.get(sk, 0) < val:
                                    e.wait_ge(sems[sk], val)
                                    seen[sk] = val
                                    nw += 1
                            self.wait_hist[nw] = self.wait_hist.get(nw, 0) + 1
                            r = ins["fn"](e)
                            if idx in sig:
                                sk, val = sig[idx]
                                r.then_inc(sems[sk], 16 if ins["dma"] else 1)
                        for l in range(self.NLANES):
                            sk = (eng, l)
                            if sk in cnt and cnt[sk] > 0 and seen.get(sk, 0) < cnt[sk]:
                                e.wait_ge(sems[sk], cnt[sk])

                    getattr(block, {"pe": "tensor", "dve": "vector", "act": "scalar", "pool": "gpsimd", "sp": "sync"}[eng])(body)


class Ctx:
    pass


def host_consts():
    c = {}
    c["ident"] = np.eye(128, dtype=np.float32)
    i = np.arange(128)
    c["m_incl"] = (i[:, None] <= i[None, :]).astype(np.float32)
    c["m_rev"] = (i[:, None] > i[None, :]).astype(np.float32)
    c["m_caus"] = (i[:, None] <= i[None, :]).astype(np.float32)
    inv = (500000.0 ** (-np.arange(0, 32, 2, dtype=np.float32) / 32.0)).astype(np.float32)
    c["invf"] = np.concatenate([inv, inv]).reshape(32, 1).astype(np.float32)
    c["sgn"] = np.concatenate([-np.ones(16), np.ones(16)]).reshape(32, 1).astype(np.float32)
    E = np.zeros((8, 8, 128), np.float32)
    for n in range(8):
        E[n, n, :] = 30000.0
    c["E"] = E.reshape(8, 1024)
    c["causneg"] = np.where(i[:, None] > i[None, :], -30000.0, 0.0).astype(np.float32)
    npast = np.where(np.arange(8)[None, :] < np.arange(8)[:, None], 0.0, -1e30).astype(np.float32)
    c["negpast"] = npast.reshape(1, 64)
    BIG = 1.0e5
    c["g_pos"] = np.where(i[None, :] >= i[:, None], BIG, 0.0).astype(np.float32)
    c["g_negt"] = np.where(i[None, :] < i[:, None], -BIG, 0.0).astype(np.float32)
    c["ones"] = np.ones((128, 128), np.float32)
    c["m_incl_gla"] = c["m_incl"] * np.float32(-1.0 / 16.0)
    c["m_rev_gla"] = c["m_rev"] * np.float32(-1.0 / 16.0)
    return c


def build(n_layers=DEPTH, dbg=None, layers=None):
    dbg = dbg or {}
    nc = bass.Bass("TRN2", target_bir_lowering=False)
    dr = {}

    def din(name, shape, dt=F32):
        dr[name] = nc.dram_tensor(name, list(shape), dt, kind="ExternalInput").ap()
        return dr[name]

    din("x", [S, D])
    din("positions", [S, 1], I32)
    din("gla_w_in", [D, 3088]); din("gla_w_gk", [16, 512]); din("gla_b_gk", [1, 512]); din("gla_norm_g", [1, 256])
    din("gla_w_out", [D, D])
    din("moba_w_in", [D, 3072]); din("moba_w_out", [D, D])
    din("gdn_w_in", [D, 4112]); din("gdn_conv_w", [4, 3072]); din("gdn_a_log", [1, 8]); din("gdn_dt_bias", [1, 8])
    din("gdn_norm_g", [1, 128]); din("gdn_w_out", [D, D])
    din("hgrn_lower_bounds", [4, 1024]); din("hgrn_w_in", [D, 4096]); din("hgrn_norm_g", [1, 128])
    din("hgrn_w_out", [D, D])
    din("ffn_w_gu", [DEPTH, D, 2 * FFN_H]); din("ffn_w_down", [DEPTH, FFN_H, D])
    din("ln_g", [DEPTH * 2, D]); din("ln_b", [DEPTH * 2, D])
    for k, v in host_consts().items():
        din("c_" + k, v.shape)
    y_out = nc.dram_tensor("y", [S, D], F32, kind="ExternalOutput").ap()

    P = Prog(nc)
    C = Ctx()
    C.nc, C.P, C.dr, C.dbg = nc, P, dr, dbg
    with contextlib.ExitStack() as es:
        C.es = es
        C.X = es.enter_context(nc.sbuf_tensor("X", [128, NT, D], F32))
        C.Xt = [T("X%d" % t) for t in range(NT)]
        C.ps = [es.enter_context(nc.psum_tensor("ps%d" % i, [128, 512], F32)) for i in range(8)]
        C.pst = [T("ps%d" % i, excl=True) for i in range(8)]
        C.ident = es.enter_context(nc.sbuf_tensor("ident", [128, 128], F32))
        C.ident_t = T("ident")
        P.dma("sp", C.ident[:, :], dr["c_ident"][:, :], writes=[C.ident_t])
        C.eps5 = es.enter_context(nc.sbuf_tensor("eps5", [128, 1], F32))
        C.eps_t = T("eps5")
        P.op("pool", lambda e: e.memset(C.eps5[:, :], 1e-5), writes=[C.eps_t])
        C.ps_rr = 0
        C.ps_pool_rr = {}
        C.cm = {}
        C.cm_t = {}
        for nm in ("m_incl", "m_rev", "m_caus", "m_incl_gla", "m_rev_gla", "g_pos", "g_negt", "ones"):
            C.cm[nm] = es.enter_context(nc.sbuf_tensor("k_" + nm, [128, 128], F32))
            C.cm_t[nm] = T("c_" + nm)
            P.dma("sp", C.cm[nm][:, :], dr["c_" + nm][:, :], writes=[C.cm_t[nm]])
        C.identb = es.enter_context(nc.sbuf_tensor("identb", [128, 128], BF16))
        C.identb_t = T("identb")
        P.op("dve", lambda e: e.tensor_copy(out=C.identb[:, :], in_=C.ident[:, :]), reads=[C.ident_t], writes=[C.identb_t])
        C.eps6 = es.enter_context(nc.sbuf_tensor("eps6", [128, 1], F32))
        C.eps6_t = T("eps6")
        P.op("pool", lambda e: e.memset(C.eps6[:, :], 1e-6), writes=[C.eps6_t])
        C.ones1 = es.enter_context(nc.sbuf_tensor("ones1", [1, 128], F32))
        C.ones1_t = T("ones1")
        P.op("pool", lambda e: e.memset(C.ones1[:, :], 1.0), writes=[C.ones1_t])
        for t in range(NT):
            P.dma("sp", C.X[:, t, :], dr["x"][t * 128:(t + 1) * 128, :], writes=[C.Xt[t]])
        for l in (layers if layers is not None else range(n_layers)):
            if not dbg.get("skip_mixer"):
                MIXERS[l % 4](C, l)
            if not dbg.get("skip_ffn"):
                ffn_phase(C, l)
        for t in range(NT):
            P.dma("sp", y_out[t * 128:(t + 1) * 128, :], C.X[:, t, :], reads=[C.Xt[t]])
        P.finalize()
    C.P = P
    build.last_prog = P
    return nc


def load_bcast_row(C, dst, dst_t, src_row_ap, eng="sp"):
    n = src_row_ap.shape[-1]
    C.P.dma(eng, dst, src_row_ap.to_broadcast([128, n]), writes=[dst_t])


def layer_norm_tile(C, t, zsrc, G, Bv, G_t, B_t, wk):
    P, nc = C.P, C.nc
    z, z_t, st, st_t, mv, mv_t, sc, sc_t = wk
    xt = C.Xt[t]
    for h, (pap, pT) in enumerate(zsrc):
        sl = slice(h * 512, (h + 1) * 512)
        P.op("dve", lambda e, sl=sl, pap=pap: e.scalar_tensor_tensor(
            out=z[:, sl], in0=C.X[:, t, sl], scalar=ALPHA, in1=pap, op0=ALU.mult, op1=ALU.add),
            reads=[xt, pT], writes=[z_t])
    for h in range(2):
        sl = slice(h * 512, (h + 1) * 512)
        P.op("dve", lambda e, sl=sl, h=h: e.bn_stats(out=st[:, h * 6:(h + 1) * 6], in_=z[:, sl]),
             reads=[z_t], writes=[st_t])
    P.op("dve", lambda e: e.bn_aggr(out=mv[:, 0:2], in_=st[:, 0:12]), reads=[st_t], writes=[mv_t])
    P.op("act", lambda e: e.activation(out=sc[:, 0:1], in_=mv[:, 1:2], func=AF.Ln, bias=C.eps5[:, 0:1], scale=1.0),
         reads=[mv_t, C.eps_t], writes=[sc_t])
    P.op("act", lambda e: e.activation(out=sc[:, 1:2], in_=sc[:, 0:1], func=AF.Exp, scale=-0.5),
         reads=[sc_t], writes=[sc_t])
    P.op("dve", lambda e: e.scalar_tensor_tensor(out=sc[:, 2:3], in0=mv[:, 0:1], scalar=-1.0, in1=sc[:, 1:2],
                                                 op0=ALU.mult, op1=ALU.mult), reads=[mv_t, sc_t], writes=[sc_t])
    P.op("act", lambda e: e.activation(out=z[:, :], in_=z[:, :], func=AF.Identity, bias=sc[:, 2:3], scale=sc[:, 1:2]),
         reads=[z_t, sc_t], writes=[z_t])
    P.op("dve", lambda e: e.tensor_tensor(out=z[:, :], in0=z[:, :], in1=G[:, :], op=ALU.mult),
         reads=[z_t, G_t], writes=[z_t])
    P.op("dve", lambda e: e.tensor_tensor(out=C.X[:, t, :], in0=z[:, :], in1=Bv[:, :], op=ALU.add),
         reads=[z_t, B_t], writes=[xt])


_ALLOC_CTR = [0]


def alloc(C, st, name, shape, dt):
    _ALLOC_CTR[0] += 1
    return st.enter_context(C.nc.sbuf_tensor("%s_%d" % (name, _ALLOC_CTR[0]), list(shape), dt))


def barrier(C):
    C.P.barrier()


def build_xT(C, XT, XT_t, tiles, evac_eng="act"):
    P = C.P
    for t in tiles:
        for half in range(2):
            pi = C.ps_rr % 8
            C.ps_rr += 1
            ps, pt = C.ps[pi], C.pst[pi]
            for c4 in range(4):
                c = half * 4 + c4
                P.op("pe", lambda e, ps=ps, c=c, c4=c4, t=t: e.transpose(
                    out=ps[:, c4 * 128:(c4 + 1) * 128], in_=C.X[:, t, c * 128:(c + 1) * 128], identity=C.ident[:, :]),
                    reads=[C.Xt[t], C.ident_t], writes=[pt])
            dst = XT[:, half * 4:(half + 1) * 4, t * 128:(t + 1) * 128]
            src = ps[:, :].rearrange("p (c n) -> p c n", c=4)
            if evac_eng == "act":
                P.op("act", lambda e, dst=dst, src=src: e.activation(out=dst, in_=src, func=AF.Copy),
                     reads=[pt], writes=[XT_t[t]])
            else:
                P.op("dve", lambda e, dst=dst, src=src: e.tensor_copy(out=dst, in_=src), reads=[pt], writes=[XT_t[t]])


def ffn_phase(C, l):
    P, nc, dr = C.P, C.nc, C.dr
    HG = 256
    NG = FFN_H // HG
    NHC = FFN_H // 128
    with contextlib.ExitStack() as st:
        XT = alloc(C, st, "f_XT", [128, 8, S], BF16)
        XT_t = {t: T("f_XT%d" % t) for t in range(NT)}
        Wd = alloc(C, st, "f_Wd", [128, NHC, D], BF16)
        Wd_t = [T("f_Wd%d" % i) for i in range(NHC)]
        Wg = [alloc(C, st, "f_Wg%d" % i, [128, 8, 2 * HG], BF16) for i in range(2)]
        Wg_t = [(T("f_Wgg%d" % i), T("f_Wgu%d" % i)) for i in range(2)]
        hT = alloc(C, st, "f_hT", [128, NHC, 512], BF16)
        hT_t = [T("f_hT%d" % i) for i in range(NHC)]
        sg = [alloc(C, st, "f_sg%d" % i, [128, 512], F32) for i in range(2)]
        sg_t = [T("f_sg%d" % i) for i in range(2)]
        G = alloc(C, st, "f_G", [128, D], F32); G_t = T("f_G")
        Bv = alloc(C, st, "f_B", [128, D], F32); B_t = T("f_B")
        z = alloc(C, st, "f_z", [128, D], F32); z_t = T("f_z")
        stt = alloc(C, st, "f_st", [128, 12], F32); st_t = T("f_st")
        mv = alloc(C, st, "f_mv", [128, 2], F32); mv_t = T("f_mv")
        sc = alloc(C, st, "f_sc", [128, 4], F32); sc_t = T("f_sc")
        wk = (z, z_t, stt, st_t, mv, mv_t, sc, sc_t)
        load_bcast_row(C, G[:, :], G_t, dr["ln_g"][2 * l + 1:2 * l + 2, :])
        load_bcast_row(C, Bv[:, :], B_t, dr["ln_b"][2 * l + 1:2 * l + 2, :])
        wd_src = dr["ffn_w_down"][l].rearrange("(c p) n -> p c n", p=128)
        for c in range(0, NHC, 2):
            P.dma("pool", Wd[:, c:c + 2, :], wd_src[:, c:c + 2, :], writes=Wd_t[c:c + 2])
        wgu = dr["ffn_w_gu"][l].rearrange("(k p) n -> p k n", p=128)
        gi = 0
        for tb in range(4):
            build_xT(C, XT, XT_t, range(tb * 4, tb * 4 + 4))
            xts = [XT_t[t] for t in range(tb * 4, tb * 4 + 4)]
            for g in range(NG):
                b = gi % 2
                gi += 1
                P.dma("pool", Wg[b][:, :, 0:HG], wgu[:, :, g * HG:(g + 1) * HG], writes=[Wg_t[b][0]])
                P.dma("pool", Wg[b][:, :, HG:2 * HG], wgu[:, :, FFN_H + g * HG:FFN_H + (g + 1) * HG], writes=[Wg_t[b][1]])
                for cc in range(HG // 128):
                    hc = g * (HG // 128) + cc
                    pg_i, pu_i = C.ps_rr % 8, (C.ps_rr + 1) % 8
                    C.ps_rr += 2
                    for (pi, off, wt) in ((pg_i, 0, Wg_t[b][0]), (pu_i, HG, Wg_t[b][1])):
                        for k in range(8):
                            P.op("pe", lambda e, pi=pi, off=off, k=k, b=b, cc=cc, tb=tb: e.matmul(
                                C.ps[pi][:, :], lhsT=Wg[b][:, k, off + cc * 128:off + (cc + 1) * 128],
                                rhs=XT[:, k, tb * 512:(tb + 1) * 512], start=(k == 0), stop=(k == 7)),
                                reads=[wt] + xts, writes=[C.pst[pi]])
                    sb = hc % 2
                    P.op("act", lambda e, sb=sb, pg_i=pg_i: e.activation(out=sg[sb][:, :], in_=C.ps[pg_i][:, :], func=AF.Silu),
                         reads=[C.pst[pg_i]], writes=[sg_t[sb]])
                    P.op("dve", lambda e, sb=sb, pu_i=pu_i, hc=hc: e.tensor_tensor(
                        out=hT[:, hc, :], in0=sg[sb][:, :], in1=C.ps[pu_i][:, :], op=ALU.mult),
                        reads=[sg_t[sb], C.pst[pu_i]], writes=[hT_t[hc]])
            for tt in range(4):
                t = tb * 4 + tt
                zs = []
                for cb in range(2):
                    pi = C.ps_rr % 8
                    C.ps_rr += 1
                    for hc in range(NHC):
                        P.op("pe", lambda e, pi=pi, hc=hc, tt=tt, cb=cb: e.matmul(
                            C.ps[pi][:, :], lhsT=hT[:, hc, tt * 128:(tt + 1) * 128],
                            rhs=Wd[:, hc, cb * 512:(cb + 1) * 512], start=(hc == 0), stop=(hc == NHC - 1)),
                            reads=[hT_t[hc], Wd_t[hc]], writes=[C.pst[pi]])
                    zs.append((C.ps[pi][:, :], C.pst[pi]))
                layer_norm_tile(C, t, zs, G, Bv, G_t, B_t, wk)
        barrier(C)


def run_pipelined(gens, width):
    it = iter(gens)
    active = []
    exhausted = False
    while True:
        if len(active) < width and not exhausted:
            try:
                active.append(next(it))
            except StopIteration:
                exhausted = True
        if not active:
            break
        for g in list(active):
            try:
                next(g)
            except StopIteration:
                active.remove(g)


def next_ps(C, pool=None):
    if pool is None:
        i = C.ps_rr % 8
        C.ps_rr += 1
    else:
        k = C.ps_pool_rr.get(pool, 0)
        C.ps_pool_rr[pool] = k + 1
        i = pool[k % len(pool)]
    return C.ps[i], C.pst[i]


def outproj_phase(C, l, O, O_t, w_out_ap):
    P, nc, dr = C.P, C.nc, C.dr
    with contextlib.ExitStack() as st:
        Wo = alloc(C, st, "o_Wo", [128, 8, D], BF16)
        Wo_t = [T("o_Wo%d" % i) for i in range(8)]
        src = w_out_ap.rearrange("(c p) n -> p c n", p=128)
        for c in range(0, 8, 2):
            P.dma("pool", Wo[:, c:c + 2, :], src[:, c:c + 2, :], writes=Wo_t[c:c + 2])
        G = alloc(C, st, "o_G", [128, D], F32); G_t = T("o_G")
        Bv = alloc(C, st, "o_B", [128, D], F32); B_t = T("o_B")
        z = alloc(C, st, "o_z", [128, D], F32); z_t = T("o_z")
        stt = alloc(C, st, "o_st", [128, 12], F32); st_t = T("o_st")
        mv = alloc(C, st, "o_mv", [128, 2], F32); mv_t = T("o_mv")
        sc = alloc(C, st, "o_sc", [128, 4], F32); sc_t = T("o_sc")
        wk = (z, z_t, stt, st_t, mv, mv_t, sc, sc_t)
        load_bcast_row(C, G[:, :], G_t, dr["ln_g"][2 * l:2 * l + 1, :])
        load_bcast_row(C, Bv[:, :], B_t, dr["ln_b"][2 * l:2 * l + 1, :])
        oT = [alloc(C, st, "o_oT%d" % i, [128, 8, 128], BF16) for i in range(2)]
        oT_t = [T("o_oT%d" % i) for i in range(2)]
        for t in range(NT):
            b = t % 2
            ps, pt = next_ps(C)
            psb = ps[:, :].bitcast(BF16)
            for c in range(8):
                P.op("pe", lambda e, psb=psb, c=c, t=t: e.transpose(
                    out=psb[:, c * 128:(c + 1) * 128], in_=O[:, t, c * 128:(c + 1) * 128], identity=C.identb[:, :]),
                    reads=[O_t[t], C.identb_t], writes=[pt])
            P.op("act", lambda e, psb=psb, b=b: e.activation(
                out=oT[b][:, :, :], in_=psb.rearrange("p (c n) -> p c n", c=8), func=AF.Copy),
                reads=[pt], writes=[oT_t[b]])
            zs = []
            for cb in range(2):
                ps2, pt2 = next_ps(C)
                for c in range(8):
                    P.op("pe", lambda e, ps2=ps2, c=c, cb=cb, b=b: e.matmul(
                        ps2[:, :], lhsT=oT[b][:, c, :], rhs=Wo[:, c, cb * 512:(cb + 1) * 512],
                        start=(c == 0), stop=(c == 7)), reads=[oT_t[b], Wo_t[c]], writes=[pt2])
                zs.append((ps2[:, :], pt2))
            layer_norm_tile(C, t, zs, G, Bv, G_t, B_t, wk)
        barrier(C)


def gla_phase(C, l):
    P, nc, dr = C.P, C.nc, C.dr
    H, DK, DV = 4, 128, 256
    with contextlib.ExitStack() as st:
        O = alloc(C, st, "g_O", [128, NT, D], BF16)
        O_t = [T("g_O%d" % t) for t in range(NT)]
        with contextlib.ExitStack() as st2:
            gla_heads(C, l, st2, O, O_t)
            barrier(C)
        outproj_phase(C, l, O, O_t, dr["gla_w_out"])


def gla_heads(C, l, st, O, O_t):
    P, nc, dr = C.P, C.nc, C.dr
    H, DK, DV = 4, 128, 256
    XT = alloc(C, st, "g_XT", [128, 8, S], BF16)
    XT_t = {t: T("g_XT%d" % t) for t in range(NT)}
    build_xT(C, XT, XT_t, range(NT))
    win = dr["gla_w_in"].rearrange("(k p) n -> p k n", p=128)
    Wlow = alloc(C, st, "g_Wlow", [128, 8, 16], BF16); Wlow_t = T("g_Wlow")
    P.dma("pool", Wlow[:, :, :], win[:, :, 3072:3088], writes=[Wlow_t])
    gkT = alloc(C, st, "g_gkT", [16, S], F32); gkT_t = T("g_gkT")
    for tb in range(4):
        ps, pt = next_ps(C)
        for k in range(8):
            P.op("pe", lambda e, ps=ps, k=k, tb=tb: e.matmul(ps[0:16, :], lhsT=Wlow[:, k, :], rhs=XT[:, k, tb * 512:(tb + 1) * 512],
                                                          start=(k == 0), stop=(k == 7)),
                 reads=[Wlow_t] + [XT_t[t] for t in range(tb * 4, tb * 4 + 4)], writes=[pt])
        P.op("act", lambda e, ps=ps, tb=tb: e.activation(out=gkT[:, tb * 512:(tb + 1) * 512], in_=ps[0:16, :], func=AF.Copy),
             reads=[pt], writes=[gkT_t])
    wgk = alloc(C, st, "g_wgk", [16, 512], F32); wgk_t = T("g_wgk")
    P.dma("sp", wgk[:, :], dr["gla_w_gk"][:, :], writes=[wgk_t])
    bgk = alloc(C, st, "g_bgk", [1, 512], F32); bgk_t = T("g_bgk")
    P.dma("sp", bgk[:, :], dr["gla_b_gk"][:, :], writes=[bgk_t])
    ng = alloc(C, st, "g_ng", [128, DV], F32); ng_t = T("g_ng")
    load_bcast_row(C, ng[:, :], ng_t, dr["gla_norm_g"][0:1, :])
    Wh = [alloc(C, st, "g_Wh%d" % i, [128, 8, 768], BF16) for i in range(2)]
    Wh_t = [[T("g_Wh%d_%d" % (i, j)) for j in range(4)] for i in range(2)]
    state = alloc(C, st, "g_state", [128, DV], F32); state_t = T("g_state")
    state_bf = alloc(C, st, "g_statebf", [128, DV], BF16); statebf_t = T("g_statebf")
    NB = 2
    def mk(name, shape, dt):
        return [alloc(C, st, "%s%d" % (name, i), shape, dt) for i in range(NB)], [T("%s%d" % (name, i)) for i in range(NB)]
    ex, ex_t = mk("g_ex", [128, 128], F32)
    lt, lt_t = mk("g_l", [128, 128], F32)
    ecT, ecT_t = mk("g_ecT", [128, 128], F32)
    eiT, eiT_t = mk("g_eiT", [128, 128], F32)
    eR, eR_t = mk("g_eR", [128, 128], F32)
    qd, qd_t = mk("g_qd", [128, 128], BF16)
    ki, ki_t = mk("g_ki", [128, 128], BF16)
    ke, ke_t = mk("g_ke", [128, 128], BF16)
    vb, vb_t = mk("g_vb", [128, DV], BF16)
    sg, sg_t = mk("g_sg", [128, DV], F32)
    sT, sT_t = mk("g_sT", [128, 128], BF16)
    junk, junk_t = mk("g_junk", [128, DV], F32)
    ss, ss_t = mk("g_ss", [128, 4], F32)
    cols = [(0, 128), (512, 128), (1024, 256), (2048, 256)]
    for h in range(H):
        wb = h % 2
        off = 0
        for j, (base, wdt) in enumerate(cols):
            P.dma("pool", Wh[wb][:, :, off:off + wdt], win[:, :, base + h * wdt:base + (h + 1) * wdt], writes=[Wh_t[wb][j]])
            off += wdt
        P.op("pool", lambda e: e.memset(state[:, :], 0.0), writes=[state_t])
        P.op("pool", lambda e: e.memset(state_bf[:, :], 0.0), writes=[statebf_t])
        def gla_tile(t, h=h, wb=wb):
            b = t % NB
            pool = (0, 1, 2, 3) if b == 0 else (4, 5, 6, 7)
            tok = slice(t * 128, (t + 1) * 128)
            p1, p1t = next_ps(C, pool)
            for j in range(2):
                for k in range(8):
                    P.op("pe", lambda e, p1=p1, j=j, k=k, wb=wb, tok=tok: e.matmul(
                        p1[:, j * 128:(j + 1) * 128], lhsT=Wh[wb][:, k, j * 128:(j + 1) * 128], rhs=XT[:, k, tok],
                        start=(k == 0), stop=(k == 7)), reads=[Wh_t[wb][j], XT_t[t]], writes=[p1t])
            p2, p2t = next_ps(C, pool)
            for k in range(8):
                P.op("pe", lambda e, p2=p2, k=k, wb=wb, tok=tok: e.matmul(
                    p2[:, 0:384], lhsT=XT[:, k, tok], rhs=Wh[wb][:, k, 128:512], start=(k == 0), stop=(k == 7)),
                    reads=[Wh_t[wb][1], Wh_t[wb][2], XT_t[t]], writes=[p2t])
            p3, p3t = next_ps(C, pool)
            for k in range(8):
                P.op("pe", lambda e, p3=p3, k=k, wb=wb, tok=tok: e.matmul(
                    p3[:, 0:256], lhsT=XT[:, k, tok], rhs=Wh[wb][:, k, 512:768], start=(k == 0), stop=(k == 7)),
                    reads=[Wh_t[wb][3], XT_t[t]], writes=[p3t])
            P.op("pe", lambda e, p3=p3, tok=tok, h=h: e.matmul(
                p3[:, 256:384], lhsT=gkT[:, tok], rhs=wgk[:, h * 128:(h + 1) * 128], start=True, stop=False),
                reads=[gkT_t, wgk_t], writes=[p3t])
            P.op("pe", lambda e, p3=p3, h=h: e.matmul(
                p3[:, 256:384], lhsT=C.ones1[:, :], rhs=bgk[:, h * 128:(h + 1) * 128], start=False, stop=True),
                reads=[C.ones1_t, bgk_t], writes=[p3t])
            yield
            P.op("act", lambda e, p3=p3, b=b: e.activation(out=ex[b][:, :], in_=p3[:, 256:384], func=AF.Exp, scale=-1.0),
                 reads=[p3t], writes=[ex_t[b]])
            P.op("act", lambda e, b=b: e.activation(out=lt[b][:, :], in_=ex[b][:, :], func=AF.Ln, bias=1.0, scale=1.0),
                 reads=[ex_t[b]], writes=[lt_t[b]])
            yield
            p4, p4t = next_ps(C, pool)
            P.op("pe", lambda e, p4=p4, b=b: e.matmul(p4[:, 0:128], lhsT=lt[b][:, :], rhs=C.cm["m_incl_gla"][:, :], start=True, stop=True),
                 reads=[lt_t[b], C.cm_t["m_incl_gla"]], writes=[p4t])
            P.op("pe", lambda e, p4=p4, b=b: e.matmul(p4[:, 128:256], lhsT=C.cm["m_rev_gla"][:, :], rhs=lt[b][:, :], start=True, stop=True),
                 reads=[lt_t[b], C.cm_t["m_rev_gla"]], writes=[p4t])
            P.op("act", lambda e, p4=p4, b=b: e.activation(out=ecT[b][:, :], in_=p4[:, 0:128], func=AF.Exp),
                 reads=[p4t], writes=[ecT_t[b]])
            P.op("act", lambda e, p4=p4, b=b: e.activation(out=eiT[b][:, :], in_=p4[:, 0:128], func=AF.Exp, scale=-1.0),
                 reads=[p4t], writes=[eiT_t[b]])
            P.op("act", lambda e, p4=p4, b=b: e.activation(out=eR[b][:, :], in_=p4[:, 128:256], func=AF.Exp),
                 reads=[p4t], writes=[eR_t[b]])
            P.op("dve", lambda e, p1=p1, b=b: e.scalar_tensor_tensor(
                out=qd[b][:, :], in0=p1[:, 0:128], scalar=float(DK ** -0.5), in1=ecT[b][:, :], op0=ALU.mult, op1=ALU.mult),
                reads=[p1t, ecT_t[b]], writes=[qd_t[b]])
            P.op("dve", lambda e, p1=p1, b=b: e.tensor_tensor(out=ki[b][:, :], in0=p1[:, 128:256], in1=eiT[b][:, :], op=ALU.mult),
                 reads=[p1t, eiT_t[b]], writes=[ki_t[b]])
            P.op("dve", lambda e, p2=p2, b=b: e.tensor_tensor(out=ke[b][:, :], in0=p2[:, 0:128], in1=eR[b][:, :], op=ALU.mult),
                 reads=[p2t, eR_t[b]], writes=[ke_t[b]])
            P.op("act", lambda e, p2=p2, b=b: e.activation(out=vb[b][:, :], in_=p2[:, 128:384], func=AF.Copy),
                 reads=[p2t], writes=[vb_t[b]])
            P.op("act", lambda e, p3=p3, b=b: e.activation(out=sg[b][:, :], in_=p3[:, 0:256], func=AF.Silu),
                 reads=[p3t], writes=[sg_t[b]])
            P.op("dve", lambda e, b=b: e.tensor_tensor(out=sg[b][:, :], in0=sg[b][:, :], in1=ng[:, :], op=ALU.mult),
                 reads=[sg_t[b], ng_t], writes=[sg_t[b]])
            yield
            p5, p5t = next_ps(C, pool)
            P.op("pe", lambda e, p5=p5, b=b: e.matmul(p5[:, 0:128], lhsT=ki[b][:, :], rhs=qd[b][:, :], start=True, stop=True),
                 reads=[ki_t[b], qd_t[b]], writes=[p5t])
            P.op("dve", lambda e, p5=p5, b=b: e.tensor_tensor(out=sT[b][:, :], in0=p5[:, 0:128], in1=C.cm["m_caus"][:, :], op=ALU.mult),
                 reads=[p5t, C.cm_t["m_caus"]], writes=[sT_t[b]])
            yield
            p6, p6t = next_ps(C, pool)
            P.op("pe", lambda e, p6=p6, b=b: e.matmul(p6[:, 0:DV], lhsT=sT[b][:, :], rhs=vb[b][:, :], start=True, stop=False),
                 reads=[sT_t[b], vb_t[b]], writes=[p6t])
            P.op("pe", lambda e, p6=p6, b=b: e.matmul(p6[:, 0:DV], lhsT=qd[b][:, :], rhs=state_bf[:, :], start=False, stop=True),
                 reads=[qd_t[b], statebf_t], writes=[p6t])
            yield
            p7, p7t = next_ps(C, pool)
            P.op("pe", lambda e, p7=p7, b=b: e.matmul(p7[:, 0:DV], lhsT=ke[b][:, :], rhs=vb[b][:, :], start=True, stop=True),
                 reads=[ke_t[b], vb_t[b]], writes=[p7t])
            P.op("dve", lambda e, p7=p7, b=b: e.scalar_tensor_tensor(
                out=state[:, :], in0=state[:, :], scalar=ecT[b][:, 127:128], in1=p7[:, 0:DV], op0=ALU.mult, op1=ALU.add),
                reads=[state_t, ecT_t[b], p7t], writes=[state_t])
            P.op("act", lambda e: e.activation(out=state_bf[:, :], in_=state[:, :], func=AF.Copy),
                 reads=[state_t], writes=[statebf_t])
            yield
            P.op("act", lambda e, p6=p6, b=b: e.activation(out=junk[b][:, :], in_=p6[:, 0:DV], func=AF.Square, accum_out=ss[b][:, 0:1]),
                 reads=[p6t], writes=[junk_t[b], ss_t[b]])
            P.op("act", lambda e, b=b: e.activation(out=ss[b][:, 1:2], in_=ss[b][:, 0:1], func=AF.Ln, bias=C.eps6[:, 0:1], scale=1.0 / DV),
                 reads=[ss_t[b], C.eps6_t], writes=[ss_t[b]])
            P.op("act", lambda e, b=b: e.activation(out=ss[b][:, 2:3], in_=ss[b][:, 1:2], func=AF.Exp, scale=-0.5),
                 reads=[ss_t[b]], writes=[ss_t[b]])
            P.op("dve", lambda e, p6=p6, b=b, t=t, h=h: e.scalar_tensor_tensor(
                out=O[:, t, h * DV:(h + 1) * DV], in0=p6[:, 0:DV], scalar=ss[b][:, 2:3], in1=sg[b][:, :], op0=ALU.mult, op1=ALU.mult),
                reads=[p6t, ss_t[b], sg_t[b]], writes=[O_t[t]])
        run_pipelined((gla_tile(t) for t in range(NT)), NB)


def moba_phase(C, l):
    P, nc, dr = C.P, C.nc, C.dr
    with contextlib.ExitStack() as st:
        O = alloc(C, st, "m_O", [128, NT, D], BF16)
        O_t = [T("m_O%d" % t) for t in range(NT)]
        with contextlib.ExitStack() as st2:
            moba_heads(C, l, st2, O, O_t)
            barrier(C)
        outproj_phase(C, l, O, O_t, dr["moba_w_out"])


def moba_heads(C, l, st, O, O_t):
    P, nc, dr = C.P, C.nc, C.dr
    H, DH = 8, 128
    PI = float(np.pi)
    XT = alloc(C, st, "m_XT", [128, 8, S], BF16)
    XT_t = {t: T("m_XT%d" % t) for t in range(NT)}
    build_xT(C, XT, XT_t, range(NT))
    win = dr["moba_w_in"].rearrange("(k p) n -> p k n", p=128)
    CT = alloc(C, st, "m_CT", [128, S], F32); CT_t = T("m_CT")
    ST = alloc(C, st, "m_ST", [128, S], F32); ST_t = T("m_ST")
    P.op("pool", lambda e: e.memset(CT[:, :], 1.0), writes=[CT_t])
    P.op("pool", lambda e: e.memset(ST[:, :], 0.0), writes=[ST_t])
    invf = alloc(C, st, "m_invf", [32, 1], F32); invf_t = T("m_invf")
    sgn = alloc(C, st, "m_sgn", [32, 1], F32); sgn_t = T("m_sgn")
    P.dma("sp", invf[:, :], dr["c_invf"][:, :], writes=[invf_t])
    P.dma("sp", sgn[:, :], dr["c_sgn"][:, :], writes=[sgn_t])
    posi = alloc(C, st, "m_posi", [32, 256], I32); posi_t = T("m_posi")
    ang = alloc(C, st, "m_ang", [32, 256], F32); ang_t = T("m_ang")
    r_ = alloc(C, st, "m_r", [32, 256], F32); r_t = T("m_r")
    ki_ = alloc(C, st, "m_ki", [32, 256], I32); ki_t = T("m_ki")
    th = alloc(C, st, "m_th", [32, 256], F32); th_t = T("m_th")
    mk_ = alloc(C, st, "m_mk", [32, 256], F32); mk_t = T("m_mk")
    pos_row = dr["positions"].rearrange("s o -> o s")
    for tb in range(8):
        cs = slice(tb * 256, (tb + 1) * 256)
        P.dma("sp", posi[:, :], pos_row[:, cs].to_broadcast([32, 256]), writes=[posi_t])
        P.op("dve", lambda e: e.tensor_copy(out=ang[:, :], in_=posi[:, :]), reads=[posi_t], writes=[ang_t])
        P.op("dve", lambda e: e.tensor_scalar(out=ang[:, :], in0=ang[:, :], scalar1=invf[:, 0:1], scalar2=None, op0=ALU.mult),
             reads=[ang_t, invf_t], writes=[ang_t])
        for (shift, dst, dst_t) in ((PI / 2, CT, CT_t), (0.0, ST, ST_t)):
            P.op("dve", lambda e, shift=shift: e.tensor_scalar(out=r_[:, :], in0=ang[:, :], scalar1=shift, scalar2=1.0 / (2 * PI),
                                                            op0=ALU.add, op1=ALU.mult), reads=[ang_t], writes=[r_t])
            P.op("dve", lambda e: e.tensor_copy(out=ki_[:, :], in_=r_[:, :]), reads=[r_t], writes=[ki_t])
            P.op("dve", lambda e: e.tensor_copy(out=r_[:, :], in_=ki_[:, :]), reads=[ki_t], writes=[r_t])
            P.op("dve", lambda e: e.scalar_tensor_tensor(out=th[:, :], in0=r_[:, :], scalar=-2 * PI, in1=ang[:, :],
                                                         op0=ALU.mult, op1=ALU.add), reads=[r_t, ang_t], writes=[th_t])
            if shift != 0.0:
                P.op("dve", lambda e, shift=shift: e.tensor_scalar(out=th[:, :], in0=th[:, :], scalar1=shift, scalar2=None, op0=ALU.add),
                     reads=[th_t], writes=[th_t])
            P.op("dve", lambda e: e.tensor_scalar(out=mk_[:, :], in0=th[:, :], scalar1=PI, scalar2=-2 * PI, op0=ALU.is_gt, op1=ALU.mult),
                 reads=[th_t], writes=[mk_t])
            P.op("dve", lambda e: e.tensor_tensor(out=th[:, :], in0=th[:, :], in1=mk_[:, :], op=ALU.add), reads=[th_t, mk_t], writes=[th_t])
            P.op("dve", lambda e: e.tensor_scalar(out=mk_[:, :], in0=th[:, :], scalar1=-PI, scalar2=2 * PI, op0=ALU.is_lt, op1=ALU.mult),
                 reads=[th_t], writes=[mk_t])
            P.op("dve", lambda e: e.tensor_tensor(out=th[:, :], in0=th[:, :], in1=mk_[:, :], op=ALU.add), reads=[th_t, mk_t], writes=[th_t])
            P.op("dve", lambda e: e.tensor_scalar(out=th[:, :], in0=th[:, :], scalar1=-PI, scalar2=PI, op0=ALU.max, op1=ALU.min),
                 reads=[th_t], writes=[th_t])
            P.op("act", lambda e, dst=dst, cs=cs: e.activation(out=dst[0:32, cs], in_=th[:, :], func=AF.Sin), reads=[th_t], writes=[dst_t])
    P.op("dve", lambda e: e.tensor_scalar(out=ST[0:32, :], in0=ST[0:32, :], scalar1=sgn[:, 0:1], scalar2=None, op0=ALU.mult),
         reads=[ST_t, sgn_t], writes=[ST_t])
    if C.dbg.get("moba_stop") == 1:
        return
    Ef = alloc(C, st, "m_Ef", [8, 1024], F32); Ef_t = T("m_Ef")
    P.dma("sp", Ef[:, :], dr["c_E"][:, :], writes=[Ef_t])
    Eb = alloc(C, st, "m_Eb", [8, 1024], BF16); Eb_t = T("m_Eb")
    P.op("dve", lambda e: e.tensor_copy(out=Eb[:, :], in_=Ef[:, :]), reads=[Ef_t], writes=[Eb_t])
    cnf = alloc(C, st, "m_cnf", [128, 128], F32); cnf_t = T("m_cnf")
    P.dma("sp", cnf[:, :], dr["c_causneg"][:, :], writes=[cnf_t])
    cnb = alloc(C, st, "m_cnb", [128, 128], BF16); cnb_t = T("m_cnb")
    P.op("dve", lambda e: e.tensor_copy(out=cnb[:, :], in_=cnf[:, :]), reads=[cnf_t], writes=[cnb_t])
    npast = alloc(C, st, "m_npast", [128, 64], F32); npast_t = T("m_npast")
    P.dma("sp", npast[:, :], dr["c_negpast"][:, :].to_broadcast([128, 64]), writes=[npast_t])
    if C.dbg.get("moba_stop") == 11:
        return
    Wh0 = alloc(C, st, "m_Wh", [128, 8, 640], BF16)
    Wh = [Wh0, Wh0]
    Wh_t0 = [T("m_Wh_%d" % j) for j in range(5)]
    Wh_t = [Wh_t0, Wh_t0]
    P.op("pool", lambda e: e.memset(Wh0[:, :, 384:640], 0.0), writes=[Wh_t0[3], Wh_t0[4]])
    qT0 = alloc(C, st, "m_qT", [128, S], BF16)
    kT0 = alloc(C, st, "m_kT", [128, S], BF16)
    qT = [qT0, qT0]
    kT = [kT0, kT0]
    qT_t0 = [T("m_qT_%d" % tb) for tb in range(4)]
    kT_t0 = [T("m_kT_%d" % tb) for tb in range(4)]
    qT_t = [qT_t0, qT_t0]
    kT_t = [kT_t0, kT_t0]
    vaug = [alloc(C, st, "m_va%d" % i, [128, NT, 130], BF16) for i in range(2)]
    va_t = [[T("m_va%d_%d" % (i, g)) for g in range(4)] for i in range(2)]
    for i in range(2):
        P.op("pool", lambda e, i=i: e.memset(vaug[i][:, :, 128:130], 1.0), writes=va_t[i])
    t1 = [alloc(C, st, "m_t1%d" % i, [128, 512], F32) for i in range(2)]
    t1_t = [T("m_t1%d" % i) for i in range(2)]
    t2 = [alloc(C, st, "m_t2%d" % i, [128, 512], F32) for i in range(2)]
    t2_t = [T("m_t2%d" % i) for i in range(2)]
    km = alloc(C, st, "m_km", [128, 8], F32); km_t = T("m_km")
    kmb = alloc(C, st, "m_kmb", [128, 8], BF16); kmb_t = T("m_kmb")
    NB = 2
    g_ = [alloc(C, st, "m_g%d" % i, [128, 8], F32) for i in range(NB)]; g_t = [T("m_g%d" % i) for i in range(NB)]
    mx = [alloc(C, st, "m_mx%d" % i, [128, 8], F32) for i in range(NB)]; mx_t = [T("m_mx%d" % i) for i in range(NB)]
    sm = [alloc(C, st, "m_sm%d" % i, [128, 8], F32) for i in range(NB)]; sm_t = [T("m_sm%d" % i) for i in range(NB)]
    selT = [alloc(C, st, "m_selT%d" % i, [8, 128], BF16) for i in range(NB)]; selT_t = [T("m_selT%d" % i) for i in range(NB)]
    rec = [alloc(C, st, "m_rec%d" % i, [128, 1], F32) for i in range(NB)]; rec_t = [T("m_rec%d" % i) for i in range(NB)]
    NPB = 4
    PT = [alloc(C, st, "m_PT%d" % i, [128, 128], BF16) for i in range(NPB)]; PT_t = [T("m_PT%d" % i) for i in range(NPB)]
    pt_rr = 0
    scale = float(DH ** -0.5)
    for h in range(H):
        wb = h % 2
        hb = h * 128
        segs = [(0, 128, hb), (128, 128, 1024 + hb), (256, 128, 2048 + hb)]
        for j, (o, w, src) in enumerate(segs):
            P.dma("pool", Wh[wb][:, :, o:o + w], win[:, :, src:src + w], writes=[Wh_t[wb][j]])
        P.dma("pool", Wh[wb][:, :, 384:400], win[:, :, hb + 16:hb + 32], writes=[Wh_t[wb][3]])
        P.dma("pool", Wh[wb][:, :, 400:416], win[:, :, hb:hb + 16], writes=[Wh_t[wb][3]])
        P.dma("pool", Wh[wb][:, :, 512:528], win[:, :, 1024 + hb + 16:1024 + hb + 32], writes=[Wh_t[wb][4]])
        P.dma("pool", Wh[wb][:, :, 528:544], win[:, :, 1024 + hb:1024 + hb + 16], writes=[Wh_t[wb][4]])
        if C.dbg.get("moba_stop") == 12:
            return
        for (dstT, dst_t, co, sw, wj, swj) in ((qT[wb], qT_t[wb], 0, 384, 0, 3), (kT[wb], kT_t[wb], 128, 512, 1, 4)):
            for tb in range(4):
                cs = slice(tb * 512, (tb + 1) * 512)
                xts = [XT_t[t] for t in range(tb * 4, tb * 4 + 4)]
                pq, pqt = next_ps(C, (2, 3, 4, 5, 6, 7))
                for k in range(8):
                    P.op("pe", lambda e, pq=pq, k=k, wb=wb, co=co, cs=cs: e.matmul(
                        pq[:, :], lhsT=Wh[wb][:, k, co:co + 128], rhs=XT[:, k, cs], start=(k == 0), stop=(k == 7)),
                        reads=[Wh_t[wb][wj]] + xts, writes=[pqt])
                pw, pwt = next_ps(C, (2, 3, 4, 5, 6, 7))
                for k in range(8):
                    P.op("pe", lambda e, pw=pw, k=k, wb=wb, sw=sw, cs=cs: e.matmul(
                        pw[:, :], lhsT=Wh[wb][:, k, sw:sw + 128], rhs=XT[:, k, cs], start=(k == 0), stop=(k == 7)),
                        reads=[Wh_t[wb][swj]] + xts, writes=[pwt])
                tbuf = tb % 2
                P.op("dve", lambda e, pq=pq, cs=cs, tbuf=tbuf: e.tensor_tensor(out=t1[tbuf][:, :], in0=pq[:, :], in1=CT[:, cs], op=ALU.mult),
                     reads=[pqt, CT_t], writes=[t1_t[tbuf]])
                P.op("dve", lambda e, pw=pw, cs=cs, tbuf=tbuf: e.tensor_tensor(out=t2[tbuf][:, :], in0=pw[:, :], in1=ST[:, cs], op=ALU.mult),
                     reads=[pwt, ST_t], writes=[t2_t[tbuf]])
                P.op("dve", lambda e, dstT=dstT, cs=cs, tbuf=tbuf: e.tensor_tensor(out=dstT[:, cs], in0=t1[tbuf][:, :], in1=t2[tbuf][:, :], op=ALU.add),
                     reads=[t1_t[tbuf], t2_t[tbuf]], writes=[dst_t[tb]])
        if C.dbg.get("moba_stop") == 2:
            return
        P.op("dve", lambda e, wb=wb: e.tensor_reduce(out=km[:, :], in_=kT[wb][:, :].rearrange("p (n b) -> p n b", b=256), axis=AX.X, op=ALU.add),
             reads=kT_t[wb], writes=[km_t])
        P.op("dve", lambda e: e.tensor_scalar(out=kmb[:, :], in0=km[:, :], scalar1=1.0 / 256.0, scalar2=None, op0=ALU.mult),
             reads=[km_t], writes=[kmb_t])
        for g4 in range(4):
            pv, pvt = next_ps(C, (2, 3, 4, 5, 6, 7))
            for j in range(4):
                t = g4 * 4 + j
                for k in range(8):
                    P.op("pe", lambda e, pv=pv, j=j, k=k, t=t, wb=wb: e.matmul(
                        pv[:, j * 128:(j + 1) * 128], lhsT=XT[:, k, t * 128:(t + 1) * 128], rhs=Wh[wb][:, k, 256:384],
                        start=(k == 0), stop=(k == 7)), reads=[Wh_t[wb][2], XT_t[t]], writes=[pvt])
            P.op("act", lambda e, pv=pv, g4=g4, wb=wb: e.activation(
                out=vaug[wb][:, g4 * 4:(g4 + 1) * 4, 0:128], in_=pv[:, :].rearrange("p (j n) -> p j n", j=4), func=AF.Copy),
                reads=[pvt], writes=[va_t[wb][g4]])
        if C.dbg.get("moba_stop") == 3:
            return
        for qt in range(C.dbg.get("moba_nqt", NT)):
            b = qt % NB
            blk = qt // 2
            qs = slice(qt * 128, (qt + 1) * 128)
            use_gate = blk >= 4
            if use_gate:
                pg, pgt = next_ps(C, (2, 3, 4, 5, 6, 7))
                P.op("pe", lambda e, pg=pg, wb=wb, qs=qs: e.matmul(pg[:, 0:8], lhsT=qT[wb][:, qs], rhs=kmb[:, :], start=True, stop=True),
                     reads=[qT_t[wb][qt // 4], kmb_t], writes=[pgt])
                P.op("dve", lambda e, pg=pg, b=b, blk=blk: e.tensor_tensor(out=g_[b][:, :], in0=pg[:, 0:8], in1=npast[:, blk * 8:(blk + 1) * 8], op=ALU.add),
                     reads=[pgt, npast_t], writes=[g_t[b]])
                P.op("dve", lambda e, b=b: e.max(out=mx[b][:, :], in_=g_[b][:, :]), reads=[g_t[b]], writes=[mx_t[b]])
                P.op("dve", lambda e, b=b: e.tensor_scalar(out=sm[b][:, :], in0=g_[b][:, :], scalar1=mx[b][:, 2:3], scalar2=-1.0,
                                                         op0=ALU.is_ge, op1=ALU.add), reads=[g_t[b], mx_t[b]], writes=[sm_t[b]])
                pt2, pt2t = next_ps(C, (2, 3, 4, 5, 6, 7))
                P.op("pe", lambda e, pt2=pt2, b=b: e.transpose(out=pt2[0:8, 0:128], in_=sm[b][:, 0:8], identity=C.ident[:, :]),
                     reads=[sm_t[b], C.ident_t], writes=[pt2t])
                P.op("act", lambda e, pt2=pt2, b=b: e.activation(out=selT[b][:, :], in_=pt2[0:8, 0:128], func=AF.Copy),
                     reads=[pt2t], writes=[selT_t[b]])
            chunks = list(range(0, 2 * blk)) + ([qt - 1, qt] if qt % 2 == 1 else [qt])
            po, pot = next_ps(C, (0, 1))
            LA = 2
            pbs = {}

            def emit_s(ci, kc, qt=qt, qs=qs, wb=wb, b=b, blk=blk, use_gate=use_gate):
                nonlocal pt_rr
                past = kc < 2 * blk
                diag = kc == qt
                ks = slice(kc * 128, (kc + 1) * 128)
                ps_, pst_ = next_ps(C, (2, 3, 4, 5, 6, 7))
                extra = (past and use_gate) or diag
                P.op("pe", lambda e, ps_=ps_, ks=ks, extra=extra, wb=wb, qs=qs: e.matmul(
                    ps_[:, 0:128], lhsT=kT[wb][:, ks], rhs=qT[wb][:, qs], start=True, stop=not extra),
                    reads=[kT_t[wb][kc // 4], qT_t[wb][qt // 4]], writes=[pst_])
                if past and use_gate:
                    n = kc // 2
                    P.op("pe", lambda e, ps_=ps_, n=n, b=b: e.matmul(
                        ps_[:, 0:128], lhsT=Eb[:, n * 128:(n + 1) * 128], rhs=selT[b][:, :], start=False, stop=True),
                        reads=[Eb_t, selT_t[b]], writes=[pst_])
                if diag:
                    P.op("pe", lambda e, ps_=ps_: e.matmul(ps_[:, 0:128], lhsT=C.identb[:, :], rhs=cnb[:, :], start=False, stop=True),
                         reads=[C.identb_t, cnb_t], writes=[pst_])
                pb = pt_rr % NPB
                pt_rr += 1
                pbs[ci] = pb
                P.op("act", lambda e, ps_=ps_, pb=pb: e.activation(out=PT[pb][:, :], in_=ps_[:, 0:128], func=AF.Exp, scale=scale),
                     reads=[pst_], writes=[PT_t[pb]])

            def emit_pv(ci, kc, po=po, pot=pot, wb=wb, pbs=pbs, chunks=chunks):
                pb = pbs[ci]
                P.op("pe", lambda e, pb=pb, kc=kc, ci=ci, nch=len(chunks), po=po, wb=wb: e.matmul(
                    po[:, 0:129], lhsT=PT[pb][:, :], rhs=vaug[wb][:, kc, 0:129], start=(ci == 0), stop=(ci == nch - 1)),
                    reads=[PT_t[pb], va_t[wb][kc // 4]], writes=[pot])

            nch_ = len(chunks)
            for ci in range(nch_ + LA):
                if ci < nch_:
                    emit_s(ci, chunks[ci])
                if ci - LA >= 0:
                    emit_pv(ci - LA, chunks[ci - LA])
            P.op("dve", lambda e, po=po, b=b: e.reciprocal(out=rec[b][:, :], in_=po[:, 128:129]), reads=[pot], writes=[rec_t[b]])
            P.op("dve", lambda e, po=po, b=b, qt=qt, hb=hb: e.tensor_scalar(
                out=O[:, qt, hb:hb + 128], in0=po[:, 0:128], scalar1=rec[b][:, 0:1], scalar2=None, op0=ALU.mult),
                reads=[pot, rec_t[b]], writes=[O_t[qt]])


def gdn_phase(C, l):
    P, nc, dr = C.P, C.nc, C.dr
    with contextlib.ExitStack() as st:
        O = alloc(C, st, "d_O", [128, NT, D], BF16)
        O_t = [T("d_O%d" % t) for t in range(NT)]
        with contextlib.ExitStack() as st2:
            gdn_heads(C, l, st2, O, O_t)
            barrier(C)
        outproj_phase(C, l, O, O_t, dr["gdn_w_out"])


def gdn_heads(C, l, st, O, O_t):
    P, nc, dr = C.P, C.nc, C.dr
    H, DK = 8, 128
    XT = alloc(C, st, "d_XT", [128, 8, S], BF16)
    XT_t = {t: T("d_XT%d" % t) for t in range(NT)}
    build_xT(C, XT, XT_t, range(NT))
    allx = [XT_t[t] for t in range(NT)]
    win = dr["gdn_w_in"].rearrange("(k p) n -> p k n", p=128)
    PS6 = (2, 3, 4, 5, 6, 7)
    Wab = alloc(C, st, "d_Wab", [128, 8, 16], BF16); Wab_t = T("d_Wab")
    P.dma("pool", Wab[:, :, :], win[:, :, 4096:4112], writes=[Wab_t])
    alog = alloc(C, st, "d_alog", [128, 8], F32); alog_t = T("d_alog")
    dtb = alloc(C, st, "d_dtb", [128, 8], F32); dtb_t = T("d_dtb")
    P.dma("sp", alog[:, :], dr["gdn_a_log"][0:1, :].to_broadcast([128, 8]), writes=[alog_t])
    P.dma("sp", dtb[:, :], dr["gdn_dt_bias"][0:1, :].to_broadcast([128, 8]), writes=[dtb_t])
    P.op("act", lambda e: e.activation(out=alog[:, :], in_=alog[:, :], func=AF.Exp), reads=[alog_t], writes=[alog_t])
    ng = alloc(C, st, "d_ng", [128, 128], F32); ng_t = T("d_ng")
    load_bcast_row(C, ng[:, :], ng_t, dr["gdn_norm_g"][0:1, :])
    def sc(name):
        return alloc(C, st, name, [128, NT, 8], F32), T(name)
    g_all, g_t = sc("d_g"); beta, beta_t = sc("d_beta"); cum, cum_t = sc("d_cum"); tot, tot_t = sc("d_tot")
    ecum, ecum_t = sc("d_ecum"); ncum, ncum_t = sc("d_ncum"); bec, bec_t = sc("d_bec"); eend, eend_t = sc("d_eend")
    ecl, ecl_t = sc("d_ecl"); tmp, tmp_t = sc("d_tmp")
    pab, pabt = next_ps(C)
    for t in range(NT):
        for k in range(8):
            P.op("pe", lambda e, t=t, k=k: e.matmul(pab[:, t * 16:(t + 1) * 16], lhsT=XT[:, k, t * 128:(t + 1) * 128], rhs=Wab[:, k, :],
                                                    start=(k == 0), stop=(k == 7)), reads=[XT_t[t], Wab_t], writes=[pabt])
    pab3 = pab[:, 0:256].rearrange("p (t c) -> p t c", c=16)
    for t in range(NT):
        P.op("dve", lambda e, t=t: e.tensor_tensor(out=tmp[:, t, :], in0=pab3[:, t, 0:8], in1=dtb[:, :], op=ALU.add),
             reads=[pabt, dtb_t], writes=[tmp_t])
    P.op("act", lambda e: e.activation(out=tmp[:, :, :], in_=tmp[:, :, :], func=AF.Exp), reads=[tmp_t], writes=[tmp_t])
    P.op("act", lambda e: e.activation(out=tmp[:, :, :], in_=tmp[:, :, :], func=AF.Ln, bias=1.0, scale=1.0), reads=[tmp_t], writes=[tmp_t])
    for t in range(NT):
        P.op("dve", lambda e, t=t: e.scalar_tensor_tensor(out=g_all[:, t, :], in0=tmp[:, t, :], scalar=-1.0, in1=alog[:, :],
                                                          op0=ALU.mult, op1=ALU.mult), reads=[tmp_t, alog_t], writes=[g_t])
    P.op("act", lambda e: e.activation(out=beta[:, :, :], in_=pab3[:, :, 8:16], func=AF.Sigmoid), reads=[pabt], writes=[beta_t])
    pcm, pcmt = next_ps(C)
    for t in range(NT):
        P.op("pe", lambda e, t=t: e.matmul(pcm[:, t * 8:(t + 1) * 8], lhsT=C.cm["m_incl"][:, :], rhs=g_all[:, t, :], start=True, stop=True),
             reads=[g_t, C.cm_t["m_incl"]], writes=[pcmt])
        P.op("pe", lambda e, t=t: e.matmul(pcm[:, 128 + t * 8:128 + (t + 1) * 8], lhsT=C.cm["ones"][:, :], rhs=g_all[:, t, :], start=True, stop=True),
             reads=[g_t, C.cm_t["ones"]], writes=[pcmt])
    P.op("act", lambda e: e.activation(out=cum[:, :, :], in_=pcm[:, 0:128].rearrange("p (t c) -> p t c", c=8), func=AF.Copy),
         reads=[pcmt], writes=[cum_t])
    P.op("act", lambda e: e.activation(out=tot[:, :, :], in_=pcm[:, 128:256].rearrange("p (t c) -> p t c", c=8), func=AF.Copy),
         reads=[pcmt], writes=[tot_t])
    P.op("act", lambda e: e.activation(out=ecum[:, :, :], in_=cum[:, :, :], func=AF.Exp), reads=[cum_t], writes=[ecum_t])
    P.op("act", lambda e: e.activation(out=ecl[:, :, :], in_=tot[:, :, :], func=AF.Exp), reads=[tot_t], writes=[ecl_t])
    P.op("dve", lambda e: e.tensor_scalar(out=ncum[:, :, :], in0=cum[:, :, :], scalar1=-1.0, scalar2=None, op0=ALU.mult),
         reads=[cum_t], writes=[ncum_t])
    P.op("dve", lambda e: e.tensor_tensor(out=bec[:, :, :], in0=beta[:, :, :], in1=ecum[:, :, :], op=ALU.mult),
         reads=[beta_t, ecum_t], writes=[bec_t])
    P.op("dve", lambda e: e.tensor_tensor(out=eend[:, :, :], in0=tot[:, :, :], in1=cum[:, :, :], op=ALU.subtract),
         reads=[tot_t, cum_t], writes=[eend_t])
    P.op("act", lambda e: e.activation(out=eend[:, :, :], in_=eend[:, :, :], func=AF.Exp), reads=[eend_t], writes=[eend_t])
    sc_reads = [g_t, beta_t, cum_t, ecum_t, ncum_t, bec_t, eend_t, ecl_t]
    if C.dbg.get("gdn_stop") == 1:
        return
    Wh = alloc(C, st, "d_Wh", [128, 8, 512], BF16)
    Wh_t = [T("d_Wh_%d" % j) for j in range(4)]
    cw = alloc(C, st, "d_cw", [128, 12], F32); cw_t = T("d_cw")
    pre = alloc(C, st, "d_pre", [128, S + 3], F32); pre_t = T("d_pre")
    P.op("pool", lambda e: e.memset(pre[:, 0:3], 0.0), writes=[pre_t])
    yb = alloc(C, st, "d_y", [128, S], F32); yb_t = T("d_y")
    vT = alloc(C, st, "d_vT", [128, S], F32); vT_t = T("d_vT")
    qTn = alloc(C, st, "d_qTn", [128, S], BF16); qTn_t = T("d_qTn")
    kTn = alloc(C, st, "d_kTn", [128, S], BF16); kTn_t = T("d_kTn")
    rs = alloc(C, st, "d_rs", [128, 512], F32); rs_t = T("d_rs")
    Sst = alloc(C, st, "d_S", [128, 128], F32); S_t = T("d_S")
    Sbf = alloc(C, st, "d_Sbf", [128, 128], BF16); Sbf_t = T("d_Sbf")
    def mk(name, shape, dt, n=2):
        return [alloc(C, st, "%s%d" % (name, i), shape, dt) for i in range(n)], [T("%s%d" % (name, i)) for i in range(n)]
    NS = C.dbg.get("gdn_ns", 4)
    Dm, Dm_t = mk("d_Dm", [128, 128], F32, NS)
    DTm, DTm_t = mk("d_DTm", [128, 128], F32, NS)
    Lm, Lm_t = mk("d_L", [128, 128], F32, NS)
    LT, LT_t = mk("d_LT", [128, 128], F32, NS)
    Pa, Pa_t = Dm, Dm_t
    PTa, PTa_t = DTm, DTm_t
    Pb, Pb_t = mk("d_Pb", [128, 128], F32, NS)
    PTb, PTb_t = mk("d_PTb", [128, 128], F32, NS)
    qkT, qkT_t = mk("d_qkT", [128, 128], BF16, NS)
    kend, kend_t = mk("d_kend", [128, 128], BF16, NS)
    Xa, Xa_t = mk("d_Xa", [128, 256], F32, NS)
    Xb, Xb_t = mk("d_Xb", [128, 256], F32, NS)
    wT, wT_t = mk("d_wT", [128, 128], BF16, NS)
    vnew, vnew_t = mk("d_vnew", [128, 128], BF16, NS)
    intra, intra_t = Pb, Pb_t
    osb, osb_t = PTb, PTb_t
    sg, sg_t = DTm, DTm_t
    junk, junk_t = Dm, Dm_t
    ss, ss_t = mk("d_ss", [128, 4], F32, NS)
    cwsrc = dr["gdn_conv_w"].rearrange("j c -> c j")
    for h in range(H):
        hb = h * 128
        for j in range(4):
            P.dma("pool", Wh[:, :, j * 128:(j + 1) * 128], win[:, :, j * 1024 + hb:j * 1024 + hb + 128], writes=[Wh_t[j]])
        for s3 in range(3):
            P.dma("sp", cw[:, s3 * 4:(s3 + 1) * 4], cwsrc[s3 * 1024 + hb:s3 * 1024 + hb + 128, :], writes=[cw_t],
                  allow_slow_non_contiguous=True)
        P.op("pool", lambda e: e.memset(Sst[:, :], 0.0), writes=[S_t])
        P.op("pool", lambda e: e.memset(Sbf[:, :], 0.0), writes=[Sbf_t])
        for s3 in range(3):
            for tb in range(4):
                cs = slice(tb * 512, (tb + 1) * 512)
                pp, ppt = next_ps(C)
                for k in range(8):
                    P.op("pe", lambda e, pp=pp, k=k, s3=s3, cs=cs: e.matmul(
                        pp[:, :], lhsT=Wh[:, k, s3 * 128:(s3 + 1) * 128], rhs=XT[:, k, cs], start=(k == 0), stop=(k == 7)),
                        reads=[Wh_t[s3]] + allx[tb * 4:tb * 4 + 4], writes=[ppt])
                P.op("act", lambda e, pp=pp, tb=tb: e.activation(out=pre[:, 3 + tb * 512:3 + (tb + 1) * 512], in_=pp[:, :], func=AF.Copy),
                     reads=[ppt], writes=[pre_t])
            dst, dst_t = (vT, vT_t) if s3 == 2 else (yb, yb_t)
            P.op("dve", lambda e, dst=dst, s3=s3: e.tensor_scalar(out=dst[:, :], in0=pre[:, 3:3 + S], scalar1=cw[:, s3 * 4 + 3:s3 * 4 + 4],
                                                                 scalar2=None, op0=ALU.mult), reads=[pre_t, cw_t], writes=[dst_t])
            for j in range(3):
                P.op("dve", lambda e, dst=dst, s3=s3, j=j: e.scalar_tensor_tensor(
                    out=dst[:, :], in0=pre[:, j:j + S], scalar=cw[:, s3 * 4 + j:s3 * 4 + j + 1], in1=dst[:, :], op0=ALU.mult, op1=ALU.add),
                    reads=[pre_t, cw_t, dst_t], writes=[dst_t])
            P.op("act", lambda e, dst=dst: e.activation(out=dst[:, :], in_=dst[:, :], func=AF.Silu), reads=[dst_t], writes=[dst_t])
            if s3 < 2:
                outT, outT_t = (qTn, qTn_t) if s3 == 0 else (kTn, kTn_t)
                scl = float(DK ** -0.5) if s3 == 0 else 1.0
                P.op("dve", lambda e: e.tensor_tensor(out=pre[:, 3:3 + S], in0=yb[:, :], in1=yb[:, :], op=ALU.mult),
                     reads=[yb_t, pre_t], writes=[pre_t])
                for tb in range(4):
                    cs = slice(tb * 512, (tb + 1) * 512)
                    pq, pqt = next_ps(C)
                    P.op("pe", lambda e, pq=pq, tb=tb: e.matmul(pq[:, :], lhsT=C.cm["ones"][:, :], rhs=pre[:, 3 + tb * 512:3 + (tb + 1) * 512],
                                                               start=True, stop=True), reads=[pre_t, C.cm_t["ones"]], writes=[pqt])
                    P.op("act", lambda e, pq=pq: e.activation(out=rs[:, :], in_=pq[:, :], func=AF.Ln, bias=C.eps6[:, 0:1], scale=1.0),
                         reads=[pqt, C.eps6_t], writes=[rs_t])
                    P.op("act", lambda e: e.activation(out=rs[:, :], in_=rs[:, :], func=AF.Exp, scale=-0.5), reads=[rs_t], writes=[rs_t])
                    P.op("dve", lambda e, outT=outT, cs=cs, scl=scl: e.scalar_tensor_tensor(
                        out=outT[:, cs], in0=yb[:, cs], scalar=scl, in1=rs[:, :], op0=ALU.mult, op1=ALU.mult),
                        reads=[yb_t, rs_t], writes=[outT_t])
        def gdn_tile(t, h=h, hb=hb):
            b = t % NS
            tok = slice(t * 128, (t + 1) * 128)
            col = lambda arr: arr[:, t, h:h + 1]
            pa, pat = next_ps(C)
            P.op("pe", lambda e, pa=pa, tok=tok: e.matmul(pa[:, 0:128], lhsT=kTn[:, tok], rhs=kTn[:, tok], start=True, stop=True),
                 reads=[kTn_t], writes=[pat])
            P.op("pe", lambda e, pa=pa, tok=tok: e.matmul(pa[:, 128:256], lhsT=kTn[:, tok], rhs=qTn[:, tok], start=True, stop=True),
                 reads=[kTn_t, qTn_t], writes=[pat])
            pb, pbt = next_ps(C)
            gbc = g_all[:, t, h:h + 1].to_broadcast([128, 128])
            for (o, mname) in ((0, "g_pos"), (128, "g_negt")):
                P.op("pe", lambda e, pb=pb, o=o, gbc=gbc: e.matmul(pb[:, o:o + 128], lhsT=gbc, rhs=C.cm["m_incl"][:, :], start=True, stop=False),
                     reads=[g_t, C.cm_t["m_incl"]], writes=[pbt])
                P.op("pe", lambda e, pb=pb, o=o, mname=mname: e.matmul(pb[:, o:o + 128], lhsT=C.ident[:, :], rhs=C.cm[mname][:, :], start=False, stop=True),
                     reads=[C.ident_t, C.cm_t[mname]], writes=[pbt])
            pc, pct = next_ps(C)
            P.op("pe", lambda e, pc=pc, tok=tok: e.transpose(out=pc[:, 0:128], in_=vT[:, tok], identity=C.ident[:, :]),
                 reads=[vT_t, C.ident_t], writes=[pct])
            pd, pdt = next_ps(C)
            pdb = pd[:, :].bitcast(BF16)
            P.op("pe", lambda e, pdb=pdb, tok=tok: e.transpose(out=pdb[:, 0:128], in_=kTn[:, tok], identity=C.identb[:, :]),
                 reads=[kTn_t, C.identb_t], writes=[pdt])
            yield
            P.op("act", lambda e, pb=pb, b=b, t=t, h=h: e.activation(out=Dm[b][:, :], in_=pb[:, 0:128], func=AF.Exp, bias=cum[:, t, h:h + 1], scale=-1.0),
                 reads=[pbt, cum_t], writes=[Dm_t[b]])
            P.op("act", lambda e, pb=pb, b=b, t=t, h=h: e.activation(out=DTm[b][:, :], in_=pb[:, 128:256], func=AF.Exp, bias=ncum[:, t, h:h + 1], scale=1.0),
                 reads=[pbt, ncum_t], writes=[DTm_t[b]])
            P.op("dve", lambda e, pa=pa, b=b, t=t, h=h: e.scalar_tensor_tensor(
                out=Lm[b][:, :], in0=pa[:, 0:128], scalar=beta[:, t, h:h + 1], in1=Dm[b][:, :], op0=ALU.mult, op1=ALU.mult),
                reads=[pat, beta_t, Dm_t[b]], writes=[Lm_t[b]])
            P.op("dve", lambda e, pa=pa, b=b: e.tensor_tensor(out=qkT[b][:, :], in0=pa[:, 128:256], in1=DTm[b][:, :], op=ALU.mult),
                 reads=[pat, DTm_t[b]], writes=[qkT_t[b]])
            P.op("dve", lambda e, pc=pc, b=b, t=t, h=h: e.tensor_scalar(out=Xa[b][:, 0:128], in0=pc[:, 0:128], scalar1=beta[:, t, h:h + 1],
                                                                     scalar2=None, op0=ALU.mult), reads=[pct, beta_t], writes=[Xa_t[b]])
            P.op("dve", lambda e, pdb=pdb, b=b, t=t, h=h: e.tensor_scalar(out=Xa[b][:, 128:256], in0=pdb[:, 0:128], scalar1=bec[:, t, h:h + 1],
                                                                      scalar2=None, op0=ALU.mult), reads=[pdt, bec_t], writes=[Xa_t[b]])
            P.op("act", lambda e, pdb=pdb, b=b, t=t, h=h: e.activation(out=kend[b][:, :], in_=pdb[:, 0:128], func=AF.Copy, scale=eend[:, t, h:h + 1]),
                 reads=[pdt, eend_t], writes=[kend_t[b]])
            yield
            pe_, pet = next_ps(C)
            P.op("pe", lambda e, pe_=pe_, b=b: e.transpose(out=pe_[:, 0:128], in_=Lm[b][:, :], identity=C.ident[:, :]),
                 reads=[Lm_t[b], C.ident_t], writes=[pet])
            P.op("act", lambda e, pe_=pe_, b=b: e.activation(out=LT[b][:, :], in_=pe_[:, 0:128], func=AF.Copy), reads=[pet], writes=[LT_t[b]])
            yield
            px, pxt = next_ps(C)
            P.op("pe", lambda e, px=px, b=b: e.matmul(px[:, 0:256], lhsT=LT[b][:, :], rhs=Xa[b][:, :], start=True, stop=True),
                 reads=[LT_t[b], Xa_t[b]], writes=[pxt])
            P.op("dve", lambda e, px=px, b=b: e.tensor_tensor(out=Xb[b][:, :], in0=Xa[b][:, :], in1=px[:, 0:256], op=ALU.subtract),
                 reads=[Xa_t[b], pxt], writes=[Xb_t[b]])
            Xc, Xc_t, Xn, Xn_t = Xb[b], Xb_t[b], Xa[b], Xa_t[b]
            Pc, Pc_t, PTc, PTc_t = Lm[b], Lm_t[b], LT[b], LT_t[b]
            for lev in range(1, 7):
                if lev % 2 == 1:
                    Pn, Pn_t, PTn, PTn_t = Pa[b], Pa_t[b], PTa[b], PTa_t[b]
                else:
                    Pn, Pn_t, PTn, PTn_t = Pb[b], Pb_t[b], PTb[b], PTb_t[b]
                yield
                pp2, pp2t = next_ps(C)
                P.op("pe", lambda e, pp2=pp2, Pc=Pc, PTc=PTc: e.matmul(pp2[:, 0:128], lhsT=Pc[:, :], rhs=PTc[:, :], start=True, stop=True),
                     reads=[Pc_t, PTc_t], writes=[pp2t])
                if lev < 6:
                    P.op("pe", lambda e, pp2=pp2, Pc=Pc, PTc=PTc: e.matmul(pp2[:, 128:256], lhsT=PTc[:, :], rhs=Pc[:, :], start=True, stop=True),
                         reads=[Pc_t, PTc_t], writes=[pp2t])
                P.op("act", lambda e, pp2=pp2, PTn=PTn: e.activation(out=PTn[:, :], in_=pp2[:, 0:128], func=AF.Copy), reads=[pp2t], writes=[PTn_t])
                if lev < 6:
                    P.op("dve", lambda e, pp2=pp2, Pn=Pn: e.tensor_scalar(out=Pn[:, :], in0=pp2[:, 128:256], scalar1=1.0, scalar2=None, op0=ALU.mult),
                         reads=[pp2t], writes=[Pn_t])
                yield
                px2, px2t = next_ps(C)
                P.op("pe", lambda e, px2=px2, PTn=PTn, Xc=Xc: e.matmul(px2[:, 0:256], lhsT=PTn[:, :], rhs=Xc[:, :], start=True, stop=True),
                     reads=[PTn_t, Xc_t], writes=[px2t])
                P.op("dve", lambda e, px2=px2, Xc=Xc, Xn=Xn: e.tensor_tensor(out=Xn[:, :], in0=Xc[:, :], in1=px2[:, 0:256], op=ALU.add),
                     reads=[Xc_t, px2t], writes=[Xn_t])
                Xc, Xc_t, Xn, Xn_t = Xn, Xn_t, Xc, Xc_t
                Pc, Pc_t, PTc, PTc_t = Pn, Pn_t, PTn, PTn_t
            yield
            pw_, pwt_ = next_ps(C)
            P.op("pe", lambda e, pw_=pw_, Xc=Xc: e.transpose(out=pw_[:, 0:128], in_=Xc[:, 128:256], identity=C.ident[:, :]),
                 reads=[Xc_t, C.ident_t], writes=[pwt_])
            P.op("act", lambda e, pw_=pw_, b=b: e.activation(out=wT[b][:, :], in_=pw_[:, 0:128], func=AF.Copy), reads=[pwt_], writes=[wT_t[b]])
            yield
            pv, pvt = next_ps(C)
            P.op("pe", lambda e, pv=pv, b=b: e.matmul(pv[:, 0:128], lhsT=wT[b][:, :], rhs=Sbf[:, :], start=True, stop=True),
                 reads=[wT_t[b], Sbf_t], writes=[pvt])
            P.op("dve", lambda e, pv=pv, b=b, Xc=Xc: e.tensor_tensor(out=vnew[b][:, :], in0=Xc[:, 0:128], in1=pv[:, 0:128], op=ALU.subtract),
                 reads=[Xc_t, pvt], writes=[vnew_t[b]])
            po, pot = next_ps(C)
            P.op("pe", lambda e, po=po, b=b: e.matmul(po[:, 0:128], lhsT=qkT[b][:, :], rhs=vnew[b][:, :], start=True, stop=True),
                 reads=[qkT_t[b], vnew_t[b]], writes=[pot])
            P.op("pe", lambda e, po=po, tok=tok: e.matmul(po[:, 128:256], lhsT=qTn[:, tok], rhs=Sbf[:, :], start=True, stop=True),
                 reads=[qTn_t, Sbf_t], writes=[pot])
            P.op("act", lambda e, po=po, b=b: e.activation(out=intra[b][:, :], in_=po[:, 0:128], func=AF.Copy), reads=[pot], writes=[intra_t[b]])
            P.op("dve", lambda e, po=po, b=b, t=t, h=h: e.scalar_tensor_tensor(
                out=osb[b][:, :], in0=po[:, 128:256], scalar=ecum[:, t, h:h + 1], in1=intra[b][:, :], op0=ALU.mult, op1=ALU.add),
                reads=[pot, ecum_t, intra_t[b]], writes=[osb_t[b]])
            pu, put = next_ps(C)
            P.op("pe", lambda e, pu=pu, b=b: e.matmul(pu[:, 0:128], lhsT=kend[b][:, :], rhs=vnew[b][:, :], start=True, stop=True),
                 reads=[kend_t[b], vnew_t[b]], writes=[put])
            P.op("dve", lambda e, pu=pu, t=t, h=h: e.scalar_tensor_tensor(
                out=Sst[:, :], in0=Sst[:, :], scalar=ecl[:, t, h:h + 1], in1=pu[:, 0:128], op0=ALU.mult, op1=ALU.add),
                reads=[S_t, ecl_t, put], writes=[S_t])
            P.op("act", lambda e: e.activation(out=Sbf[:, :], in_=Sst[:, :], func=AF.Copy), reads=[S_t], writes=[Sbf_t])
            yield
            pz, pzt = next_ps(C)
            for k in range(8):
                P.op("pe", lambda e, pz=pz, k=k, tok=tok: e.matmul(pz[:, 0:128], lhsT=XT[:, k, tok], rhs=Wh[:, k, 384:512], start=(k == 0), stop=(k == 7)),
                     reads=[XT_t[t], Wh_t[3]], writes=[pzt])
            P.op("act", lambda e, pz=pz, b=b: e.activation(out=sg[b][:, :], in_=pz[:, 0:128], func=AF.Silu), reads=[pzt], writes=[sg_t[b]])
            P.op("dve", lambda e, b=b: e.tensor_tensor(out=sg[b][:, :], in0=sg[b][:, :], in1=ng[:, :], op=ALU.mult),
                 reads=[sg_t[b], ng_t], writes=[sg_t[b]])
            P.op("act", lambda e, b=b: e.activation(out=junk[b][:, :], in_=osb[b][:, :], func=AF.Square, accum_out=ss[b][:, 0:1]),
                 reads=[osb_t[b]], writes=[junk_t[b], ss_t[b]])
            P.op("act", lambda e, b=b: e.activation(out=ss[b][:, 1:2], in_=ss[b][:, 0:1], func=AF.Ln, bias=C.eps6[:, 0:1], scale=1.0 / 128),
                 reads=[ss_t[b], C.eps6_t], writes=[ss_t[b]])
            P.op("act", lambda e, b=b: e.activation(out=ss[b][:, 2:3], in_=ss[b][:, 1:2], func=AF.Exp, scale=-0.5),
                 reads=[ss_t[b]], writes=[ss_t[b]])
            P.op("dve", lambda e, b=b, t=t, hb=hb: e.scalar_tensor_tensor(
                out=O[:, t, hb:hb + 128], in0=osb[b][:, :], scalar=ss[b][:, 2:3], in1=sg[b][:, :], op0=ALU.mult, op1=ALU.mult),
                reads=[osb_t[b], ss_t[b], sg_t[b]], writes=[O_t[t]])


        run_pipelined((gdn_tile(t) for t in range(NT)), NS)
def hgrn_phase(C, l):
    P, nc, dr = C.P, C.nc, C.dr
    with contextlib.ExitStack() as st:
        O = alloc(C, st, "h_O", [128, NT, D], BF16)
        O_t = [T("h_O%d" % t) for t in range(NT)]
        with contextlib.ExitStack() as st2:
            hgrn_heads(C, l, st2, O, O_t)
            barrier(C)
        outproj_phase(C, l, O, O_t, dr["hgrn_w_out"])


def hgrn_lb(C, st, l, src_ap, shape, name):
    P = C.P
    pn, n = shape
    hb = alloc(C, st, name + "_hb", [pn, 4, n], F32); hb_t = T(name + "_hb")
    P.dma("sp", hb[:, :, :], src_ap, writes=[hb_t], allow_slow_non_contiguous=True)
    mx = alloc(C, st, name + "_mx", [pn, n], F32); mx_t = T(name + "_mx")
    P.op("dve", lambda e: e.tensor_tensor(out=mx[:, :], in0=hb[:, 0, :], in1=hb[:, 1, :], op=ALU.max), reads=[hb_t], writes=[mx_t])
    for j in (2, 3):
        P.op("dve", lambda e, j=j: e.tensor_tensor(out=mx[:, :], in0=mx[:, :], in1=hb[:, j, :], op=ALU.max), reads=[hb_t, mx_t], writes=[mx_t])
    for j in range(4):
        P.op("dve", lambda e, j=j: e.tensor_tensor(out=hb[:, j, :], in0=hb[:, j, :], in1=mx[:, :], op=ALU.subtract), reads=[hb_t, mx_t], writes=[hb_t])
    P.op("act", lambda e: e.activation(out=hb[:, :, :], in_=hb[:, :, :], func=AF.Exp), reads=[hb_t], writes=[hb_t])
    den = mx
    P.op("dve", lambda e: e.tensor_tensor(out=den[:, :], in0=hb[:, 0, :], in1=hb[:, 1, :], op=ALU.add), reads=[hb_t, mx_t], writes=[mx_t])
    for j in (2, 3):
        P.op("dve", lambda e, j=j: e.tensor_tensor(out=den[:, :], in0=den[:, :], in1=hb[:, j, :], op=ALU.add), reads=[hb_t, mx_t], writes=[mx_t])
    P.op("dve", lambda e: e.reciprocal(out=den[:, :], in_=den[:, :]), reads=[mx_t], writes=[mx_t])
    lb = alloc(C, st, name + "_lb", [pn, n], F32); lb_t = T(name + "_lb")
    oml = alloc(C, st, name + "_oml", [pn, n], F32)
    if l == 0:
        P.op("dve", lambda e: e.memset(lb[:, :], 0.0), writes=[lb_t])
    else:
        P.op("dve", lambda e: e.tensor_copy(out=lb[:, :], in_=hb[:, 1, :]), reads=[hb_t], writes=[lb_t])
        for j in range(2, l + 1):
            P.op("dve", lambda e, j=j: e.tensor_tensor(out=lb[:, :], in0=lb[:, :], in1=hb[:, j, :], op=ALU.add), reads=[hb_t, lb_t], writes=[lb_t])
        P.op("dve", lambda e: e.tensor_tensor(out=lb[:, :], in0=lb[:, :], in1=den[:, :], op=ALU.mult), reads=[mx_t, lb_t], writes=[lb_t])
    P.op("dve", lambda e: e.tensor_scalar(out=oml[:, :], in0=lb[:, :], scalar1=-1.0, scalar2=1.0, op0=ALU.mult, op1=ALU.add),
         reads=[lb_t], writes=[lb_t])
    return lb, oml, lb_t


def hgrn_heads(C, l, st, O, O_t):
    P, nc, dr = C.P, C.nc, C.dr
    H, DK, DV = 8, 128, 128
    XT = alloc(C, st, "h_XT", [128, 8, S], BF16)
    XT_t = {t: T("h_XT%d" % t) for t in range(NT)}
    build_xT(C, XT, XT_t, range(NT))
    win = dr["hgrn_w_in"].rearrange("(k p) n -> p k n", p=128)
    lbb, omlb, lbb_t = hgrn_lb(C, st, l, dr["hgrn_lower_bounds"].rearrange("(o l) n -> o l n", o=1).to_broadcast([128, 4, D]),
                               (128, D), "h_b")
    lbT, omlT, lbT_t = hgrn_lb(C, st, l, dr["hgrn_lower_bounds"].rearrange("l (c p) -> p l c", p=128), (128, 8), "h_T")
    ng = alloc(C, st, "h_ng", [128, DV], F32); ng_t = T("h_ng")
    load_bcast_row(C, ng[:, :], ng_t, dr["hgrn_norm_g"][0:1, :])
    Wh = [alloc(C, st, "h_Wh%d" % i, [128, 8, 512], BF16) for i in range(2)]
    Wh_t = [[T("h_Wh%d_%d" % (i, j)) for j in range(4)] for i in range(2)]
    state = alloc(C, st, "h_state", [128, DV], F32); state_t = T("h_state")
    state_bf = alloc(C, st, "h_statebf", [128, DV], BF16); statebf_t = T("h_statebf")
    NB = 2
    def mk(name, shape, dt):
        return [alloc(C, st, "%s%d" % (name, i), shape, dt) for i in range(NB)], [T("%s%d" % (name, i)) for i in range(NB)]
    sig, sig_t = mk("h_sig", [128, 128], F32)
    a_, a_t = mk("h_a", [128, 128], F32)
    fg, fg_t = mk("h_fg", [128, 128], F32)
    kin, kin_t = mk("h_kin", [128, 128], F32)
    la, la_t = mk("h_la", [128, 128], F32)
    sgT, sgT_t = mk("h_sgT", [128, 128], F32)
    ecT, ecT_t = mk("h_ecT", [128, 128], F32)
    eiT, eiT_t = mk("h_eiT", [128, 128], F32)
    eR, eR_t = mk("h_eR", [128, 128], F32)
    qd, qd_t = mk("h_qd", [128, 128], BF16)
    ki, ki_t = mk("h_ki", [128, 128], BF16)
    ke, ke_t = mk("h_ke", [128, 128], BF16)
    vb, vb_t = mk("h_vb", [128, DV], BF16)
    sg, sg_t = mk("h_sg", [128, DV], F32)
    sT, sT_t = mk("h_sT", [128, 128], BF16)
    junk, junk_t = mk("h_junk", [128, DV], F32)
    ss, ss_t = mk("h_ss", [128, 4], F32)
    for h in range(H):
        wb = h % 2
        hs = slice(h * 128, (h + 1) * 128)
        for j in range(4):
            P.dma("pool", Wh[wb][:, :, j * 128:(j + 1) * 128], win[:, :, j * 1024 + h * 128:j * 1024 + (h + 1) * 128], writes=[Wh_t[wb][j]])
        P.op("pool", lambda e: e.memset(state[:, :], 0.0), writes=[state_t])
        P.op("pool", lambda e: e.memset(state_bf[:, :], 0.0), writes=[statebf_t])
        def hgrn_tile(t, h=h, wb=wb):
            b = t % NB
            pool = (0, 1, 2, 3) if b == 0 else (4, 5, 6, 7)
            tok = slice(t * 128, (t + 1) * 128)
            p1, p1t = next_ps(C, pool)
            for j in range(2):
                for k in range(8):
                    P.op("pe", lambda e, p1=p1, j=j, k=k, wb=wb, tok=tok: e.matmul(
                        p1[:, j * 128:(j + 1) * 128], lhsT=Wh[wb][:, k, j * 128:(j + 1) * 128], rhs=XT[:, k, tok],
                        start=(k == 0), stop=(k == 7)), reads=[Wh_t[wb][j], XT_t[t]], writes=[p1t])
            p2, p2t = next_ps(C, pool)
            for k in range(8):
                P.op("pe", lambda e, p2=p2, k=k, wb=wb, tok=tok: e.matmul(
                    p2[:, 0:384], lhsT=XT[:, k, tok], rhs=Wh[wb][:, k, 128:512], start=(k == 0), stop=(k == 7)),
                    reads=[Wh_t[wb][1], Wh_t[wb][2], Wh_t[wb][3], XT_t[t]], writes=[p2t])
            yield
            P.op("act", lambda e, p2=p2, b=b: e.activation(out=sig[b][:, :], in_=p2[:, 0:128], func=AF.Sigmoid),
                 reads=[p2t], writes=[sig_t[b]])
            P.op("dve", lambda e, b=b, hs=hs: e.tensor_tensor(out=a_[b][:, :], in0=sig[b][:, :], in1=omlb[:, hs], op=ALU.mult),
                 reads=[sig_t[b], lbb_t], writes=[a_t[b]])
            P.op("dve", lambda e, b=b, hs=hs: e.tensor_tensor(out=fg[b][:, :], in0=a_[b][:, :], in1=lbb[:, hs], op=ALU.add),
                 reads=[a_t[b], lbb_t], writes=[fg_t[b]])
            P.op("dve", lambda e, b=b, hs=hs: e.tensor_tensor(out=kin[b][:, :], in0=omlb[:, hs], in1=a_[b][:, :], op=ALU.subtract),
                 reads=[a_t[b], lbb_t], writes=[kin_t[b]])
            P.op("act", lambda e, b=b: e.activation(out=la[b][:, :], in_=fg[b][:, :], func=AF.Ln),
                 reads=[fg_t[b]], writes=[la_t[b]])
            P.op("act", lambda e, p1=p1, b=b: e.activation(out=sgT[b][:, :], in_=p1[:, 128:256], func=AF.Sigmoid, scale=-1.0),
                 reads=[p1t], writes=[sgT_t[b]])
            yield
            p4, p4t = next_ps(C, pool)
            P.op("pe", lambda e, p4=p4, b=b: e.matmul(p4[:, 0:128], lhsT=la[b][:, :], rhs=C.cm["m_incl"][:, :], start=True, stop=True),
                 reads=[la_t[b], C.cm_t["m_incl"]], writes=[p4t])
            P.op("pe", lambda e, p4=p4, b=b: e.matmul(p4[:, 128:256], lhsT=C.cm["m_rev"][:, :], rhs=la[b][:, :], start=True, stop=True),
                 reads=[la_t[b], C.cm_t["m_rev"]], writes=[p4t])
            P.op("act", lambda e, p4=p4, b=b: e.activation(out=ecT[b][:, :], in_=p4[:, 0:128], func=AF.Exp),
                 reads=[p4t], writes=[ecT_t[b]])
            P.op("act", lambda e, p4=p4, b=b: e.activation(out=eiT[b][:, :], in_=p4[:, 0:128], func=AF.Exp, scale=-1.0),
                 reads=[p4t], writes=[eiT_t[b]])
            P.op("act", lambda e, p4=p4, b=b: e.activation(out=eR[b][:, :], in_=p4[:, 128:256], func=AF.Exp),
                 reads=[p4t], writes=[eR_t[b]])
            P.op("dve", lambda e, p1=p1, b=b: e.tensor_tensor(out=qd[b][:, :], in0=p1[:, 0:128], in1=ecT[b][:, :], op=ALU.mult),
                 reads=[p1t, ecT_t[b]], writes=[qd_t[b]])
            P.op("dve", lambda e, b=b, h=h: e.scalar_tensor_tensor(
                out=ki[b][:, :], in0=sgT[b][:, :], scalar=omlT[:, h:h + 1], in1=eiT[b][:, :], op0=ALU.mult, op1=ALU.mult),
                reads=[sgT_t[b], lbT_t, eiT_t[b]], writes=[ki_t[b]])
            P.op("dve", lambda e, b=b: e.tensor_tensor(out=ke[b][:, :], in0=kin[b][:, :], in1=eR[b][:, :], op=ALU.mult),
                 reads=[kin_t[b], eR_t[b]], writes=[ke_t[b]])
            P.op("act", lambda e, p2=p2, b=b: e.activation(out=vb[b][:, :], in_=p2[:, 128:256], func=AF.Copy),
                 reads=[p2t], writes=[vb_t[b]])
            P.op("act", lambda e, p2=p2, b=b: e.activation(out=sg[b][:, :], in_=p2[:, 256:384], func=AF.Silu),
                 reads=[p2t], writes=[sg_t[b]])
            P.op("dve", lambda e, b=b: e.tensor_tensor(out=sg[b][:, :], in0=sg[b][:, :], in1=ng[:, :], op=ALU.mult),
                 reads=[sg_t[b], ng_t], writes=[sg_t[b]])
            yield
            p5, p5t = next_ps(C, pool)
            P.op("pe", lambda e, p5=p5, b=b: e.matmul(p5[:, 0:128], lhsT=ki[b][:, :], rhs=qd[b][:, :], start=True, stop=True),
                 reads=[ki_t[b], qd_t[b]], writes=[p5t])
            P.op("dve", lambda e, p5=p5, b=b: e.tensor_tensor(out=sT[b][:, :], in0=p5[:, 0:128], in1=C.cm["m_caus"][:, :], op=ALU.mult),
                 reads=[p5t, C.cm_t["m_caus"]], writes=[sT_t[b]])
            yield
            p6, p6t = next_ps(C, pool)
            P.op("pe", lambda e, p6=p6, b=b: e.matmul(p6[:, 0:DV], lhsT=sT[b][:, :], rhs=vb[b][:, :], start=True, stop=False),
                 reads=[sT_t[b], vb_t[b]], writes=[p6t])
            P.op("pe", lambda e, p6=p6, b=b: e.matmul(p6[:, 0:DV], lhsT=qd[b][:, :], rhs=state_bf[:, :], start=False, stop=True),
                 reads=[qd_t[b], statebf_t], writes=[p6t])
            yield
            p7, p7t = next_ps(C, pool)
            P.op("pe", lambda e, p7=p7, b=b: e.matmul(p7[:, 0:DV], lhsT=ke[b][:, :], rhs=vb[b][:, :], start=True, stop=True),
                 reads=[ke_t[b], vb_t[b]], writes=[p7t])
            P.op("dve", lambda e, p7=p7, b=b: e.scalar_tensor_tensor(
                out=state[:, :], in0=state[:, :], scalar=ecT[b][:, 127:128], in1=p7[:, 0:DV], op0=ALU.mult, op1=ALU.add),
                reads=[state_t, ecT_t[b], p7t], writes=[state_t])
            P.op("act", lambda e: e.activation(out=state_bf[:, :], in_=state[:, :], func=AF.Copy),
                 reads=[state_t], writes=[statebf_t])
            yield
            P.op("act", lambda e, p6=p6, b=b: e.activation(out=junk[b][:, :], in_=p6[:, 0:DV], func=AF.Square, accum_out=ss[b][:, 0:1]),
                 reads=[p6t], writes=[junk_t[b], ss_t[b]])
            P.op("act", lambda e, b=b: e.activation(out=ss[b][:, 1:2], in_=ss[b][:, 0:1], func=AF.Ln, bias=C.eps6[:, 0:1], scale=1.0 / DV),
                 reads=[ss_t[b], C.eps6_t], writes=[ss_t[b]])
            P.op("act", lambda e, b=b: e.activation(out=ss[b][:, 2:3], in_=ss[b][:, 1:2], func=AF.Exp, scale=-0.5),
                 reads=[ss_t[b]], writes=[ss_t[b]])
            P.op("dve", lambda e, p6=p6, b=b, t=t, h=h: e.scalar_tensor_tensor(
                out=O[:, t, h * DV:(h + 1) * DV], in0=p6[:, 0:DV], scalar=ss[b][:, 2:3], in1=sg[b][:, :], op0=ALU.mult, op1=ALU.mult),
                reads=[p6t, ss_t[b], sg_t[b]], writes=[O_t[t]])
        run_pipelined((hgrn_tile(t) for t in range(NT)), NB)


def make_in_map(inputs, b):
    m = {}
    m["x"] = np.ascontiguousarray(inputs["x"][b])
    m["positions"] = np.ascontiguousarray(inputs["positions"][b].reshape(S, 1).astype(np.int32))
    for k in ("gla_w_in", "gla_w_gk", "gla_b_gk", "gla_norm_g", "gla_w_out", "moba_w_in", "moba_w_out", "gdn_w_in",
              "gdn_conv_w", "gdn_a_log", "gdn_dt_bias", "gdn_norm_g", "gdn_w_out", "hgrn_w_in", "hgrn_norm_g",
              "hgrn_w_out"):
        v = np.asarray(inputs[k])[0]
        if v.ndim == 1:
            v = v.reshape(1, -1)
        m[k] = np.ascontiguousarray(v)
    m["hgrn_lower_bounds"] = np.ascontiguousarray(inputs["hgrn_lower_bounds"])
    m["ffn_w_gu"] = np.ascontiguousarray(inputs["ffn_w_gu"])
    m["ffn_w_down"] = np.ascontiguousarray(inputs["ffn_w_down"])
    m["ln_g"] = np.ascontiguousarray(np.asarray(inputs["ln_g"]).reshape(DEPTH * 2, D))
    m["ln_b"] = np.ascontiguousarray(np.asarray(inputs["ln_b"]).reshape(DEPTH * 2, D))
    for k, v in host_consts().items():
        m["c_" + k] = v
    return m


def _todo(C, l):
    raise NotImplementedError


MIXERS = [gla_phase, moba_phase, gdn_phase, hgrn_phase]
_NC_CACHE = {}


def kernel(**inputs):
    if "nc" not in _NC_CACHE:
        _NC_CACHE["nc"] = build()
    nc = _NC_CACHE["nc"]
    in_maps = [make_in_map(inputs, b) for b in range(NCORES)]
    res = run_bass_kernel_spmd(nc, in_maps, core_ids=list(range(NCORES)))
    return np.stack([np.asarray(r["y"]) for r in res.results], axis=0).astype(np.float32)
```

```python
import contextlib
import numpy as np
import concourse.bass as bass
import concourse.mybir as mybir
from concourse.bass_utils import run_bass_kernel_spmd

F32 = mybir.dt.float32
BF16 = mybir.dt.bfloat16
I32 = mybir.dt.int32
AF = mybir.ActivationFunctionType
ALU = mybir.AluOpType
AX = mybir.AxisListType

D = 1024
S = 2048
NT = S // 128
DEPTH = 4
FFN_H = 2816
ALPHA = (2.0 * DEPTH) ** 0.25
NCORES = 8


class T:
    __slots__ = ("name", "w", "r", "excl")

    def __init__(self, name, excl=False):
        self.name = name
        self.w = None
        self.r = []
        self.excl = excl


class Prog:
    ENGS = ("pe", "dve", "act", "pool", "sp")
    NLANES = 6

    def __init__(self, nc):
        self.nc = nc
        self.ins = []
        self.pending = {e: set() for e in self.ENGS}
        self.last_barrier = 0
        self.eng_obj = {"pe": nc.tensor, "dve": nc.vector, "act": nc.scalar, "pool": nc.gpsimd, "sp": nc.sync}

    def op(self, eng, fn, reads=(), writes=(), dma=False):
        idx = len(self.ins)
        deps = set()
        for t in reads:
            if t.w is not None:
                deps.add(t.w)
            if t.excl:
                deps.update(r for r in t.r if self.ins[r]["eng"] != eng)
        for t in writes:
            if t.w is not None:
                deps.add(t.w)
            deps.update(t.r)
        if self.pending[eng]:
            deps |= self.pending[eng]
            self.pending[eng] = set()
        if eng == "pe":
            deps = {d for d in deps if self.ins[d]["eng"] != "pe" or self.ins[d]["dma"]}
        self.ins.append(dict(eng=eng, fn=fn, deps=deps, dma=dma))
        for t in reads:
            t.r.append(idx)
        for t in writes:
            t.w = idx
            t.r = []
        return idx

    def barrier(self):
        last = {}
        deps = set()
        for i in range(self.last_barrier, len(self.ins)):
            ins = self.ins[i]
            if ins["dma"]:
                deps.add(i)
            else:
                last[ins["eng"]] = i
        deps |= set(last.values())
        for e in self.ENGS:
            self.pending[e] |= deps
        self.last_barrier = len(self.ins)

    def dma(self, eng, out, in_, reads=(), writes=(), **kw):
        return self.op(eng, lambda e: e.dma_start(out=out, in_=in_, **kw), reads, writes, dma=True)

    def finalize(self):
        nc = self.nc
        waited = set()
        for ins in self.ins:
            waited.update(ins["deps"])
        sems = {}
        with contextlib.ExitStack() as es:
            for e in self.ENGS:
                sems[e] = es.enter_context(nc.semaphore("s_" + e))
            for q in ("sp", "act", "pool"):
                for l in range(self.NLANES):
                    sems[(q, l)] = es.enter_context(nc.semaphore("d_%s%d" % (q, l)))
            cnt = {k: 0 for k in sems}
            sig = {}
            lane_rr = {"sp": 0, "act": 0, "pool": 0}
            lane_prev = {}
            for idx, ins in enumerate(self.ins):
                if ins["dma"]:
                    q = ins["eng"]
                    lane = (q, lane_rr[q] % self.NLANES)
                    lane_rr[q] += 1
                    ins["lane_prev"] = cnt[lane]
                    cnt[lane] += 16
                    sig[idx] = (lane, cnt[lane])
                elif idx in waited:
                    cnt[ins["eng"]] += 1
                    sig[idx] = (ins["eng"], cnt[ins["eng"]])
            self.sig_counts = dict(cnt)
            self.wait_hist = {}
            with nc.Block() as block:
                for eng in self.ENGS:
                    my = [(i, ins) for i, ins in enumerate(self.ins) if ins["eng"] == eng]
                    if not my:
                        continue

                    def body(e, my=my, eng=eng):
                        seen = {}
                        for idx, ins in my:
                            needs = {}
                            for d in ins["deps"]:
                                sk, val = sig[d]
                                if needs.get(sk, 0) < val:
                                    needs[sk] = val
                            if ins["dma"]:
                                sk, val = sig[idx]
                                if ins["lane_prev"] > 0 and needs.get(sk, 0) < ins["lane_prev"]:
                                    needs[sk] = ins["lane_prev"]
                            nw = 0
                            for sk, val in needs.items():
                                if seen.get(sk, 0) < val:
                                    e.wait_ge(sems[sk], val)
                                    seen[sk] = val
                                    nw += 1
                            self.wait_hist[nw] = self.wait_hist.get(nw, 0) + 1
                            r = ins["fn"](e)
                            if idx in sig:
                                sk, val = sig[idx]
                                r.then_inc(sems[sk], 16 if ins["dma"] else 1)
                        for l in range(self.NLANES):
                            sk = (eng, l)
                            if sk in cnt and cnt[sk] > 0 and seen.get(sk, 0) < cnt[sk]:
                                e.wait_ge(sems[sk], cnt[sk])

                    getattr(block, {"pe": "tensor", "dve": "vector", "act": "scalar", "pool": "gpsimd", "sp": "sync"}[eng])(body)


class Ctx:
    pass


def host_consts():
    c = {}
    c["ident"] = np.eye(128, dtype=np.float32)
    i = np.arange(128)
    c["m_incl"] = (i[:, None] <= i[None, :]).astype(np.float32)
    c["m_rev"] = (i[:, None] > i[None, :]).astype(np.float32)
    c["m_caus"] = (i[:, None] <= i[None, :]).astype(np.float32)
    inv = (500000.0 ** (-np.arange(0, 32, 2, dtype=np.float32) / 32.0)).astype(np.float32)
    c["invf"] = np.concatenate([inv, inv]).reshape(32, 1).astype(np.float32)
    c["sgn"] = np.concatenate([-np.ones(16), np.ones(16)]).reshape(32, 1).astype(np.float32)
    E = np.zeros((8, 8, 128), np.float32)
    for n in range(8):
        E[n, n, :] = 30000.0
    c["E"] = E.reshape(8, 1024)
    c["causneg"] = np.where(i[:, None] > i[None, :], -30000.0, 0.0).astype(np.float32)
    npast = np.where(np.arange(8)[None, :] < np.arange(8)[:, None], 0.0, -1e30).astype(np.float32)
    c["negpast"] = npast.reshape(1, 64)
    BIG = 1.0e5
    c["g_pos"] = np.where(i[None, :] >= i[:, None], BIG, 0.0).astype(np.float32)
    c["g_negt"] = np.where(i[None, :] < i[:, None], -BIG, 0.0).astype(np.float32)
    c["ones"] = np.ones((128, 128), np.float32)
    c["m_incl_gla"] = c["m_incl"] * np.float32(-1.0 / 16.0)
    c["m_rev_gla"] = c["m_rev"] * np.float32(-1.0 / 16.0)
    return c


def build(n_layers=DEPTH, dbg=None, layers=None):
    dbg = dbg or {}
    nc = bass.Bass("TRN2", target_bir_lowering=False)
    dr = {}

    def din(name, shape, dt=F32):
        dr[name] = nc.dram_tensor(name, list(shape), dt, kind="ExternalInput").ap()
        return dr[name]

    din("x", [S, D])
    din("positions", [S, 1], I32)
    din("gla_w_in", [D, 3088]); din("gla_w_gk", [16, 512]); din("gla_b_gk", [1, 512]); din("gla_norm_g", [1, 256])
    din("gla_w_out", [D, D])
    din("moba_w_in", [D, 3072]); din("moba_w_out", [D, D])
    din("gdn_w_in", [D, 4112]); din("gdn_conv_w", [4, 3072]); din("gdn_a_log", [1, 8]); din("gdn_dt_bias", [1, 8])
    din("gdn_norm_g", [1, 128]); din("gdn_w_out", [D, D])
    din("hgrn_lower_bounds", [4, 1024]); din("hgrn_w_in", [D, 4096]); din("hgrn_norm_g", [1, 128])
    din("hgrn_w_out", [D, D])
    din("ffn_w_gu", [DEPTH, D, 2 * FFN_H]); din("ffn_w_down", [DEPTH, FFN_H, D])
    din("ln_g", [DEPTH * 2, D]); din("ln_b", [DEPTH * 2, D])
    for k, v in host_consts().items():
        din("c_" + k, v.shape)
    y_out = nc.dram_tensor("y", [S, D], F32, kind="ExternalOutput").ap()

    P = Prog(nc)
    C = Ctx()
    C.nc, C.P, C.dr, C.dbg = nc, P, dr, dbg
    with contextlib.ExitStack() as es:
        C.es = es
        C.X = es.enter_context(nc.sbuf_tensor("X", [128, NT, D], F32))
        C.Xt = [T("X%d" % t) for t in range(NT)]
        C.ps = [es.enter_context(nc.psum_tensor("ps%d" % i, [128, 512], F32)) for i in range(8)]
        C.pst = [T("ps%d" % i, excl=True) for i in range(8)]
        C.ident = es.enter_context(nc.sbuf_tensor("ident", [128, 128], F32))
        C.ident_t = T("ident")
        P.dma("sp", C.ident[:, :], dr["c_ident"][:, :], writes=[C.ident_t])
        C.eps5 = es.enter_context(nc.sbuf_tensor("eps5", [128, 1], F32))
        C.eps_t = T("eps5")
        P.op("pool", lambda e: e.memset(C.eps5[:, :], 1e-5), writes=[C.eps_t])
        C.ps_rr = 0
        C.ps_pool_rr = {}
        C.cm = {}
        C.cm_t = {}
        for nm in ("m_incl", "m_rev", "m_caus", "m_incl_gla", "m_rev_gla", "g_pos", "g_negt", "ones"):
            C.cm[nm] = es.enter_context(nc.sbuf_tensor("k_" + nm, [128, 128], F32))
            C.cm_t[nm] = T("c_" + nm)
            P.dma("sp", C.cm[nm][:, :], dr["c_" + nm][:, :], writes=[C.cm_t[nm]])
        C.identb = es.enter_context(nc.sbuf_tensor("identb", [128, 128], BF16))
        C.identb_t = T("identb")
        P.op("dve", lambda e: e.tensor_copy(out=C.identb[:, :], in_=C.ident[:, :]), reads=[C.ident_t], writes=[C.identb_t])
        C.eps6 = es.enter_context(nc.sbuf_tensor("eps6", [128, 1], F32))
        C.eps6_t = T("eps6")
        P.op("pool", lambda e: e.memset(C.eps6[:, :], 1e-6), writes=[C.eps6_t])
        C.ones1 = es.enter_context(nc.sbuf_tensor("ones1", [1, 128], F32))
        C.ones1_t = T("ones1")
        P.op("pool", lambda e: e.memset(C.ones1[:, :], 1.0), writes=[C.ones1_t])
        for t in range(NT):
            P.dma("sp", C.X[:, t, :], dr["x"][t * 128:(t + 1) * 128, :], writes=[C.Xt[t]])
        for l in (layers if layers is not None else range(n_layers)):
            if not dbg.get("skip_mixer"):
                MIXERS[l % 4](C, l)
            if not dbg.get("skip_ffn"):
                ffn_phase(C, l)
        for t in range(NT):
            P.dma("sp", y_out[t * 128:(t + 1) * 128, :], C.X[:, t, :], reads=[C.Xt[t]])
        P.finalize()
    C.P = P
    build.last_prog = P
    return nc


def load_bcast_row(C, dst, dst_t, src_row_ap, eng="sp"):
    n = src_row_ap.shape[-1]
    C.P.dma(eng, dst, src_row_ap.to_broadcast([128, n]), writes=[dst_t])


def layer_norm_tile(C, t, zsrc, G, Bv, G_t, B_t, wk):
    P, nc = C.P, C.nc
    z, z_t, st, st_t, mv, mv_t, sc, sc_t = wk
    xt = C.Xt[t]
    for h, (pap, pT) in enumerate(zsrc):
        sl = slice(h * 512, (h + 1) * 512)
        P.op("dve", lambda e, sl=sl, pap=pap: e.scalar_tensor_tensor(
            out=z[:, sl], in0=C.X[:, t, sl], scalar=ALPHA, in1=pap, op0=ALU.mult, op1=ALU.add),
            reads=[xt, pT], writes=[z_t])
    for h in range(2):
        sl = slice(h * 512, (h + 1) * 512)
        P.op("dve", lambda e, sl=sl, h=h: e.bn_stats(out=st[:, h * 6:(h + 1) * 6], in_=z[:, sl]),
             reads=[z_t], writes=[st_t])
    P.op("dve", lambda e: e.bn_aggr(out=mv[:, 0:2], in_=st[:, 0:12]), reads=[st_t], writes=[mv_t])
    P.op("act", lambda e: e.activation(out=sc[:, 0:1], in_=mv[:, 1:2], func=AF.Ln, bias=C.eps5[:, 0:1], scale=1.0),
         reads=[mv_t, C.eps_t], writes=[sc_t])
    P.op("act", lambda e: e.activation(out=sc[:, 1:2], in_=sc[:, 0:1], func=AF.Exp, scale=-0.5),
         reads=[sc_t], writes=[sc_t])
    P.op("dve", lambda e: e.scalar_tensor_tensor(out=sc[:, 2:3], in0=mv[:, 0:1], scalar=-1.0, in1=sc[:, 1:2],
                                                 op0=ALU.mult, op1=ALU.mult), reads=[mv_t, sc_t], writes=[sc_t])
    P.op("act", lambda e: e.activation(out=z[:, :], in_=z[:, :], func=AF.Identity, bias=sc[:, 2:3], scale=sc[:, 1:2]),
         reads=[z_t, sc_t], writes=[z_t])
    P.op("dve", lambda e: e.tensor_tensor(out=z[:, :], in0=z[:, :], in1=G[:, :], op=ALU.mult),
         reads=[z_t, G_t], writes=[z_t])
    P.op("dve", lambda e: e.tensor_tensor(out=C.X[:, t, :], in0=z[:, :], in1=Bv[:, :], op=ALU.add),
         reads=[z_t, B_t], writes=[xt])


_ALLOC_CTR = [0]


def alloc(C, st, name, shape, dt):
    _ALLOC_CTR[0] += 1
    return st.enter_context(C.nc.sbuf_tensor("%s_%d" % (name, _ALLOC_CTR[0]), list(shape), dt))


def barrier(C):
    C.P.barrier()


def build_xT(C, XT, XT_t, tiles, evac_eng="act"):
    P = C.P
    for t in tiles:
        for half in range(2):
            pi = C.ps_rr % 8
            C.ps_rr += 1
            ps, pt = C.ps[pi], C.pst[pi]
            for c4 in range(4):
                c = half * 4 + c4
                P.op("pe", lambda e, ps=ps, c=c, c4=c4, t=t: e.transpose(
                    out=ps[:, c4 * 128:(c4 + 1) * 128], in_=C.X[:, t, c * 128:(c + 1) * 128], identity=C.ident[:, :]),
                    reads=[C.Xt[t], C.ident_t], writes=[pt])
            dst = XT[:, half * 4:(half + 1) * 4, t * 128:(t + 1) * 128]
            src = ps[:, :].rearrange("p (c n) -> p c n", c=4)
            if evac_eng == "act":
                P.op("act", lambda e, dst=dst, src=src: e.activation(out=dst, in_=src, func=AF.Copy),
                     reads=[pt], writes=[XT_t[t]])
            else:
                P.op("dve", lambda e, dst=dst, src=src: e.tensor_copy(out=dst, in_=src), reads=[pt], writes=[XT_t[t]])


def ffn_phase(C, l):
    P, nc, dr = C.P, C.nc, C.dr
    HG = 256
    NG = FFN_H // HG
    NHC = FFN_H // 128
    with contextlib.ExitStack() as st:
        XT = alloc(C, st, "f_XT", [128, 8, S], BF16)
        XT_t = {t: T("f_XT%d" % t) for t in range(NT)}
        Wd = alloc(C, st, "f_Wd", [128, NHC, D], BF16)
        Wd_t = [T("f_Wd%d" % i) for i in range(NHC)]
        Wg = [alloc(C, st, "f_Wg%d" % i, [128, 8, 2 * HG], BF16) for i in range(2)]
        Wg_t = [(T("f_Wgg%d" % i), T("f_Wgu%d" % i)) for i in range(2)]
        hT = alloc(C, st, "f_hT", [128, NHC, 512], BF16)
        hT_t = [T("f_hT%d" % i) for i in range(NHC)]
        sg = [alloc(C, st, "f_sg%d" % i, [128, 512], F32) for i in range(2)]
        sg_t = [T("f_sg%d" % i) for i in range(2)]
        G = alloc(C, st, "f_G", [128, D], F32); G_t = T("f_G")
        Bv = alloc(C, st, "f_B", [128, D], F32); B_t = T("f_B")
        z = alloc(C, st, "f_z", [128, D], F32); z_t = T("f_z")
        stt = alloc(C, st, "f_st", [128, 12], F32); st_t = T("f_st")
        mv = alloc(C, st, "f_mv", [128, 2], F32); mv_t = T("f_mv")
        sc = alloc(C, st, "f_sc", [128, 4], F32); sc_t = T("f_sc")
        wk = (z, z_t, stt, st_t, mv, mv_t, sc, sc_t)
        load_bcast_row(C, G[:, :], G_t, dr["ln_g"][2 * l + 1:2 * l + 2, :])
        load_bcast_row(C, Bv[:, :], B_t, dr["ln_b"][2 * l + 1:2 * l + 2, :])
        wd_src = dr["ffn_w_down"][l].rearrange("(c p) n -> p c n", p=128)
        for c in range(0, NHC, 2):
            P.dma("pool", Wd[:, c:c + 2, :], wd_src[:, c:c + 2, :], writes=Wd_t[c:c + 2])
        wgu = dr["ffn_w_gu"][l].rearrange("(k p) n -> p k n", p=128)
        gi = 0
        for tb in range(4):
            build_xT(C, XT, XT_t, range(tb * 4, tb * 4 + 4))
            xts = [XT_t[t] for t in range(tb * 4, tb * 4 + 4)]
            for g in range(NG):
                b = gi % 2
                gi += 1
                P.dma("pool", Wg[b][:, :, 0:HG], wgu[:, :, g * HG:(g + 1) * HG], writes=[Wg_t[b][0]])
                P.dma("pool", Wg[b][:, :, HG:2 * HG], wgu[:, :, FFN_H + g * HG:FFN_H + (g + 1) * HG], writes=[Wg_t[b][1]])
                for cc in range(HG // 128):
                    hc = g * (HG // 128) + cc
                    pg_i, pu_i = C.ps_rr % 8, (C.ps_rr + 1) % 8
                    C.ps_rr += 2
                    for (pi, off, wt) in ((pg_i, 0, Wg_t[b][0]), (pu_i, HG, Wg_t[b][1])):
                        for k in range(8):
                            P.op("pe", lambda e, pi=pi, off=off, k=k, b=b, cc=cc, tb=tb: e.matmul(
                                C.ps[pi][:, :], lhsT=Wg[b][:, k, off + cc * 128:off + (cc + 1) * 128],
                                rhs=XT[:, k, tb * 512:(tb + 1) * 512], start=(k == 0), stop=(k == 7)),
                                reads=[wt] + xts, writes=[C.pst[pi]])
                    sb = hc % 2
                    P.op("act", lambda e, sb=sb, pg_i=pg_i: e.activation(out=sg[sb][:, :], in_=C.ps[pg_i][:, :], func=AF.Silu),
                         reads=[C.pst[pg_i]], writes=[sg_t[sb]])
                    P.op("dve", lambda e, sb=sb, pu_i=pu_i, hc=hc: e.tensor_tensor(
                        out=hT[:, hc, :], in0=sg[sb][:, :], in1=C.ps[pu_i][:, :], op=ALU.mult),
                        reads=[sg_t[sb], C.pst[pu_i]], writes=[hT_t[hc]])
            for tt in range(4):
                t = tb * 4 + tt
                zs = []
                for cb in range(2):
                    pi = C.ps_rr % 8
                    C.ps_rr += 1
                    for hc in range(NHC):
                        P.op("pe", lambda e, pi=pi, hc=hc, tt=tt, cb=cb: e.matmul(
                            C.ps[pi][:, :], lhsT=hT[:, hc, tt * 128:(tt + 1) * 128],
                            rhs=Wd[:, hc, cb * 512:(cb + 1) * 512], start=(hc == 0), stop=(hc == NHC - 1)),
                            reads=[hT_t[hc], Wd_t[hc]], writes=[C.pst[pi]])
                    zs.append((C.ps[pi][:, :], C.pst[pi]))
                layer_norm_tile(C, t, zs, G, Bv, G_t, B_t, wk)
        barrier(C)


def run_pipelined(gens, width):
    it = iter(gens)
    active = []
    exhausted = False
    while True:
        if len(active) < width and not exhausted:
            try:
                active.append(next(it))
            except StopIteration:
                exhausted = True
        if not active:
            break
        for g in list(active):
            try:
                next(g)
            except StopIteration:
                active.remove(g)


def next_ps(C, pool=None):
    if pool is None:
        i = C.ps_rr % 8
        C.ps_rr += 1
    else:
        k = C.ps_pool_rr.get(pool, 0)
        C.ps_pool_rr[pool] = k + 1
        i = pool[k % len(pool)]
    return C.ps[i], C.pst[i]


def outproj_phase(C, l, O, O_t, w_out_ap):
    P, nc, dr = C.P, C.nc, C.dr
    with contextlib.ExitStack() as st:
        Wo = alloc(C, st, "o_Wo", [128, 8, D], BF16)
        Wo_t = [T("o_Wo%d" % i) for i in range(8)]
        src = w_out_ap.rearrange("(c p) n -> p c n", p=128)
        for c in range(0, 8, 2):
            P.dma("pool", Wo[:, c:c + 2, :], src[:, c:c + 2, :], writes=Wo_t[c:c + 2])
        G = alloc(C, st, "o_G", [128, D], F32); G_t = T("o_G")
        Bv = alloc(C, st, "o_B", [128, D], F32); B_t = T("o_B")
        z = alloc(C, st, "o_z", [128, D], F32); z_t = T("o_z")
        stt = alloc(C, st, "o_st", [128, 12], F32); st_t = T("o_st")
        mv = alloc(C, st, "o_mv", [128, 2], F32); mv_t = T("o_mv")
        sc = alloc(C, st, "o_sc", [128, 4], F32); sc_t = T("o_sc")
        wk = (z, z_t, stt, st_t, mv, mv_t, sc, sc_t)
        load_bcast_row(C, G[:, :], G_t, dr["ln_g"][2 * l:2 * l + 1, :])
        load_bcast_row(C, Bv[:, :], B_t, dr["ln_b"][2 * l:2 * l + 1, :])
        oT = [alloc(C, st, "o_oT%d" % i, [128, 8, 128], BF16) for i in range(2)]
        oT_t = [T("o_oT%d" % i) for i in range(2)]
        for t in range(NT):
            b = t % 2
            ps, pt = next_ps(C)
            psb = ps[:, :].bitcast(BF16)
            for c in range(8):
                P.op("pe", lambda e, psb=psb, c=c, t=t: e.transpose(
                    out=psb[:, c * 128:(c + 1) * 128], in_=O[:, t, c * 128:(c + 1) * 128], identity=C.identb[:, :]),
                    reads=[O_t[t], C.identb_t], writes=[pt])
            P.op("act", lambda e, psb=psb, b=b: e.activation(
                out=oT[b][:, :, :], in_=psb.rearrange("p (c n) -> p c n", c=8), func=AF.Copy),
                reads=[pt], writes=[oT_t[b]])
            zs = []
            for cb in range(2):
                ps2, pt2 = next_ps(C)
                for c in range(8):
                    P.op("pe", lambda e, ps2=ps2, c=c, cb=cb, b=b: e.matmul(
                        ps2[:, :], lhsT=oT[b][:, c, :], rhs=Wo[:, c, cb * 512:(cb + 1) * 512],
                        start=(c == 0), stop=(c == 7)), reads=[oT_t[b], Wo_t[c]], writes=[pt2])
                zs.append((ps2[:, :], pt2))
            layer_norm_tile(C, t, zs, G, Bv, G_t, B_t, wk)
        barrier(C)


def gla_phase(C, l):
    P, nc, dr = C.P, C.nc, C.dr
    H, DK, DV = 4, 128, 256
    with contextlib.ExitStack() as st:
        O = alloc(C, st, "g_O", [128, NT, D], BF16)
        O_t = [T("g_O%d" % t) for t in range(NT)]
        with contextlib.ExitStack() as st2:
            gla_heads(C, l, st2, O, O_t)
            barrier(C)
        outproj_phase(C, l, O, O_t, dr["gla_w_out"])


def gla_heads(C, l, st, O, O_t):
    P, nc, dr = C.P, C.nc, C.dr
    H, DK, DV = 4, 128, 256
    XT = alloc(C, st, "g_XT", [128, 8, S], BF16)
    XT_t = {t: T("g_XT%d" % t) for t in range(NT)}
    build_xT(C, XT, XT_t, range(NT))
    win = dr["gla_w_in"].rearrange("(k p) n -> p k n", p=128)
    Wlow = alloc(C, st, "g_Wlow", [128, 8, 16], BF16); Wlow_t = T("g_Wlow")
    P.dma("pool", Wlow[:, :, :], win[:, :, 3072:3088], writes=[Wlow_t])
    gkT = alloc(C, st, "g_gkT", [16, S], F32); gkT_t = T("g_gkT")
    for tb in range(4):
        ps, pt = next_ps(C)
        for k in range(8):
            P.op("pe", lambda e, ps=ps, k=k, tb=tb: e.matmul(ps[0:16, :], lhsT=Wlow[:, k, :], rhs=XT[:, k, tb * 512:(tb + 1) * 512],
                                                          start=(k == 0), stop=(k == 7)),
                 reads=[Wlow_t] + [XT_t[t] for t in range(tb * 4, tb * 4 + 4)], writes=[pt])
        P.op("act", lambda e, ps=ps, tb=tb: e.activation(out=gkT[:, tb * 512:(tb + 1) * 512], in_=ps[0:16, :], func=AF.Copy),
             reads=[pt], writes=[gkT_t])
    wgk = alloc(C, st, "g_wgk", [16, 512], F32); wgk_t = T("g_wgk")
    P.dma("sp", wgk[:, :], dr["gla_w_gk"][:, :], writes=[wgk_t])
    bgk = alloc(C, st, "g_bgk", [1, 512], F32); bgk_t = T("g_bgk")
    P.dma("sp", bgk[:, :], dr["gla_b_gk"][:, :], writes=[bgk_t])
    ng = alloc(C, st, "g_ng", [128, DV], F32); ng_t = T("g_ng")
    load_bcast_row(C, ng[:, :], ng_t, dr["gla_norm_g"][0:1, :])
    Wh = [alloc(C, st, "g_Wh%d" % i, [128, 8, 768], BF16) for i in range(2)]
    Wh_t = [[T("g_Wh%d_%d" % (i, j)) for j in range(4)] for i in range(2)]
    state = alloc(C, st, "g_state", [128, DV], F32); state_t = T("g_state")
    state_bf = alloc(C, st, "g_statebf", [128, DV], BF16); statebf_t = T("g_statebf")
    NB = 2
    def mk(name, shape, dt):
        return [alloc(C, st, "%s%d" % (name, i), shape, dt) for i in range(NB)], [T("%s%d" % (name, i)) for i in range(NB)]
    ex, ex_t = mk("g_ex", [128, 128], F32)
    lt, lt_t = mk("g_l", [128, 128], F32)
    ecT, ecT_t = mk("g_ecT", [128, 128], F32)
    eiT, eiT_t = mk("g_eiT", [128, 128], F32)
    eR, eR_t = mk("g_eR", [128, 128], F32)
    qd, qd_t = mk("g_qd", [128, 128], BF16)
    ki, ki_t = mk("g_ki", [128, 128], BF16)
    ke, ke_t = mk("g_ke", [128, 128], BF16)
    vb, vb_t = mk("g_vb", [128, DV], BF16)
    sg, sg_t = mk("g_sg", [128, DV], F32)
    sT, sT_t = mk("g_sT", [128, 128], BF16)
    junk, junk_t = mk("g_junk", [128, DV], F32)
    osq, osq_t = mk("g_osq", [128, DV], F32)
    ss, ss_t = mk("g_ss", [128, 4], F32)
    cols = [(0, 128), (512, 128), (1024, 256), (2048, 256)]
    for h in range(H):
        wb = h % 2
        off = 0
        for j, (base, wdt) in enumerate(cols):
            P.dma("pool", Wh[wb][:, :, off:off + wdt], win[:, :, base + h * wdt:base + (h + 1) * wdt], writes=[Wh_t[wb][j]])
            off += wdt
        P.op("pool", lambda e: e.memset(state[:, :], 0.0), writes=[state_t])
        P.op("pool", lambda e: e.memset(state_bf[:, :], 0.0), writes=[statebf_t])
        def gla_tile(t, h=h, wb=wb):
            b = t % NB
            pool = (0, 1, 2, 3) if b == 0 else (4, 5, 6, 7)
            tok = slice(t * 128, (t + 1) * 128)
            p1, p1t = next_ps(C, pool)
            for j in range(2):
                for k in range(8):
                    P.op("pe", lambda e, p1=p1, j=j, k=k, wb=wb, tok=tok: e.matmul(
                        p1[:, j * 128:(j + 1) * 128], lhsT=Wh[wb][:, k, j * 128:(j + 1) * 128], rhs=XT[:, k, tok],
                        start=(k == 0), stop=(k == 7)), reads=[Wh_t[wb][j], XT_t[t]], writes=[p1t])
            p2, p2t = next_ps(C, pool)
            for k in range(8):
                P.op("pe", lambda e, p2=p2, k=k, wb=wb, tok=tok: e.matmul(
                    p2[:, 0:384], lhsT=XT[:, k, tok], rhs=Wh[wb][:, k, 128:512], start=(k == 0), stop=(k == 7)),
                    reads=[Wh_t[wb][1], Wh_t[wb][2], XT_t[t]], writes=[p2t])
            p3, p3t = next_ps(C, pool)
            for k in range(8):
                P.op("pe", lambda e, p3=p3, k=k, wb=wb, tok=tok: e.matmul(
                    p3[:, 0:256], lhsT=XT[:, k, tok], rhs=Wh[wb][:, k, 512:768], start=(k == 0), stop=(k == 7)),
                    reads=[Wh_t[wb][3], XT_t[t]], writes=[p3t])
            P.op("pe", lambda e, p3=p3, tok=tok, h=h: e.matmul(
                p3[:, 256:384], lhsT=gkT[:, tok], rhs=wgk[:, h * 128:(h + 1) * 128], start=True, stop=False),
                reads=[gkT_t, wgk_t], writes=[p3t])
            P.op("pe", lambda e, p3=p3, h=h: e.matmul(
                p3[:, 256:384], lhsT=C.ones1[:, :], rhs=bgk[:, h * 128:(h + 1) * 128], start=False, stop=True),
                reads=[C.ones1_t, bgk_t], writes=[p3t])
            yield
            P.op("act", lambda e, p3=p3, b=b: e.activation(out=ex[b][:, :], in_=p3[:, 256:384], func=AF.Exp, scale=-1.0),
                 reads=[p3t], writes=[ex_t[b]])
            P.op("act", lambda e, b=b: e.activation(out=lt[b][:, :], in_=ex[b][:, :], func=AF.Ln, bias=1.0, scale=1.0),
                 reads=[ex_t[b]], writes=[lt_t[b]])
            yield
            p4, p4t = next_ps(C, pool)
            P.op("pe", lambda e, p4=p4, b=b: e.matmul(p4[:, 0:128], lhsT=lt[b][:, :], rhs=C.cm["m_incl_gla"][:, :], start=True, stop=True),
                 reads=[lt_t[b], C.cm_t["m_incl_gla"]], writes=[p4t])
            P.op("pe", lambda e, p4=p4, b=b: e.matmul(p4[:, 128:256], lhsT=C.cm["m_rev_gla"][:, :], rhs=lt[b][:, :], start=True, stop=True),
                 reads=[lt_t[b], C.cm_t["m_rev_gla"]], writes=[p4t])
            P.op("act", lambda e, p4=p4, b=b: e.activation(out=ecT[b][:, :], in_=p4[:, 0:128], func=AF.Exp),
                 reads=[p4t], writes=[ecT_t[b]])
            P.op("act", lambda e, p4=p4, b=b: e.activation(out=eiT[b][:, :], in_=p4[:, 0:128], func=AF.Exp, scale=-1.0),
                 reads=[p4t], writes=[eiT_t[b]])
            P.op("act", lambda e, p4=p4, b=b: e.activation(out=eR[b][:, :], in_=p4[:, 128:256], func=AF.Exp),
                 reads=[p4t], writes=[eR_t[b]])
            P.op("dve", lambda e, p1=p1, b=b: e.scalar_tensor_tensor(
                out=qd[b][:, :], in0=p1[:, 0:128], scalar=float(DK ** -0.5), in1=ecT[b][:, :], op0=ALU.mult, op1=ALU.mult),
                reads=[p1t, ecT_t[b]], writes=[qd_t[b]])
            P.op("dve", lambda e, p1=p1, b=b: e.tensor_tensor(out=ki[b][:, :], in0=p1[:, 128:256], in1=eiT[b][:, :], op=ALU.mult),
                 reads=[p1t, eiT_t[b]], writes=[ki_t[b]])
            P.op("dve", lambda e, p2=p2, b=b: e.tensor_tensor(out=ke[b][:, :], in0=p2[:, 0:128], in1=eR[b][:, :], op=ALU.mult),
                 reads=[p2t, eR_t[b]], writes=[ke_t[b]])
            P.op("act", lambda e, p2=p2, b=b: e.activation(out=vb[b][:, :], in_=p2[:, 128:384], func=AF.Copy),
                 reads=[p2t], writes=[vb_t[b]])
            P.op("act", lambda e, p3=p3, b=b: e.activation(out=sg[b][:, :], in_=p3[:, 0:256], func=AF.Exp, scale=-1.0),
                 reads=[p3t], writes=[sg_t[b]])
            P.op("act", lambda e, b=b: e.activation(out=sg[b][:, :], in_=sg[b][:, :], func=AF.Ln, bias=1.0, scale=1.0),
                 reads=[sg_t[b]], writes=[sg_t[b]])
            P.op("act", lambda e, b=b: e.activation(out=sg[b][:, :], in_=sg[b][:, :], func=AF.Exp, scale=-1.0),
                 reads=[sg_t[b]], writes=[sg_t[b]])
            P.op("dve", lambda e, p3=p3, b=b: e.tensor_tensor(out=sg[b][:, :], in0=p3[:, 0:256], in1=sg[b][:, :], op=ALU.mult),
                 reads=[p3t, sg_t[b]], writes=[sg_t[b]])
            P.op("dve", lambda e, b=b: e.tensor_tensor(out=sg[b][:, :], in0=sg[b][:, :], in1=ng[:, :], op=ALU.mult),
                 reads=[sg_t[b], ng_t], writes=[sg_t[b]])
            yield
            p5, p5t = next_ps(C, pool)
            P.op("pe", lambda e, p5=p5, b=b: e.matmul(p5[:, 0:128], lhsT=ki[b][:, :], rhs=qd[b][:, :], start=True, stop=True),
                 reads=[ki_t[b], qd_t[b]], writes=[p5t])
            P.op("dve", lambda e, p5=p5, b=b: e.tensor_tensor(out=sT[b][:, :], in0=p5[:, 0:128], in1=C.cm["m_caus"][:, :], op=ALU.mult),
                 reads=[p5t, C.cm_t["m_caus"]], writes=[sT_t[b]])
            yield
            p6, p6t = next_ps(C, pool)
            P.op("pe", lambda e, p6=p6, b=b: e.matmul(p6[:, 0:DV], lhsT=sT[b][:, :], rhs=vb[b][:, :], start=True, stop=False),
                 reads=[sT_t[b], vb_t[b]], writes=[p6t])
            P.op("pe", lambda e, p6=p6, b=b: e.matmul(p6[:, 0:DV], lhsT=qd[b][:, :], rhs=state_bf[:, :], start=False, stop=True),
                 reads=[qd_t[b], statebf_t], writes=[p6t])
            yield
            p7, p7t = next_ps(C, pool)
            P.op("pe", lambda e, p7=p7, b=b: e.matmul(p7[:, 0:DV], lhsT=ke[b][:, :], rhs=vb[b][:, :], start=True, stop=True),
                 reads=[ke_t[b], vb_t[b]], writes=[p7t])
            P.op("dve", lambda e, p7=p7, b=b: e.scalar_tensor_tensor(
                out=state[:, :], in0=state[:, :], scalar=ecT[b][:, 127:128], in1=p7[:, 0:DV], op0=ALU.mult, op1=ALU.add),
                reads=[state_t, ecT_t[b], p7t], writes=[state_t])
            P.op("act", lambda e: e.activation(out=state_bf[:, :], in_=state[:, :], func=AF.Copy),
                 reads=[state_t], writes=[statebf_t])
            yield
            P.op("act", lambda e, p6=p6, b=b: e.activation(out=junk[b][:, :], in_=p6[:, 0:DV], func=AF.Copy),
                 reads=[p6t], writes=[junk_t[b]])
            P.op("dve", lambda e, b=b: e.scalar_tensor_tensor(out=osq[b][:, :], in0=junk[b][:, :], scalar=1.0, in1=junk[b][:, :],
                                                              op0=ALU.mult, op1=ALU.mult, accum_out=ss[b][:, 0:1]),
                 reads=[junk_t[b]], writes=[osq_t[b], ss_t[b]])
            P.op("act", lambda e, b=b: e.activation(out=ss[b][:, 1:2], in_=ss[b][:, 0:1], func=AF.Ln, bias=C.eps6[:, 0:1], scale=1.0 / DV),
                 reads=[ss_t[b], C.eps6_t], writes=[ss_t[b]])
            P.op("act", lambda e, b=b: e.activation(out=ss[b][:, 2:3], in_=ss[b][:, 1:2], func=AF.Exp, scale=-0.5),
                 reads=[ss_t[b]], writes=[ss_t[b]])
            P.op("dve", lambda e, p6=p6, b=b, t=t, h=h: e.scalar_tensor_tensor(
                out=O[:, t, h * DV:(h + 1) * DV], in0=junk[b][:, :], scalar=ss[b][:, 2:3], in1=sg[b][:, :], op0=ALU.mult, op1=ALU.mult),
                reads=[junk_t[b], ss_t[b], sg_t[b]], writes=[O_t[t]])
        run_pipelined((gla_tile(t) for t in range(NT)), NB)


def moba_phase(C, l):
    P, nc, dr = C.P, C.nc, C.dr
    with contextlib.ExitStack() as st:
        O = alloc(C, st, "m_O", [128, NT, D], BF16)
        O_t = [T("m_O%d" % t) for t in range(NT)]
        with contextlib.ExitStack() as st2:
            moba_heads(C, l, st2, O, O_t)
            barrier(C)
        outproj_phase(C, l, O, O_t, dr["moba_w_out"])


def moba_heads(C, l, st, O, O_t):
    P, nc, dr = C.P, C.nc, C.dr
    H, DH = 8, 128
    PI = float(np.pi)
    XT = alloc(C, st, "m_XT", [128, 8, S], BF16)
    XT_t = {t: T("m_XT%d" % t) for t in range(NT)}
    build_xT(C, XT, XT_t, range(NT))
    win = dr["moba_w_in"].rearrange("(k p) n -> p k n", p=128)
    CT = alloc(C, st, "m_CT", [128, S], F32); CT_t = T("m_CT")
    ST = alloc(C, st, "m_ST", [128, S], F32); ST_t = T("m_ST")
    P.op("pool", lambda e: e.memset(CT[:, :], 1.0), writes=[CT_t])
    P.op("pool", lambda e: e.memset(ST[:, :], 0.0), writes=[ST_t])
    invf = alloc(C, st, "m_invf", [32, 1], F32); invf_t = T("m_invf")
    sgn = alloc(C, st, "m_sgn", [32, 1], F32); sgn_t = T("m_sgn")
    P.dma("sp", invf[:, :], dr["c_invf"][:, :], writes=[invf_t])
    P.dma("sp", sgn[:, :], dr["c_sgn"][:, :], writes=[sgn_t])
    posi = alloc(C, st, "m_posi", [32, 256], I32); posi_t = T("m_posi")
    ang = alloc(C, st, "m_ang", [32, 256], F32); ang_t = T("m_ang")
    r_ = alloc(C, st, "m_r", [32, 256], F32); r_t = T("m_r")
    ki_ = alloc(C, st, "m_ki", [32, 256], I32); ki_t = T("m_ki")
    th = alloc(C, st, "m_th", [32, 256], F32); th_t = T("m_th")
    mk_ = alloc(C, st, "m_mk", [32, 256], F32); mk_t = T("m_mk")
    pos_row = dr["positions"].rearrange("s o -> o s")
    for tb in range(8):
        cs = slice(tb * 256, (tb + 1) * 256)
        P.dma("sp", posi[:, :], pos_row[:, cs].to_broadcast([32, 256]), writes=[posi_t])
        P.op("dve", lambda e: e.tensor_copy(out=ang[:, :], in_=posi[:, :]), reads=[posi_t], writes=[ang_t])
        P.op("dve", lambda e: e.tensor_scalar(out=ang[:, :], in0=ang[:, :], scalar1=invf[:, 0:1], scalar2=None, op0=ALU.mult),
             reads=[ang_t, invf_t], writes=[ang_t])
        for (shift, dst, dst_t) in ((PI / 2, CT, CT_t), (0.0, ST, ST_t)):
            P.op("dve", lambda e, shift=shift: e.tensor_scalar(out=r_[:, :], in0=ang[:, :], scalar1=shift, scalar2=1.0 / (2 * PI),
                                                            op0=ALU.add, op1=ALU.mult), reads=[ang_t], writes=[r_t])
            P.op("dve", lambda e: e.tensor_copy(out=ki_[:, :], in_=r_[:, :]), reads=[r_t], writes=[ki_t])
            P.op("dve", lambda e: e.tensor_copy(out=r_[:, :], in_=ki_[:, :]), reads=[ki_t], writes=[r_t])
            P.op("dve", lambda e: e.scalar_tensor_tensor(out=th[:, :], in0=r_[:, :], scalar=-2 * PI, in1=ang[:, :],
                                                         op0=ALU.mult, op1=ALU.add), reads=[r_t, ang_t], writes=[th_t])
            if shift != 0.0:
                P.op("dve", lambda e, shift=shift: e.tensor_scalar(out=th[:, :], in0=th[:, :], scalar1=shift, scalar2=None, op0=ALU.add),
                     reads=[th_t], writes=[th_t])
            P.op("dve", lambda e: e.tensor_scalar(out=mk_[:, :], in0=th[:, :], scalar1=PI, scalar2=-2 * PI, op0=ALU.is_gt, op1=ALU.mult),
                 reads=[th_t], writes=[mk_t])
            P.op("dve", lambda e: e.tensor_tensor(out=th[:, :], in0=th[:, :], in1=mk_[:, :], op=ALU.add), reads=[th_t, mk_t], writes=[th_t])
            P.op("dve", lambda e: e.tensor_scalar(out=mk_[:, :], in0=th[:, :], scalar1=-PI, scalar2=2 * PI, op0=ALU.is_lt, op1=ALU.mult),
                 reads=[th_t], writes=[mk_t])
            P.op("dve", lambda e: e.tensor_tensor(out=th[:, :], in0=th[:, :], in1=mk_[:, :], op=ALU.add), reads=[th_t, mk_t], writes=[th_t])
            P.op("dve", lambda e: e.tensor_scalar(out=th[:, :], in0=th[:, :], scalar1=-PI, scalar2=PI, op0=ALU.max, op1=ALU.min),
                 reads=[th_t], writes=[th_t])
            P.op("act", lambda e, dst=dst, cs=cs: e.activation(out=dst[0:32, cs], in_=th[:, :], func=AF.Sin), reads=[th_t], writes=[dst_t])
    P.op("dve", lambda e: e.tensor_scalar(out=ST[0:32, :], in0=ST[0:32, :], scalar1=sgn[:, 0:1], scalar2=None, op0=ALU.mult),
         reads=[ST_t, sgn_t], writes=[ST_t])
    if C.dbg.get("moba_stop") == 1:
        return
    Ef = alloc(C, st, "m_Ef", [8, 1024], F32); Ef_t = T("m_Ef")
    P.dma("sp", Ef[:, :], dr["c_E"][:, :], writes=[Ef_t])
    Eb = alloc(C, st, "m_Eb", [8, 1024], BF16); Eb_t = T("m_Eb")
    P.op("dve", lambda e: e.tensor_copy(out=Eb[:, :], in_=Ef[:, :]), reads=[Ef_t], writes=[Eb_t])
    cnf = alloc(C, st, "m_cnf", [128, 128], F32); cnf_t = T("m_cnf")
    P.dma("sp", cnf[:, :], dr["c_causneg"][:, :], writes=[cnf_t])
    cnb = alloc(C, st, "m_cnb", [128, 128], BF16); cnb_t = T("m_cnb")
    P.op("dve", lambda e: e.tensor_copy(out=cnb[:, :], in_=cnf[:, :]), reads=[cnf_t], writes=[cnb_t])
    npast = alloc(C, st, "m_npast", [128, 64], F32); npast_t = T("m_npast")
    P.dma("sp", npast[:, :], dr["c_negpast"][:, :].to_broadcast([128, 64]), writes=[npast_t])
    if C.dbg.get("moba_stop") == 11:
        return
    Wh0 = alloc(C, st, "m_Wh", [128, 8, 640], BF16)
    Wh = [Wh0, Wh0]
    Wh_t0 = [T("m_Wh_%d" % j) for j in range(5)]
    Wh_t = [Wh_t0, Wh_t0]
    P.op("pool", lambda e: e.memset(Wh0[:, :, 384:640], 0.0), writes=[Wh_t0[3], Wh_t0[4]])
    qT0 = alloc(C, st, "m_qT", [128, S], BF16)
    kT0 = alloc(C, st, "m_kT", [128, S], BF16)
    qT = [qT0, qT0]
    kT = [kT0, kT0]
    qT_t0 = [T("m_qT_%d" % tb) for tb in range(4)]
    kT_t0 = [T("m_kT_%d" % tb) for tb in range(4)]
    qT_t = [qT_t0, qT_t0]
    kT_t = [kT_t0, kT_t0]
    vaug = [alloc(C, st, "m_va%d" % i, [128, NT, 130], BF16) for i in range(2)]
    va_t = [[T("m_va%d_%d" % (i, g)) for g in range(4)] for i in range(2)]
    for i in range(2):
        P.op("pool", lambda e, i=i: e.memset(vaug[i][:, :, 128:130], 1.0), writes=va_t[i])
    t1 = [alloc(C, st, "m_t1%d" % i, [128, 512], F32) for i in range(2)]
    t1_t = [T("m_t1%d" % i) for i in range(2)]
    t2 = [alloc(C, st, "m_t2%d" % i, [128, 512], F32) for i in range(2)]
    t2_t = [T("m_t2%d" % i) for i in range(2)]
    km = alloc(C, st, "m_km", [128, 8], F32); km_t = T("m_km")
    kmb = alloc(C, st, "m_kmb", [128, 8], BF16); kmb_t = T("m_kmb")
    NB = 2
    g_ = [alloc(C, st, "m_g%d" % i, [128, 8], F32) for i in range(NB)]; g_t = [T("m_g%d" % i) for i in range(NB)]
    mx = [alloc(C, st, "m_mx%d" % i, [128, 8], F32) for i in range(NB)]; mx_t = [T("m_mx%d" % i) for i in range(NB)]
    sm = [alloc(C, st, "m_sm%d" % i, [128, 8], F32) for i in range(NB)]; sm_t = [T("m_sm%d" % i) for i in range(NB)]
    selT = [alloc(C, st, "m_selT%d" % i, [8, 128], BF16) for i in range(NB)]; selT_t = [T("m_selT%d" % i) for i in range(NB)]
    rec = [alloc(C, st, "m_rec%d" % i, [128, 1], F32) for i in range(NB)]; rec_t = [T("m_rec%d" % i) for i in range(NB)]
    NPB = 4
    PT = [alloc(C, st, "m_PT%d" % i, [128, 128], BF16) for i in range(NPB)]; PT_t = [T("m_PT%d" % i) for i in range(NPB)]
    pt_rr = 0
    scale = float(DH ** -0.5)
    for h in range(H):
        wb = h % 2
        hb = h * 128
        segs = [(0, 128, hb), (128, 128, 1024 + hb), (256, 128, 2048 + hb)]
        for j, (o, w, src) in enumerate(segs):
            P.dma("pool", Wh[wb][:, :, o:o + w], win[:, :, src:src + w], writes=[Wh_t[wb][j]])
        P.dma("pool", Wh[wb][:, :, 384:400], win[:, :, hb + 16:hb + 32], writes=[Wh_t[wb][3]])
        P.dma("pool", Wh[wb][:, :, 400:416], win[:, :, hb:hb + 16], writes=[Wh_t[wb][3]])
        P.dma("pool", Wh[wb][:, :, 512:528], win[:, :, 1024 + hb + 16:1024 + hb + 32], writes=[Wh_t[wb][4]])
        P.dma("pool", Wh[wb][:, :, 528:544], win[:, :, 1024 + hb:1024 + hb + 16], writes=[Wh_t[wb][4]])
        if C.dbg.get("moba_stop") == 12:
            return
        for (dstT, dst_t, co, sw, wj, swj) in ((qT[wb], qT_t[wb], 0, 384, 0, 3), (kT[wb], kT_t[wb], 128, 512, 1, 4)):
            for tb in range(4):
                cs = slice(tb * 512, (tb + 1) * 512)
                xts = [XT_t[t] for t in range(tb * 4, tb * 4 + 4)]
                pq, pqt = next_ps(C, (2, 3, 4, 5, 6, 7))
                for k in range(8):
                    P.op("pe", lambda e, pq=pq, k=k, wb=wb, co=co, cs=cs: e.matmul(
                        pq[:, :], lhsT=Wh[wb][:, k, co:co + 128], rhs=XT[:, k, cs], start=(k == 0), stop=(k == 7)),
                        reads=[Wh_t[wb][wj]] + xts, writes=[pqt])
                pw, pwt = next_ps(C, (2, 3, 4, 5, 6, 7))
                for k in range(8):
                    P.op("pe", lambda e, pw=pw, k=k, wb=wb, sw=sw, cs=cs: e.matmul(
                        pw[:, :], lhsT=Wh[wb][:, k, sw:sw + 128], rhs=XT[:, k, cs], start=(k == 0), stop=(k == 7)),
                        reads=[Wh_t[wb][swj]] + xts, writes=[pwt])
                tbuf = tb % 2
                P.op("dve", lambda e, pq=pq, cs=cs, tbuf=tbuf: e.tensor_tensor(out=t1[tbuf][:, :], in0=pq[:, :], in1=CT[:, cs], op=ALU.mult),
                     reads=[pqt, CT_t], writes=[t1_t[tbuf]])
                P.op("dve", lambda e, pw=pw, cs=cs, tbuf=tbuf: e.tensor_tensor(out=t2[tbuf][:, :], in0=pw[:, :], in1=ST[:, cs], op=ALU.mult),
                     reads=[pwt, ST_t], writes=[t2_t[tbuf]])
                P.op("dve", lambda e, dstT=dstT, cs=cs, tbuf=tbuf: e.tensor_tensor(out=dstT[:, cs], in0=t1[tbuf][:, :], in1=t2[tbuf][:, :], op=ALU.add),
                     reads=[t1_t[tbuf], t2_t[tbuf]], writes=[dst_t[tb]])
        if C.dbg.get("moba_stop") == 2:
            return
        P.op("dve", lambda e, wb=wb: e.tensor_reduce(out=km[:, :], in_=kT[wb][:, :].rearrange("p (n b) -> p n b", b=256), axis=AX.X, op=ALU.add),
             reads=kT_t[wb], writes=[km_t])
        P.op("dve", lambda e: e.tensor_scalar(out=kmb[:, :], in0=km[:, :], scalar1=1.0 / 256.0, scalar2=None, op0=ALU.mult),
             reads=[km_t], writes=[kmb_t])
        for g4 in range(4):
            pv, pvt = next_ps(C, (2, 3, 4, 5, 6, 7))
            for j in range(4):
                t = g4 * 4 + j
                for k in range(8):
                    P.op("pe", lambda e, pv=pv, j=j, k=k, t=t, wb=wb: e.matmul(
                        pv[:, j * 128:(j + 1) * 128], lhsT=XT[:, k, t * 128:(t + 1) * 128], rhs=Wh[wb][:, k, 256:384],
                        start=(k == 0), stop=(k == 7)), reads=[Wh_t[wb][2], XT_t[t]], writes=[pvt])
            P.op("act", lambda e, pv=pv, g4=g4, wb=wb: e.activation(
                out=vaug[wb][:, g4 * 4:(g4 + 1) * 4, 0:128], in_=pv[:, :].rearrange("p (j n) -> p j n", j=4), func=AF.Copy),
                reads=[pvt], writes=[va_t[wb][g4]])
        if C.dbg.get("moba_stop") == 3:
            return
        for qt in range(C.dbg.get("moba_nqt", NT)):
            b = qt % NB
            blk = qt // 2
            qs = slice(qt * 128, (qt + 1) * 128)
            use_gate = blk >= 4
            if use_gate:
                pg, pgt = next_ps(C, (2, 3, 4, 5, 6, 7))
                P.op("pe", lambda e, pg=pg, wb=wb, qs=qs: e.matmul(pg[:, 0:8], lhsT=qT[wb][:, qs], rhs=kmb[:, :], start=True, stop=True),
                     reads=[qT_t[wb][qt // 4], kmb_t], writes=[pgt])
                P.op("dve", lambda e, pg=pg, b=b, blk=blk: e.tensor_tensor(out=g_[b][:, :], in0=pg[:, 0:8], in1=npast[:, blk * 8:(blk + 1) * 8], op=ALU.add),
                     reads=[pgt, npast_t], writes=[g_t[b]])
                P.op("dve", lambda e, b=b: e.max(out=mx[b][:, :], in_=g_[b][:, :]), reads=[g_t[b]], writes=[mx_t[b]])
                P.op("dve", lambda e, b=b: e.tensor_scalar(out=sm[b][:, :], in0=g_[b][:, :], scalar1=mx[b][:, 2:3], scalar2=-1.0,
                                                         op0=ALU.is_ge, op1=ALU.add), reads=[g_t[b], mx_t[b]], writes=[sm_t[b]])
                pt2, pt2t = next_ps(C, (2, 3, 4, 5, 6, 7))
                P.op("pe", lambda e, pt2=pt2, b=b: e.transpose(out=pt2[0:8, 0:128], in_=sm[b][:, 0:8], identity=C.ident[:, :]),
                     reads=[sm_t[b], C.ident_t], writes=[pt2t])
                P.op("act", lambda e, pt2=pt2, b=b: e.activation(out=selT[b][:, :], in_=pt2[0:8, 0:128], func=AF.Copy),
                     reads=[pt2t], writes=[selT_t[b]])
            chunks = list(range(0, 2 * blk)) + ([qt - 1, qt] if qt % 2 == 1 else [qt])
            po, pot = next_ps(C, (0, 1))
            LA = 2
            pbs = {}

            def emit_s(ci, kc, qt=qt, qs=qs, wb=wb, b=b, blk=blk, use_gate=use_gate):
                nonlocal pt_rr
                past = kc < 2 * blk
                diag = kc == qt
                ks = slice(kc * 128, (kc + 1) * 128)
                ps_, pst_ = next_ps(C, (2, 3, 4, 5, 6, 7))
                extra = (past and use_gate) or diag
                P.op("pe", lambda e, ps_=ps_, ks=ks, extra=extra, wb=wb, qs=qs: e.matmul(
                    ps_[:, 0:128], lhsT=kT[wb][:, ks], rhs=qT[wb][:, qs], start=True, stop=not extra),
                    reads=[kT_t[wb][kc // 4], qT_t[wb][qt // 4]], writes=[pst_])
                if past and use_gate:
                    n = kc // 2
                    P.op("pe", lambda e, ps_=ps_, n=n, b=b: e.matmul(
                        ps_[:, 0:128], lhsT=Eb[:, n * 128:(n + 1) * 128], rhs=selT[b][:, :], start=False, stop=True),
                        reads=[Eb_t, selT_t[b]], writes=[pst_])
                if diag:
                    P.op("pe", lambda e, ps_=ps_: e.matmul(ps_[:, 0:128], lhsT=C.identb[:, :], rhs=cnb[:, :], start=False, stop=True),
                         reads=[C.identb_t, cnb_t], writes=[pst_])
                pb = pt_rr % NPB
                pt_rr += 1
                pbs[ci] = pb
                P.op("act", lambda e, ps_=ps_, pb=pb: e.activation(out=PT[pb][:, :], in_=ps_[:, 0:128], func=AF.Exp, scale=scale),
                     reads=[pst_], writes=[PT_t[pb]])

            def emit_pv(ci, kc, po=po, pot=pot, wb=wb, pbs=pbs, chunks=chunks):
                pb = pbs[ci]
                P.op("pe", lambda e, pb=pb, kc=kc, ci=ci, nch=len(chunks), po=po, wb=wb: e.matmul(
                    po[:, 0:129], lhsT=PT[pb][:, :], rhs=vaug[wb][:, kc, 0:129], start=(ci == 0), stop=(ci == nch - 1)),
                    reads=[PT_t[pb], va_t[wb][kc // 4]], writes=[pot])

            nch_ = len(chunks)
            for ci in range(nch_ + LA):
                if ci < nch_:
                    emit_s(ci, chunks[ci])
                if ci - LA >= 0:
                    emit_pv(ci - LA, chunks[ci - LA])
            P.op("dve", lambda e, po=po, b=b: e.reciprocal(out=rec[b][:, :], in_=po[:, 128:129]), reads=[pot], writes=[rec_t[b]])
            P.op("dve", lambda e, po=po, b=b, qt=qt, hb=hb: e.tensor_scalar(
                out=O[:, qt, hb:hb + 128], in0=po[:, 0:128], scalar1=rec[b][:, 0:1], scalar2=None, op0=ALU.mult),
                reads=[pot, rec_t[b]], writes=[O_t[qt]])


def gdn_phase(C, l):
    P, nc, dr = C.P, C.nc, C.dr
    with contextlib.ExitStack() as st:
        O = alloc(C, st, "d_O", [128, NT, D], BF16)
        O_t = [T("d_O%d" % t) for t in range(NT)]
        with contextlib.ExitStack() as st2:
            gdn_heads(C, l, st2, O, O_t)
            barrier(C)
        outproj_phase(C, l, O, O_t, dr["gdn_w_out"])


def gdn_heads(C, l, st, O, O_t):
    P, nc, dr = C.P, C.nc, C.dr
    H, DK = 8, 128
    XT = alloc(C, st, "d_XT", [128, 8, S], BF16)
    XT_t = {t: T("d_XT%d" % t) for t in range(NT)}
    build_xT(C, XT, XT_t, range(NT))
    allx = [XT_t[t] for t in range(NT)]
    win = dr["gdn_w_in"].rearrange("(k p) n -> p k n", p=128)
    PS6 = (2, 3, 4, 5, 6, 7)
    Wab = alloc(C, st, "d_Wab", [128, 8, 16], BF16); Wab_t = T("d_Wab")
    P.dma("pool", Wab[:, :, :], win[:, :, 4096:4112], writes=[Wab_t])
    alog = alloc(C, st, "d_alog", [128, 8], F32); alog_t = T("d_alog")
    dtb = alloc(C, st, "d_dtb", [128, 8], F32); dtb_t = T("d_dtb")
    P.dma("sp", alog[:, :], dr["gdn_a_log"][0:1, :].to_broadcast([128, 8]), writes=[alog_t])
    P.dma("sp", dtb[:, :], dr["gdn_dt_bias"][0:1, :].to_broadcast([128, 8]), writes=[dtb_t])
    P.op("act", lambda e: e.activation(out=alog[:, :], in_=alog[:, :], func=AF.Exp), reads=[alog_t], writes=[alog_t])
    ng = alloc(C, st, "d_ng", [128, 128], F32); ng_t = T("d_ng")
    load_bcast_row(C, ng[:, :], ng_t, dr["gdn_norm_g"][0:1, :])
    def sc(name):
        return alloc(C, st, name, [128, NT, 8], F32), T(name)
    g_all, g_t = sc("d_g"); beta, beta_t = sc("d_beta"); cum, cum_t = sc("d_cum"); tot, tot_t = sc("d_tot")
    ecum, ecum_t = sc("d_ecum"); ncum, ncum_t = sc("d_ncum"); bec, bec_t = sc("d_bec"); eend, eend_t = sc("d_eend")
    ecl, ecl_t = sc("d_ecl"); tmp, tmp_t = sc("d_tmp")
    pab, pabt = next_ps(C)
    for t in range(NT):
        for k in range(8):
            P.op("pe", lambda e, t=t, k=k: e.matmul(pab[:, t * 16:(t + 1) * 16], lhsT=XT[:, k, t * 128:(t + 1) * 128], rhs=Wab[:, k, :],
                                                    start=(k == 0), stop=(k == 7)), reads=[XT_t[t], Wab_t], writes=[pabt])
    pab3 = pab[:, 0:256].rearrange("p (t c) -> p t c", c=16)
    for t in range(NT):
        P.op("dve", lambda e, t=t: e.tensor_tensor(out=tmp[:, t, :], in0=pab3[:, t, 0:8], in1=dtb[:, :], op=ALU.add),
             reads=[pabt, dtb_t], writes=[tmp_t])
    P.op("act", lambda e: e.activation(out=tmp[:, :, :], in_=tmp[:, :, :], func=AF.Exp), reads=[tmp_t], writes=[tmp_t])
    P.op("act", lambda e: e.activation(out=tmp[:, :, :], in_=tmp[:, :, :], func=AF.Ln, bias=1.0, scale=1.0), reads=[tmp_t], writes=[tmp_t])
    for t in range(NT):
        P.op("dve", lambda e, t=t: e.scalar_tensor_tensor(out=g_all[:, t, :], in0=tmp[:, t, :], scalar=-1.0, in1=alog[:, :],
                                                          op0=ALU.mult, op1=ALU.mult), reads=[tmp_t, alog_t], writes=[g_t])
    P.op("act", lambda e: e.activation(out=beta[:, :, :], in_=pab3[:, :, 8:16], func=AF.Exp, scale=-1.0), reads=[pabt], writes=[beta_t])
    P.op("act", lambda e: e.activation(out=beta[:, :, :], in_=beta[:, :, :], func=AF.Ln, bias=1.0, scale=1.0), reads=[beta_t], writes=[beta_t])
    P.op("act", lambda e: e.activation(out=beta[:, :, :], in_=beta[:, :, :], func=AF.Exp, scale=-1.0), reads=[beta_t], writes=[beta_t])
    pcm, pcmt = next_ps(C)
    for t in range(NT):
        P.op("pe", lambda e, t=t: e.matmul(pcm[:, t * 8:(t + 1) * 8], lhsT=C.cm["m_incl"][:, :], rhs=g_all[:, t, :], start=True, stop=True),
             reads=[g_t, C.cm_t["m_incl"]], writes=[pcmt])
        P.op("pe", lambda e, t=t: e.matmul(pcm[:, 128 + t * 8:128 + (t + 1) * 8], lhsT=C.cm["ones"][:, :], rhs=g_all[:, t, :], start=True, stop=True),
             reads=[g_t, C.cm_t["ones"]], writes=[pcmt])
    P.op("act", lambda e: e.activation(out=cum[:, :, :], in_=pcm[:, 0:128].rearrange("p (t c) -> p t c", c=8), func=AF.Copy),
         reads=[pcmt], writes=[cum_t])
    P.op("act", lambda e: e.activation(out=tot[:, :, :], in_=pcm[:, 128:256].rearrange("p (t c) -> p t c", c=8), func=AF.Copy),
         reads=[pcmt], writes=[tot_t])
    P.op("act", lambda e: e.activation(out=ecum[:, :, :], in_=cum[:, :, :], func=AF.Exp), reads=[cum_t], writes=[ecum_t])
    P.op("act", lambda e: e.activation(out=ecl[:, :, :], in_=tot[:, :, :], func=AF.Exp), reads=[tot_t], writes=[ecl_t])
    P.op("dve", lambda e: e.tensor_scalar(out=ncum[:, :, :], in0=cum[:, :, :], scalar1=-1.0, scalar2=None, op0=ALU.mult),
         reads=[cum_t], writes=[ncum_t])
    P.op("dve", lambda e: e.tensor_tensor(out=bec[:, :, :], in0=beta[:, :, :], in1=ecum[:, :, :], op=ALU.mult),
         reads=[beta_t, ecum_t], writes=[bec_t])
    P.op("dve", lambda e: e.tensor_tensor(out=eend[:, :, :], in0=tot[:, :, :], in1=cum[:, :, :], op=ALU.subtract),
         reads=[tot_t, cum_t], writes=[eend_t])
    P.op("act", lambda e: e.activation(out=eend[:, :, :], in_=eend[:, :, :], func=AF.Exp), reads=[eend_t], writes=[eend_t])
    sc_reads = [g_t, beta_t, cum_t, ecum_t, ncum_t, bec_t, eend_t, ecl_t]
    if C.dbg.get("gdn_stop") == 1:
        return
    Wh = alloc(C, st, "d_Wh", [128, 8, 512], BF16)
    Wh_t = [T("d_Wh_%d" % j) for j in range(4)]
    cw = alloc(C, st, "d_cw", [128, 12], F32); cw_t = T("d_cw")
    pre = alloc(C, st, "d_pre", [128, S + 3], F32); pre_t = T("d_pre")
    P.op("pool", lambda e: e.memset(pre[:, 0:3], 0.0), writes=[pre_t])
    yb = alloc(C, st, "d_y", [128, S], F32); yb_t = T("d_y")
    vT = alloc(C, st, "d_vT", [128, S], F32); vT_t = T("d_vT")
    qTn = alloc(C, st, "d_qTn", [128, S], BF16); qTn_t = T("d_qTn")
    kTn = alloc(C, st, "d_kTn", [128, S], BF16); kTn_t = T("d_kTn")
    rs = alloc(C, st, "d_rs", [128, 512], F32); rs_t = T("d_rs")
    Sst = alloc(C, st, "d_S", [128, 128], F32); S_t = T("d_S")
    Sbf = alloc(C, st, "d_Sbf", [128, 128], BF16); Sbf_t = T("d_Sbf")
    def mk(name, shape, dt, n=2):
        return [alloc(C, st, "%s%d" % (name, i), shape, dt) for i in range(n)], [T("%s%d" % (name, i)) for i in range(n)]
    NS = C.dbg.get("gdn_ns", 4)
    Dm, Dm_t = mk("d_Dm", [128, 128], F32, NS)
    DTm, DTm_t = mk("d_DTm", [128, 128], F32, NS)
    Lm, Lm_t = mk("d_L", [128, 128], F32, NS)
    LT, LT_t = mk("d_LT", [128, 128], F32, NS)
    Pa, Pa_t = Dm, Dm_t
    PTa, PTa_t = DTm, DTm_t
    Pb, Pb_t = mk("d_Pb", [128, 128], F32, NS)
    PTb, PTb_t = mk("d_PTb", [128, 128], F32, NS)
    qkT, qkT_t = mk("d_qkT", [128, 128], BF16, NS)
    kend, kend_t = mk("d_kend", [128, 128], BF16, NS)
    Xa, Xa_t = mk("d_Xa", [128, 256], F32, NS)
    Xb, Xb_t = mk("d_Xb", [128, 256], F32, NS)
    wT, wT_t = mk("d_wT", [128, 128], BF16, NS)
    vnew, vnew_t = mk("d_vnew", [128, 128], BF16, NS)
    intra, intra_t = Pb, Pb_t
    osb, osb_t = PTb, PTb_t
    sg, sg_t = DTm, DTm_t
    junk, junk_t = Dm, Dm_t
    ss, ss_t = mk("d_ss", [128, 4], F32, NS)
    cwsrc = dr["gdn_conv_w"].rearrange("j c -> c j")
    for h in range(H):
        hb = h * 128
        for j in range(4):
            P.dma("pool", Wh[:, :, j * 128:(j + 1) * 128], win[:, :, j * 1024 + hb:j * 1024 + hb + 128], writes=[Wh_t[j]])
        for s3 in range(3):
            P.dma("sp", cw[:, s3 * 4:(s3 + 1) * 4], cwsrc[s3 * 1024 + hb:s3 * 1024 + hb + 128, :], writes=[cw_t],
                  allow_slow_non_contiguous=True)
        P.op("pool", lambda e: e.memset(Sst[:, :], 0.0), writes=[S_t])
        P.op("pool", lambda e: e.memset(Sbf[:, :], 0.0), writes=[Sbf_t])
        for s3 in range(3):
            for tb in range(4):
                cs = slice(tb * 512, (tb + 1) * 512)
                pp, ppt = next_ps(C)
                for k in range(8):
                    P.op("pe", lambda e, pp=pp, k=k, s3=s3, cs=cs: e.matmul(
                        pp[:, :], lhsT=Wh[:, k, s3 * 128:(s3 + 1) * 128], rhs=XT[:, k, cs], start=(k == 0), stop=(k == 7)),
                        reads=[Wh_t[s3]] + allx[tb * 4:tb * 4 + 4], writes=[ppt])
                P.op("act", lambda e, pp=pp, tb=tb: e.activation(out=pre[:, 3 + tb * 512:3 + (tb + 1) * 512], in_=pp[:, :], func=AF.Copy),
                     reads=[ppt], writes=[pre_t])
            dst, dst_t = (vT, vT_t) if s3 == 2 else (yb, yb_t)
            P.op("dve", lambda e, dst=dst, s3=s3: e.tensor_scalar(out=dst[:, :], in0=pre[:, 3:3 + S], scalar1=cw[:, s3 * 4 + 3:s3 * 4 + 4],
                                                                 scalar2=None, op0=ALU.mult), reads=[pre_t, cw_t], writes=[dst_t])
            for j in range(3):
                P.op("dve", lambda e, dst=dst, s3=s3, j=j: e.scalar_tensor_tensor(
                    out=dst[:, :], in0=pre[:, j:j + S], scalar=cw[:, s3 * 4 + j:s3 * 4 + j + 1], in1=dst[:, :], op0=ALU.mult, op1=ALU.add),
                    reads=[pre_t, cw_t, dst_t], writes=[dst_t])
            sgm = pre[:, 3:3 + S]
            P.op("act", lambda e, dst=dst, sgm=sgm: e.activation(out=sgm, in_=dst[:, :], func=AF.Exp, scale=-1.0), reads=[dst_t, pre_t], writes=[pre_t])
            P.op("act", lambda e, sgm=sgm: e.activation(out=sgm, in_=sgm, func=AF.Ln, bias=1.0, scale=1.0), reads=[pre_t], writes=[pre_t])
            P.op("act", lambda e, sgm=sgm: e.activation(out=sgm, in_=sgm, func=AF.Exp, scale=-1.0), reads=[pre_t], writes=[pre_t])
            P.op("dve", lambda e, dst=dst, sgm=sgm: e.tensor_tensor(out=dst[:, :], in0=dst[:, :], in1=sgm, op=ALU.mult),
                 reads=[dst_t, pre_t], writes=[dst_t])
            if s3 < 2:
                outT, outT_t = (qTn, qTn_t) if s3 == 0 else (kTn, kTn_t)
                scl = float(DK ** -0.5) if s3 == 0 else 1.0
                P.op("dve", lambda e: e.tensor_tensor(out=pre[:, 3:3 + S], in0=yb[:, :], in1=yb[:, :], op=ALU.mult),
                     reads=[yb_t, pre_t], writes=[pre_t])
                for tb in range(4):
                    cs = slice(tb * 512, (tb + 1) * 512)
                    pq, pqt = next_ps(C)
                    P.op("pe", lambda e, pq=pq, tb=tb: e.matmul(pq[:, :], lhsT=C.cm["ones"][:, :], rhs=pre[:, 3 + tb * 512:3 + (tb + 1) * 512],
                                                               start=True, stop=True), reads=[pre_t, C.cm_t["ones"]], writes=[pqt])
                    P.op("act", lambda e, pq=pq: e.activation(out=rs[:, :], in_=pq[:, :], func=AF.Ln, bias=C.eps6[:, 0:1], scale=1.0),
                         reads=[pqt, C.eps6_t], writes=[rs_t])
                    P.op("act", lambda e: e.activation(out=rs[:, :], in_=rs[:, :], func=AF.Exp, scale=-0.5), reads=[rs_t], writes=[rs_t])
                    P.op("dve", lambda e, outT=outT, cs=cs, scl=scl: e.scalar_tensor_tensor(
                        out=outT[:, cs], in0=yb[:, cs], scalar=scl, in1=rs[:, :], op0=ALU.mult, op1=ALU.mult),
                        reads=[yb_t, rs_t], writes=[outT_t])
        def gdn_tile(t, h=h, hb=hb):
            b = t % NS
            tok = slice(t * 128, (t + 1) * 128)
            col = lambda arr: arr[:, t, h:h + 1]
            pa, pat = next_ps(C)
            P.op("pe", lambda e, pa=pa, tok=tok: e.matmul(pa[:, 0:128], lhsT=kTn[:, tok], rhs=kTn[:, tok], start=True, stop=True),
                 reads=[kTn_t], writes=[pat])
            P.op("pe", lambda e, pa=pa, tok=tok: e.matmul(pa[:, 128:256], lhsT=kTn[:, tok], rhs=qTn[:, tok], start=True, stop=True),
                 reads=[kTn_t, qTn_t], writes=[pat])
            pb, pbt = next_ps(C)
            gbc = g_all[:, t, h:h + 1].to_broadcast([128, 128])
            for (o, mname) in ((0, "g_pos"), (128, "g_negt")):
                P.op("pe", lambda e, pb=pb, o=o, gbc=gbc: e.matmul(pb[:, o:o + 128], lhsT=gbc, rhs=C.cm["m_incl"][:, :], start=True, stop=False),
                     reads=[g_t, C.cm_t["m_incl"]], writes=[pbt])
                P.op("pe", lambda e, pb=pb, o=o, mname=mname: e.matmul(pb[:, o:o + 128], lhsT=C.ident[:, :], rhs=C.cm[mname][:, :], start=False, stop=True),
                     reads=[C.ident_t, C.cm_t[mname]], writes=[pbt])
            pc, pct = next_ps(C)
            P.op("pe", lambda e, pc=pc, tok=tok: e.transpose(out=pc[:, 0:128], in_=vT[:, tok], identity=C.ident[:, :]),
                 reads=[vT_t, C.ident_t], writes=[pct])
            pd, pdt = next_ps(C)
            pdb = pd[:, :].bitcast(BF16)
            P.op("pe", lambda e, pdb=pdb, tok=tok: e.transpose(out=pdb[:, 0:128], in_=kTn[:, tok], identity=C.identb[:, :]),
                 reads=[kTn_t, C.identb_t], writes=[pdt])
            yield
            P.op("act", lambda e, pb=pb, b=b, t=t, h=h: e.activation(out=Dm[b][:, :], in_=pb[:, 0:128], func=AF.Exp, bias=cum[:, t, h:h + 1], scale=-1.0),
                 reads=[pbt, cum_t], writes=[Dm_t[b]])
            P.op("act", lambda e, pb=pb, b=b, t=t, h=h: e.activation(out=DTm[b][:, :], in_=pb[:, 128:256], func=AF.Exp, bias=ncum[:, t, h:h + 1], scale=1.0),
                 reads=[pbt, ncum_t], writes=[DTm_t[b]])
            P.op("dve", lambda e, pa=pa, b=b, t=t, h=h: e.scalar_tensor_tensor(
                out=Lm[b][:, :], in0=pa[:, 0:128], scalar=beta[:, t, h:h + 1], in1=Dm[b][:, :], op0=ALU.mult, op1=ALU.mult),
                reads=[pat, beta_t, Dm_t[b]], writes=[Lm_t[b]])
            P.op("dve", lambda e, pa=pa, b=b: e.tensor_tensor(out=qkT[b][:, :], in0=pa[:, 128:256], in1=DTm[b][:, :], op=ALU.mult),
                 reads=[pat, DTm_t[b]], writes=[qkT_t[b]])
            P.op("dve", lambda e, pc=pc, b=b, t=t, h=h: e.tensor_scalar(out=Xa[b][:, 0:128], in0=pc[:, 0:128], scalar1=beta[:, t, h:h + 1],
                                                                     scalar2=None, op0=ALU.mult), reads=[pct, beta_t], writes=[Xa_t[b]])
            P.op("dve", lambda e, pdb=pdb, b=b, t=t, h=h: e.tensor_scalar(out=Xa[b][:, 128:256], in0=pdb[:, 0:128], scalar1=bec[:, t, h:h + 1],
                                                                      scalar2=None, op0=ALU.mult), reads=[pdt, bec_t], writes=[Xa_t[b]])
            P.op("act", lambda e, pdb=pdb, b=b, t=t, h=h: e.activation(out=kend[b][:, :], in_=pdb[:, 0:128], func=AF.Copy, scale=eend[:, t, h:h + 1]),
                 reads=[pdt, eend_t], writes=[kend_t[b]])
            yield
            pe_, pet = next_ps(C)
            P.op("pe", lambda e, pe_=pe_, b=b: e.transpose(out=pe_[:, 0:128], in_=Lm[b][:, :], identity=C.ident[:, :]),
                 reads=[Lm_t[b], C.ident_t], writes=[pet])
            P.op("act", lambda e, pe_=pe_, b=b: e.activation(out=LT[b][:, :], in_=pe_[:, 0:128], func=AF.Copy), reads=[pet], writes=[LT_t[b]])
            yield
            px, pxt = next_ps(C)
            P.op("pe", lambda e, px=px, b=b: e.matmul(px[:, 0:256], lhsT=LT[b][:, :], rhs=Xa[b][:, :], start=True, stop=True),
                 reads=[LT_t[b], Xa_t[b]], writes=[pxt])
            P.op("dve", lambda e, px=px, b=b: e.tensor_tensor(out=Xb[b][:, :], in0=Xa[b][:, :], in1=px[:, 0:256], op=ALU.subtract),
                 reads=[Xa_t[b], pxt], writes=[Xb_t[b]])
            Xc, Xc_t, Xn, Xn_t = Xb[b], Xb_t[b], Xa[b], Xa_t[b]
            Pc, Pc_t, PTc, PTc_t = Lm[b], Lm_t[b], LT[b], LT_t[b]
            for lev in range(1, 7):
                if lev % 2 == 1:
                    Pn, Pn_t, PTn, PTn_t = Pa[b], Pa_t[b], PTa[b], PTa_t[b]
                else:
                    Pn, Pn_t, PTn, PTn_t = Pb[b], Pb_t[b], PTb[b], PTb_t[b]
                yield
                pp2, pp2t = next_ps(C)
                P.op("pe", lambda e, pp2=pp2, Pc=Pc, PTc=PTc: e.matmul(pp2[:, 0:128], lhsT=Pc[:, :], rhs=PTc[:, :], start=True, stop=True),
                     reads=[Pc_t, PTc_t], writes=[pp2t])
                if lev < 6:
                    P.op("pe", lambda e, pp2=pp2, Pc=Pc, PTc=PTc: e.matmul(pp2[:, 128:256], lhsT=PTc[:, :], rhs=Pc[:, :], start=True, stop=True),
                         reads=[Pc_t, PTc_t], writes=[pp2t])
                P.op("act", lambda e, pp2=pp2, PTn=PTn: e.activation(out=PTn[:, :], in_=pp2[:, 0:128], func=AF.Copy), reads=[pp2t], writes=[PTn_t])
                if lev < 6:
                    P.op("dve", lambda e, pp2=pp2, Pn=Pn: e.tensor_scalar(out=Pn[:, :], in0=pp2[:, 128:256], scalar1=1.0, scalar2=None, op0=ALU.mult),
                         reads=[pp2t], writes=[Pn_t])
                yield
                px2, px2t = next_ps(C)
                P.op("pe", lambda e, px2=px2, PTn=PTn, Xc=Xc: e.matmul(px2[:, 0:256], lhsT=PTn[:, :], rhs=Xc[:, :], start=True, stop=True),
                     reads=[PTn_t, Xc_t], writes=[px2t])
                P.op("dve", lambda e, px2=px2, Xc=Xc, Xn=Xn: e.tensor_tensor(out=Xn[:, :], in0=Xc[:, :], in1=px2[:, 0:256], op=ALU.add),
                     reads=[Xc_t, px2t], writes=[Xn_t])
                Xc, Xc_t, Xn, Xn_t = Xn, Xn_t, Xc, Xc_t
                Pc, Pc_t, PTc, PTc_t = Pn, Pn_t, PTn, PTn_t
            yield
            pw_, pwt_ = next_ps(C)
            P.op("pe", lambda e, pw_=pw_, Xc=Xc: e.transpose(out=pw_[:, 0:128], in_=Xc[:, 128:256], identity=C.ident[:, :]),
                 reads=[Xc_t, C.ident_t], writes=[pwt_])
            P.op("act", lambda e, pw_=pw_, b=b: e.activation(out=wT[b][:, :], in_=pw_[:, 0:128], func=AF.Copy), reads=[pwt_], writes=[wT_t[b]])
            yield
            pv, pvt = next_ps(C)
            P.op("pe", lambda e, pv=pv, b=b: e.matmul(pv[:, 0:128], lhsT=wT[b][:, :], rhs=Sbf[:, :], start=True, stop=True),
                 reads=[wT_t[b], Sbf_t], writes=[pvt])
            P.op("dve", lambda e, pv=pv, b=b, Xc=Xc: e.tensor_tensor(out=vnew[b][:, :], in0=Xc[:, 0:128], in1=pv[:, 0:128], op=ALU.subtract),
                 reads=[Xc_t, pvt], writes=[vnew_t[b]])
            po, pot = next_ps(C)
            P.op("pe", lambda e, po=po, b=b: e.matmul(po[:, 0:128], lhsT=qkT[b][:, :], rhs=vnew[b][:, :], start=True, stop=True),
                 reads=[qkT_t[b], vnew_t[b]], writes=[pot])
            P.op("pe", lambda e, po=po, tok=tok: e.matmul(po[:, 128:256], lhsT=qTn[:, tok], rhs=Sbf[:, :], start=True, stop=True),
                 reads=[qTn_t, Sbf_t], writes=[pot])
            P.op("act", lambda e, po=po, b=b: e.activation(out=intra[b][:, :], in_=po[:, 0:128], func=AF.Copy), reads=[pot], writes=[intra_t[b]])
            P.op("dve", lambda e, po=po, b=b, t=t, h=h: e.scalar_tensor_tensor(
                out=osb[b][:, :], in0=po[:, 128:256], scalar=ecum[:, t, h:h + 1], in1=intra[b][:, :], op0=ALU.mult, op1=ALU.add),
                reads=[pot, ecum_t, intra_t[b]], writes=[osb_t[b]])
            pu, put = next_ps(C)
            P.op("pe", lambda e, pu=pu, b=b: e.matmul(pu[:, 0:128], lhsT=kend[b][:, :], rhs=vnew[b][:, :], start=True, stop=True),
                 reads=[kend_t[b], vnew_t[b]], writes=[put])
            P.op("dve", lambda e, pu=pu, t=t, h=h: e.scalar_tensor_tensor(
                out=Sst[:, :], in0=Sst[:, :], scalar=ecl[:, t, h:h + 1], in1=pu[:, 0:128], op0=ALU.mult, op1=ALU.add),
                reads=[S_t, ecl_t, put], writes=[S_t])
            P.op("act", lambda e: e.activation(out=Sbf[:, :], in_=Sst[:, :], func=AF.Copy), reads=[S_t], writes=[Sbf_t])
            yield
            pz, pzt = next_ps(C)
            for k in range(8):
                P.op("pe", lambda e, pz=pz, k=k, tok=tok: e.matmul(pz[:, 0:128], lhsT=XT[:, k, tok], rhs=Wh[:, k, 384:512], start=(k == 0), stop=(k == 7)),
                     reads=[XT_t[t], Wh_t[3]], writes=[pzt])
            P.op("act", lambda e, pz=pz, b=b: e.activation(out=sg[b][:, :], in_=pz[:, 0:128], func=AF.Exp, scale=-1.0), reads=[pzt], writes=[sg_t[b]])
            P.op("act", lambda e, b=b: e.activation(out=sg[b][:, :], in_=sg[b][:, :], func=AF.Ln, bias=1.0, scale=1.0), reads=[sg_t[b]], writes=[sg_t[b]])
            P.op("act", lambda e, b=b: e.activation(out=sg[b][:, :], in_=sg[b][:, :], func=AF.Exp, scale=-1.0), reads=[sg_t[b]], writes=[sg_t[b]])
            P.op("dve", lambda e, pz=pz, b=b: e.tensor_tensor(out=sg[b][:, :], in0=pz[:, 0:128], in1=sg[b][:, :], op=ALU.mult),
                 reads=[pzt, sg_t[b]], writes=[sg_t[b]])
            P.op("dve", lambda e, b=b: e.tensor_tensor(out=sg[b][:, :], in0=sg[b][:, :], in1=ng[:, :], op=ALU.mult),
                 reads=[sg_t[b], ng_t], writes=[sg_t[b]])
            P.op("dve", lambda e, b=b: e.scalar_tensor_tensor(out=junk[b][:, :], in0=osb[b][:, :], scalar=1.0, in1=osb[b][:, :],
                                                              op0=ALU.mult, op1=ALU.mult, accum_out=ss[b][:, 0:1]),
                 reads=[osb_t[b]], writes=[junk_t[b], ss_t[b]])
            P.op("act", lambda e, b=b: e.activation(out=ss[b][:, 1:2], in_=ss[b][:, 0:1], func=AF.Ln, bias=C.eps6[:, 0:1], scale=1.0 / 128),
                 reads=[ss_t[b], C.eps6_t], writes=[ss_t[b]])
            P.op("act", lambda e, b=b: e.activation(out=ss[b][:, 2:3], in_=ss[b][:, 1:2], func=AF.Exp, scale=-0.5),
                 reads=[ss_t[b]], writes=[ss_t[b]])
            P.op("dve", lambda e, b=b, t=t, hb=hb: e.scalar_tensor_tensor(
                out=O[:, t, hb:hb + 128], in0=osb[b][:, :], scalar=ss[b][:, 2:3], in1=sg[b][:, :], op0=ALU.mult, op1=ALU.mult),
                reads=[osb_t[b], ss_t[b], sg_t[b]], writes=[O_t[t]])


        run_pipelined((gdn_tile(t) for t in range(NT)), NS)
def hgrn_phase(C, l):
    P, nc, dr = C.P, C.nc, C.dr
    with contextlib.ExitStack() as st:
        O = alloc(C, st, "h_O", [128, NT, D], BF16)
        O_t = [T("h_O%d" % t) for t in range(NT)]
        with contextlib.ExitStack() as st2:
            hgrn_heads(C, l, st2, O, O_t)
            barrier(C)
        outproj_phase(C, l, O, O_t, dr["hgrn_w_out"])


def hgrn_lb(C, st, l, src_ap, shape, name):
    P = C.P
    pn, n = shape
    hb = alloc(C, st, name + "_hb", [pn, 4, n], F32); hb_t = T(name + "_hb")
    P.dma("sp", hb[:, :, :], src_ap, writes=[hb_t], allow_slow_non_contiguous=True)
    mx = alloc(C, st, name + "_mx", [pn, n], F32); mx_t = T(name + "_mx")
    P.op("dve", lambda e: e.tensor_tensor(out=mx[:, :], in0=hb[:, 0, :], in1=hb[:, 1, :], op=ALU.max), reads=[hb_t], writes=[mx_t])
    for j in (2, 3):
        P.op("dve", lambda e, j=j: e.tensor_tensor(out=mx[:, :], in0=mx[:, :], in1=hb[:, j, :], op=ALU.max), reads=[hb_t, mx_t], writes=[mx_t])
    for j in range(4):
        P.op("dve", lambda e, j=j: e.tensor_tensor(out=hb[:, j, :], in0=hb[:, j, :], in1=mx[:, :], op=ALU.subtract), reads=[hb_t, mx_t], writes=[hb_t])
    P.op("act", lambda e: e.activation(out=hb[:, :, :], in_=hb[:, :, :], func=AF.Exp), reads=[hb_t], writes=[hb_t])
    den = mx
    P.op("dve", lambda e: e.tensor_tensor(out=den[:, :], in0=hb[:, 0, :], in1=hb[:, 1, :], op=ALU.add), reads=[hb_t, mx_t], writes=[mx_t])
    for j in (2, 3):
        P.op("dve", lambda e, j=j: e.tensor_tensor(out=den[:, :], in0=den[:, :], in1=hb[:, j, :], op=ALU.add), reads=[hb_t, mx_t], writes=[mx_t])
    P.op("dve", lambda e: e.reciprocal(out=den[:, :], in_=den[:, :]), reads=[mx_t], writes=[mx_t])
    lb = alloc(C, st, name + "_lb", [pn, n], F32); lb_t = T(name + "_lb")
    oml = alloc(C, st, name + "_oml", [pn, n], F32)
    if l == 0:
        P.op("dve", lambda e: e.memset(lb[:, :], 0.0), writes=[lb_t])
    else:
        P.op("dve", lambda e: e.tensor_copy(out=lb[:, :], in_=hb[:, 1, :]), reads=[hb_t], writes=[lb_t])
        for j in range(2, l + 1):
            P.op("dve", lambda e, j=j: e.tensor_tensor(out=lb[:, :], in0=lb[:, :], in1=hb[:, j, :], op=ALU.add), reads=[hb_t, lb_t], writes=[lb_t])
        P.op("dve", lambda e: e.tensor_tensor(out=lb[:, :], in0=lb[:, :], in1=den[:, :], op=ALU.mult), reads=[mx_t, lb_t], writes=[lb_t])
    P.op("dve", lambda e: e.tensor_scalar(out=oml[:, :], in0=lb[:, :], scalar1=-1.0, scalar2=1.0, op0=ALU.mult, op1=ALU.add),
         reads=[lb_t], writes=[lb_t])
    return lb, oml, lb_t


def hgrn_heads(C, l, st, O, O_t):
    P, nc, dr = C.P, C.nc, C.dr
    H, DK, DV = 8, 128, 128
    XT = alloc(C, st, "h_XT", [128, 8, S], BF16)
    XT_t = {t: T("h_XT%d" % t) for t in range(NT)}
    build_xT(C, XT, XT_t, range(NT))
    win = dr["hgrn_w_in"].rearrange("(k p) n -> p k n", p=128)
    lbb, omlb, lbb_t = hgrn_lb(C, st, l, dr["hgrn_lower_bounds"].rearrange("(o l) n -> o l n", o=1).to_broadcast([128, 4, D]),
                               (128, D), "h_b")
    lbT, omlT, lbT_t = hgrn_lb(C, st, l, dr["hgrn_lower_bounds"].rearrange("l (c p) -> p l c", p=128), (128, 8), "h_T")
    ng = alloc(C, st, "h_ng", [128, DV], F32); ng_t = T("h_ng")
    load_bcast_row(C, ng[:, :], ng_t, dr["hgrn_norm_g"][0:1, :])
    Wh = [alloc(C, st, "h_Wh%d" % i, [128, 8, 512], BF16) for i in range(2)]
    Wh_t = [[T("h_Wh%d_%d" % (i, j)) for j in range(4)] for i in range(2)]
    state = alloc(C, st, "h_state", [128, DV], F32); state_t = T("h_state")
    state_bf = alloc(C, st, "h_statebf", [128, DV], BF16); statebf_t = T("h_statebf")
    NB = 2
    def mk(name, shape, dt):
        return [alloc(C, st, "%s%d" % (name, i), shape, dt) for i in range(NB)], [T("%s%d" % (name, i)) for i in range(NB)]
    sig, sig_t = mk("h_sig", [128, 128], F32)
    a_, a_t = mk("h_a", [128, 128], F32)
    fg, fg_t = mk("h_fg", [128, 128], F32)
    kin, kin_t = mk("h_kin", [128, 128], F32)
    la, la_t = mk("h_la", [128, 128], F32)
    sgT, sgT_t = mk("h_sgT", [128, 128], F32)
    ecT, ecT_t = mk("h_ecT", [128, 128], F32)
    eiT, eiT_t = mk("h_eiT", [128, 128], F32)
    eR, eR_t = mk("h_eR", [128, 128], F32)
    qd, qd_t = mk("h_qd", [128, 128], BF16)
    ki, ki_t = mk("h_ki", [128, 128], BF16)
    ke, ke_t = mk("h_ke", [128, 128], BF16)
    vb, vb_t = mk("h_vb", [128, DV], BF16)
    sg, sg_t = mk("h_sg", [128, DV], F32)
    sT, sT_t = mk("h_sT", [128, 128], BF16)
    junk, junk_t = mk("h_junk", [128, DV], F32)
    osq, osq_t = mk("h_osq", [128, DV], F32)
    ss, ss_t = mk("h_ss", [128, 4], F32)
    for h in range(H):
        wb = h % 2
        hs = slice(h * 128, (h + 1) * 128)
        for j in range(4):
            P.dma("pool", Wh[wb][:, :, j * 128:(j + 1) * 128], win[:, :, j * 1024 + h * 128:j * 1024 + (h + 1) * 128], writes=[Wh_t[wb][j]])
        P.op("pool", lambda e: e.memset(state[:, :], 0.0), writes=[state_t])
        P.op("pool", lambda e: e.memset(state_bf[:, :], 0.0), writes=[statebf_t])
        def hgrn_tile(t, h=h, wb=wb):
            b = t % NB
            pool = (0, 1, 2, 3) if b == 0 else (4, 5, 6, 7)
            tok = slice(t * 128, (t + 1) * 128)
            p1, p1t = next_ps(C, pool)
            for j in range(2):
                for k in range(8):
                    P.op("pe", lambda e, p1=p1, j=j, k=k, wb=wb, tok=tok: e.matmul(
                        p1[:, j * 128:(j + 1) * 128], lhsT=Wh[wb][:, k, j * 128:(j + 1) * 128], rhs=XT[:, k, tok],
                        start=(k == 0), stop=(k == 7)), reads=[Wh_t[wb][j], XT_t[t]], writes=[p1t])
            p2, p2t = next_ps(C, pool)
            for k in range(8):
                P.op("pe", lambda e, p2=p2, k=k, wb=wb, tok=tok: e.matmul(
                    p2[:, 0:384], lhsT=XT[:, k, tok], rhs=Wh[wb][:, k, 128:512], start=(k == 0), stop=(k == 7)),
                    reads=[Wh_t[wb][1], Wh_t[wb][2], Wh_t[wb][3], XT_t[t]], writes=[p2t])
            yield
            P.op("act", lambda e, p2=p2, b=b: e.activation(out=sig[b][:, :], in_=p2[:, 0:128], func=AF.Exp, scale=-1.0),
                 reads=[p2t], writes=[sig_t[b]])
            P.op("act", lambda e, b=b: e.activation(out=sig[b][:, :], in_=sig[b][:, :], func=AF.Ln, bias=1.0, scale=1.0),
                 reads=[sig_t[b]], writes=[sig_t[b]])
            P.op("act", lambda e, b=b: e.activation(out=sig[b][:, :], in_=sig[b][:, :], func=AF.Exp, scale=-1.0),
                 reads=[sig_t[b]], writes=[sig_t[b]])
            P.op("dve", lambda e, b=b, hs=hs: e.tensor_tensor(out=a_[b][:, :], in0=sig[b][:, :], in1=omlb[:, hs], op=ALU.mult),
                 reads=[sig_t[b], lbb_t], writes=[a_t[b]])
            P.op("dve", lambda e, b=b, hs=hs: e.tensor_tensor(out=fg[b][:, :], in0=a_[b][:, :], in1=lbb[:, hs], op=ALU.add),
                 reads=[a_t[b], lbb_t], writes=[fg_t[b]])
            P.op("dve", lambda e, b=b, hs=hs: e.tensor_tensor(out=kin[b][:, :], in0=omlb[:, hs], in1=a_[b][:, :], op=ALU.subtract),
                 reads=[a_t[b], lbb_t], writes=[kin_t[b]])
            P.op("act", lambda e, b=b: e.activation(out=la[b][:, :], in_=fg[b][:, :], func=AF.Ln),
                 reads=[fg_t[b]], writes=[la_t[b]])
            P.op("act", lambda e, p1=p1, b=b: e.activation(out=sgT[b][:, :], in_=p1[:, 128:256], func=AF.Exp, scale=1.0),
                 reads=[p1t], writes=[sgT_t[b]])
            P.op("act", lambda e, b=b: e.activation(out=sgT[b][:, :], in_=sgT[b][:, :], func=AF.Ln, bias=1.0, scale=1.0),
                 reads=[sgT_t[b]], writes=[sgT_t[b]])
            P.op("act", lambda e, b=b: e.activation(out=sgT[b][:, :], in_=sgT[b][:, :], func=AF.Exp, scale=-1.0),
                 reads=[sgT_t[b]], writes=[sgT_t[b]])
            yield
            p4, p4t = next_ps(C, pool)
            P.op("pe", lambda e, p4=p4, b=b: e.matmul(p4[:, 0:128], lhsT=la[b][:, :], rhs=C.cm["m_incl"][:, :], start=True, stop=True),
                 reads=[la_t[b], C.cm_t["m_incl"]], writes=[p4t])
            P.op("pe", lambda e, p4=p4, b=b: e.matmul(p4[:, 128:256], lhsT=C.cm["m_rev"][:, :], rhs=la[b][:, :], start=True, stop=True),
                 reads=[la_t[b], C.cm_t["m_rev"]], writes=[p4t])
            P.op("act", lambda e, p4=p4, b=b: e.activation(out=ecT[b][:, :], in_=p4[:, 0:128], func=AF.Exp),
                 reads=[p4t], writes=[ecT_t[b]])
            P.op("act", lambda e, p4=p4, b=b: e.activation(out=eiT[b][:, :], in_=p4[:, 0:128], func=AF.Exp, scale=-1.0),
                 reads=[p4t], writes=[eiT_t[b]])
            P.op("act", lambda e, p4=p4, b=b: e.activation(out=eR[b][:, :], in_=p4[:, 128:256], func=AF.Exp),
                 reads=[p4t], writes=[eR_t[b]])
            P.op("dve", lambda e, p1=p1, b=b: e.tensor_tensor(out=qd[b][:, :], in0=p1[:, 0:128], in1=ecT[b][:, :], op=ALU.mult),
                 reads=[p1t, ecT_t[b]], writes=[qd_t[b]])
            P.op("dve", lambda e, b=b, h=h: e.scalar_tensor_tensor(
                out=ki[b][:, :], in0=sgT[b][:, :], scalar=omlT[:, h:h + 1], in1=eiT[b][:, :], op0=ALU.mult, op1=ALU.mult),
                reads=[sgT_t[b], lbT_t, eiT_t[b]], writes=[ki_t[b]])
            P.op("dve", lambda e, b=b: e.tensor_tensor(out=ke[b][:, :], in0=kin[b][:, :], in1=eR[b][:, :], op=ALU.mult),
                 reads=[kin_t[b], eR_t[b]], writes=[ke_t[b]])
            P.op("act", lambda e, p2=p2, b=b: e.activation(out=vb[b][:, :], in_=p2[:, 128:256], func=AF.Copy),
                 reads=[p2t], writes=[vb_t[b]])
            P.op("act", lambda e, p2=p2, b=b: e.activation(out=sg[b][:, :], in_=p2[:, 256:384], func=AF.Exp, scale=-1.0),
                 reads=[p2t], writes=[sg_t[b]])
            P.op("act", lambda e, b=b: e.activation(out=sg[b][:, :], in_=sg[b][:, :], func=AF.Ln, bias=1.0, scale=1.0),
                 reads=[sg_t[b]], writes=[sg_t[b]])
            P.op("act", lambda e, b=b: e.activation(out=sg[b][:, :], in_=sg[b][:, :], func=AF.Exp, scale=-1.0),
                 reads=[sg_t[b]], writes=[sg_t[b]])
            P.op("dve", lambda e, p2=p2, b=b: e.tensor_tensor(out=sg[b][:, :], in0=p2[:, 256:384], in1=sg[b][:, :], op=ALU.mult),
                 reads=[p2t, sg_t[b]], writes=[sg_t[b]])
            P.op("dve", lambda e, b=b: e.tensor_tensor(out=sg[b][:, :], in0=sg[b][:, :], in1=ng[:, :], op=ALU.mult),
                 reads=[sg_t[b], ng_t], writes=[sg_t[b]])
            yield
            p5, p5t = next_ps(C, pool)
            P.op("pe", lambda e, p5=p5, b=b: e.matmul(p5[:, 0:128], lhsT=ki[b][:, :], rhs=qd[b][:, :], start=True, stop=True),
                 reads=[ki_t[b], qd_t[b]], writes=[p5t])
            P.op("dve", lambda e, p5=p5, b=b: e.tensor_tensor(out=sT[b][:, :], in0=p5[:, 0:128], in1=C.cm["m_caus"][:, :], op=ALU.mult),
                 reads=[p5t, C.cm_t["m_caus"]], writes=[sT_t[b]])
            yield
            p6, p6t = next_ps(C, pool)
            P.op("pe", lambda e, p6=p6, b=b: e.matmul(p6[:, 0:DV], lhsT=sT[b][:, :], rhs=vb[b][:, :], start=True, stop=False),
                 reads=[sT_t[b], vb_t[b]], writes=[p6t])
            P.op("pe", lambda e, p6=p6, b=b: e.matmul(p6[:, 0:DV], lhsT=qd[b][:, :], rhs=state_bf[:, :], start=False, stop=True),
                 reads=[qd_t[b], statebf_t], writes=[p6t])
            yield
            p7, p7t = next_ps(C, pool)
            P.op("pe", lambda e, p7=p7, b=b: e.matmul(p7[:, 0:DV], lhsT=ke[b][:, :], rhs=vb[b][:, :], start=True, stop=True),
                 reads=[ke_t[b], vb_t[b]], writes=[p7t])
            P.op("dve", lambda e, p7=p7, b=b: e.scalar_tensor_tensor(
                out=state[:, :], in0=state[:, :], scalar=ecT[b][:, 127:128], in1=p7[:, 0:DV], op0=ALU.mult, op1=ALU.add),
                reads=[state_t, ecT_t[b], p7t], writes=[state_t])
            P.op("act", lambda e: e.activation(out=state_bf[:, :], in_=state[:, :], func=AF.Copy),
                 reads=[state_t], writes=[statebf_t])
            yield
            P.op("act", lambda e, p6=p6, b=b: e.activation(out=junk[b][:, :], in_=p6[:, 0:DV], func=AF.Copy),
                 reads=[p6t], writes=[junk_t[b]])
            P.op("dve", lambda e, b=b: e.scalar_tensor_tensor(out=osq[b][:, :], in0=junk[b][:, :], scalar=1.0, in1=junk[b][:, :],
                                                              op0=ALU.mult, op1=ALU.mult, accum_out=ss[b][:, 0:1]),
                 reads=[junk_t[b]], writes=[osq_t[b], ss_t[b]])
            P.op("act", lambda e, b=b: e.activation(out=ss[b][:, 1:2], in_=ss[b][:, 0:1], func=AF.Ln, bias=C.eps6[:, 0:1], scale=1.0 / DV),
                 reads=[ss_t[b], C.eps6_t], writes=[ss_t[b]])
            P.op("act", lambda e, b=b: e.activation(out=ss[b][:, 2:3], in_=ss[b][:, 1:2], func=AF.Exp, scale=-0.5),
                 reads=[ss_t[b]], writes=[ss_t[b]])
            P.op("dve", lambda e, p6=p6, b=b, t=t, h=h: e.scalar_tensor_tensor(
                out=O[:, t, h * DV:(h + 1) * DV], in0=junk[b][:, :], scalar=ss[b][:, 2:3], in1=sg[b][:, :], op0=ALU.mult, op1=ALU.mult),
                reads=[junk_t[b], ss_t[b], sg_t[b]], writes=[O_t[t]])
        run_pipelined((hgrn_tile(t) for t in range(NT)), NB)


def make_in_map(inputs, b):
    m = {}
    m["x"] = np.ascontiguousarray(inputs["x"][b])
    m["positions"] = np.ascontiguousarray(inputs["positions"][b].reshape(S, 1).astype(np.int32))
    for k in ("gla_w_in", "gla_w_gk", "gla_b_gk", "gla_norm_g", "gla_w_out", "moba_w_in", "moba_w_out", "gdn_w_in",
              "gdn_conv_w", "gdn_a_log", "gdn_dt_bias", "gdn_norm_g", "gdn_w_out", "hgrn_w_in", "hgrn_norm_g",
              "hgrn_w_out"):
        v = np.asarray(inputs[k])[0]
        if v.ndim == 1:
            v = v.reshape(1, -1)
        m[k] = np.ascontiguousarray(v)
    m["hgrn_lower_bounds"] = np.ascontiguousarray(inputs["hgrn_lower_bounds"])
    m["ffn_w_gu"] = np.ascontiguousarray(inputs["ffn_w_gu"])
    m["ffn_w_down"] = np.ascontiguousarray(inputs["ffn_w_down"])
    m["ln_g"] = np.ascontiguousarray(np.asarray(inputs["ln_g"]).reshape(DEPTH * 2, D))
    m["ln_b"] = np.ascontiguousarray(np.asarray(inputs["ln_b"]).reshape(DEPTH * 2, D))
    for k, v in host_consts().items():
        m["c_" + k] = v
    return m


def _todo(C, l):
    raise NotImplementedError


MIXERS = [gla_phase, moba_phase, gdn_phase, hgrn_phase]
_NC_CACHE = {}


def kernel(**inputs):
    if "nc" not in _NC_CACHE:
        _NC_CACHE["nc"] = build()
    nc = _NC_CACHE["nc"]
    in_maps = [make_in_map(inputs, b) for b in range(NCORES)]
    res = run_bass_kernel_spmd(nc, in_maps, core_ids=list(range(NCORES)))
    return np.stack([np.asarray(r["y"]) for r in res.results], axis=0).astype(np.float32)
```

```python
import contextlib
import numpy as np
import concourse.bass as bass
import concourse.mybir as mybir
from concourse.bass_utils import run_bass_kernel_spmd

F32 = mybir.dt.float32
BF16 = mybir.dt.bfloat16
I32 = mybir.dt.int32
AF = mybir.ActivationFunctionType
ALU = mybir.AluOpType
AX = mybir.AxisListType

D = 1024
S = 2048
NT = S // 128
DEPTH = 4
FFN_H = 2816
ALPHA = (2.0 * DEPTH) ** 0.25
NCORES = 8


class T:
    __slots__ = ("name", "w", "r", "excl")

    def __init__(self, name, excl=False):
        self.name = name
        self.w = None
        self.r = []
        self.excl = excl


class Prog:
    ENGS = ("pe", "dve", "act", "pool", "sp")
    NLANES = 6

    def __init__(self, nc):
        self.nc = nc
        self.ins = []
        self.pending = {e: set() for e in self.ENGS}
        self.last_barrier = 0
        self.eng_obj = {"pe": nc.tensor, "dve": nc.vector, "act": nc.scalar, "pool": nc.gpsimd, "sp": nc.sync}

    def op(self, eng, fn, reads=(), writes=(), dma=False):
        idx = len(self.ins)
        deps = set()
        for t in reads:
            if t.w is not None:
                deps.add(t.w)
            if t.excl:
                deps.update(r for r in t.r if self.ins[r]["eng"] != eng)
        for t in writes:
            if t.w is not None:
                deps.add(t.w)
            deps.update(t.r)
        if self.pending[eng]:
            deps |= self.pending[eng]
            self.pending[eng] = set()
        if eng == "pe":
            deps = {d for d in deps if self.ins[d]["eng"] != "pe" or self.ins[d]["dma"]}
        self.ins.append(dict(eng=eng, fn=fn, deps=deps, dma=dma))
        for t in reads:
            t.r.append(idx)
        for t in writes:
            t.w = idx
            t.r = []
        return idx

    def barrier(self):
        last = {}
        deps = set()
        for i in range(self.last_barrier, len(self.ins)):
            ins = self.ins[i]
            if ins["dma"]:
                deps.add(i)
            else:
                last[ins["eng"]] = i
        deps |= set(last.values())
        for e in self.ENGS:
            self.pending[e] |= deps
        self.last_barrier = len(self.ins)

    def dma(self, eng, out, in_, reads=(), writes=(), **kw):
        return self.op(eng, lambda e: e.dma_start(out=out, in_=in_, **kw), reads, writes, dma=True)

    def finalize(self):
        nc = self.nc
        waited = set()
        for ins in self.ins:
            waited.update(ins["deps"])
        sems = {}
        with contextlib.ExitStack() as es:
            for e in self.ENGS:
                sems[e] = es.enter_context(nc.semaphore("s_" + e))
            for q in ("sp", "act", "pool"):
                for l in range(self.NLANES):
                    sems[(q, l)] = es.enter_context(nc.semaphore("d_%s%d" % (q, l)))
            cnt = {k: 0 for k in sems}
            sig = {}
            lane_rr = {"sp": 0, "act": 0, "pool": 0}
            lane_prev = {}
            for idx, ins in enumerate(self.ins):
                if ins["dma"]:
                    q = ins["eng"]
                    lane = (q, lane_rr[q] % self.NLANES)
                    lane_rr[q] += 1
                    ins["lane_prev"] = cnt[lane]
                    cnt[lane] += 16
                    sig[idx] = (lane, cnt[lane])
                elif idx in waited:
                    cnt[ins["eng"]] += 1
                    sig[idx] = (ins["eng"], cnt[ins["eng"]])
            self.sig_counts = dict(cnt)
            self.wait_hist = {}
            with nc.Block() as block:
                for eng in self.ENGS:
                    my = [(i, ins) for i, ins in enumerate(self.ins) if ins["eng"] == eng]
                    if not my:
                        continue

                    def body(e, my=my, eng=eng):
                        seen = {}
                        for idx, ins in my:
                            needs = {}
                            for d in ins["deps"]:
                                sk, val = sig[d]
                                if needs.get(sk, 0) < val:
                                    needs[sk] = val
                            if ins["dma"]:
                                sk, val = sig[idx]
                                if ins["lane_prev"] > 0 and needs.get(sk, 0) < ins["lane_prev"]:
                                    needs[sk] = ins["lane_prev"]
                            nw = 0
                            for sk, val in needs.items():
                                if seen.get(sk, 0) < val:
                                    e.wait_ge(sems[sk], val)
                                    seen[sk] = val
                                    nw += 1
                            self.wait_hist[nw] = self.wait_hist.get(nw, 0) + 1
                            r = ins["fn"](e)
                            if idx in sig:
                                sk, val = sig[idx]
                                r.then_inc(sems[sk], 16 if ins["dma"] else 1)
                        for l in range(self.NLANES):
                            sk = (eng, l)
                            if sk in cnt and cnt[sk] > 0 and seen.get(sk, 0) < cnt[sk]:
                                e.wait_ge(sems[sk], cnt[sk])

                    getattr(block, {"pe": "tensor", "dve": "vector", "act": "scalar", "pool": "gpsimd", "sp": "sync"}[eng])(body)


class Ctx:
    pass


def host_consts():
    c = {}
    c["ident"] = np.eye(128, dtype=np.float32)
    i = np.arange(128)
    c["m_incl"] = (i[:, None] <= i[None, :]).astype(np.float32)
    c["m_rev"] = (i[:, None] > i[None, :]).astype(np.float32)
    c["m_caus"] = (i[:, None] <= i[None, :]).astype(np.float32)
    inv = (500000.0 ** (-np.arange(0, 32, 2, dtype=np.float32) / 32.0)).astype(np.float32)
    c["invf"] = np.concatenate([inv, inv]).reshape(32, 1).astype(np.float32)
    c["sgn"] = np.concatenate([-np.ones(16), np.ones(16)]).reshape(32, 1).astype(np.float32)
    E = np.zeros((8, 8, 128), np.float32)
    for n in range(8):
        E[n, n, :] = 30000.0
    c["E"] = E.reshape(8, 1024)
    c["causneg"] = np.where(i[:, None] > i[None, :], -30000.0, 0.0).astype(np.float32)
    npast = np.where(np.arange(8)[None, :] < np.arange(8)[:, None], 0.0, -1e30).astype(np.float32)
    c["negpast"] = npast.reshape(1, 64)
    BIG = 1.0e5
    c["g_pos"] = np.where(i[None, :] >= i[:, None], BIG, 0.0).astype(np.float32)
    c["g_negt"] = np.where(i[None, :] < i[:, None], -BIG, 0.0).astype(np.float32)
    c["ones"] = np.ones((128, 128), np.float32)
    c["m_incl_gla"] = c["m_incl"] * np.float32(-1.0 / 16.0)
    c["m_rev_gla"] = c["m_rev"] * np.float32(-1.0 / 16.0)
    return c


def build(n_layers=DEPTH, dbg=None, layers=None):
    dbg = dbg or {}
    nc = bass.Bass("TRN2", target_bir_lowering=False)
    dr = {}

    def din(name, shape, dt=F32):
        dr[name] = nc.dram_tensor(name, list(shape), dt, kind="ExternalInput").ap()
        return dr[name]

    din("x", [S, D])
    din("positions", [S, 1], I32)
    din("gla_w_in", [D, 3088]); din("gla_w_gk", [16, 512]); din("gla_b_gk", [1, 512]); din("gla_norm_g", [1, 256])
    din("gla_w_out", [D, D])
    din("moba_w_in", [D, 3072]); din("moba_w_out", [D, D])
    din("gdn_w_in", [D, 4112]); din("gdn_conv_w", [4, 3072]); din("gdn_a_log", [1, 8]); din("gdn_dt_bias", [1, 8])
    din("gdn_norm_g", [1, 128]); din("gdn_w_out", [D, D])
    din("hgrn_lower_bounds", [4, 1024]); din("hgrn_w_in", [D, 4096]); din("hgrn_norm_g", [1, 128])
    din("hgrn_w_out", [D, D])
    din("ffn_w_gu", [DEPTH, D, 2 * FFN_H]); din("ffn_w_down", [DEPTH, FFN_H, D])
    din("ln_g", [DEPTH * 2, D]); din("ln_b", [DEPTH * 2, D])
    for k, v in host_consts().items():
        din("c_" + k, v.shape)
    y_out = nc.dram_tensor("y", [S, D], F32, kind="ExternalOutput").ap()

    P = Prog(nc)
    C = Ctx()
    C.nc, C.P, C.dr, C.dbg = nc, P, dr, dbg
    with contextlib.ExitStack() as es:
        C.es = es
        C.X = es.enter_context(nc.sbuf_tensor("X", [128, NT, D], F32))
        C.Xt = [T("X%d" % t) for t in range(NT)]
        C.ps = [es.enter_context(nc.psum_tensor("ps%d" % i, [128, 512], F32)) for i in range(8)]
        C.pst = [T("ps%d" % i, excl=True) for i in range(8)]
        C.ident = es.enter_context(nc.sbuf_tensor("ident", [128, 128], F32))
        C.ident_t = T("ident")
        P.dma("sp", C.ident[:, :], dr["c_ident"][:, :], writes=[C.ident_t])
        C.eps5 = es.enter_context(nc.sbuf_tensor("eps5", [128, 1], F32))
        C.eps_t = T("eps5")
        P.op("pool", lambda e: e.memset(C.eps5[:, :], 1e-5), writes=[C.eps_t])
        C.ps_rr = 0
        C.ps_pool_rr = {}
        C.cm = {}
        C.cm_t = {}
        for nm in ("m_incl", "m_rev", "m_caus", "m_incl_gla", "m_rev_gla", "g_pos", "g_negt", "ones"):
            C.cm[nm] = es.enter_context(nc.sbuf_tensor("k_" + nm, [128, 128], F32))
            C.cm_t[nm] = T("c_" + nm)
            P.dma("sp", C.cm[nm][:, :], dr["c_" + nm][:, :], writes=[C.cm_t[nm]])
        C.identb = es.enter_context(nc.sbuf_tensor("identb", [128, 128], BF16))
        C.identb_t = T("identb")
        P.op("dve", lambda e: e.tensor_copy(out=C.identb[:, :], in_=C.ident[:, :]), reads=[C.ident_t], writes=[C.identb_t])
        C.eps6 = es.enter_context(nc.sbuf_tensor("eps6", [128, 1], F32))
        C.eps6_t = T("eps6")
        P.op("pool", lambda e: e.memset(C.eps6[:, :], 1e-6), writes=[C.eps6_t])
        C.ones1 = es.enter_context(nc.sbuf_tensor("ones1", [1, 128], F32))
        C.ones1_t = T("ones1")
        P.op("pool", lambda e: e.memset(C.ones1[:, :], 1.0), writes=[C.ones1_t])
        for t in range(NT):
            P.dma("sp", C.X[:, t, :], dr["x"][t * 128:(t + 1) * 128, :], writes=[C.Xt[t]])
        for l in (layers if layers is not None else range(n_layers)):
            if not dbg.get("skip_mixer"):
                MIXERS[l % 4](C, l)
            if not dbg.get("skip_ffn"):
                ffn_phase(C, l)
        for t in range(NT):
            P.dma("sp", y_out[t * 128:(t + 1) * 128, :], C.X[:, t, :], reads=[C.Xt[t]])
        P.finalize()
    C.P = P
    build.last_prog = P
    return nc


def load_bcast_row(C, dst, dst_t, src_row_ap, eng="sp"):
    n = src_row_ap.shape[-1]
    C.P.dma(eng, dst, src_row_ap.to_broadcast([128, n]), writes=[dst_t])


def layer_norm_tile(C, t, zsrc, G, Bv, G_t, B_t, wk):
    P, nc = C.P, C.nc
    z, z_t, st, st_t, mv, mv_t, sc, sc_t = wk
    xt = C.Xt[t]
    for h, (pap, pT) in enumerate(zsrc):
        sl = slice(h * 512, (h + 1) * 512)
        P.op("dve", lambda e, sl=sl, pap=pap: e.scalar_tensor_tensor(
            out=z[:, sl], in0=C.X[:, t, sl], scalar=ALPHA, in1=pap, op0=ALU.mult, op1=ALU.add),
            reads=[xt, pT], writes=[z_t])
    for h in range(2):
        sl = slice(h * 512, (h + 1) * 512)
        P.op("dve", lambda e, sl=sl, h=h: e.bn_stats(out=st[:, h * 6:(h + 1) * 6], in_=z[:, sl]),
             reads=[z_t], writes=[st_t])
    P.op("dve", lambda e: e.bn_aggr(out=mv[:, 0:2], in_=st[:, 0:12]), reads=[st_t], writes=[mv_t])
    P.op("act", lambda e: e.activation(out=sc[:, 0:1], in_=mv[:, 1:2], func=AF.Ln, bias=C.eps5[:, 0:1], scale=1.0),
         reads=[mv_t, C.eps_t], writes=[sc_t])
    P.op("act", lambda e: e.activation(out=sc[:, 1:2], in_=sc[:, 0:1], func=AF.Exp, scale=-0.5),
         reads=[sc_t], writes=[sc_t])
    P.op("dve", lambda e: e.scalar_tensor_tensor(out=sc[:, 2:3], in0=mv[:, 0:1], scalar=-1.0, in1=sc[:, 1:2],
                                                 op0=ALU.mult, op1=ALU.mult), reads=[mv_t, sc_t], writes=[sc_t])
    P.op("act", lambda e: e.activation(out=z[:, :], in_=z[:, :], func=AF.Identity, bias=sc[:, 2:3], scale=sc[:, 1:2]),
         reads=[z_t, sc_t], writes=[z_t])
    P.op("dve", lambda e: e.tensor_tensor(out=z[:, :], in0=z[:, :], in1=G[:, :], op=ALU.mult),
         reads=[z_t, G_t], writes=[z_t])
    P.op("dve", lambda e: e.tensor_tensor(out=C.X[:, t, :], in0=z[:, :], in1=Bv[:, :], op=ALU.add),
         reads=[z_t, B_t], writes=[xt])


_ALLOC_CTR = [0]


def alloc(C, st, name, shape, dt):
    _ALLOC_CTR[0] += 1
    return st.enter_context(C.nc.sbuf_tensor("%s_%d" % (name, _ALLOC_CTR[0]), list(shape), dt))


def barrier(C):
    C.P.barrier()


def build_xT(C, XT, XT_t, tiles, evac_eng="act"):
    P = C.P
    for t in tiles:
        for half in range(2):
            pi = C.ps_rr % 8
            C.ps_rr += 1
            ps, pt = C.ps[pi], C.pst[pi]
            for c4 in range(4):
                c = half * 4 + c4
                P.op("pe", lambda e, ps=ps, c=c, c4=c4, t=t: e.transpose(
                    out=ps[:, c4 * 128:(c4 + 1) * 128], in_=C.X[:, t, c * 128:(c + 1) * 128], identity=C.ident[:, :]),
                    reads=[C.Xt[t], C.ident_t], writes=[pt])
            dst = XT[:, half * 4:(half + 1) * 4, t * 128:(t + 1) * 128]
            src = ps[:, :].rearrange("p (c n) -> p c n", c=4)
            if evac_eng == "act":
                P.op("act", lambda e, dst=dst, src=src: e.activation(out=dst, in_=src, func=AF.Copy),
                     reads=[pt], writes=[XT_t[t]])
            else:
                P.op("dve", lambda e, dst=dst, src=src: e.tensor_copy(out=dst, in_=src), reads=[pt], writes=[XT_t[t]])


def ffn_phase(C, l):
    P, nc, dr = C.P, C.nc, C.dr
    HG = 256
    NG = FFN_H // HG
    NHC = FFN_H // 128
    with contextlib.ExitStack() as st:
        XT = alloc(C, st, "f_XT", [128, 8, S], BF16)
        XT_t = {t: T("f_XT%d" % t) for t in range(NT)}
        Wd = alloc(C, st, "f_Wd", [128, NHC, D], BF16)
        Wd_t = [T("f_Wd%d" % i) for i in range(NHC)]
        Wg = [alloc(C, st, "f_Wg%d" % i, [128, 8, 2 * HG], BF16) for i in range(2)]
        Wg_t = [(T("f_Wgg%d" % i), T("f_Wgu%d" % i)) for i in range(2)]
        hT = alloc(C, st, "f_hT", [128, NHC, 512], BF16)
        hT_t = [T("f_hT%d" % i) for i in range(NHC)]
        sg = [alloc(C, st, "f_sg%d" % i, [128, 512], F32) for i in range(2)]
        sg_t = [T("f_sg%d" % i) for i in range(2)]
        G = alloc(C, st, "f_G", [128, D], F32); G_t = T("f_G")
        Bv = alloc(C, st, "f_B", [128, D], F32); B_t = T("f_B")
        z = alloc(C, st, "f_z", [128, D], F32); z_t = T("f_z")
        stt = alloc(C, st, "f_st", [128, 12], F32); st_t = T("f_st")
        mv = alloc(C, st, "f_mv", [128, 2], F32); mv_t = T("f_mv")
        sc = alloc(C, st, "f_sc", [128, 4], F32); sc_t = T("f_sc")
        wk = (z, z_t, stt, st_t, mv, mv_t, sc, sc_t)
        load_bcast_row(C, G[:, :], G_t, dr["ln_g"][2 * l + 1:2 * l + 2, :])
        load_bcast_row(C, Bv[:, :], B_t, dr["ln_b"][2 * l + 1:2 * l + 2, :])
        wd_src = dr["ffn_w_down"][l].rearrange("(c p) n -> p c n", p=128)
        for c in range(0, NHC, 2):
            P.dma("pool", Wd[:, c:c + 2, :], wd_src[:, c:c + 2, :], writes=Wd_t[c:c + 2])
        wgu = dr["ffn_w_gu"][l].rearrange("(k p) n -> p k n", p=128)
        gi = 0
        for tb in range(4):
            build_xT(C, XT, XT_t, range(tb * 4, tb * 4 + 4))
            xts = [XT_t[t] for t in range(tb * 4, tb * 4 + 4)]
            for g in range(NG):
                b = gi % 2
                gi += 1
                P.dma("pool", Wg[b][:, :, 0:HG], wgu[:, :, g * HG:(g + 1) * HG], writes=[Wg_t[b][0]])
                P.dma("pool", Wg[b][:, :, HG:2 * HG], wgu[:, :, FFN_H + g * HG:FFN_H + (g + 1) * HG], writes=[Wg_t[b][1]])
                for cc in range(HG // 128):
                    hc = g * (HG // 128) + cc
                    pg_i, pu_i = C.ps_rr % 8, (C.ps_rr + 1) % 8
                    C.ps_rr += 2
                    for (pi, off, wt) in ((pg_i, 0, Wg_t[b][0]), (pu_i, HG, Wg_t[b][1])):
                        for k in range(8):
                            P.op("pe", lambda e, pi=pi, off=off, k=k, b=b, cc=cc, tb=tb: e.matmul(
                                C.ps[pi][:, :], lhsT=Wg[b][:, k, off + cc * 128:off + (cc + 1) * 128],
                                rhs=XT[:, k, tb * 512:(tb + 1) * 512], start=(k == 0), stop=(k == 7)),
                                reads=[wt] + xts, writes=[C.pst[pi]])
                    sb = hc % 2
                    P.op("act", lambda e, sb=sb, pg_i=pg_i: e.activation(out=sg[sb][:, :], in_=C.ps[pg_i][:, :], func=AF.Silu),
                         reads=[C.pst[pg_i]], writes=[sg_t[sb]])
                    P.op("dve", lambda e, sb=sb, pu_i=pu_i, hc=hc: e.tensor_tensor(
                        out=hT[:, hc, :], in0=sg[sb][:, :], in1=C.ps[pu_i][:, :], op=ALU.mult),
                        reads=[sg_t[sb], C.pst[pu_i]], writes=[hT_t[hc]])
            for tt in range(4):
                t = tb * 4 + tt
                zs = []
                for cb in range(2):
                    pi = C.ps_rr % 8
                    C.ps_rr += 1
                    for hc in range(NHC):
                        P.op("pe", lambda e, pi=pi, hc=hc, tt=tt, cb=cb: e.matmul(
                            C.ps[pi][:, :], lhsT=hT[:, hc, tt * 128:(tt + 1) * 128],
                            rhs=Wd[:, hc, cb * 512:(cb + 1) * 512], start=(hc == 0), stop=(hc == NHC - 1)),
                            reads=[hT_t[hc], Wd_t[hc]], writes=[C.pst[pi]])
                    zs.append((C.ps[pi][:, :], C.pst[pi]))
                layer_norm_tile(C, t, zs, G, Bv, G_t, B_t, wk)
        barrier(C)


def run_pipelined(gens, width):
    it = iter(gens)
    active = []
    exhausted = False
    while True:
        if len(active) < width and not exhausted:
            try:
                active.append(next(it))
            except StopIteration:
                exhausted = True
        if not active:
            break
        for g in list(active):
            try:
                next(g)
            except StopIteration:
                active.remove(g)


def next_ps(C, pool=None):
    if pool is None:
        i = C.ps_rr % 8
        C.ps_rr += 1
    else:
        k = C.ps_pool_rr.get(pool, 0)
        C.ps_pool_rr[pool] = k + 1
        i = pool[k % len(pool)]
    return C.ps[i], C.pst[i]


def outproj_phase(C, l, O, O_t, w_out_ap):
    P, nc, dr = C.P, C.nc, C.dr
    with contextlib.ExitStack() as st:
        Wo = alloc(C, st, "o_Wo", [128, 8, D], BF16)
        Wo_t = [T("o_Wo%d" % i) for i in range(8)]
        src = w_out_ap.rearrange("(c p) n -> p c n", p=128)
        for c in range(0, 8, 2):
            P.dma("pool", Wo[:, c:c + 2, :], src[:, c:c + 2, :], writes=Wo_t[c:c + 2])
        G = alloc(C, st, "o_G", [128, D], F32); G_t = T("o_G")
        Bv = alloc(C, st, "o_B", [128, D], F32); B_t = T("o_B")
        z = alloc(C, st, "o_z", [128, D], F32); z_t = T("o_z")
        stt = alloc(C, st, "o_st", [128, 12], F32); st_t = T("o_st")
        mv = alloc(C, st, "o_mv", [128, 2], F32); mv_t = T("o_mv")
        sc = alloc(C, st, "o_sc", [128, 4], F32); sc_t = T("o_sc")
        wk = (z, z_t, stt, st_t, mv, mv_t, sc, sc_t)
        load_bcast_row(C, G[:, :], G_t, dr["ln_g"][2 * l:2 * l + 1, :])
        load_bcast_row(C, Bv[:, :], B_t, dr["ln_b"][2 * l:2 * l + 1, :])
        oT = [alloc(C, st, "o_oT%d" % i, [128, 8, 128], BF16) for i in range(2)]
        oT_t = [T("o_oT%d" % i) for i in range(2)]
        for t in range(NT):
            b = t % 2
            ps, pt = next_ps(C)
            psb = ps[:, :].bitcast(BF16)
            for c in range(8):
                P.op("pe", lambda e, psb=psb, c=c, t=t: e.transpose(
                    out=psb[:, c * 128:(c + 1) * 128], in_=O[:, t, c * 128:(c + 1) * 128], identity=C.identb[:, :]),
                    reads=[O_t[t], C.identb_t], writes=[pt])
            P.op("act", lambda e, psb=psb, b=b: e.activation(
                out=oT[b][:, :, :], in_=psb.rearrange("p (c n) -> p c n", c=8), func=AF.Copy),
                reads=[pt], writes=[oT_t[b]])
            zs = []
            for cb in range(2):
                ps2, pt2 = next_ps(C)
                for c in range(8):
                    P.op("pe", lambda e, ps2=ps2, c=c, cb=cb, b=b: e.matmul(
                        ps2[:, :], lhsT=oT[b][:, c, :], rhs=Wo[:, c, cb * 512:(cb + 1) * 512],
                        start=(c == 0), stop=(c == 7)), reads=[oT_t[b], Wo_t[c]], writes=[pt2])
                zs.append((ps2[:, :], pt2))
            layer_norm_tile(C, t, zs, G, Bv, G_t, B_t, wk)
        barrier(C)


def gla_phase(C, l):
    P, nc, dr = C.P, C.nc, C.dr
    H, DK, DV = 4, 128, 256
    with contextlib.ExitStack() as st:
        O = alloc(C, st, "g_O", [128, NT, D], BF16)
        O_t = [T("g_O%d" % t) for t in range(NT)]
        with contextlib.ExitStack() as st2:
            gla_heads(C, l, st2, O, O_t)
            barrier(C)
        outproj_phase(C, l, O, O_t, dr["gla_w_out"])


def gla_heads(C, l, st, O, O_t):
    P, nc, dr = C.P, C.nc, C.dr
    H, DK, DV = 4, 128, 256
    XT = alloc(C, st, "g_XT", [128, 8, S], BF16)
    XT_t = {t: T("g_XT%d" % t) for t in range(NT)}
    build_xT(C, XT, XT_t, range(NT))
    win = dr["gla_w_in"].rearrange("(k p) n -> p k n", p=128)
    Wlow = alloc(C, st, "g_Wlow", [128, 8, 16], BF16); Wlow_t = T("g_Wlow")
    P.dma("pool", Wlow[:, :, :], win[:, :, 3072:3088], writes=[Wlow_t])
    gkT = alloc(C, st, "g_gkT", [16, S], F32); gkT_t = T("g_gkT")
    for tb in range(4):
        ps, pt = next_ps(C)
        for k in range(8):
            P.op("pe", lambda e, ps=ps, k=k, tb=tb: e.matmul(ps[0:16, :], lhsT=Wlow[:, k, :], rhs=XT[:, k, tb * 512:(tb + 1) * 512],
                                                          start=(k == 0), stop=(k == 7)),
                 reads=[Wlow_t] + [XT_t[t] for t in range(tb * 4, tb * 4 + 4)], writes=[pt])
        P.op("act", lambda e, ps=ps, tb=tb: e.activation(out=gkT[:, tb * 512:(tb + 1) * 512], in_=ps[0:16, :], func=AF.Copy),
             reads=[pt], writes=[gkT_t])
    wgk = alloc(C, st, "g_wgk", [16, 512], F32); wgk_t = T("g_wgk")
    P.dma("sp", wgk[:, :], dr["gla_w_gk"][:, :], writes=[wgk_t])
    bgk = alloc(C, st, "g_bgk", [1, 512], F32); bgk_t = T("g_bgk")
    P.dma("sp", bgk[:, :], dr["gla_b_gk"][:, :], writes=[bgk_t])
    ng = alloc(C, st, "g_ng", [128, DV], F32); ng_t = T("g_ng")
    load_bcast_row(C, ng[:, :], ng_t, dr["gla_norm_g"][0:1, :])
    Wh = [alloc(C, st, "g_Wh%d" % i, [128, 8, 768], BF16) for i in range(2)]
    Wh_t = [[T("g_Wh%d_%d" % (i, j)) for j in range(4)] for i in range(2)]
    state = alloc(C, st, "g_state", [128, DV], F32); state_t = T("g_state")
    state_bf = alloc(C, st, "g_statebf", [128, DV], BF16); statebf_t = T("g_statebf")
    NB = 2
    def mk(name, shape, dt):
        return [alloc(C, st, "%s%d" % (name, i), shape, dt) for i in range(NB)], [T("%s%d" % (name, i)) for i in range(NB)]
    ex, ex_t = mk("g_ex", [128, 128], F32)
    lt, lt_t = mk("g_l", [128, 128], F32)
    ecT, ecT_t = mk("g_ecT", [128, 128], F32)
    eiT, eiT_t = mk("g_eiT", [128, 128], F32)
    eR, eR_t = mk("g_eR", [128, 128], F32)
    qd, qd_t = mk("g_qd", [128, 128], BF16)
    ki, ki_t = mk("g_ki", [128, 128], BF16)
    ke, ke_t = mk("g_ke", [128, 128], BF16)
    vb, vb_t = mk("g_vb", [128, DV], BF16)
    sg, sg_t = mk("g_sg", [128, DV], F32)
    sT, sT_t = mk("g_sT", [128, 128], BF16)
    junk, junk_t = mk("g_junk", [128, DV], F32)
    osq, osq_t = mk("g_osq", [128, DV], F32)
    ss, ss_t = mk("g_ss", [128, 4], F32)
    cols = [(0, 128), (512, 128), (1024, 256), (2048, 256)]
    for h in range(H):
        wb = h % 2
        off = 0
        for j, (base, wdt) in enumerate(cols):
            P.dma("pool", Wh[wb][:, :, off:off + wdt], win[:, :, base + h * wdt:base + (h + 1) * wdt], writes=[Wh_t[wb][j]])
            off += wdt
        P.op("pool", lambda e: e.memset(state[:, :], 0.0), writes=[state_t])
        P.op("pool", lambda e: e.memset(state_bf[:, :], 0.0), writes=[statebf_t])
        def gla_tile(t, h=h, wb=wb):
            b = t % NB
            pool = (0, 1, 2, 3) if b == 0 else (4, 5, 6, 7)
            tok = slice(t * 128, (t + 1) * 128)
            p1, p1t = next_ps(C, pool)
            for j in range(2):
                for k in range(8):
                    P.op("pe", lambda e, p1=p1, j=j, k=k, wb=wb, tok=tok: e.matmul(
                        p1[:, j * 128:(j + 1) * 128], lhsT=Wh[wb][:, k, j * 128:(j + 1) * 128], rhs=XT[:, k, tok],
                        start=(k == 0), stop=(k == 7)), reads=[Wh_t[wb][j], XT_t[t]], writes=[p1t])
            p2, p2t = next_ps(C, pool)
            for k in range(8):
                P.op("pe", lambda e, p2=p2, k=k, wb=wb, tok=tok: e.matmul(
                    p2[:, 0:384], lhsT=XT[:, k, tok], rhs=Wh[wb][:, k, 128:512], start=(k == 0), stop=(k == 7)),
                    reads=[Wh_t[wb][1], Wh_t[wb][2], XT_t[t]], writes=[p2t])
            p3, p3t = next_ps(C, pool)
            for k in range(8):
                P.op("pe", lambda e, p3=p3, k=k, wb=wb, tok=tok: e.matmul(
                    p3[:, 0:256], lhsT=XT[:, k, tok], rhs=Wh[wb][:, k, 512:768], start=(k == 0), stop=(k == 7)),
                    reads=[Wh_t[wb][3], XT_t[t]], writes=[p3t])
            P.op("pe", lambda e, p3=p3, tok=tok, h=h: e.matmul(
                p3[:, 256:384], lhsT=gkT[:, tok], rhs=wgk[:, h * 128:(h + 1) * 128], start=True, stop=False),
                reads=[gkT_t, wgk_t], writes=[p3t])
            P.op("pe", lambda e, p3=p3, h=h: e.matmul(
                p3[:, 256:384], lhsT=C.ones1[:, :], rhs=bgk[:, h * 128:(h + 1) * 128], start=False, stop=True),
                reads=[C.ones1_t, bgk_t], writes=[p3t])
            yield
            P.op("act", lambda e, p3=p3, b=b: e.activation(out=ex[b][:, :], in_=p3[:, 256:384], func=AF.Exp, scale=-1.0),
                 reads=[p3t], writes=[ex_t[b]])
            P.op("act", lambda e, b=b: e.activation(out=lt[b][:, :], in_=ex[b][:, :], func=AF.Ln, bias=1.0, scale=1.0),
                 reads=[ex_t[b]], writes=[lt_t[b]])
            yield
            p4, p4t = next_ps(C, pool)
            P.op("pe", lambda e, p4=p4, b=b: e.matmul(p4[:, 0:128], lhsT=lt[b][:, :], rhs=C.cm["m_incl_gla"][:, :], start=True, stop=True),
                 reads=[lt_t[b], C.cm_t["m_incl_gla"]], writes=[p4t])
            P.op("pe", lambda e, p4=p4, b=b: e.matmul(p4[:, 128:256], lhsT=C.cm["m_rev_gla"][:, :], rhs=lt[b][:, :], start=True, stop=True),
                 reads=[lt_t[b], C.cm_t["m_rev_gla"]], writes=[p4t])
            P.op("act", lambda e, p4=p4, b=b: e.activation(out=ecT[b][:, :], in_=p4[:, 0:128], func=AF.Exp),
                 reads=[p4t], writes=[ecT_t[b]])
            P.op("act", lambda e, p4=p4, b=b: e.activation(out=eiT[b][:, :], in_=p4[:, 0:128], func=AF.Exp, scale=-1.0),
                 reads=[p4t], writes=[eiT_t[b]])
            P.op("act", lambda e, p4=p4, b=b: e.activation(out=eR[b][:, :], in_=p4[:, 128:256], func=AF.Exp),
                 reads=[p4t], writes=[eR_t[b]])
            P.op("dve", lambda e, p1=p1, b=b: e.scalar_tensor_tensor(
                out=qd[b][:, :], in0=p1[:, 0:128], scalar=float(DK ** -0.5), in1=ecT[b][:, :], op0=ALU.mult, op1=ALU.mult),
                reads=[p1t, ecT_t[b]], writes=[qd_t[b]])
            P.op("dve", lambda e, p1=p1, b=b: e.tensor_tensor(out=ki[b][:, :], in0=p1[:, 128:256], in1=eiT[b][:, :], op=ALU.mult),
                 reads=[p1t, eiT_t[b]], writes=[ki_t[b]])
            P.op("dve", lambda e, p2=p2, b=b: e.tensor_tensor(out=ke[b][:, :], in0=p2[:, 0:128], in1=eR[b][:, :], op=ALU.mult),
                 reads=[p2t, eR_t[b]], writes=[ke_t[b]])
            P.op("act", lambda e, p2=p2, b=b: e.activation(out=vb[b][:, :], in_=p2[:, 128:384], func=AF.Copy),
                 reads=[p2t], writes=[vb_t[b]])
            P.op("act", lambda e, p3=p3, b=b: e.activation(out=sg[b][:, :], in_=p3[:, 0:256], func=AF.Exp, scale=-1.0),
                 reads=[p3t], writes=[sg_t[b]])
            P.op("act", lambda e, b=b: e.activation(out=sg[b][:, :], in_=sg[b][:, :], func=AF.Ln, bias=1.0, scale=1.0),
                 reads=[sg_t[b]], writes=[sg_t[b]])
            P.op("act", lambda e, b=b: e.activation(out=sg[b][:, :], in_=sg[b][:, :], func=AF.Exp, scale=-1.0),
                 reads=[sg_t[b]], writes=[sg_t[b]])
            P.op("dve", lambda e, p3=p3, b=b: e.tensor_tensor(out=sg[b][:, :], in0=p3[:, 0:256], in1=sg[b][:, :], op=ALU.mult),
                 reads=[p3t, sg_t[b]], writes=[sg_t[b]])
            P.op("dve", lambda e, b=b: e.tensor_tensor(out=sg[b][:, :], in0=sg[b][:, :], in1=ng[:, :], op=ALU.mult),
                 reads=[sg_t[b], ng_t], writes=[sg_t[b]])
            yield
            p5, p5t = next_ps(C, pool)
            P.op("pe", lambda e, p5=p5, b=b: e.matmul(p5[:, 0:128], lhsT=ki[b][:, :], rhs=qd[b][:, :], start=True, stop=True),
                 reads=[ki_t[b], qd_t[b]], writes=[p5t])
            P.op("dve", lambda e, p5=p5, b=b: e.tensor_tensor(out=sT[b][:, :], in0=p5[:, 0:128], in1=C.cm["m_caus"][:, :], op=ALU.mult),
                 reads=[p5t, C.cm_t["m_caus"]], writes=[sT_t[b]])
            yield
            p6, p6t = next_ps(C, pool)
            P.op("pe", lambda e, p6=p6, b=b: e.matmul(p6[:, 0:DV], lhsT=sT[b][:, :], rhs=vb[b][:, :], start=True, stop=False),
                 reads=[sT_t[b], vb_t[b]], writes=[p6t])
            P.op("pe", lambda e, p6=p6, b=b: e.matmul(p6[:, 0:DV], lhsT=qd[b][:, :], rhs=state_bf[:, :], start=False, stop=True),
                 reads=[qd_t[b], statebf_t], writes=[p6t])
            yield
            p7, p7t = next_ps(C, pool)
            P.op("pe", lambda e, p7=p7, b=b: e.matmul(p7[:, 0:DV], lhsT=ke[b][:, :], rhs=vb[b][:, :], start=True, stop=True),
                 reads=[ke_t[b], vb_t[b]], writes=[p7t])
            P.op("dve", lambda e, p7=p7, b=b: e.scalar_tensor_tensor(
                out=state[:, :], in0=state[:, :], scalar=ecT[b][:, 127:128], in1=p7[:, 0:DV], op0=ALU.mult, op1=ALU.add),
                reads=[state_t, ecT_t[b], p7t], writes=[state_t])
            P.op("act", lambda e: e.activation(out=state_bf[:, :], in_=state[:, :], func=AF.Copy),
                 reads=[state_t], writes=[statebf_t])
            yield
            P.op("act", lambda e, p6=p6, b=b: e.activation(out=junk[b][:, :], in_=p6[:, 0:DV], func=AF.Copy),
                 reads=[p6t], writes=[junk_t[b]])
            P.op("dve", lambda e, b=b: e.scalar_tensor_tensor(out=osq[b][:, :], in0=junk[b][:, :], scalar=1.0, in1=junk[b][:, :],
                                                              op0=ALU.mult, op1=ALU.mult, accum_out=ss[b][:, 0:1]),
                 reads=[junk_t[b]], writes=[osq_t[b], ss_t[b]])
            P.op("act", lambda e, b=b: e.activation(out=ss[b][:, 1:2], in_=ss[b][:, 0:1], func=AF.Ln, bias=C.eps6[:, 0:1], scale=1.0 / DV),
                 reads=[ss_t[b], C.eps6_t], writes=[ss_t[b]])
            P.op("act", lambda e, b=b: e.activation(out=ss[b][:, 2:3], in_=ss[b][:, 1:2], func=AF.Exp, scale=-0.5),
                 reads=[ss_t[b]], writes=[ss_t[b]])
            P.op("dve", lambda e, p6=p6, b=b, t=t, h=h: e.scalar_tensor_tensor(
                out=O[:, t, h * DV:(h + 1) * DV], in0=junk[b][:, :], scalar=ss[b][:, 2:3], in1=sg[b][:, :], op0=ALU.mult, op1=ALU.mult),
                reads=[junk_t[b], ss_t[b], sg_t[b]], writes=[O_t[t]])
        run_pipelined((gla_tile(t) for t in range(NT)), NB)


def moba_phase(C, l):
    P, nc, dr = C.P, C.nc, C.dr
    with contextlib.ExitStack() as st:
        O = alloc(C, st, "m_O", [128, NT, D], BF16)
        O_t = [T("m_O%d" % t) for t in range(NT)]
        with contextlib.ExitStack() as st2:
            moba_heads(C, l, st2, O, O_t)
            barrier(C)
        outproj_phase(C, l, O, O_t, dr["moba_w_out"])


def moba_heads(C, l, st, O, O_t):
    P, nc, dr = C.P, C.nc, C.dr
    H, DH = 8, 128
    PI = float(np.pi)
    XT = alloc(C, st, "m_XT", [128, 8, S], BF16)
    XT_t = {t: T("m_XT%d" % t) for t in range(NT)}
    build_xT(C, XT, XT_t, range(NT))
    win = dr["moba_w_in"].rearrange("(k p) n -> p k n", p=128)
    CT = alloc(C, st, "m_CT", [128, S], F32); CT_t = T("m_CT")
    ST = alloc(C, st, "m_ST", [128, S], F32); ST_t = T("m_ST")
    P.op("pool", lambda e: e.memset(CT[:, :], 1.0), writes=[CT_t])
    P.op("pool", lambda e: e.memset(ST[:, :], 0.0), writes=[ST_t])
    invf = alloc(C, st, "m_invf", [32, 1], F32); invf_t = T("m_invf")
    sgn = alloc(C, st, "m_sgn", [32, 1], F32); sgn_t = T("m_sgn")
    P.dma("sp", invf[:, :], dr["c_invf"][:, :], writes=[invf_t])
    P.dma("sp", sgn[:, :], dr["c_sgn"][:, :], writes=[sgn_t])
    posi = alloc(C, st, "m_posi", [32, 256], I32); posi_t = T("m_posi")
    ang = alloc(C, st, "m_ang", [32, 256], F32); ang_t = T("m_ang")
    r_ = alloc(C, st, "m_r", [32, 256], F32); r_t = T("m_r")
    ki_ = alloc(C, st, "m_ki", [32, 256], I32); ki_t = T("m_ki")
    th = alloc(C, st, "m_th", [32, 256], F32); th_t = T("m_th")
    mk_ = alloc(C, st, "m_mk", [32, 256], F32); mk_t = T("m_mk")
    pos_row = dr["positions"].rearrange("s o -> o s")
    for tb in range(8):
        cs = slice(tb * 256, (tb + 1) * 256)
        P.dma("sp", posi[:, :], pos_row[:, cs].to_broadcast([32, 256]), writes=[posi_t])
        P.op("dve", lambda e: e.tensor_copy(out=ang[:, :], in_=posi[:, :]), reads=[posi_t], writes=[ang_t])
        P.op("dve", lambda e: e.tensor_scalar(out=ang[:, :], in0=ang[:, :], scalar1=invf[:, 0:1], scalar2=None, op0=ALU.mult),
             reads=[ang_t, invf_t], writes=[ang_t])
        for (shift, dst, dst_t) in ((PI / 2, CT, CT_t), (0.0, ST, ST_t)):
            P.op("dve", lambda e, shift=shift: e.tensor_scalar(out=r_[:, :], in0=ang[:, :], scalar1=shift, scalar2=1.0 / (2 * PI),
                                                            op0=ALU.add, op1=ALU.mult), reads=[ang_t], writes=[r_t])
            P.op("dve", lambda e: e.tensor_copy(out=ki_[:, :], in_=r_[:, :]), reads=[r_t], writes=[ki_t])
            P.op("dve", lambda e: e.tensor_copy(out=r_[:, :], in_=ki_[:, :]), reads=[ki_t], writes=[r_t])
            P.op("dve", lambda e: e.scalar_tensor_tensor(out=th[:, :], in0=r_[:, :], scalar=-2 * PI, in1=ang[:, :],
                                                         op0=ALU.mult, op1=ALU.add), reads=[r_t, ang_t], writes=[th_t])
            if shift != 0.0:
                P.op("dve", lambda e, shift=shift: e.tensor_scalar(out=th[:, :], in0=th[:, :], scalar1=shift, scalar2=None, op0=ALU.add),
                     reads=[th_t], writes=[th_t])
            P.op("dve", lambda e: e.tensor_scalar(out=mk_[:, :], in0=th[:, :], scalar1=PI, scalar2=-2 * PI, op0=ALU.is_gt, op1=ALU.mult),
                 reads=[th_t], writes=[mk_t])
            P.op("dve", lambda e: e.tensor_tensor(out=th[:, :], in0=th[:, :], in1=mk_[:, :], op=ALU.add), reads=[th_t, mk_t], writes=[th_t])
            P.op("dve", lambda e: e.tensor_scalar(out=mk_[:, :], in0=th[:, :], scalar1=-PI, scalar2=2 * PI, op0=ALU.is_lt, op1=ALU.mult),
                 reads=[th_t], writes=[mk_t])
            P.op("dve", lambda e: e.tensor_tensor(out=th[:, :], in0=th[:, :], in1=mk_[:, :], op=ALU.add), reads=[th_t, mk_t], writes=[th_t])
            P.op("dve", lambda e: e.tensor_scalar(out=th[:, :], in0=th[:, :], scalar1=-PI, scalar2=PI, op0=ALU.max, op1=ALU.min),
                 reads=[th_t], writes=[th_t])
            P.op("act", lambda e, dst=dst, cs=cs: e.activation(out=dst[0:32, cs], in_=th[:, :], func=AF.Sin), reads=[th_t], writes=[dst_t])
    P.op("dve", lambda e: e.tensor_scalar(out=ST[0:32, :], in0=ST[0:32, :], scalar1=sgn[:, 0:1], scalar2=None, op0=ALU.mult),
         reads=[ST_t, sgn_t], writes=[ST_t])
    if C.dbg.get("moba_stop") == 1:
        return
    Ef = alloc(C, st, "m_Ef", [8, 1024], F32); Ef_t = T("m_Ef")
    P.dma("sp", Ef[:, :], dr["c_E"][:, :], writes=[Ef_t])
    Eb = alloc(C, st, "m_Eb", [8, 1024], BF16); Eb_t = T("m_Eb")
    P.op("dve", lambda e: e.tensor_copy(out=Eb[:, :], in_=Ef[:, :]), reads=[Ef_t], writes=[Eb_t])
    cnf = alloc(C, st, "m_cnf", [128, 128], F32); cnf_t = T("m_cnf")
    P.dma("sp", cnf[:, :], dr["c_causneg"][:, :], writes=[cnf_t])
    cnb = alloc(C, st, "m_cnb", [128, 128], BF16); cnb_t = T("m_cnb")
    P.op("dve", lambda e: e.tensor_copy(out=cnb[:, :], in_=cnf[:, :]), reads=[cnf_t], writes=[cnb_t])
    npast = alloc(C, st, "m_npast", [128, 64], F32); npast_t = T("m_npast")
    P.dma("sp", npast[:, :], dr["c_negpast"][:, :].to_broadcast([128, 64]), writes=[npast_t])
    if C.dbg.get("moba_stop") == 11:
        return
    Wh0 = alloc(C, st, "m_Wh", [128, 8, 640], BF16)
    Wh = [Wh0, Wh0]
    Wh_t0 = [T("m_Wh_%d" % j) for j in range(5)]
    Wh_t = [Wh_t0, Wh_t0]
    P.op("pool", lambda e: e.memset(Wh0[:, :, 384:640], 0.0), writes=[Wh_t0[3], Wh_t0[4]])
    qT0 = alloc(C, st, "m_qT", [128, S], BF16)
    kT0 = alloc(C, st, "m_kT", [128, S], BF16)
    qT = [qT0, qT0]
    kT = [kT0, kT0]
    qT_t0 = [T("m_qT_%d" % tb) for tb in range(4)]
    kT_t0 = [T("m_kT_%d" % tb) for tb in range(4)]
    qT_t = [qT_t0, qT_t0]
    kT_t = [kT_t0, kT_t0]
    vaug = [alloc(C, st, "m_va%d" % i, [128, NT, 130], BF16) for i in range(2)]
    va_t = [[T("m_va%d_%d" % (i, g)) for g in range(4)] for i in range(2)]
    for i in range(2):
        P.op("pool", lambda e, i=i: e.memset(vaug[i][:, :, 128:130], 1.0), writes=va_t[i])
    t1 = [alloc(C, st, "m_t1%d" % i, [128, 512], F32) for i in range(2)]
    t1_t = [T("m_t1%d" % i) for i in range(2)]
    t2 = [alloc(C, st, "m_t2%d" % i, [128, 512], F32) for i in range(2)]
    t2_t = [T("m_t2%d" % i) for i in range(2)]
    km = alloc(C, st, "m_km", [128, 8], F32); km_t = T("m_km")
    kmb = alloc(C, st, "m_kmb", [128, 8], BF16); kmb_t = T("m_kmb")
    NB = 2
    g_ = [alloc(C, st, "m_g%d" % i, [128, 8], F32) for i in range(NB)]; g_t = [T("m_g%d" % i) for i in range(NB)]
    mx = [alloc(C, st, "m_mx%d" % i, [128, 8], F32) for i in range(NB)]; mx_t = [T("m_mx%d" % i) for i in range(NB)]
    sm = [alloc(C, st, "m_sm%d" % i, [128, 8], F32) for i in range(NB)]; sm_t = [T("m_sm%d" % i) for i in range(NB)]
    selT = [alloc(C, st, "m_selT%d" % i, [8, 128], BF16) for i in range(NB)]; selT_t = [T("m_selT%d" % i) for i in range(NB)]
    rec = [alloc(C, st, "m_rec%d" % i, [128, 1], F32) for i in range(NB)]; rec_t = [T("m_rec%d" % i) for i in range(NB)]
    NPB = 6
    PT = [alloc(C, st, "m_PT%d" % i, [128, 128], BF16) for i in range(NPB)]; PT_t = [T("m_PT%d" % i) for i in range(NPB)]
    pt_rr = 0
    scale = float(DH ** -0.5)
    for h in range(H):
        wb = h % 2
        hb = h * 128
        segs = [(0, 128, hb), (128, 128, 1024 + hb), (256, 128, 2048 + hb)]
        for j, (o, w, src) in enumerate(segs):
            P.dma("pool", Wh[wb][:, :, o:o + w], win[:, :, src:src + w], writes=[Wh_t[wb][j]])
        P.dma("pool", Wh[wb][:, :, 384:400], win[:, :, hb + 16:hb + 32], writes=[Wh_t[wb][3]])
        P.dma("pool", Wh[wb][:, :, 400:416], win[:, :, hb:hb + 16], writes=[Wh_t[wb][3]])
        P.dma("pool", Wh[wb][:, :, 512:528], win[:, :, 1024 + hb + 16:1024 + hb + 32], writes=[Wh_t[wb][4]])
        P.dma("pool", Wh[wb][:, :, 528:544], win[:, :, 1024 + hb:1024 + hb + 16], writes=[Wh_t[wb][4]])
        if C.dbg.get("moba_stop") == 12:
            return
        for (dstT, dst_t, co, sw, wj, swj) in ((qT[wb], qT_t[wb], 0, 384, 0, 3), (kT[wb], kT_t[wb], 128, 512, 1, 4)):
            for tb in range(4):
                cs = slice(tb * 512, (tb + 1) * 512)
                xts = [XT_t[t] for t in range(tb * 4, tb * 4 + 4)]
                pq, pqt = next_ps(C, (2, 3, 4, 5, 6, 7))
                for k in range(8):
                    P.op("pe", lambda e, pq=pq, k=k, wb=wb, co=co, cs=cs: e.matmul(
                        pq[:, :], lhsT=Wh[wb][:, k, co:co + 128], rhs=XT[:, k, cs], start=(k == 0), stop=(k == 7)),
                        reads=[Wh_t[wb][wj]] + xts, writes=[pqt])
                pw, pwt = next_ps(C, (2, 3, 4, 5, 6, 7))
                for k in range(8):
                    P.op("pe", lambda e, pw=pw, k=k, wb=wb, sw=sw, cs=cs: e.matmul(
                        pw[:, :], lhsT=Wh[wb][:, k, sw:sw + 128], rhs=XT[:, k, cs], start=(k == 0), stop=(k == 7)),
                        reads=[Wh_t[wb][swj]] + xts, writes=[pwt])
                tbuf = tb % 2
                P.op("dve", lambda e, pq=pq, cs=cs, tbuf=tbuf: e.tensor_tensor(out=t1[tbuf][:, :], in0=pq[:, :], in1=CT[:, cs], op=ALU.mult),
                     reads=[pqt, CT_t], writes=[t1_t[tbuf]])
                P.op("dve", lambda e, pw=pw, cs=cs, tbuf=tbuf: e.tensor_tensor(out=t2[tbuf][:, :], in0=pw[:, :], in1=ST[:, cs], op=ALU.mult),
                     reads=[pwt, ST_t], writes=[t2_t[tbuf]])
                P.op("dve", lambda e, dstT=dstT, cs=cs, tbuf=tbuf: e.tensor_tensor(out=dstT[:, cs], in0=t1[tbuf][:, :], in1=t2[tbuf][:, :], op=ALU.add),
                     reads=[t1_t[tbuf], t2_t[tbuf]], writes=[dst_t[tb]])
        if C.dbg.get("moba_stop") == 2:
            return
        P.op("dve", lambda e, wb=wb: e.tensor_reduce(out=km[:, :], in_=kT[wb][:, :].rearrange("p (n b) -> p n b", b=256), axis=AX.X, op=ALU.add),
             reads=kT_t[wb], writes=[km_t])
        P.op("dve", lambda e: e.tensor_scalar(out=kmb[:, :], in0=km[:, :], scalar1=1.0 / 256.0, scalar2=None, op0=ALU.mult),
             reads=[km_t], writes=[kmb_t])
        for g4 in range(4):
            pv, pvt = next_ps(C, (2, 3, 4, 5, 6, 7))
            for j in range(4):
                t = g4 * 4 + j
                for k in range(8):
                    P.op("pe", lambda e, pv=pv, j=j, k=k, t=t, wb=wb: e.matmul(
                        pv[:, j * 128:(j + 1) * 128], lhsT=XT[:, k, t * 128:(t + 1) * 128], rhs=Wh[wb][:, k, 256:384],
                        start=(k == 0), stop=(k == 7)), reads=[Wh_t[wb][2], XT_t[t]], writes=[pvt])
            P.op("act", lambda e, pv=pv, g4=g4, wb=wb: e.activation(
                out=vaug[wb][:, g4 * 4:(g4 + 1) * 4, 0:128], in_=pv[:, :].rearrange("p (j n) -> p j n", j=4), func=AF.Copy),
                reads=[pvt], writes=[va_t[wb][g4]])
        if C.dbg.get("moba_stop") == 3:
            return
        for qt in range(C.dbg.get("moba_nqt", NT)):
            b = qt % NB
            blk = qt // 2
            qs = slice(qt * 128, (qt + 1) * 128)
            use_gate = blk >= 4
            if use_gate:
                pg, pgt = next_ps(C, (2, 3, 4, 5, 6, 7))
                P.op("pe", lambda e, pg=pg, wb=wb, qs=qs: e.matmul(pg[:, 0:8], lhsT=qT[wb][:, qs], rhs=kmb[:, :], start=True, stop=True),
                     reads=[qT_t[wb][qt // 4], kmb_t], writes=[pgt])
                P.op("dve", lambda e, pg=pg, b=b, blk=blk: e.tensor_tensor(out=g_[b][:, :], in0=pg[:, 0:8], in1=npast[:, blk * 8:(blk + 1) * 8], op=ALU.add),
                     reads=[pgt, npast_t], writes=[g_t[b]])
                P.op("dve", lambda e, b=b: e.max(out=mx[b][:, :], in_=g_[b][:, :]), reads=[g_t[b]], writes=[mx_t[b]])
                P.op("dve", lambda e, b=b: e.tensor_scalar(out=sm[b][:, :], in0=g_[b][:, :], scalar1=mx[b][:, 2:3], scalar2=-1.0,
                                                         op0=ALU.is_ge, op1=ALU.add), reads=[g_t[b], mx_t[b]], writes=[sm_t[b]])
                pt2, pt2t = next_ps(C, (2, 3, 4, 5, 6, 7))
                P.op("pe", lambda e, pt2=pt2, b=b: e.transpose(out=pt2[0:8, 0:128], in_=sm[b][:, 0:8], identity=C.ident[:, :]),
                     reads=[sm_t[b], C.ident_t], writes=[pt2t])
                P.op("act", lambda e, pt2=pt2, b=b: e.activation(out=selT[b][:, :], in_=pt2[0:8, 0:128], func=AF.Copy),
                     reads=[pt2t], writes=[selT_t[b]])
            chunks = list(range(0, 2 * blk)) + ([qt - 1, qt] if qt % 2 == 1 else [qt])
            po, pot = next_ps(C, (0, 1))
            LA = 4
            pbs = {}

            def emit_s(ci, kc, qt=qt, qs=qs, wb=wb, b=b, blk=blk, use_gate=use_gate):
                nonlocal pt_rr
                past = kc < 2 * blk
                diag = kc == qt
                ks = slice(kc * 128, (kc + 1) * 128)
                ps_, pst_ = next_ps(C, (2, 3, 4, 5, 6, 7))
                extra = (past and use_gate) or diag
                P.op("pe", lambda e, ps_=ps_, ks=ks, extra=extra, wb=wb, qs=qs: e.matmul(
                    ps_[:, 0:128], lhsT=kT[wb][:, ks], rhs=qT[wb][:, qs], start=True, stop=not extra),
                    reads=[kT_t[wb][kc // 4], qT_t[wb][qt // 4]], writes=[pst_])
                if past and use_gate:
                    n = kc // 2
                    P.op("pe", lambda e, ps_=ps_, n=n, b=b: e.matmul(
                        ps_[:, 0:128], lhsT=Eb[:, n * 128:(n + 1) * 128], rhs=selT[b][:, :], start=False, stop=True),
                        reads=[Eb_t, selT_t[b]], writes=[pst_])
                if diag:
                    P.op("pe", lambda e, ps_=ps_: e.matmul(ps_[:, 0:128], lhsT=C.identb[:, :], rhs=cnb[:, :], start=False, stop=True),
                         reads=[C.identb_t, cnb_t], writes=[pst_])
                pb = pt_rr % NPB
                pt_rr += 1
                pbs[ci] = pb
                P.op("act", lambda e, ps_=ps_, pb=pb: e.activation(out=PT[pb][:, :], in_=ps_[:, 0:128], func=AF.Exp, scale=scale),
                     reads=[pst_], writes=[PT_t[pb]])

            def emit_pv(ci, kc, po=po, pot=pot, wb=wb, pbs=pbs, chunks=chunks):
                pb = pbs[ci]
                P.op("pe", lambda e, pb=pb, kc=kc, ci=ci, nch=len(chunks), po=po, wb=wb: e.matmul(
                    po[:, 0:129], lhsT=PT[pb][:, :], rhs=vaug[wb][:, kc, 0:129], start=(ci == 0), stop=(ci == nch - 1)),
                    reads=[PT_t[pb], va_t[wb][kc // 4]], writes=[pot])

            nch_ = len(chunks)
            for ci in range(nch_ + LA):
                if ci < nch_:
                    emit_s(ci, chunks[ci])
                if ci - LA >= 0:
                    emit_pv(ci - LA, chunks[ci - LA])
            P.op("dve", lambda e, po=po, b=b: e.reciprocal(out=rec[b][:, :], in_=po[:, 128:129]), reads=[pot], writes=[rec_t[b]])
            P.op("dve", lambda e, po=po, b=b, qt=qt, hb=hb: e.tensor_scalar(
                out=O[:, qt, hb:hb + 128], in0=po[:, 0:128], scalar1=rec[b][:, 0:1], scalar2=None, op0=ALU.mult),
                reads=[pot, rec_t[b]], writes=[O_t[qt]])


def gdn_phase(C, l):
    P, nc, dr = C.P, C.nc, C.dr
    with contextlib.ExitStack() as st:
        O = alloc(C, st, "d_O", [128, NT, D], BF16)
        O_t = [T("d_O%d" % t) for t in range(NT)]
        with contextlib.ExitStack() as st2:
            gdn_heads(C, l, st2, O, O_t)
            barrier(C)
        outproj_phase(C, l, O, O_t, dr["gdn_w_out"])


def gdn_heads(C, l, st, O, O_t):
    P, nc, dr = C.P, C.nc, C.dr
    H, DK = 8, 128
    XT = alloc(C, st, "d_XT", [128, 8, S], BF16)
    XT_t = {t: T("d_XT%d" % t) for t in range(NT)}
    build_xT(C, XT, XT_t, range(NT))
    allx = [XT_t[t] for t in range(NT)]
    win = dr["gdn_w_in"].rearrange("(k p) n -> p k n", p=128)
    PS6 = (2, 3, 4, 5, 6, 7)
    Wab = alloc(C, st, "d_Wab", [128, 8, 16], BF16); Wab_t = T("d_Wab")
    P.dma("pool", Wab[:, :, :], win[:, :, 4096:4112], writes=[Wab_t])
    alog = alloc(C, st, "d_alog", [128, 8], F32); alog_t = T("d_alog")
    dtb = alloc(C, st, "d_dtb", [128, 8], F32); dtb_t = T("d_dtb")
    P.dma("sp", alog[:, :], dr["gdn_a_log"][0:1, :].to_broadcast([128, 8]), writes=[alog_t])
    P.dma("sp", dtb[:, :], dr["gdn_dt_bias"][0:1, :].to_broadcast([128, 8]), writes=[dtb_t])
    P.op("act", lambda e: e.activation(out=alog[:, :], in_=alog[:, :], func=AF.Exp), reads=[alog_t], writes=[alog_t])
    ng = alloc(C, st, "d_ng", [128, 128], F32); ng_t = T("d_ng")
    load_bcast_row(C, ng[:, :], ng_t, dr["gdn_norm_g"][0:1, :])
    def sc(name):
        return alloc(C, st, name, [128, NT, 8], F32), T(name)
    g_all, g_t = sc("d_g"); beta, beta_t = sc("d_beta"); cum, cum_t = sc("d_cum"); tot, tot_t = sc("d_tot")
    ecum, ecum_t = sc("d_ecum"); ncum, ncum_t = sc("d_ncum"); bec, bec_t = sc("d_bec"); eend, eend_t = sc("d_eend")
    ecl, ecl_t = sc("d_ecl"); tmp, tmp_t = sc("d_tmp")
    pab, pabt = next_ps(C)
    for t in range(NT):
        for k in range(8):
            P.op("pe", lambda e, t=t, k=k: e.matmul(pab[:, t * 16:(t + 1) * 16], lhsT=XT[:, k, t * 128:(t + 1) * 128], rhs=Wab[:, k, :],
                                                    start=(k == 0), stop=(k == 7)), reads=[XT_t[t], Wab_t], writes=[pabt])
    pab3 = pab[:, 0:256].rearrange("p (t c) -> p t c", c=16)
    for t in range(NT):
        P.op("dve", lambda e, t=t: e.tensor_tensor(out=tmp[:, t, :], in0=pab3[:, t, 0:8], in1=dtb[:, :], op=ALU.add),
             reads=[pabt, dtb_t], writes=[tmp_t])
    P.op("act", lambda e: e.activation(out=tmp[:, :, :], in_=tmp[:, :, :], func=AF.Exp), reads=[tmp_t], writes=[tmp_t])
    P.op("act", lambda e: e.activation(out=tmp[:, :, :], in_=tmp[:, :, :], func=AF.Ln, bias=1.0, scale=1.0), reads=[tmp_t], writes=[tmp_t])
    for t in range(NT):
        P.op("dve", lambda e, t=t: e.scalar_tensor_tensor(out=g_all[:, t, :], in0=tmp[:, t, :], scalar=-1.0, in1=alog[:, :],
                                                          op0=ALU.mult, op1=ALU.mult), reads=[tmp_t, alog_t], writes=[g_t])
    P.op("act", lambda e: e.activation(out=beta[:, :, :], in_=pab3[:, :, 8:16], func=AF.Exp, scale=-1.0), reads=[pabt], writes=[beta_t])
    P.op("act", lambda e: e.activation(out=beta[:, :, :], in_=beta[:, :, :], func=AF.Ln, bias=1.0, scale=1.0), reads=[beta_t], writes=[beta_t])
    P.op("act", lambda e: e.activation(out=beta[:, :, :], in_=beta[:, :, :], func=AF.Exp, scale=-1.0), reads=[beta_t], writes=[beta_t])
    pcm, pcmt = next_ps(C)
    for t in range(NT):
        P.op("pe", lambda e, t=t: e.matmul(pcm[:, t * 8:(t + 1) * 8], lhsT=C.cm["m_incl"][:, :], rhs=g_all[:, t, :], start=True, stop=True),
             reads=[g_t, C.cm_t["m_incl"]], writes=[pcmt])
        P.op("pe", lambda e, t=t: e.matmul(pcm[:, 128 + t * 8:128 + (t + 1) * 8], lhsT=C.cm["ones"][:, :], rhs=g_all[:, t, :], start=True, stop=True),
             reads=[g_t, C.cm_t["ones"]], writes=[pcmt])
    P.op("act", lambda e: e.activation(out=cum[:, :, :], in_=pcm[:, 0:128].rearrange("p (t c) -> p t c", c=8), func=AF.Copy),
         reads=[pcmt], writes=[cum_t])
    P.op("act", lambda e: e.activation(out=tot[:, :, :], in_=pcm[:, 128:256].rearrange("p (t c) -> p t c", c=8), func=AF.Copy),
         reads=[pcmt], writes=[tot_t])
    P.op("act", lambda e: e.activation(out=ecum[:, :, :], in_=cum[:, :, :], func=AF.Exp), reads=[cum_t], writes=[ecum_t])
    P.op("act", lambda e: e.activation(out=ecl[:, :, :], in_=tot[:, :, :], func=AF.Exp), reads=[tot_t], writes=[ecl_t])
    P.op("dve", lambda e: e.tensor_scalar(out=ncum[:, :, :], in0=cum[:, :, :], scalar1=-1.0, scalar2=None, op0=ALU.mult),
         reads=[cum_t], writes=[ncum_t])
    P.op("dve", lambda e: e.tensor_tensor(out=bec[:, :, :], in0=beta[:, :, :], in1=ecum[:, :, :], op=ALU.mult),
         reads=[beta_t, ecum_t], writes=[bec_t])
    P.op("dve", lambda e: e.tensor_tensor(out=eend[:, :, :], in0=tot[:, :, :], in1=cum[:, :, :], op=ALU.subtract),
         reads=[tot_t, cum_t], writes=[eend_t])
    P.op("act", lambda e: e.activation(out=eend[:, :, :], in_=eend[:, :, :], func=AF.Exp), reads=[eend_t], writes=[eend_t])
    sc_reads = [g_t, beta_t, cum_t, ecum_t, ncum_t, bec_t, eend_t, ecl_t]
    if C.dbg.get("gdn_stop") == 1:
        return
    Wh = alloc(C, st, "d_Wh", [128, 8, 512], BF16)
    Wh_t = [T("d_Wh_%d" % j) for j in range(4)]
    cw = alloc(C, st, "d_cw", [128, 12], F32); cw_t = T("d_cw")
    pre = alloc(C, st, "d_pre", [128, S + 3], F32); pre_t = T("d_pre")
    P.op("pool", lambda e: e.memset(pre[:, 0:3], 0.0), writes=[pre_t])
    yb = alloc(C, st, "d_y", [128, S], F32); yb_t = T("d_y")
    vT = alloc(C, st, "d_vT", [128, S], F32); vT_t = T("d_vT")
    qTn = alloc(C, st, "d_qTn", [128, S], BF16); qTn_t = T("d_qTn")
    kTn = alloc(C, st, "d_kTn", [128, S], BF16); kTn_t = T("d_kTn")
    rs = alloc(C, st, "d_rs", [128, 512], F32); rs_t = T("d_rs")
    Sst = alloc(C, st, "d_S", [128, 128], F32); S_t = T("d_S")
    Sbf = alloc(C, st, "d_Sbf", [128, 128], BF16); Sbf_t = T("d_Sbf")
    def mk(name, shape, dt, n=2):
        return [alloc(C, st, "%s%d" % (name, i), shape, dt) for i in range(n)], [T("%s%d" % (name, i)) for i in range(n)]
    NS = C.dbg.get("gdn_ns", 4)
    Dm, Dm_t = mk("d_Dm", [128, 128], F32, NS)
    DTm, DTm_t = mk("d_DTm", [128, 128], F32, NS)
    Lm, Lm_t = mk("d_L", [128, 128], F32, NS)
    LT, LT_t = mk("d_LT", [128, 128], F32, NS)
    Pa, Pa_t = Dm, Dm_t
    PTa, PTa_t = DTm, DTm_t
    Pb, Pb_t = mk("d_Pb", [128, 128], F32, NS)
    PTb, PTb_t = mk("d_PTb", [128, 128], F32, NS)
    qkT, qkT_t = mk("d_qkT", [128, 128], BF16, NS)
    kend, kend_t = mk("d_kend", [128, 128], BF16, NS)
    Xa, Xa_t = mk("d_Xa", [128, 256], F32, NS)
    Xb, Xb_t = mk("d_Xb", [128, 256], F32, NS)
    wT, wT_t = mk("d_wT", [128, 128], BF16, NS)
    vnew, vnew_t = mk("d_vnew", [128, 128], BF16, NS)
    intra, intra_t = Pb, Pb_t
    osb, osb_t = PTb, PTb_t
    sg, sg_t = DTm, DTm_t
    junk, junk_t = Dm, Dm_t
    ss, ss_t = mk("d_ss", [128, 4], F32, NS)
    cwsrc = dr["gdn_conv_w"].rearrange("j c -> c j")
    for h in range(H):
        hb = h * 128
        for j in range(4):
            P.dma("pool", Wh[:, :, j * 128:(j + 1) * 128], win[:, :, j * 1024 + hb:j * 1024 + hb + 128], writes=[Wh_t[j]])
        for s3 in range(3):
            P.dma("sp", cw[:, s3 * 4:(s3 + 1) * 4], cwsrc[s3 * 1024 + hb:s3 * 1024 + hb + 128, :], writes=[cw_t],
                  allow_slow_non_contiguous=True)
        P.op("pool", lambda e: e.memset(Sst[:, :], 0.0), writes=[S_t])
        P.op("pool", lambda e: e.memset(Sbf[:, :], 0.0), writes=[Sbf_t])
        for s3 in range(3):
            for tb in range(4):
                cs = slice(tb * 512, (tb + 1) * 512)
                pp, ppt = next_ps(C)
                for k in range(8):
                    P.op("pe", lambda e, pp=pp, k=k, s3=s3, cs=cs: e.matmul(
                        pp[:, :], lhsT=Wh[:, k, s3 * 128:(s3 + 1) * 128], rhs=XT[:, k, cs], start=(k == 0), stop=(k == 7)),
                        reads=[Wh_t[s3]] + allx[tb * 4:tb * 4 + 4], writes=[ppt])
                P.op("act", lambda e, pp=pp, tb=tb: e.activation(out=pre[:, 3 + tb * 512:3 + (tb + 1) * 512], in_=pp[:, :], func=AF.Copy),
                     reads=[ppt], writes=[pre_t])
            dst, dst_t = (vT, vT_t) if s3 == 2 else (yb, yb_t)
            P.op("dve", lambda e, dst=dst, s3=s3: e.tensor_scalar(out=dst[:, :], in0=pre[:, 3:3 + S], scalar1=cw[:, s3 * 4 + 3:s3 * 4 + 4],
                                                                 scalar2=None, op0=ALU.mult), reads=[pre_t, cw_t], writes=[dst_t])
            for j in range(3):
                P.op("dve", lambda e, dst=dst, s3=s3, j=j: e.scalar_tensor_tensor(
                    out=dst[:, :], in0=pre[:, j:j + S], scalar=cw[:, s3 * 4 + j:s3 * 4 + j + 1], in1=dst[:, :], op0=ALU.mult, op1=ALU.add),
                    reads=[pre_t, cw_t, dst_t], writes=[dst_t])
            sgm = pre[:, 3:3 + S]
            P.op("act", lambda e, dst=dst, sgm=sgm: e.activation(out=sgm, in_=dst[:, :], func=AF.Exp, scale=-1.0), reads=[dst_t, pre_t], writes=[pre_t])
            P.op("act", lambda e, sgm=sgm: e.activation(out=sgm, in_=sgm, func=AF.Ln, bias=1.0, scale=1.0), reads=[pre_t], writes=[pre_t])
            P.op("act", lambda e, sgm=sgm: e.activation(out=sgm, in_=sgm, func=AF.Exp, scale=-1.0), reads=[pre_t], writes=[pre_t])
            P.op("dve", lambda e, dst=dst, sgm=sgm: e.tensor_tensor(out=dst[:, :], in0=dst[:, :], in1=sgm, op=ALU.mult),
                 reads=[dst_t, pre_t], writes=[dst_t])
            if s3 < 2:
                outT, outT_t = (qTn, qTn_t) if s3 == 0 else (kTn, kTn_t)
                scl = float(DK ** -0.5) if s3 == 0 else 1.0
                P.op("dve", lambda e: e.tensor_tensor(out=pre[:, 3:3 + S], in0=yb[:, :], in1=yb[:, :], op=ALU.mult),
                     reads=[yb_t, pre_t], writes=[pre_t])
                for tb in range(4):
                    cs = slice(tb * 512, (tb + 1) * 512)
                    pq, pqt = next_ps(C)
                    P.op("pe", lambda e, pq=pq, tb=tb: e.matmul(pq[:, :], lhsT=C.cm["ones"][:, :], rhs=pre[:, 3 + tb * 512:3 + (tb + 1) * 512],
                                                               start=True, stop=True), reads=[pre_t, C.cm_t["ones"]], writes=[pqt])
                    P.op("act", lambda e, pq=pq: e.activation(out=rs[:, :], in_=pq[:, :], func=AF.Ln, bias=C.eps6[:, 0:1], scale=1.0),
                         reads=[pqt, C.eps6_t], writes=[rs_t])
                    P.op("act", lambda e: e.activation(out=rs[:, :], in_=rs[:, :], func=AF.Exp, scale=-0.5), reads=[rs_t], writes=[rs_t])
                    P.op("dve", lambda e, outT=outT, cs=cs, scl=scl: e.scalar_tensor_tensor(
                        out=outT[:, cs], in0=yb[:, cs], scalar=scl, in1=rs[:, :], op0=ALU.mult, op1=ALU.mult),
                        reads=[yb_t, rs_t], writes=[outT_t])
        def gdn_tile(t, h=h, hb=hb):
            b = t % NS
            tok = slice(t * 128, (t + 1) * 128)
            col = lambda arr: arr[:, t, h:h + 1]
            pa, pat = next_ps(C)
            P.op("pe", lambda e, pa=pa, tok=tok: e.matmul(pa[:, 0:128], lhsT=kTn[:, tok], rhs=kTn[:, tok], start=True, stop=True),
                 reads=[kTn_t], writes=[pat])
            P.op("pe", lambda e, pa=pa, tok=tok: e.matmul(pa[:, 128:256], lhsT=kTn[:, tok], rhs=qTn[:, tok], start=True, stop=True),
                 reads=[kTn_t, qTn_t], writes=[pat])
            pb, pbt = next_ps(C)
            gbc = g_all[:, t, h:h + 1].to_broadcast([128, 128])
            for (o, mname) in ((0, "g_pos"), (128, "g_negt")):
                P.op("pe", lambda e, pb=pb, o=o, gbc=gbc: e.matmul(pb[:, o:o + 128], lhsT=gbc, rhs=C.cm["m_incl"][:, :], start=True, stop=False),
                     reads=[g_t, C.cm_t["m_incl"]], writes=[pbt])
                P.op("pe", lambda e, pb=pb, o=o, mname=mname: e.matmul(pb[:, o:o + 128], lhsT=C.ident[:, :], rhs=C.cm[mname][:, :], start=False, stop=True),
                     reads=[C.ident_t, C.cm_t[mname]], writes=[pbt])
            pc, pct = next_ps(C)
            P.op("pe", lambda e, pc=pc, tok=tok: e.transpose(out=pc[:, 0:128], in_=vT[:, tok], identity=C.ident[:, :]),
                 reads=[vT_t, C.ident_t], writes=[pct])
            pd, pdt = next_ps(C)
            pdb = pd[:, :].bitcast(BF16)
            P.op("pe", lambda e, pdb=pdb, tok=tok: e.transpose(out=pdb[:, 0:128], in_=kTn[:, tok], identity=C.identb[:, :]),
                 reads=[kTn_t, C.identb_t], writes=[pdt])
            yield
            P.op("act", lambda e, pb=pb, b=b, t=t, h=h: e.activation(out=Dm[b][:, :], in_=pb[:, 0:128], func=AF.Exp, bias=cum[:, t, h:h + 1], scale=-1.0),
                 reads=[pbt, cum_t], writes=[Dm_t[b]])
            P.op("act", lambda e, pb=pb, b=b, t=t, h=h: e.activation(out=DTm[b][:, :], in_=pb[:, 128:256], func=AF.Exp, bias=ncum[:, t, h:h + 1], scale=1.0),
                 reads=[pbt, ncum_t], writes=[DTm_t[b]])
            P.op("dve", lambda e, pa=pa, b=b, t=t, h=h: e.scalar_tensor_tensor(
                out=Lm[b][:, :], in0=pa[:, 0:128], scalar=beta[:, t, h:h + 1], in1=Dm[b][:, :], op0=ALU.mult, op1=ALU.mult),
                reads=[pat, beta_t, Dm_t[b]], writes=[Lm_t[b]])
            P.op("dve", lambda e, pa=pa, b=b: e.tensor_tensor(out=qkT[b][:, :], in0=pa[:, 128:256], in1=DTm[b][:, :], op=ALU.mult),
                 reads=[pat, DTm_t[b]], writes=[qkT_t[b]])
            P.op("dve", lambda e, pc=pc, b=b, t=t, h=h: e.tensor_scalar(out=Xa[b][:, 0:128], in0=pc[:, 0:128], scalar1=beta[:, t, h:h + 1],
                                                                     scalar2=None, op0=ALU.mult), reads=[pct, beta_t], writes=[Xa_t[b]])
            P.op("dve", lambda e, pdb=pdb, b=b, t=t, h=h: e.tensor_scalar(out=Xa[b][:, 128:256], in0=pdb[:, 0:128], scalar1=bec[:, t, h:h + 1],
                                                                      scalar2=None, op0=ALU.mult), reads=[pdt, bec_t], writes=[Xa_t[b]])
            P.op("act", lambda e, pdb=pdb, b=b, t=t, h=h: e.activation(out=kend[b][:, :], in_=pdb[:, 0:128], func=AF.Copy, scale=eend[:, t, h:h + 1]),
                 reads=[pdt, eend_t], writes=[kend_t[b]])
            yield
            pe_, pet = next_ps(C)
            P.op("pe", lambda e, pe_=pe_, b=b: e.transpose(out=pe_[:, 0:128], in_=Lm[b][:, :], identity=C.ident[:, :]),
                 reads=[Lm_t[b], C.ident_t], writes=[pet])
            P.op("act", lambda e, pe_=pe_, b=b: e.activation(out=LT[b][:, :], in_=pe_[:, 0:128], func=AF.Copy), reads=[pet], writes=[LT_t[b]])
            yield
            px, pxt = next_ps(C)
            P.op("pe", lambda e, px=px, b=b: e.matmul(px[:, 0:256], lhsT=LT[b][:, :], rhs=Xa[b][:, :], start=True, stop=True),
                 reads=[LT_t[b], Xa_t[b]], writes=[pxt])
            P.op("dve", lambda e, px=px, b=b: e.tensor_tensor(out=Xb[b][:, :], in0=Xa[b][:, :], in1=px[:, 0:256], op=ALU.subtract),
                 reads=[Xa_t[b], pxt], writes=[Xb_t[b]])
            Xc, Xc_t, Xn, Xn_t = Xb[b], Xb_t[b], Xa[b], Xa_t[b]
            Pc, Pc_t, PTc, PTc_t = Lm[b], Lm_t[b], LT[b], LT_t[b]
            for lev in range(1, 7):
                if lev % 2 == 1:
                    Pn, Pn_t, PTn, PTn_t = Pa[b], Pa_t[b], PTa[b], PTa_t[b]
                else:
                    Pn, Pn_t, PTn, PTn_t = Pb[b], Pb_t[b], PTb[b], PTb_t[b]
                yield
                pp2, pp2t = next_ps(C)
                P.op("pe", lambda e, pp2=pp2, Pc=Pc, PTc=PTc: e.matmul(pp2[:, 0:128], lhsT=Pc[:, :], rhs=PTc[:, :], start=True, stop=True),
                     reads=[Pc_t, PTc_t], writes=[pp2t])
                if lev < 6:
                    P.op("pe", lambda e, pp2=pp2, Pc=Pc, PTc=PTc: e.matmul(pp2[:, 128:256], lhsT=PTc[:, :], rhs=Pc[:, :], start=True, stop=True),
                         reads=[Pc_t, PTc_t], writes=[pp2t])
                P.op("act", lambda e, pp2=pp2, PTn=PTn: e.activation(out=PTn[:, :], in_=pp2[:, 0:128], func=AF.Copy), reads=[pp2t], writes=[PTn_t])
                if lev < 6:
                    P.op("dve", lambda e, pp2=pp2, Pn=Pn: e.tensor_scalar(out=Pn[:, :], in0=pp2[:, 128:256], scalar1=1.0, scalar2=None, op0=ALU.mult),
                         reads=[pp2t], writes=[Pn_t])
                yield
                px2, px2t = next_ps(C)
                P.op("pe", lambda e, px2=px2, PTn=PTn, Xc=Xc: e.matmul(px2[:, 0:256], lhsT=PTn[:, :], rhs=Xc[:, :], start=True, stop=True),
                     reads=[PTn_t, Xc_t], writes=[px2t])
                P.op("dve", lambda e, px2=px2, Xc=Xc, Xn=Xn: e.tensor_tensor(out=Xn[:, :], in0=Xc[:, :], in1=px2[:, 0:256], op=ALU.add),
                     reads=[Xc_t, px2t], writes=[Xn_t])
                Xc, Xc_t, Xn, Xn_t = Xn, Xn_t, Xc, Xc_t
                Pc, Pc_t, PTc, PTc_t = Pn, Pn_t, PTn, PTn_t
            yield
            pw_, pwt_ = next_ps(C)
            P.op("pe", lambda e, pw_=pw_, Xc=Xc: e.transpose(out=pw_[:, 0:128], in_=Xc[:, 128:256], identity=C.ident[:, :]),
                 reads=[Xc_t, C.ident_t], writes=[pwt_])
            P.op("act", lambda e, pw_=pw_, b=b: e.activation(out=wT[b][:, :], in_=pw_[:, 0:128], func=AF.Copy), reads=[pwt_], writes=[wT_t[b]])
            yield
            pv, pvt = next_ps(C)
            P.op("pe", lambda e, pv=pv, b=b: e.matmul(pv[:, 0:128], lhsT=wT[b][:, :], rhs=Sbf[:, :], start=True, stop=True),
                 reads=[wT_t[b], Sbf_t], writes=[pvt])
            P.op("dve", lambda e, pv=pv, b=b, Xc=Xc: e.tensor_tensor(out=vnew[b][:, :], in0=Xc[:, 0:128], in1=pv[:, 0:128], op=ALU.subtract),
                 reads=[Xc_t, pvt], writes=[vnew_t[b]])
            po, pot = next_ps(C)
            P.op("pe", lambda e, po=po, b=b: e.matmul(po[:, 0:128], lhsT=qkT[b][:, :], rhs=vnew[b][:, :], start=True, stop=True),
                 reads=[qkT_t[b], vnew_t[b]], writes=[pot])
            P.op("pe", lambda e, po=po, tok=tok: e.matmul(po[:, 128:256], lhsT=qTn[:, tok], rhs=Sbf[:, :], start=True, stop=True),
                 reads=[qTn_t, Sbf_t], writes=[pot])
            P.op("act", lambda e, po=po, b=b: e.activation(out=intra[b][:, :], in_=po[:, 0:128], func=AF.Copy), reads=[pot], writes=[intra_t[b]])
            P.op("dve", lambda e, po=po, b=b, t=t, h=h: e.scalar_tensor_tensor(
                out=osb[b][:, :], in0=po[:, 128:256], scalar=ecum[:, t, h:h + 1], in1=intra[b][:, :], op0=ALU.mult, op1=ALU.add),
                reads=[pot, ecum_t, intra_t[b]], writes=[osb_t[b]])
            pu, put = next_ps(C)
            P.op("pe", lambda e, pu=pu, b=b: e.matmul(pu[:, 0:128], lhsT=kend[b][:, :], rhs=vnew[b][:, :], start=True, stop=True),
                 reads=[kend_t[b], vnew_t[b]], writes=[put])
            P.op("dve", lambda e, pu=pu, t=t, h=h: e.scalar_tensor_tensor(
                out=Sst[:, :], in0=Sst[:, :], scalar=ecl[:, t, h:h + 1], in1=pu[:, 0:128], op0=ALU.mult, op1=ALU.add),
                reads=[S_t, ecl_t, put], writes=[S_t])
            P.op("act", lambda e: e.activation(out=Sbf[:, :], in_=Sst[:, :], func=AF.Copy), reads=[S_t], writes=[Sbf_t])
            yield
            pz, pzt = next_ps(C)
            for k in range(8):
                P.op("pe", lambda e, pz=pz, k=k, tok=tok: e.matmul(pz[:, 0:128], lhsT=XT[:, k, tok], rhs=Wh[:, k, 384:512], start=(k == 0), stop=(k == 7)),
                     reads=[XT_t[t], Wh_t[3]], writes=[pzt])
            P.op("act", lambda e, pz=pz, b=b: e.activation(out=sg[b][:, :], in_=pz[:, 0:128], func=AF.Exp, scale=-1.0), reads=[pzt], writes=[sg_t[b]])
            P.op("act", lambda e, b=b: e.activation(out=sg[b][:, :], in_=sg[b][:, :], func=AF.Ln, bias=1.0, scale=1.0), reads=[sg_t[b]], writes=[sg_t[b]])
            P.op("act", lambda e, b=b: e.activation(out=sg[b][:, :], in_=sg[b][:, :], func=AF.Exp, scale=-1.0), reads=[sg_t[b]], writes=[sg_t[b]])
            P.op("dve", lambda e, pz=pz, b=b: e.tensor_tensor(out=sg[b][:, :], in0=pz[:, 0:128], in1=sg[b][:, :], op=ALU.mult),
                 reads=[pzt, sg_t[b]], writes=[sg_t[b]])
            P.op("dve", lambda e, b=b: e.tensor_tensor(out=sg[b][:, :], in0=sg[b][:, :], in1=ng[:, :], op=ALU.mult),
                 reads=[sg_t[b], ng_t], writes=[sg_t[b]])
            P.op("dve", lambda e, b=b: e.scalar_tensor_tensor(out=junk[b][:, :], in0=osb[b][:, :], scalar=1.0, in1=osb[b][:, :],
                                                              op0=ALU.mult, op1=ALU.mult, accum_out=ss[b][:, 0:1]),
                 reads=[osb_t[b]], writes=[junk_t[b], ss_t[b]])
            P.op("act", lambda e, b=b: e.activation(out=ss[b][:, 1:2], in_=ss[b][:, 0:1], func=AF.Ln, bias=C.eps6[:, 0:1], scale=1.0 / 128),
                 reads=[ss_t[b], C.eps6_t], writes=[ss_t[b]])
            P.op("act", lambda e, b=b: e.activation(out=ss[b][:, 2:3], in_=ss[b][:, 1:2], func=AF.Exp, scale=-0.5),
                 reads=[ss_t[b]], writes=[ss_t[b]])
            P.op("dve", lambda e, b=b, t=t, hb=hb: e.scalar_tensor_tensor(
                out=O[:, t, hb:hb + 128], in0=osb[b][:, :], scalar=ss[b][:, 2:3], in1=sg[b][:, :], op0=ALU.mult, op1=ALU.mult),
                reads=[osb_t[b], ss_t[b], sg_t[b]], writes=[O_t[t]])


        run_pipelined((gdn_tile(t) for t in range(NT)), NS)
def hgrn_phase(C, l):
    P, nc, dr = C.P, C.nc, C.dr
    with contextlib.ExitStack() as st:
        O = alloc(C, st, "h_O", [128, NT, D], BF16)
        O_t = [T("h_O%d" % t) for t in range(NT)]
        with contextlib.ExitStack() as st2:
            hgrn_heads(C, l, st2, O, O_t)
            barrier(C)
        outproj_phase(C, l, O, O_t, dr["hgrn_w_out"])


def hgrn_lb(C, st, l, src_ap, shape, name):
    P = C.P
    pn, n = shape
    hb = alloc(C, st, name + "_hb", [pn, 4, n], F32); hb_t = T(name + "_hb")
    P.dma("sp", hb[:, :, :], src_ap, writes=[hb_t], allow_slow_non_contiguous=True)
    mx = alloc(C, st, name + "_mx", [pn, n], F32); mx_t = T(name + "_mx")
    P.op("dve", lambda e: e.tensor_tensor(out=mx[:, :], in0=hb[:, 0, :], in1=hb[:, 1, :], op=ALU.max), reads=[hb_t], writes=[mx_t])
    for j in (2, 3):
        P.op("dve", lambda e, j=j: e.tensor_tensor(out=mx[:, :], in0=mx[:, :], in1=hb[:, j, :], op=ALU.max), reads=[hb_t, mx_t], writes=[mx_t])
    for j in range(4):
        P.op("dve", lambda e, j=j: e.tensor_tensor(out=hb[:, j, :], in0=hb[:, j, :], in1=mx[:, :], op=ALU.subtract), reads=[hb_t, mx_t], writes=[hb_t])
    P.op("act", lambda e: e.activation(out=hb[:, :, :], in_=hb[:, :, :], func=AF.Exp), reads=[hb_t], writes=[hb_t])
    den = mx
    P.op("dve", lambda e: e.tensor_tensor(out=den[:, :], in0=hb[:, 0, :], in1=hb[:, 1, :], op=ALU.add), reads=[hb_t, mx_t], writes=[mx_t])
    for j in (2, 3):
        P.op("dve", lambda e, j=j: e.tensor_tensor(out=den[:, :], in0=den[:, :], in1=hb[:, j, :], op=ALU.add), reads=[hb_t, mx_t], writes=[mx_t])
    P.op("dve", lambda e: e.reciprocal(out=den[:, :], in_=den[:, :]), reads=[mx_t], writes=[mx_t])
    lb = alloc(C, st, name + "_lb", [pn, n], F32); lb_t = T(name + "_lb")
    oml = alloc(C, st, name + "_oml", [pn, n], F32)
    if l == 0:
        P.op("dve", lambda e: e.memset(lb[:, :], 0.0), writes=[lb_t])
    else:
        P.op("dve", lambda e: e.tensor_copy(out=lb[:, :], in_=hb[:, 1, :]), reads=[hb_t], writes=[lb_t])
        for j in range(2, l + 1):
            P.op("dve", lambda e, j=j: e.tensor_tensor(out=lb[:, :], in0=lb[:, :], in1=hb[:, j, :], op=ALU.add), reads=[hb_t, lb_t], writes=[lb_t])
        P.op("dve", lambda e: e.tensor_tensor(out=lb[:, :], in0=lb[:, :], in1=den[:, :], op=ALU.mult), reads=[mx_t, lb_t], writes=[lb_t])
    P.op("dve", lambda e: e.tensor_scalar(out=oml[:, :], in0=lb[:, :], scalar1=-1.0, scalar2=1.0, op0=ALU.mult, op1=ALU.add),
         reads=[lb_t], writes=[lb_t])
    return lb, oml, lb_t


def hgrn_heads(C, l, st, O, O_t):
    P, nc, dr = C.P, C.nc, C.dr
    H, DK, DV = 8, 128, 128
    XT = alloc(C, st, "h_XT", [128, 8, S], BF16)
    XT_t = {t: T("h_XT%d" % t) for t in range(NT)}
    build_xT(C, XT, XT_t, range(NT))
    win = dr["hgrn_w_in"].rearrange("(k p) n -> p k n", p=128)
    lbb, omlb, lbb_t = hgrn_lb(C, st, l, dr["hgrn_lower_bounds"].rearrange("(o l) n -> o l n", o=1).to_broadcast([128, 4, D]),
                               (128, D), "h_b")
    lbT, omlT, lbT_t = hgrn_lb(C, st, l, dr["hgrn_lower_bounds"].rearrange("l (c p) -> p l c", p=128), (128, 8), "h_T")
    ng = alloc(C, st, "h_ng", [128, DV], F32); ng_t = T("h_ng")
    load_bcast_row(C, ng[:, :], ng_t, dr["hgrn_norm_g"][0:1, :])
    Wh = [alloc(C, st, "h_Wh%d" % i, [128, 8, 512], BF16) for i in range(2)]
    Wh_t = [[T("h_Wh%d_%d" % (i, j)) for j in range(4)] for i in range(2)]
    state = alloc(C, st, "h_state", [128, DV], F32); state_t = T("h_state")
    state_bf = alloc(C, st, "h_statebf", [128, DV], BF16); statebf_t = T("h_statebf")
    NB = 2
    def mk(name, shape, dt):
        return [alloc(C, st, "%s%d" % (name, i), shape, dt) for i in range(NB)], [T("%s%d" % (name, i)) for i in range(NB)]
    sig, sig_t = mk("h_sig", [128, 128], F32)
    a_, a_t = mk("h_a", [128, 128], F32)
    fg, fg_t = mk("h_fg", [128, 128], F32)
    kin, kin_t = mk("h_kin", [128, 128], F32)
    la, la_t = mk("h_la", [128, 128], F32)
    sgT, sgT_t = mk("h_sgT", [128, 128], F32)
    ecT, ecT_t = mk("h_ecT", [128, 128], F32)
    eiT, eiT_t = mk("h_eiT", [128, 128], F32)
    eR, eR_t = mk("h_eR", [128, 128], F32)
    qd, qd_t = mk("h_qd", [128, 128], BF16)
    ki, ki_t = mk("h_ki", [128, 128], BF16)
    ke, ke_t = mk("h_ke", [128, 128], BF16)
    vb, vb_t = mk("h_vb", [128, DV], BF16)
    sg, sg_t = mk("h_sg", [128, DV], F32)
    sT, sT_t = mk("h_sT", [128, 128], BF16)
    junk, junk_t = mk("h_junk", [128, DV], F32)
    osq, osq_t = mk("h_osq", [128, DV], F32)
    ss, ss_t = mk("h_ss", [128, 4], F32)
    for h in range(H):
        wb = h % 2
        hs = slice(h * 128, (h + 1) * 128)
        for j in range(4):
            P.dma("pool", Wh[wb][:, :, j * 128:(j + 1) * 128], win[:, :, j * 1024 + h * 128:j * 1024 + (h + 1) * 128], writes=[Wh_t[wb][j]])
        P.op("pool", lambda e: e.memset(state[:, :], 0.0), writes=[state_t])
        P.op("pool", lambda e: e.memset(state_bf[:, :], 0.0), writes=[statebf_t])
        def hgrn_tile(t, h=h, wb=wb):
            b = t % NB
            pool = (0, 1, 2, 3) if b == 0 else (4, 5, 6, 7)
            tok = slice(t * 128, (t + 1) * 128)
            p1, p1t = next_ps(C, pool)
            for j in range(2):
                for k in range(8):
                    P.op("pe", lambda e, p1=p1, j=j, k=k, wb=wb, tok=tok: e.matmul(
                        p1[:, j * 128:(j + 1) * 128], lhsT=Wh[wb][:, k, j * 128:(j + 1) * 128], rhs=XT[:, k, tok],
                        start=(k == 0), stop=(k == 7)), reads=[Wh_t[wb][j], XT_t[t]], writes=[p1t])
            p2, p2t = next_ps(C, pool)
            for k in range(8):
                P.op("pe", lambda e, p2=p2, k=k, wb=wb, tok=tok: e.matmul(
                    p2[:, 0:384], lhsT=XT[:, k, tok], rhs=Wh[wb][:, k, 128:512], start=(k == 0), stop=(k == 7)),
                    reads=[Wh_t[wb][1], Wh_t[wb][2], Wh_t[wb][3], XT_t[t]], writes=[p2t])
            yield
            P.op("act", lambda e, p2=p2, b=b: e.activation(out=sig[b][:, :], in_=p2[:, 0:128], func=AF.Exp, scale=-1.0),
                 reads=[p2t], writes=[sig_t[b]])
            P.op("act", lambda e, b=b: e.activation(out=sig[b][:, :], in_=sig[b][:, :], func=AF.Ln, bias=1.0, scale=1.0),
                 reads=[sig_t[b]], writes=[sig_t[b]])
            P.op("act", lambda e, b=b: e.activation(out=sig[b][:, :], in_=sig[b][:, :], func=AF.Exp, scale=-1.0),
                 reads=[sig_t[b]], writes=[sig_t[b]])
            P.op("dve", lambda e, b=b, hs=hs: e.tensor_tensor(out=a_[b][:, :], in0=sig[b][:, :], in1=omlb[:, hs], op=ALU.mult),
                 reads=[sig_t[b], lbb_t], writes=[a_t[b]])
            P.op("dve", lambda e, b=b, hs=hs: e.tensor_tensor(out=fg[b][:, :], in0=a_[b][:, :], in1=lbb[:, hs], op=ALU.add),
                 reads=[a_t[b], lbb_t], writes=[fg_t[b]])
            P.op("dve", lambda e, b=b, hs=hs: e.tensor_tensor(out=kin[b][:, :], in0=omlb[:, hs], in1=a_[b][:, :], op=ALU.subtract),
                 reads=[a_t[b], lbb_t], writes=[kin_t[b]])
            P.op("act", lambda e, b=b: e.activation(out=la[b][:, :], in_=fg[b][:, :], func=AF.Ln),
                 reads=[fg_t[b]], writes=[la_t[b]])
            P.op("act", lambda e, p1=p1, b=b: e.activation(out=sgT[b][:, :], in_=p1[:, 128:256], func=AF.Exp, scale=1.0),
                 reads=[p1t], writes=[sgT_t[b]])
            P.op("act", lambda e, b=b: e.activation(out=sgT[b][:, :], in_=sgT[b][:, :], func=AF.Ln, bias=1.0, scale=1.0),
                 reads=[sgT_t[b]], writes=[sgT_t[b]])
            P.op("act", lambda e, b=b: e.activation(out=sgT[b][:, :], in_=sgT[b][:, :], func=AF.Exp, scale=-1.0),
                 reads=[sgT_t[b]], writes=[sgT_t[b]])
            yield
            p4, p4t = next_ps(C, pool)
            P.op("pe", lambda e, p4=p4, b=b: e.matmul(p4[:, 0:128], lhsT=la[b][:, :], rhs=C.cm["m_incl"][:, :], start=True, stop=True),
                 reads=[la_t[b], C.cm_t["m_incl"]], writes=[p4t])
            P.op("pe", lambda e, p4=p4, b=b: e.matmul(p4[:, 128:256], lhsT=C.cm["m_rev"][:, :], rhs=la[b][:, :], start=True, stop=True),
                 reads=[la_t[b], C.cm_t["m_rev"]], writes=[p4t])
            P.op("act", lambda e, p4=p4, b=b: e.activation(out=ecT[b][:, :], in_=p4[:, 0:128], func=AF.Exp),
                 reads=[p4t], writes=[ecT_t[b]])
            P.op("act", lambda e, p4=p4, b=b: e.activation(out=eiT[b][:, :], in_=p4[:, 0:128], func=AF.Exp, scale=-1.0),
                 reads=[p4t], writes=[eiT_t[b]])
            P.op("act", lambda e, p4=p4, b=b: e.activation(out=eR[b][:, :], in_=p4[:, 128:256], func=AF.Exp),
                 reads=[p4t], writes=[eR_t[b]])
            P.op("dve", lambda e, p1=p1, b=b: e.tensor_tensor(out=qd[b][:, :], in0=p1[:, 0:128], in1=ecT[b][:, :], op=ALU.mult),
                 reads=[p1t, ecT_t[b]], writes=[qd_t[b]])
            P.op("dve", lambda e, b=b, h=h: e.scalar_tensor_tensor(
                out=ki[b][:, :], in0=sgT[b][:, :], scalar=omlT[:, h:h + 1], in1=eiT[b][:, :], op0=ALU.mult, op1=ALU.mult),
                reads=[sgT_t[b], lbT_t, eiT_t[b]], writes=[ki_t[b]])
            P.op("dve", lambda e, b=b: e.tensor_tensor(out=ke[b][:, :], in0=kin[b][:, :], in1=eR[b][:, :], op=ALU.mult),
                 reads=[kin_t[b], eR_t[b]], writes=[ke_t[b]])
            P.op("act", lambda e, p2=p2, b=b: e.activation(out=vb[b][:, :], in_=p2[:, 128:256], func=AF.Copy),
                 reads=[p2t], writes=[vb_t[b]])
            P.op("act", lambda e, p2=p2, b=b: e.activation(out=sg[b][:, :], in_=p2[:, 256:384], func=AF.Exp, scale=-1.0),
                 reads=[p2t], writes=[sg_t[b]])
            P.op("act", lambda e, b=b: e.activation(out=sg[b][:, :], in_=sg[b][:, :], func=AF.Ln, bias=1.0, scale=1.0),
                 reads=[sg_t[b]], writes=[sg_t[b]])
            P.op("act", lambda e, b=b: e.activation(out=sg[b][:, :], in_=sg[b][:, :], func=AF.Exp, scale=-1.0),
                 reads=[sg_t[b]], writes=[sg_t[b]])
            P.op("dve", lambda e, p2=p2, b=b: e.tensor_tensor(out=sg[b][:, :], in0=p2[:, 256:384], in1=sg[b][:, :], op=ALU.mult),
                 reads=[p2t, sg_t[b]], writes=[sg_t[b]])
            P.op("dve", lambda e, b=b: e.tensor_tensor(out=sg[b][:, :], in0=sg[b][:, :], in1=ng[:, :], op=ALU.mult),
                 reads=[sg_t[b], ng_t], writes=[sg_t[b]])
            yield
            p5, p5t = next_ps(C, pool)
            P.op("pe", lambda e, p5=p5, b=b: e.matmul(p5[:, 0:128], lhsT=ki[b][:, :], rhs=qd[b][:, :], start=True, stop=True),
                 reads=[ki_t[b], qd_t[b]], writes=[p5t])
            P.op("dve", lambda e, p5=p5, b=b: e.tensor_tensor(out=sT[b][:, :], in0=p5[:, 0:128], in1=C.cm["m_caus"][:, :], op=ALU.mult),
                 reads=[p5t, C.cm_t["m_caus"]], writes=[sT_t[b]])
            yield
            p6, p6t = next_ps(C, pool)
            P.op("pe", lambda e, p6=p6, b=b: e.matmul(p6[:, 0:DV], lhsT=sT[b][:, :], rhs=vb[b][:, :], start=True, stop=False),
                 reads=[sT_t[b], vb_t[b]], writes=[p6t])
            P.op("pe", lambda e, p6=p6, b=b: e.matmul(p6[:, 0:DV], lhsT=qd[b][:, :], rhs=state_bf[:, :], start=False, stop=True),
                 reads=[qd_t[b], statebf_t], writes=[p6t])
            yield
            p7, p7t = next_ps(C, pool)
            P.op("pe", lambda e, p7=p7, b=b: e.matmul(p7[:, 0:DV], lhsT=ke[b][:, :], rhs=vb[b][:, :], start=True, stop=True),
                 reads=[ke_t[b], vb_t[b]], writes=[p7t])
            P.op("dve", lambda e, p7=p7, b=b: e.scalar_tensor_tensor(
                out=state[:, :], in0=state[:, :], scalar=ecT[b][:, 127:128], in1=p7[:, 0:DV], op0=ALU.mult, op1=ALU.add),
                reads=[state_t, ecT_t[b], p7t], writes=[state_t])
            P.op("act", lambda e: e.activation(out=state_bf[:, :], in_=state[:, :], func=AF.Copy),
                 reads=[state_t], writes=[statebf_t])
            yield
            P.op("act", lambda e, p6=p6, b=b: e.activation(out=junk[b][:, :], in_=p6[:, 0:DV], func=AF.Copy),
                 reads=[p6t], writes=[junk_t[b]])
            P.op("dve", lambda e, b=b: e.scalar_tensor_tensor(out=osq[b][:, :], in0=junk[b][:, :], scalar=1.0, in1=junk[b][:, :],
                                                              op0=ALU.mult, op1=ALU.mult, accum_out=ss[b][:, 0:1]),
                 reads=[junk_t[b]], writes=[osq_t[b], ss_t[b]])
            P.op("act", lambda e, b=b: e.activation(out=ss[b][:, 1:2], in_=ss[b][:, 0:1], func=AF.Ln, bias=C.eps6[:, 0:1], scale=1.0 / DV),
                 reads=[ss_t[b], C.eps6_t], writes=[ss_t[b]])
            P.op("act", lambda e, b=b: e.activation(out=ss[b][:, 2:3], in_=ss[b][:, 1:2], func=AF.Exp, scale=-0.5),
                 reads=[ss_t[b]], writes=[ss_t[b]])
            P.op("dve", lambda e, p6=p6, b=b, t=t, h=h: e.scalar_tensor_tensor(
                out=O[:, t, h * DV:(h + 1) * DV], in0=junk[b][:, :], scalar=ss[b][:, 2:3], in1=sg[b][:, :], op0=ALU.mult, op1=ALU.mult),
                reads=[junk_t[b], ss_t[b], sg_t[b]], writes=[O_t[t]])
        run_pipelined((hgrn_tile(t) for t in range(NT)), NB)


def make_in_map(inputs, b):
    m = {}
    m["x"] = np.ascontiguousarray(inputs["x"][b])
    m["positions"] = np.ascontiguousarray(inputs["positions"][b].reshape(S, 1).astype(np.int32))
    for k in ("gla_w_in", "gla_w_gk", "gla_b_gk", "gla_norm_g", "gla_w_out", "moba_w_in", "moba_w_out", "gdn_w_in",
              "gdn_conv_w", "gdn_a_log", "gdn_dt_bias", "gdn_norm_g", "gdn_w_out", "hgrn_w_in", "hgrn_norm_g",
              "hgrn_w_out"):
        v = np.asarray(inputs[k])[0]
        if v.ndim == 1:
            v = v.reshape(1, -1)
        m[k] = np.ascontiguousarray(v)
    m["hgrn_lower_bounds"] = np.ascontiguousarray(inputs["hgrn_lower_bounds"])
    m["ffn_w_gu"] = np.ascontiguousarray(inputs["ffn_w_gu"])
    m["ffn_w_down"] = np.ascontiguousarray(inputs["ffn_w_down"])
    m["ln_g"] = np.ascontiguousarray(np.asarray(inputs["ln_g"]).reshape(DEPTH * 2, D))
    m["ln_b"] = np.ascontiguousarray(np.asarray(inputs["ln_b"]).reshape(DEPTH * 2, D))
    for k, v in host_consts().items():
        m["c_" + k] = v
    return m


def _todo(C, l):
    raise NotImplementedError


MIXERS = [gla_phase, moba_phase, gdn_phase, hgrn_phase]
_NC_CACHE = {}


def kernel(**inputs):
    if "nc" not in _NC_CACHE:
        _NC_CACHE["nc"] = build()
    nc = _NC_CACHE["nc"]
    in_maps = [make_in_map(inputs, b) for b in range(NCORES)]
    res = run_bass_kernel_spmd(nc, in_maps, core_ids=list(range(NCORES)))
    return np.stack([np.asarray(r["y"]) for r in res.results], axis=0).astype(np.float32)
```
